# Optimizing a Trainium2 kernel written in Bass

```python
import jax
import jax.numpy as jnp
from jax import lax
import numpy as np

D_MODEL = 1024
BATCH = 32
SEQ = 2048
DEPTH = 2

GRID_W = 64
CTX_LEN = 256
MIX_W = D_MODEL
N_MIXERS = 4
GROUP_W = MIX_W // N_MIXERS
HEAD_DIM = 64
N_Q_HEADS = GROUP_W // HEAD_DIM
N_KV_HEADS = N_Q_HEADS // 2
WINDOW = 128
ATT_BLOCK = WINDOW
ROPE_BASE = 10000.0
CONV_WIDTH = 31
CHUNK = 128
GMLP_HEADS = 4
GMLP_HEAD_W = GROUP_W // GMLP_HEADS
POOL_WINDOWS = (2, 4, 8, 16)
POOL_GROUP_W = GROUP_W // len(POOL_WINDOWS)
N_EXPERTS = 32
TOP_K = 4
D_FF = D_MODEL
SWIGLU_ALPHA = 1.702
SWIGLU_LIMIT = 7.0
MOE_BLOCK = 256
EPS = 1e-6
NEG_INF = -1e30

OFF_Q = 0
OFF_K = OFF_Q + N_Q_HEADS * HEAD_DIM
OFF_V = OFF_K + N_KV_HEADS * HEAD_DIM
OFF_CONV = OFF_V + N_KV_HEADS * HEAD_DIM
OFF_GMLP = OFF_CONV + 2 * GROUP_W
OFF_POOL = OFF_GMLP + 2 * GROUP_W
IN_W = OFF_POOL + GROUP_W

kernel_name = 'hybrid_parallel_dit_block'


def rmsnorm(x, g):
    xf = x.astype(jnp.float32)
    y = xf * lax.rsqrt(jnp.mean(xf * xf, axis=-1, keepdims=True) + EPS)
    return (y * g.astype(jnp.float32)).astype(x.dtype)


def layernorm(x, g, b):
    xf = x.astype(jnp.float32)
    mu = jnp.mean(xf, axis=-1, keepdims=True)
    xc = xf - mu
    y = xc * lax.rsqrt(jnp.mean(xc * xc, axis=-1, keepdims=True) + EPS)
    return (y * g.astype(jnp.float32) + b.astype(jnp.float32)).astype(x.dtype)


def group_rmsnorm(y, g):
    shp = y.shape
    yg = y.reshape(shp[:-1] + (N_MIXERS, GROUP_W))
    return rmsnorm(yg, g.reshape(N_MIXERS, GROUP_W)).reshape(shp)


def adaln(cond, w, b):
    m = jax.nn.silu(cond) @ w + b
    return m.reshape(m.shape[:-1] + (6, D_MODEL))


def axial_rope_tables(L):
    rows = L // GRID_W
    t = jnp.arange(rows * GRID_W)
    row = (t // GRID_W).astype(jnp.float32)
    col = (t % GRID_W).astype(jnp.float32)
    half = HEAD_DIM // 2
    inv = ROPE_BASE ** (-jnp.arange(0, half, 2, dtype=jnp.float32) / half)
    ang_r = row[:, None] * inv[None, :]
    ang_c = col[:, None] * inv[None, :]
    return (jnp.cos(ang_r), jnp.sin(ang_r), jnp.cos(ang_c), jnp.sin(ang_c))


def rotate(x, cos, sin):
    cs = cos[:, None, :].astype(x.dtype)
    sn = sin[:, None, :].astype(x.dtype)
    x1, x2 = jnp.split(x, 2, axis=-1)
    return jnp.concatenate([x1 * cs - x2 * sn, x2 * cs + x1 * sn], axis=-1)


def apply_axial_rope(x, tabs):
    cos_r, sin_r, cos_c, sin_c = tabs
    half = HEAD_DIM // 2
    return jnp.concatenate([rotate(x[..., :half], cos_r, sin_r),
                            rotate(x[..., half:], cos_c, sin_c)], axis=-1)


def window_attention(q, k, v, kc, vc, sink):
    b, L = q.shape[0], q.shape[1]
    nb = L // ATT_BLOCK
    grp = N_Q_HEADS // N_KV_HEADS
    scale = HEAD_DIM ** -0.5
    qb = q.reshape(b, nb, ATT_BLOCK, N_KV_HEADS, grp, HEAD_DIM)

    def band(t):
        tp = jnp.pad(t, ((0, 0), (ATT_BLOCK, ATT_BLOCK), (0, 0), (0, 0)))
        tp = tp.reshape(b, nb + 2, ATT_BLOCK, N_KV_HEADS, HEAD_DIM)
        return jnp.concatenate([tp[:, :-2], tp[:, 1:-1], tp[:, 2:]], axis=2)

    kw = band(k)
    vw = band(v)
    s_loc = jnp.einsum('bnqkgd,bnjkd->bnkgqj', qb, kw).astype(jnp.float32) * scale
    s_ctx = jnp.einsum('bnqkgd,bckd->bnkgqc', qb, kc).astype(jnp.float32) * scale
    qi = jnp.arange(ATT_BLOCK)[:, None]
    kj = jnp.arange(3 * ATT_BLOCK)[None, :]
    in_band = (kj >= qi) & (kj <= qi + 2 * WINDOW)
    key_pos = (jnp.arange(nb)[:, None] - 1) * ATT_BLOCK + jnp.arange(3 * ATT_BLOCK)[None, :]
    in_seq = (key_pos >= 0) & (key_pos < L)
    mask = in_band[None, :, :] & in_seq[:, None, :]
    s_loc = jnp.where(mask[None, :, None, None], s_loc, NEG_INF)
    s_sink = jnp.broadcast_to(
        sink.astype(jnp.float32).reshape(N_KV_HEADS, grp)[None, None, :, :, None, None],
        s_loc.shape[:-1] + (1,))
    p = jax.nn.softmax(jnp.concatenate([s_sink, s_ctx, s_loc], axis=-1), axis=-1).astype(q.dtype)
    lc = kc.shape[1]
    o = (jnp.einsum('bnkgqc,bckd->bnqkgd', p[..., 1:1 + lc], vc)
         + jnp.einsum('bnkgqj,bnjkd->bnqkgd', p[..., 1 + lc:], vw))
    return o.reshape(b, L, N_Q_HEADS * HEAD_DIM)


def ctx_attention(qc, kc, vc, sink):
    b, lc = qc.shape[0], qc.shape[1]
    grp = N_Q_HEADS // N_KV_HEADS
    scale = HEAD_DIM ** -0.5
    qg = qc.reshape(b, lc, N_KV_HEADS, grp, HEAD_DIM)
    s = jnp.einsum('bqkgd,bckd->bkgqc', qg, kc).astype(jnp.float32) * scale
    s_sink = jnp.broadcast_to(
        sink.astype(jnp.float32).reshape(N_KV_HEADS, grp)[None, :, :, None, None],
        s.shape[:-1] + (1,))
    p = jax.nn.softmax(jnp.concatenate([s_sink, s], axis=-1), axis=-1).astype(qc.dtype)
    o = jnp.einsum('bkgqc,bckd->bqkgd', p[..., 1:], vc)
    return o.reshape(b, lc, N_Q_HEADS * HEAD_DIM)


def conformer_conv(h2, dw_w, dw_b, ln_g, ln_b, pw):
    a, gte = jnp.split(h2, 2, axis=-1)
    h = a * jax.nn.sigmoid(gte)
    h = lax.conv_general_dilated(
        h, dw_w[:, None, :], window_strides=(1,),
        padding=[(CONV_WIDTH // 2, CONV_WIDTH // 2)],
        dimension_numbers=('NWC', 'WIO', 'NWC'),
        feature_group_count=GROUP_W) + dw_b
    h = jax.nn.silu(layernorm(h, ln_g, ln_b))
    return h @ pw


def chunk_gating(h2, ln_g, ln_b, ws, bs):
    u, v = jnp.split(h2, 2, axis=-1)
    v = layernorm(v, ln_g, ln_b)
    b, L = v.shape[0], v.shape[1]
    nc = L // CHUNK
    vr = v.reshape(b, nc, CHUNK, GMLP_HEADS, GMLP_HEAD_W)
    mixed = jnp.einsum('hpq,bnqhc->bnphc', ws, vr) + bs.T[None, None, :, :, None]
    return u * mixed.reshape(b, L, GROUP_W)


def pool_mix(h, pw, pscale):
    b, L = h.shape[0], h.shape[1]
    hf = h.astype(jnp.float32)
    csum = jnp.concatenate([jnp.zeros((b, 1, GROUP_W), jnp.float32), jnp.cumsum(hf, axis=1)], axis=1)
    t = jnp.arange(L)
    outs = []
    for gi, w in enumerate(POOL_WINDOWS):
        lo = jnp.clip(t - w // 2, 0, L)
        hi = jnp.clip(t + w // 2, 0, L)
        sl = slice(gi * POOL_GROUP_W, (gi + 1) * POOL_GROUP_W)
        cs = csum[..., sl]
        mean = (cs[:, hi] - cs[:, lo]) / (hi - lo).astype(jnp.float32)[:, None]
        y = (mean - hf[..., sl]).astype(h.dtype)
        outs.append(y @ pw[gi])
    return jnp.concatenate(outs, axis=-1) * pscale


def local_mixers(p, dw_w, dw_b, cln_g, cln_b, cpw, gln_g, gln_b, gws, gbs, pw, ps):
    conv = conformer_conv(p[..., OFF_CONV:OFF_GMLP], dw_w, dw_b, cln_g, cln_b, cpw)
    gm = chunk_gating(p[..., OFF_GMLP:OFF_POOL], gln_g, gln_b, gws, gbs)
    pl = pool_mix(p[..., OFF_POOL:IN_W], pw, ps)
    return jnp.concatenate([conv, gm, pl], axis=-1)


def merge_heads(att, local, gn_g, wo):
    return group_rmsnorm(jnp.concatenate([att, local], axis=-1), gn_g) @ wo


def clamped_swiglu(h):
    glu, lin = jnp.split(h, 2, axis=-1)
    glu = jnp.minimum(glu, SWIGLU_LIMIT)
    lin = jnp.clip(lin, -SWIGLU_LIMIT, SWIGLU_LIMIT)
    return glu * jax.nn.sigmoid(SWIGLU_ALPHA * glu) * (lin + 1.0)


def moe(h, rw, rb, w1, b1, w2, b2):
    n_tok, d = h.shape
    logits = (h @ rw + rb).astype(jnp.float32)
    top_val, top_idx = lax.top_k(logits, TOP_K)
    gates = jax.nn.softmax(top_val, axis=-1)
    n_asg = n_tok * TOP_K
    flat_e = top_idx.reshape(-1)
    order = jnp.argsort(flat_e)
    sorted_e = flat_e[order]
    src_tok = order // TOP_K
    counts = jnp.bincount(flat_e, length=N_EXPERTS)
    padded = (counts + MOE_BLOCK - 1) // MOE_BLOCK * MOE_BLOCK
    pad_end = jnp.cumsum(padded)
    pad_start = pad_end - padded
    start = jnp.cumsum(counts) - counts
    dest = pad_start[sorted_e] + jnp.arange(n_asg) - start[sorted_e]
    n_rows = -(-n_asg // MOE_BLOCK) * MOE_BLOCK + N_EXPERTS * MOE_BLOCK
    n_blk = n_rows // MOE_BLOCK
    row_tok = jnp.zeros((n_rows,), jnp.int32).at[dest].set(src_tok.astype(jnp.int32))
    row_w = jnp.zeros((n_rows,), h.dtype).at[dest].set(gates.reshape(-1)[order].astype(h.dtype))
    blk_e = jnp.minimum(jnp.searchsorted(pad_end, jnp.arange(n_blk) * MOE_BLOCK, side='right'),
                        N_EXPERTS - 1)
    xp = h[row_tok].reshape(n_blk, MOE_BLOCK, d)

    def run_block(args):
        xb, e = args
        return clamped_swiglu(xb @ w1[e] + b1[e]) @ w2[e] + b2[e]

    yp = lax.map(run_block, (xp, blk_e)).reshape(n_rows, d)
    return jax.ops.segment_sum(yp * row_w[:, None], row_tok, num_segments=n_tok)


def setup_inputs(seed: int = 0) -> dict:
    key = jax.random.key(seed)
    ks = jax.random.split(key, 32)

    def nrm(k, shape, scale):
        return jax.random.normal(k, shape, jnp.float32) * scale

    D = D_MODEL
    return {
        'x': nrm(ks[0], (BATCH, SEQ, D), 1.0),
        'c': nrm(ks[1], (BATCH, D), 1.0),
        'ctx': nrm(ks[2], (BATCH, CTX_LEN, D), 1.0),
        'c_ctx': nrm(ks[3], (D,), 1.0),
        'ada_w': nrm(ks[4], (DEPTH, D, 6 * D), 0.5 * D ** -0.5),
        'ada_b': nrm(ks[5], (DEPTH, 6 * D), 0.02),
        'norm1_g': 1.0 + nrm(ks[6], (DEPTH, D), 0.05),
        'norm2_g': 1.0 + nrm(ks[7], (DEPTH, D), 0.05),
        'w_in': nrm(ks[8], (DEPTH, D, IN_W), D ** -0.5),
        'attn_sink': nrm(ks[9], (DEPTH, N_Q_HEADS), 1.0),
        'conv_dw_w': nrm(ks[10], (DEPTH, CONV_WIDTH, GROUP_W), CONV_WIDTH ** -0.5),
        'conv_dw_b': nrm(ks[11], (DEPTH, GROUP_W), 0.02),
        'conv_ln_g': 1.0 + nrm(ks[12], (DEPTH, GROUP_W), 0.05),
        'conv_ln_b': nrm(ks[13], (DEPTH, GROUP_W), 0.02),
        'conv_pw_w': nrm(ks[14], (DEPTH, GROUP_W, GROUP_W), GROUP_W ** -0.5),
        'gmlp_ln_g': 1.0 + nrm(ks[15], (DEPTH, GROUP_W), 0.05),
        'gmlp_ln_b': nrm(ks[16], (DEPTH, GROUP_W), 0.02),
        'gmlp_ws': nrm(ks[17], (DEPTH, GMLP_HEADS, CHUNK, CHUNK), CHUNK ** -0.5),
        'gmlp_bs': 1.0 + nrm(ks[18], (DEPTH, GMLP_HEADS, CHUNK), 0.02),
        'pool_w': nrm(ks[19], (DEPTH, len(POOL_WINDOWS), POOL_GROUP_W, POOL_GROUP_W), POOL_GROUP_W ** -0.5),
        'pool_scale': 1.0 + nrm(ks[20], (DEPTH, GROUP_W), 0.05),
        'group_norm_g': 1.0 + nrm(ks[21], (DEPTH, MIX_W), 0.05),
        'w_out': nrm(ks[22], (DEPTH, MIX_W, D), MIX_W ** -0.5),
        'router_w': nrm(ks[23], (DEPTH, D, N_EXPERTS), D ** -0.5),
        'router_b': nrm(ks[24], (DEPTH, N_EXPERTS), 0.01),
        'exp_w1': nrm(ks[25], (DEPTH, N_EXPERTS, D, 2 * D_FF), D ** -0.5),
        'exp_b1': nrm(ks[26], (DEPTH, N_EXPERTS, 2 * D_FF), 0.02),
        'exp_w2': nrm(ks[27], (DEPTH, N_EXPERTS, D_FF, D), D_FF ** -0.5),
        'exp_b2': nrm(ks[28], (DEPTH, N_EXPERTS, D), 0.02),
        'final_norm_g': 1.0 + nrm(ks[29], (D,), 0.05),
    }


def reference(x, c, ctx, c_ctx, ada_w, ada_b, norm1_g, norm2_g, w_in, attn_sink,
              conv_dw_w, conv_dw_b, conv_ln_g, conv_ln_b, conv_pw_w,
              gmlp_ln_g, gmlp_ln_b, gmlp_ws, gmlp_bs, pool_w, pool_scale,
              group_norm_g, w_out, router_w, router_b, exp_w1, exp_b1, exp_w2, exp_b2,
              final_norm_g):
    b, L, d = x.shape
    lc = ctx.shape[1]
    kv_w = N_KV_HEADS * HEAD_DIM
    tabs = axial_rope_tables(L)
    for l in range(DEPTH):
        last = l == DEPTH - 1
        mx = adaln(c, ada_w[l], ada_b[l])[:, :, None, :]
        sh1, sc1, g1, sh2, sc2, g2 = [mx[:, i] for i in range(6)]
        mc = adaln(c_ctx, ada_w[l], ada_b[l])
        sh1c, sc1c, g1c, sh2c, sc2c, g2c = [mc[i] for i in range(6)]
        lp = (conv_dw_w[l], conv_dw_b[l], conv_ln_g[l], conv_ln_b[l], conv_pw_w[l],
              gmlp_ln_g[l], gmlp_ln_b[l], gmlp_ws[l], gmlp_bs[l], pool_w[l], pool_scale[l])

        hx = rmsnorm(x, norm1_g[l]) * (1.0 + sc1) + sh1
        hc = rmsnorm(ctx, norm1_g[l]) * (1.0 + sc1c) + sh1c
        px = hx @ w_in[l]
        if last:
            kvc = hc @ w_in[l][:, OFF_K:OFF_CONV]
        else:
            pc = hc @ w_in[l]
            kvc = pc[..., OFF_K:OFF_CONV]
        kc = kvc[..., :kv_w].reshape(b, lc, N_KV_HEADS, HEAD_DIM)
        vc = kvc[..., kv_w:].reshape(b, lc, N_KV_HEADS, HEAD_DIM)

        q = apply_axial_rope(px[..., OFF_Q:OFF_K].reshape(b, L, N_Q_HEADS, HEAD_DIM), tabs)
        k = apply_axial_rope(px[..., OFF_K:OFF_V].reshape(b, L, N_KV_HEADS, HEAD_DIM), tabs)
        v = px[..., OFF_V:OFF_CONV].reshape(b, L, N_KV_HEADS, HEAD_DIM)
        att_x = window_attention(q, k, v, kc, vc, attn_sink[l])
        x = x + g1 * merge_heads(att_x, local_mixers(px, *lp), group_norm_g[l], w_out[l])
        if not last:
            qc = pc[..., OFF_Q:OFF_K].reshape(b, lc, N_Q_HEADS, HEAD_DIM)
            att_c = ctx_attention(qc, kc, vc, attn_sink[l])
            ctx = ctx + g1c * merge_heads(att_c, local_mixers(pc, *lp), group_norm_g[l], w_out[l])

        h2x = (rmsnorm(x, norm2_g[l]) * (1.0 + sc2) + sh2).reshape(b * L, d)
        if last:
            ff = moe(h2x, router_w[l], router_b[l], exp_w1[l], exp_b1[l], exp_w2[l], exp_b2[l])
            x = x + g2 * ff.reshape(b, L, d)
        else:
            h2c = (rmsnorm(ctx, norm2_g[l]) * (1.0 + sc2c) + sh2c).reshape(b * lc, d)
            ff = moe(jnp.concatenate([h2x, h2c], axis=0), router_w[l], router_b[l],
                     exp_w1[l], exp_b1[l], exp_w2[l], exp_b2[l])
            x = x + g2 * ff[:b * L].reshape(b, L, d)
            ctx = ctx + g2c * ff[b * L:].reshape(b, lc, d)
    return rmsnorm(x, final_norm_g)
```

```python
import contextlib
import numpy as np
import concourse.bass as bass
import concourse.mybir as mybir
from concourse.bass_utils import run_bass_kernel_spmd

F32 = mybir.dt.float32
BF16 = mybir.dt.bfloat16
I32 = mybir.dt.int32
AF = mybir.ActivationFunctionType
ALU = mybir.AluOpType
AX = mybir.AxisListType
POOL_ENG = mybir.EngineType.Pool

EPOCH = 16000
NSLOT = 8
NCORE = 8
SPC = 4
D = 1024
L = 2048
LC = 256
INW = 2432
EPS = 1e-6
TS = 512
import os as _os
SKIPBIG = float(_os.environ.get("SKIPBIG", "1.0e6"))


class Res:
    __slots__ = ("w", "r")

    def __init__(self):
        self.w = {}
        self.r = {}


class TT:
    __slots__ = ("t", "res", "excl")

    def __init__(self, t, excl=False):
        self.t = t
        self.res = Res()
        self.excl = excl

    def __getitem__(self, k):
        return self.t[k]


def _res(b):
    return b.res if isinstance(b, TT) else b


class Sched:
    def __init__(self, nc, stack):
        self.nc = nc
        self.stack = stack
        self.gstack = stack
        self.E = {}
        for name, e in (("pe", nc.tensor), ("act", nc.scalar), ("dve", nc.vector),
                        ("pool", nc.gpsimd), ("sp", nc.sync)):
            self.E[name] = dict(e=e, name=name, sems=[], cnt=0, waited={}, slots=None, slot_i=0)
        self.n_inst = 0
        self.n_wait = 0
        self.uid = 0

    def sem(self, name):
        return self.gstack.enter_context(self.nc.semaphore(name))

    def sb(self, shape, dt, name=None):
        self.uid += 1
        return TT(self.stack.enter_context(self.nc.sbuf_tensor(f"{name or 't'}_{self.uid}", list(shape), dt)))

    def ps(self, shape, dt=F32, name=None):
        self.uid += 1
        shape = [128, 512 if dt == F32 else 1024]
        return TT(excl=True, t=self.stack.enter_context(self.nc.psum_tensor(f"{name or 'p'}_{self.uid}", list(shape), dt)))

    def _wait(self, E, sem, val):
        key = id(sem)
        if E["waited"].get(key, 0) >= val:
            return
        E["waited"][key] = val
        E["e"].wait_ge(sem, val)
        self.n_wait += 1

    def _deps(self, E, reads, writes):
        deps = {}
        for b in reads:
            for k, v in _res(b).w.items():
                if k not in deps or deps[k][1] < v[1]:
                    deps[k] = v
        for b in writes:
            r = _res(b)
            for dd in (r.w, r.r):
                for k, v in dd.items():
                    if k not in deps or deps[k][1] < v[1]:
                        deps[k] = v
        own = E["sems"]
        for k, (sem, val) in deps.items():
            if E["name"] == "pe" and any(sem is s for s in own):
                continue
            self._wait(E, sem, val)

    def _record(self, tok, reads, writes):
        k = id(tok[0])
        for b in reads:
            _res(b).r[k] = tok
        for b in writes:
            r = _res(b)
            r.w = {k: tok}
            r.r = {}

    def _next_tok(self, E):
        n = E["cnt"]
        ep = n // EPOCH
        while len(E["sems"]) <= ep:
            E["sems"].append(self.sem(f"c_{E['name']}_{len(E['sems'])}"))
        E["cnt"] = n + 1
        return (E["sems"][ep], n % EPOCH + 1)

    def op(self, eng, fn, reads=(), writes=()):
        E = self.E[eng]
        ex = [b for b in reads if isinstance(b, TT) and b.excl]
        if ex:
            reads = [b for b in reads if not (isinstance(b, TT) and b.excl)]
            writes = list(writes) + ex
        self._deps(E, reads, writes)
        inst = fn(E["e"])
        tok = self._next_tok(E)
        inst.then_inc(tok[0], 1)
        self._record(tok, reads, writes)
        self.n_inst += 1

    def wait_for(self, eng, reads=(), writes=()):
        self._deps(self.E[eng], reads, writes)

    def dma(self, q, fn, reads=(), writes=()):
        E = self.E[q]
        if E["slots"] is None:
            E["slots"] = [[self.sem(f"d_{q}_{i}"), 0] for i in range(NSLOT)]
        self._deps(E, reads, writes)
        sl = E["slots"][E["slot_i"] % NSLOT]
        E["slot_i"] += 1
        if sl[1] > 0:
            self._wait(E, sl[0], sl[1])
        inst = fn(E["e"])
        sl[1] += 16
        inst.then_inc(sl[0], 16)
        self._record((sl[0], sl[1]), reads, writes)
        self.n_inst += 1

    def _all_toks(self):
        toks = []
        for E in self.E.values():
            if E["slots"]:
                for s, v in E["slots"]:
                    if v:
                        toks.append((s, v))
            n = E["cnt"]
            if n:
                toks.append((E["sems"][(n - 1) // EPOCH], (n - 1) % EPOCH + 1))
        return toks

    def barrier(self):
        toks = self._all_toks()
        for E in self.E.values():
            for s, v in toks:
                self._wait(E, s, v)

    def finish(self):
        self.barrier()


def _consts():
    c = {}
    c["ident"] = np.eye(128, dtype=np.float32)
    tq = np.arange(128)
    c["upper"] = (tq[:, None] < tq[None, :]).astype(np.float32)
    kj = np.arange(128)[:, None]
    qi = np.arange(128)[None, :]
    mL = (qi <= kj).astype(np.float32)
    mR = (kj <= qi).astype(np.float32)
    c["masks"] = np.stack([np.concatenate([mL, mL], 1), np.concatenate([mR, mR], 1)], 1)
    t = np.arange(L)
    row = (t // 64).astype(np.float32)
    col = (t % 64).astype(np.float32)
    inv = (10000.0 ** (-np.arange(0, 32, 2, dtype=np.float32) / 32)).astype(np.float32)
    ang_r = row[:, None] * inv[None, :]
    ang_c = col[:, None] * inv[None, :]
    cs = np.zeros((64, L), np.float32)
    sn = np.zeros((64, L), np.float32)
    for d in range(64):
        ang = ang_r if d < 32 else ang_c
        i = d % 16
        cs[d] = np.cos(ang[:, i])
        sn[d] = np.sin(ang[:, i]) * (-1.0 if (d % 32) < 16 else 1.0)
    c["ropeC"] = np.concatenate([cs, cs], 0)
    c["ropeS"] = np.concatenate([sn, sn], 0)
    PM = np.zeros((128, 20, 128), np.float32)
    LL = 512
    for gi, w in enumerate((2, 4, 8, 16)):
        M = np.zeros((LL, LL), np.float32)
        for tt in range(LL):
            lo = max(tt - w // 2, 0)
            hi = min(tt + w // 2, LL)
            M[lo:hi, tt] = 1.0 / (hi - lo)
            M[tt, tt] -= 1.0
        PM[:, gi * 5 + 0] = M[0:128, 128:256]
        PM[:, gi * 5 + 1] = M[256:384, 128:256]
        PM[:, gi * 5 + 2] = M[128:256, 128:256]
        PM[:, gi * 5 + 3] = M[0:128, 0:128]
        PM[:, gi * 5 + 4] = M[384:512, 384:512]
    c["poolM"] = PM
    c["slotthr"] = np.tile((np.arange(128, dtype=np.float32) * TS)[None, :], (32, 1))
    c["piota"] = np.arange(128, dtype=np.float32)[:, None]
    return c


def _ext_cols():
    def sw(off, n_heads):
        idx = []
        for h in range(n_heads):
            for d in range(64):
                sd = d + 16 if (d % 32) < 16 else d - 16
                idx.append(off + h * 64 + sd)
        return idx
    q = list(range(0, 256))
    qs = sw(0, 4)
    k0 = list(range(256, 320))
    k1 = list(range(320, 384))
    ks = sw(256, 2)
    k0s, k1s = ks[:64], ks[64:]
    cols = q + qs + k0 + k0 + k1 + k1 + k0s + k0s + k1s + k1s
    cols += list(range(512, 1024))
    cols += list(range(1024, 1536))
    cols += list(range(384, 512))
    cols += list(range(1536, 1792))
    assert len(cols) == INW
    return np.array(cols)


def build(stop_after=None):
    nc = bass.Bass("TRN2", target_bir_lowering=False)

    def din(name, shape, dt=F32):
        return nc.dram_tensor(name, list(shape), dt, kind="ExternalInput").ap()

    def dscr(name, shape, dt=F32):
        return TT(nc.dram_tensor(name, list(shape), dt, kind="Internal").ap())

    x_in = TT(din("x", [SPC * L, D]))
    c_in = TT(din("ctx", [SPC * LC, D]))
    cT_in = din("cT", [128, 8, 5])
    ada_w = din("ada_w", [2, D, 6 * D])
    ada_b = din("ada_b", [2, 6 * D])
    norm1_g = din("norm1_g", [2, D])
    norm2_g = din("norm2_g", [2, D])
    w_in = din("w_in_ext", [2, D, INW])
    attn_sink = din("attn_sink", [2, 4])
    dwT = din("conv_dwT", [2, 128, 2, 31])
    convcols = din("convcols", [2, 128, 3, 2])
    conv_pw = din("conv_pw_w", [2, 256, 256])
    gln_g = din("gmlp_ln_g", [2, 256])
    gln_b = din("gmlp_ln_b", [2, 256])
    gwsT = din("gmlp_wsT", [2, 4, 128, 128])
    gbsT = din("gmlp_bsT", [2, 128, 4])
    pool_w = din("pool_w", [2, 4, 64, 64])
    pool_scale = din("pool_scale", [2, 256])
    gncol = din("gncol", [2, 128, 8])
    w_out = din("w_out", [2, D, D])
    router_w = din("router_w", [2, D, 32])
    router_b = din("router_b", [2, 32])
    exp_w1 = din("exp_w1", [2, 32, D, 2 * D])
    exp_b1T = din("exp_b1T", [2, 32, 128, 16])
    exp_w2 = din("exp_w2r", [2, 32, 4, 128, 2 * D])
    exp_b2 = din("exp_b2", [2, 32, D])
    final_g = din("final_norm_g", [1, D])
    k_ident = din("k_ident", [128, 128])
    k_upper = din("k_upper", [128, 128])
    k_masks = din("k_masks", [128, 2, 256])
    k_ropeC = din("k_ropeC", [128, L])
    k_ropeS = din("k_ropeS", [128, L])
    k_poolM = din("k_poolM", [128, 20, 128])
    k_slotthr = din("k_slotthr", [32, 128])
    k_piota = din("k_piota", [128, 1])
    out_t = TT(nc.dram_tensor("out", [SPC * L, D], F32, kind="ExternalOutput").ap())

    xs1 = dscr("xs1", [SPC * L, D])
    xs2 = dscr("xs2", [SPC * L, D])
    cs1 = dscr("cs1", [SPC * LC, D])
    cs2 = dscr("cs2", [SPC * LC, D])
    modrows = dscr("modrows", [5, 6 * D])
    NTMAX = (SPC * (L + LC)) // 128
    NSLMAX = NTMAX * 128 * 4 // TS + 32
    h2d = dscr("h2d", [NTMAX * 128, D], BF16)
    xsort = dscr("xsort", [NSLMAX * TS, D], BF16)
    ysort = dscr("ysort", [NSLMAX * TS, D], F32)

    with contextlib.ExitStack() as gst:
        S = Sched(nc, gst)

        def ACT(out, in_, func, reads, writes, **kw):
            S.op("act", lambda e: e.activation(out=out, in_=in_, func=func, **kw), reads, writes)

        def MM(out, lhsT, rhs, start, stop, reads, writes):
            S.op("pe", lambda e: e.matmul(out, lhsT=lhsT, rhs=rhs, start=start, stop=stop), reads, writes)

        def TR(out, in_, ident, reads, writes):
            S.op("pe", lambda e: e.transpose(out=out, in_=in_, identity=ident), reads, writes)

        def TTOP(eng, out, in0, in1, op, reads, writes):
            S.op(eng, lambda e: e.tensor_tensor(out=out, in0=in0, in1=in1, op=op), reads, writes)

        def TSC(eng, out, in0, s1, s2, op0, op1, reads, writes):
            if op1 is None:
                S.op(eng, lambda e: e.tensor_scalar(out=out, in0=in0, scalar1=s1, scalar2=None, op0=op0), reads, writes)
            else:
                S.op(eng, lambda e: e.tensor_scalar(out=out, in0=in0, scalar1=s1, scalar2=s2, op0=op0, op1=op1),
                     reads, writes)

        def STT(eng, out, in0, scalar, in1, op0, op1, reads, writes):
            eng = "dve"
            S.op(eng, lambda e: e.scalar_tensor_tensor(out=out, in0=in0, scalar=scalar, in1=in1, op0=op0, op1=op1),
                 reads, writes)

        def CP(eng, out, in_, reads, writes):
            if eng == "act":
                S.op("act", lambda e: e.copy(out=out, in_=in_), reads, writes)
            else:
                S.op(eng, lambda e: e.tensor_copy(out=out, in_=in_), reads, writes)

        def DMA(q, out, in_, reads, writes):
            S.dma(q, lambda e: e.dma_start(out=out, in_=in_), reads, writes)

        def rstd_from(ssum, n, tmp):
            TSC("dve", tmp[:], ssum[:], 1.0 / n, EPS, ALU.mult, ALU.add, [ssum], [tmp])
            ACT(tmp[:], tmp[:], AF.Sqrt, [tmp], [tmp])
            S.op("dve", lambda e: e.reciprocal(out=tmp[:], in_=tmp[:]), [tmp], [tmp])

        ident32 = S.sb([128, 128], F32, "ident32")
        identb = S.sb([128, 128], BF16, "identb")
        DMA("sp", ident32[:], k_ident, [], [ident32])
        DMA("pool", identb[:], k_ident, [], [identb])

        def phase_ada(l):
            with contextlib.ExitStack() as ph:
                S.stack = ph
                cT = S.sb([128, 8, 5], F32)
                sc = S.sb([128, 8, 5], F32)
                DMA("sp", cT[:], cT_in, [], [cT])
                ACT(sc[:], cT[:], AF.Silu, [cT], [sc])
                rows = S.sb([5, 6 * D], F32)
                bb = S.sb([5, 6 * D], F32)
                DMA("sp", bb[:], ada_b[l:l + 1, :].partition_broadcast(5), [], [bb])
                wts = [S.sb([128, 8, 512], F32) for _ in range(2)]
                pms = [S.ps([128, 512]) for _ in range(2)]
                for n in range(12):
                    wt = wts[n % 2]
                    pm = pms[n % 2]
                    DMA("sp", wt[:], ada_w[l, :, n * 512:(n + 1) * 512].rearrange("(c p) n -> p c n", p=128), [], [wt])
                    for c in range(8):
                        MM(pm[0:5, :], sc[:, c, :], wt[:, c, :], c == 0, c == 7, [sc, wt], [pm])
                    TTOP("dve", rows[:, n * 512:(n + 1) * 512], pm[0:5, :], bb[:, n * 512:(n + 1) * 512], ALU.add,
                         [pm, bb], [rows])
                DMA("sp", modrows[:, :], rows[:], [rows], [modrows])
            S.stack = gst
            S.barrier()

        def phase_mix(l, last, xsrc, xdst, csrc, cdst):
            with contextlib.ExitStack() as ph:
                S.stack = ph
                win = S.sb([128, 8, INW], BF16, "win")
                for c in range(8):
                    for hf in range(2):
                        DMA("pool", win[:, c, hf * 1216:(hf + 1) * 1216], w_in[l, c * 128:(c + 1) * 128, hf * 1216:(hf + 1) * 1216],
                            [], [win])
                wout = S.sb([128, 8, D], BF16, "wout")
                gnc = S.sb([128, 8], F32)
                DMA("sp", gnc[:], gncol[l], [], [gnc])
                dg = S.sb([128, 2, 31, 128], BF16)
                dw = S.sb([128, 2, 31], F32)
                DMA("sp", dw[:], dwT[l], [], [dw])
                for cc_ in range(2):
                    for j_ in range(31):
                        TSC("pool" if j_ % 2 else "dve", dg[:, cc_, j_, :], identb[:], dw[:, cc_, j_:j_ + 1], None, ALU.mult, None,
                            [identb, dw], [dg])
                ccols = S.sb([128, 3, 2], F32)
                DMA("sp", ccols[:], convcols[l], [], [ccols])
                cpw = S.sb([128, 2, 256], BF16)
                DMA("pool", cpw[:], conv_pw[l].rearrange("(c p) n -> p c n", p=128), [], [cpw])
                wsT = S.sb([128, 4, 128], BF16)
                DMA("pool", wsT[:], gwsT[l].rearrange("h q p -> q h p"), [], [wsT])
                bsT = S.sb([128, 4], F32)
                DMA("sp", bsT[:], gbsT[l], [], [bsT])
                glg = S.sb([128, 256], F32)
                glb = S.sb([128, 256], F32)
                DMA("sp", glg[:], gln_g[l:l + 1, :].partition_broadcast(128), [], [glg])
                DMA("sp", glb[:], gln_b[l:l + 1, :].partition_broadcast(128), [], [glb])
                pw = S.sb([64, 4, 64], BF16)
                DMA("pool", pw[:], pool_w[l].rearrange("g c j -> c g j"), [], [pw])
                psc = S.sb([128, 256], F32)
                DMA("sp", psc[:], pool_scale[l:l + 1, :].partition_broadcast(128), [], [psc])
                poolM = S.sb([128, 20, 128], BF16)
                for hf in range(2):
                    DMA("pool", poolM[:, hf * 10:(hf + 1) * 10, :], k_poolM[:, hf * 10:(hf + 1) * 10, :], [], [poolM])
                masks = S.sb([128, 2, 256], BF16)
                DMA("pool", masks[:], k_masks, [], [masks])
                esink = S.sb([128, 4], F32)
                DMA("sp", esink[:], attn_sink[l:l + 1, :].partition_broadcast(128), [], [esink])
                ACT(esink[:], esink[:], AF.Exp, [esink], [esink])
                ropeC = S.sb([128, 512], F32)
                ropeS = S.sb([128, 512], F32)
                gn1 = S.sb([128, D], F32)
                DMA("sp", gn1[:], norm1_g[l:l + 1, :].partition_broadcast(128), [], [gn1])
                onesm = S.sb([128, 128], F32)
                S.op("dve", lambda e: e.memset(onesm[:], 1.0 / 256.0), [], [onesm])
                A1 = S.sb([128, D], F32)
                B1 = S.sb([128, D], F32)
                G1 = S.sb([128, D], F32)

                qT = [S.sb([128, L], BF16) for _ in range(2)]
                kT = [S.sb([128, L], BF16) for _ in range(2)]
                kcT = [S.sb([128, LC], BF16) for _ in range(2)]
                vaug = S.sb([128, 16, 2, 65], BF16)
                vcaug = S.sb([128, 2, 2, 65], BF16)
                S.op("pool", lambda e: e.memset(vaug[:], 1.0), [], [vaug])
                S.op("pool", lambda e: e.memset(vcaug[:], 1.0), [], [vcaug])
                convT = S.sb([128, 2, L + 30], BF16)
                S.op("pool", lambda e: e.memset(convT[:], 0.0), [], [convT])
                sTb = S.sb([128, 2, L], BF16)
                gmb = S.sb([128, 16, 256], BF16)
                poolh = S.sb([128, 16, 256], BF16)
                xt1_ = S.sb([128, D], F32)
                xt = [xt1_, xt1_]
                ss = S.sb([128, 1], F32)
                rs = S.sb([128, 1], F32)
                h32 = S.sb([128, D], F32)
                hb = S.sb([128, D], BF16)
                hT = S.sb([128, 8, 512], BF16)
                ftmp = [S.sb([128, 512], F32) for _ in range(3)]
                u32 = S.sb([128, 256], F32)
                v32 = S.sb([128, 256], F32)
                vnb = S.sb([128, 256], BF16)
                st2 = S.sb([128, 2], F32)
                st3 = S.sb([128, 2], F32)
                acc = S.sb([128, 2, 512], F32)
                mean_sb = S.sb([128, 512], F32)
                rstd_sb = S.sb([128, 512], F32)
                PT = [S.sb([128, 5, 256], BF16) for _ in range(2)]
                den = S.sb([128, 4], F32)
                mix = S.sb([128, D], F32)
                gss = S.sb([128, 4], F32)
                grs = S.sb([128, 4], F32)
                yb = S.sb([128, D], BF16)
                yT = S.sb([128, 8, 128], BF16)
                yTp = S.sb([64, 4, 128], BF16)
                xn = h32
                wtmp = [h32, mix]
                for c in range(8):
                    DMA("sp", wtmp[c % 2][:], w_out[l, c * 128:(c + 1) * 128, :], [], [wtmp[c % 2]])
                    TSC("dve", wout[:, c, :], wtmp[c % 2][:], gnc[:, c:c + 1], None, ALU.mult, None,
                        [wtmp[c % 2], gnc], [wout])
                pT = [S.ps([128, 128], BF16) for _ in range(2)]
                pf = [S.ps([128, 512]) for _ in range(2)]
                pa = S.ps([128, 512])
                pb = S.ps([128, 512])
                pg = S.ps([128, 512])
                pq = S.ps([128, 512])

                def load_mod(r):
                    DMA("sp", A1[:], modrows[r:r + 1, D:2 * D].partition_broadcast(128), [modrows], [A1])
                    DMA("sp", B1[:], modrows[r:r + 1, 0:D].partition_broadcast(128), [modrows], [B1])
                    DMA("sp", G1[:], modrows[r:r + 1, 2 * D:3 * D].partition_broadcast(128), [modrows], [G1])
                    STT("dve", A1[:], A1[:], 1.0, gn1[:], ALU.add, ALU.mult, [A1, gn1], [A1])

                cp_i = [0]

                def cp_alt(out, in_, reads, writes):
                    cp_i[0] += 1
                    CP("act" if cp_i[0] % 2 else "dve", out, in_, reads, writes)

                def seq(src, dst, row0, LL, is_ctx, do_pass2):
                    NT = LL // 128
                    GT = min(512, LL)
                    TG = GT // 128
                    KT = kcT if is_ctx else kT
                    VA = vcaug if is_ctx else vaug
                    for g in range(LL // GT):
                        for ti in range(TG):
                            t = g * TG + ti
                            x_ = xt[t % 2]
                            DMA("sp", x_[:], src[row0 + t * 128: row0 + (t + 1) * 128, :], [src], [x_])
                            ACT(yb[:], x_[:], AF.Square, [x_], [yb, ss], accum_out=ss[:])
                            rstd_from(ss, D, rs)
                            STT("dve", h32[:], x_[:], rs[:, 0:1], A1[:], ALU.mult, ALU.mult, [x_, rs, A1], [h32])
                            TTOP("pool", hb[:], h32[:], B1[:], ALU.add, [h32, B1], [hb])
                            p_ = pT[t % 2]
                            for c in range(8):
                                TR(p_[:, c * 128:(c + 1) * 128], hb[:, c * 128:(c + 1) * 128], identb[:], [hb, identb], [p_])
                            cp_alt(hT[:, :, ti * 128:(ti + 1) * 128], p_[:, 0:1024].rearrange("p (c t) -> p c t", c=8), [p_], [hT])
                        if not is_ctx:
                            DMA("sp", ropeC[:, :GT], k_ropeC[:, g * GT:(g + 1) * GT], [], [ropeC])
                            DMA("sp", ropeS[:, :GT], k_ropeS[:, g * GT:(g + 1) * GT], [], [ropeS])
                        order = [0, 2, 1, 3, 4, 6, 5, 7, 10, 8, 11, 9] if not is_ctx else [0, 1, 4, 5, 10, 8, 11, 9]
                        tsl = slice(g * GT, (g + 1) * GT)
                        for oi, fc in enumerate(order):
                            p_ = pf[oi % 2]
                            for c in range(8):
                                MM(p_[:, :GT], win[:, c, fc * 128:(fc + 1) * 128], hT[:, c, :GT], c == 0, c == 7,
                                   [win, hT], [p_])
                            if fc in (0, 1, 4, 5):
                                dstT = (qT if not is_ctx else qT)[fc] if fc < 2 else KT[fc - 4]
                                if is_ctx:
                                    cp_alt(dstT[:, tsl], p_[:, :GT], [p_], [dstT])
                                else:
                                    TTOP("dve", ftmp[0][:, :GT], p_[:, :GT], ropeC[:, :GT], ALU.mult, [p_, ropeC], [ftmp[0]])
                            elif fc in (2, 3, 6, 7):
                                dstT = qT[fc - 2] if fc < 4 else KT[fc - 6]
                                TTOP("dve", ftmp[1][:, :GT], p_[:, :GT], ropeS[:, :GT], ALU.mult, [p_, ropeS], [ftmp[1]])
                                TTOP("pool", dstT[:, tsl], ftmp[0][:, :GT], ftmp[1][:, :GT], ALU.add,
                                     [ftmp[0], ftmp[1]], [dstT])
                            elif fc in (10, 11):
                                ACT(ftmp[2][:, :GT], p_[:, :GT], AF.Sigmoid, [p_], [ftmp[2]])
                            else:
                                cc = fc - 8
                                TTOP("dve", convT[:, cc, 15 + g * GT: 15 + (g + 1) * GT], p_[:, :GT], ftmp[2][:, :GT],
                                     ALU.mult, [p_, ftmp[2]], [convT])
                        for ti in range(TG):
                            t = g * TG + ti
                            for c in range(8):
                                MM(pa[:, :], hT[:, c, ti * 128:(ti + 1) * 128], win[:, c, 1536:2048], c == 0, c == 7,
                                   [hT, win], [pa])
                            for c in range(8):
                                MM(pb[:, 0:384], hT[:, c, ti * 128:(ti + 1) * 128], win[:, c, 2048:2432], c == 0, c == 7,
                                   [hT, win], [pb])
                            S.op("act", lambda e: e.copy(out=VA[:, t, :, 0:64],
                                                          in_=pb[:, 0:128].rearrange("p (j d) -> p j d", j=2)), [pb], [VA])
                            CP("dve", poolh[:, t, :], pb[:, 128:384], [pb], [poolh])
                            CP("act", u32[:], pa[:, 0:256], [pa], [u32])
                            ACT(v32[:], pa[:, 256:512], AF.Copy, [pa], [v32, st2], accum_out=st2[:, 0:1])
                            ACT(yb[:, 0:256], pa[:, 256:512], AF.Square, [pa], [yb, st2], accum_out=st2[:, 1:2])
                            TSC("dve", st3[:, 0:1], st2[:, 0:1], 1.0 / 256, None, ALU.mult, None, [st2], [st3])
                            STT("dve", st3[:, 1:2], st3[:, 0:1], -1.0, st3[:, 0:1], ALU.mult, ALU.mult, [st3], [st3])
                            STT("dve", st3[:, 1:2], st2[:, 1:2], 1.0 / 256, st3[:, 1:2], ALU.mult, ALU.add, [st2, st3], [st3])
                            TSC("dve", st3[:, 1:2], st3[:, 1:2], EPS, None, ALU.add, None, [st3], [st3])
                            ACT(st3[:, 1:2], st3[:, 1:2], AF.Sqrt, [st3], [st3])
                            S.op("dve", lambda e: e.reciprocal(out=st3[:, 1:2], in_=st3[:, 1:2]), [st3], [st3])
                            TSC("dve", v32[:], v32[:], st3[:, 0:1], st3[:, 1:2], ALU.subtract, ALU.mult, [v32, st3], [v32])
                            TTOP("pool", v32[:], v32[:], glg[:], ALU.mult, [v32, glg], [v32])
                            TTOP("pool", vnb[:], v32[:], glb[:], ALU.add, [v32, glb], [vnb])
                            for hh in range(4):
                                MM(pg[:, hh * 64:(hh + 1) * 64], wsT[:, hh, :], vnb[:, hh * 64:(hh + 1) * 64], True, True,
                                   [wsT, vnb], [pg])
                            for hh in range(4):
                                STT("dve", gmb[:, t, hh * 64:(hh + 1) * 64], pg[:, hh * 64:(hh + 1) * 64], bsT[:, hh:hh + 1],
                                    u32[:, hh * 64:(hh + 1) * 64], ALU.add, ALU.mult, [pg, bsT, u32], [gmb])
                    if not do_pass2:
                        return
                    for g in range(LL // GT):
                        tsl = slice(g * GT, (g + 1) * GT)
                        for cc in range(2):
                            pc_ = pq if cc == 0 else pg
                            for j in range(31):
                                MM(pc_[:, :GT], dg[:, cc, j, :], convT[:, cc, g * GT + j: g * GT + j + GT], j == 0, j == 30,
                                   [dg, convT], [pc_])
                            TSC("dve", acc[:, cc, :GT], pc_[:, :GT], ccols[:, 0, cc:cc + 1], None, ALU.add, None, [pc_, ccols], [acc])
                        for cc in range(2):
                            MM(pq[:, :GT], onesm[:], acc[:, cc, :GT], cc == 0, cc == 1, [onesm, acc], [pq])
                        CP("act", mean_sb[:, :GT], pq[:, :GT], [pq], [mean_sb])
                        for cc in range(2):
                            ACT(ftmp[cc][:, :GT], acc[:, cc, :GT], AF.Square, [acc], [ftmp[cc]])
                        for cc in range(2):
                            MM(pq[:, :GT], onesm[:], ftmp[cc][:, :GT], cc == 0, cc == 1, [onesm, ftmp[cc]], [pq])
                        TTOP("pool", rstd_sb[:, :GT], mean_sb[:, :GT], mean_sb[:, :GT], ALU.mult, [mean_sb], [rstd_sb])
                        TTOP("dve", rstd_sb[:, :GT], pq[:, :GT], rstd_sb[:, :GT], ALU.subtract, [pq, rstd_sb], [rstd_sb])
                        TSC("dve", rstd_sb[:, :GT], rstd_sb[:, :GT], EPS, None, ALU.add, None, [rstd_sb], [rstd_sb])
                        ACT(rstd_sb[:, :GT], rstd_sb[:, :GT], AF.Sqrt, [rstd_sb], [rstd_sb])
                        S.op("dve", lambda e: e.reciprocal(out=rstd_sb[:, :GT], in_=rstd_sb[:, :GT]), [rstd_sb], [rstd_sb])
                        for cc in range(2):
                            TTOP("dve", acc[:, cc, :GT], acc[:, cc, :GT], mean_sb[:, :GT], ALU.subtract, [acc, mean_sb], [acc])
                            TTOP("pool", acc[:, cc, :GT], acc[:, cc, :GT], rstd_sb[:, :GT], ALU.mult, [acc, rstd_sb], [acc])
                            ACT(sTb[:, cc, tsl], acc[:, cc, :GT], AF.Silu, [acc, ccols], [sTb],
                                scale=ccols[:, 1, cc:cc + 1], bias=ccols[:, 2, cc:cc + 1])
                        for ti in range(TG):
                            t = g * TG + ti
                            qs = slice(t * 128, (t + 1) * 128)
                            x_ = xt[t % 2]
                            DMA("sp", x_[:], src[row0 + t * 128: row0 + (t + 1) * 128, :], [src], [x_])
                            blocks = []
                            for cb in range(2):
                                blocks.append((kcT, slice(cb * 128, (cb + 1) * 128), vcaug, cb, None))
                            if not is_ctx:
                                if t > 0:
                                    blocks.append((kT, slice((t - 1) * 128, t * 128), vaug, t - 1, 0))
                                blocks.append((kT, qs, vaug, t, None))
                                if t < NT - 1:
                                    blocks.append((kT, slice((t + 1) * 128, (t + 2) * 128), vaug, t + 1, 1))
                            nb = len(blocks)
                            for j in range(2):
                                P_ = PT[j]
                                for lo in range(0, nb, 4):
                                    hi = min(nb, lo + 4)
                                    for gq in range(2):
                                        pr = slice(64 * gq, 64 * gq + 64)
                                        psc_ = pa if gq == 0 else pq
                                        for bi in range(lo, hi):
                                            Ks, ksl, Vt, vi, mi = blocks[bi]
                                            MM(psc_[:, (bi - lo) * 128:(bi - lo + 1) * 128], Ks[j][pr, ksl], qT[j][pr, qs], True, True,
                                               [Ks[j], qT[j]], [psc_])
                                    for gq in range(2):
                                        psc_ = pa if gq == 0 else pq
                                        ACT(P_[:, lo:hi, gq * 128:(gq + 1) * 128],
                                            psc_[:, 0:(hi - lo) * 128].rearrange("p (b q) -> p b q", q=128), AF.Exp, [psc_], [P_], scale=0.125)
                                for bi, (Ks, ksl, Vt, vi, mi) in enumerate(blocks):
                                    if mi is not None:
                                        TTOP("pool", P_[:, bi, :], P_[:, bi, :], masks[:, mi, :], ALU.mult, [P_, masks], [P_])
                                for gq in range(2):
                                    hh = 2 * j + gq
                                    for bi, (Ks, ksl, Vt, vi, mi) in enumerate(blocks):
                                        MM(pb[:, hh * 65:(hh + 1) * 65], P_[:, bi, gq * 128:(gq + 1) * 128], Vt[:, vi, j, :],
                                           bi == 0, bi == nb - 1, [P_, Vt], [pb])
                            pb3 = pb[:, 0:260].rearrange("p (h d) -> p h d", h=4)
                            TTOP("dve", den[:], pb3[:, :, 64], esink[:], ALU.add, [pb, esink], [den])
                            S.op("dve", lambda e: e.reciprocal(out=den[:], in_=den[:]), [den], [den])
                            for hh in range(4):
                                if hh % 2 == 0:
                                    TSC("dve", mix[:, hh * 64:(hh + 1) * 64], pb[:, hh * 65: hh * 65 + 64], den[:, hh:hh + 1], None,
                                        ALU.mult, None, [pb, den], [mix])
                                else:
                                    ACT(mix[:, hh * 64:(hh + 1) * 64], pb[:, hh * 65: hh * 65 + 64], AF.Copy, [pb, den], [mix],
                                        scale=den[:, hh:hh + 1])
                            for cc in range(2):
                                MM(pg[:, 0:256], sTb[:, cc, qs], cpw[:, cc, :], cc == 0, cc == 1, [sTb, cpw], [pg])
                            CP("act", mix[:, 256:512], pg[:, 0:256], [pg], [mix])
                            CP("pool", mix[:, 512:768], gmb[:, t, :], [gmb], [mix])
                            for gi in range(4):
                                srcs = []
                                if t > 0:
                                    srcs.append((t - 1, gi * 5 + 0))
                                srcs.append((t, gi * 5 + (3 if t == 0 else (4 if t == NT - 1 else 2))))
                                if t < NT - 1:
                                    srcs.append((t + 1, gi * 5 + 1))
                                for si, (tt_, mi) in enumerate(srcs):
                                    MM(pq[0:64, gi * 128:(gi + 1) * 128], poolh[:, tt_, gi * 64:(gi + 1) * 64], poolM[:, mi, :],
                                       si == 0, si == len(srcs) - 1, [poolh, poolM], [pq])
                            CP("dve", yTp[:, :, :], pq[0:64, :].rearrange("p (g t) -> p g t", g=4), [pq], [yTp])
                            for gi in range(4):
                                MM(pg[:, 256 + gi * 64: 256 + (gi + 1) * 64], yTp[:, gi, :], pw[:, gi, :], True, True,
                                   [yTp, pw], [pg])
                            TTOP("dve", mix[:, 768:1024], pg[:, 256:512], psc[:], ALU.mult, [pg, psc], [mix])
                            for gi in range(4):
                                ACT(hb[:, 0:256], mix[:, gi * 256:(gi + 1) * 256], AF.Square, [mix], [hb, gss],
                                    accum_out=gss[:, gi:gi + 1])
                            rstd_from(gss, 256, grs)
                            for gi in range(4):
                                if gi % 2 == 0:
                                    TSC("dve", yb[:, gi * 256:(gi + 1) * 256], mix[:, gi * 256:(gi + 1) * 256], grs[:, gi:gi + 1],
                                        None, ALU.mult, None, [mix, grs], [yb])
                                else:
                                    TSC("pool", yb[:, gi * 256:(gi + 1) * 256], mix[:, gi * 256:(gi + 1) * 256], grs[:, gi:gi + 1],
                                        None, ALU.mult, None, [mix, grs], [yb])
                            p_ = pT[t % 2]
                            for c in range(8):
                                TR(p_[:, c * 128:(c + 1) * 128], yb[:, c * 128:(c + 1) * 128], identb[:], [yb, identb], [p_])
                            cp_alt(yT[:, :, :], p_[:, 0:1024].rearrange("p (c t) -> p c t", c=8), [p_], [yT])
                            for nbk in range(2):
                                p_ = pf[nbk]
                                for c in range(8):
                                    MM(p_[:, :], yT[:, c, :], wout[:, c, nbk * 512:(nbk + 1) * 512], c == 0, c == 7, [yT, wout], [p_])
                                TTOP("dve", xn[:, nbk * 512:(nbk + 1) * 512], p_[:, :], G1[:, nbk * 512:(nbk + 1) * 512], ALU.mult,
                                     [p_, G1], [xn])
                            TTOP("pool", xn[:], xn[:], x_[:], ALU.add, [xn, x_], [xn])
                            DMA("sp", dst[row0 + t * 128: row0 + (t + 1) * 128, :], xn[:], [xn], [dst])

                import os
                lim = os.environ.get("MIXLIM", "")
                for s in range(SPC):
                    load_mod(4)
                    seq(csrc, cdst, s * LC, LC, True, not last)
                    if lim == "c":
                        break
                    load_mod(s)
                    seq(xsrc, xdst, s * L, L, False, True)
                    if lim == "cx":
                        break
            S.stack = gst
            S.barrier()

        def phase_moe(l, last, tiles, final):
            NT = len(tiles)
            NSL = NT * 128 * 4 // TS + 32
            with contextlib.ExitStack() as ph:
                S.stack = ph
                idxi = S.sb([128, NT, 4], I32, "idxi")
                gate4 = S.sb([128, NT, 4], F32, "gate4")
                widx = S.sb([128, 128], I32, "widx")
                w2idx = S.sb([128, 128], I32, "w2idx")
                bidx = S.sb([128, 128], I32, "bidx")
                Gd = S.sb([128, NT, 32], F32, "Gd")
                A2 = S.sb([128, D], F32)
                B2 = S.sb([128, D], F32)
                G2 = S.sb([128, D], F32)
                gn2 = S.sb([128, D], F32)
                DMA("sp", gn2[:], norm2_g[l:l + 1, :].partition_broadcast(128), [], [gn2])
                cur = [None]

                def load_mod(r, which):
                    if cur[0] == (r, which):
                        return
                    cur[0] = (r, which)
                    if which == 0:
                        DMA("sp", A2[:], modrows[r:r + 1, 4 * D:5 * D].partition_broadcast(128), [modrows], [A2])
                        DMA("sp", B2[:], modrows[r:r + 1, 3 * D:4 * D].partition_broadcast(128), [modrows], [B2])
                        STT("dve", A2[:], A2[:], 1.0, gn2[:], ALU.add, ALU.mult, [A2, gn2], [A2])
                    else:
                        DMA("sp", G2[:], modrows[r:r + 1, 5 * D:6 * D].partition_broadcast(128), [modrows], [G2])

                with contextlib.ExitStack() as ph1:
                    S.stack = ph1
                    rw = S.sb([128, 8, 32], F32)
                    DMA("sp", rw[:], router_w[l].rearrange("(c p) n -> p c n", p=128), [], [rw])
                    rbb = S.sb([128, 32], F32)
                    DMA("sp", rbb[:], router_b[l:l + 1, :].partition_broadcast(128), [], [rbb])
                    upper = S.sb([128, 128], BF16)
                    DMA("pool", upper[:], k_upper, [], [upper])
                    onesb = S.sb([128, 128], BF16)
                    S.op("dve", lambda e: e.memset(onesb[:], 1.0), [], [onesb])
                    ones32 = S.sb([128, 1], F32)
                    S.op("dve", lambda e: e.memset(ones32[:], 1.0), [], [ones32])
                    thr = S.sb([32, 128], F32)
                    DMA("sp", thr[:], k_slotthr, [], [thr])
                    lg = S.sb([128, NT, 32], F32)
                    t8 = S.sb([128, NT, 8], F32)
                    pos = S.sb([128, NT, 32], F32)
                    base = S.sb([128, 32], F32)
                    S.op("dve", lambda e: e.memset(base[:], 0.0), [], [base])
                    xt = [S.sb([128, D], F32) for _ in range(2)]
                    junk = S.sb([128, D], BF16)
                    ss = S.sb([128, 1], F32)
                    rs = S.sb([128, 1], F32)
                    h32 = S.sb([128, D], F32)
                    hb = [S.sb([128, D], BF16) for _ in range(2)]
                    hT32 = S.sb([128, 8, 128], F32)
                    maskb = S.sb([128, 32], BF16)
                    zt = S.sb([128, 2048], BF16)
                    S.op("pool", lambda e: e.memset(zt[:], 0.0), [], [zt])
                    pT32 = [S.ps([128, 128], F32) for _ in range(2)]
                    pl = S.ps([128, 512])
                    pp = S.ps([128, 512])
                    xz = xsort[0:NSL * TS, :].rearrange("(n p r) d -> n p (r d)", p=128, r=2)
                    for n in range(NSL * TS // 256):
                        DMA("sp", xz[n], zt[:], [zt], [xsort])
                    for t, (src, dst, row0, r) in enumerate(tiles):
                        load_mod(r, 0)
                        x_ = xt[t % 2]
                        DMA("sp", x_[:], src[row0:row0 + 128, :], [src], [x_])
                        ACT(junk[:], x_[:], AF.Square, [x_], [junk, ss], accum_out=ss[:])
                        rstd_from(ss, D, rs)
                        STT("dve", h32[:], x_[:], rs[:, 0:1], A2[:], ALU.mult, ALU.mult, [x_, rs, A2], [h32])
                        TTOP("pool", h32[:], h32[:], B2[:], ALU.add, [h32, B2], [h32])
                        hb_ = hb[t % 2]
                        CP("act", hb_[:], h32[:], [h32], [hb_])
                        DMA("sp", h2d[t * 128:(t + 1) * 128, :], hb_[:], [hb_], [h2d])
                        for hf in range(2):
                            p_ = pT32[hf]
                            for cc in range(4):
                                c = hf * 4 + cc
                                TR(p_[:, cc * 128:(cc + 1) * 128], h32[:, c * 128:(c + 1) * 128], ident32[:], [h32, ident32], [p_])
                            CP("act" if hf else "dve", hT32[:, hf * 4:(hf + 1) * 4, :],
                               p_[:, 0:512].rearrange("p (c t) -> p c t", c=4), [p_], [hT32])
                        for c in range(8):
                            MM(pl[:, 0:32], hT32[:, c, :], rw[:, c, :], c == 0, c == 7, [hT32, rw], [pl])
                        TTOP("dve", lg[:, t, :], pl[:, 0:32], rbb[:], ALU.add, [pl, rbb], [lg])
                        S.op("dve", lambda e: e.max(out=t8[:, t, :], in_=lg[:, t, :]), [lg], [t8])
                        TSC("dve", maskb[:], lg[:, t, :], t8[:, t, 3:4], None, ALU.is_ge, None, [lg, t8], [maskb])
                        MM(pp[:, 0:32], upper[:], maskb[:], True, True, [upper, maskb], [pp])
                        MM(pp[:, 32:64], onesb[:], maskb[:], True, True, [onesb, maskb], [pp])
                        TTOP("dve", pos[:, t, :], pp[:, 0:32], base[:], ALU.add, [pp, base], [pos])
                        TTOP("dve", base[:], pp[:, 32:64], base[:], ALU.add, [pp, base], [base])
                        TSC("dve", ss[:], t8[:, t, 0:1], -1.0, None, ALU.mult, None, [t8], [ss])
                        ACT(gate4[:, t, :], t8[:, t, 0:4], AF.Exp, [t8, ss], [gate4, rs], bias=ss[:, 0:1], accum_out=rs[:, 0:1])
                        S.op("dve", lambda e: e.reciprocal(out=rs[:], in_=rs[:]), [rs], [rs])
                        TSC("dve", gate4[:, t, :], gate4[:, t, :], rs[:, 0:1], None, ALU.mult, None, [gate4, rs], [gate4])
                        ACT(Gd[:, t, :], lg[:, t, :], AF.Exp, [lg, ss], [Gd], bias=ss[:, 0:1])
                        TTOP("dve", Gd[:, t, :], Gd[:, t, :], maskb[:], ALU.mult, [Gd, maskb], [Gd])
                        TSC("dve", Gd[:, t, :], Gd[:, t, :], rs[:, 0:1], None, ALU.mult, None, [Gd, rs], [Gd])
                    padded = S.sb([128, 32], F32)
                    pend = S.sb([128, 32], F32)
                    pstart = S.sb([128, 32], F32)
                    tmpc = S.sb([128, 32], F32)
                    TSC("dve", padded[:], base[:], 0.0, None, ALU.is_gt, None, [base], [padded])
                    for jj in range(1, NT * 128 // TS + 1):
                        STT("dve", padded[:], base[:], float(TS * jj), padded[:], ALU.is_gt, ALU.add, [base, padded], [padded])
                    TSC("dve", padded[:], padded[:], float(TS), None, ALU.mult, None, [padded], [padded])
                    CP("dve", pend[:, 0:1], padded[:, 0:1], [padded], [pend])
                    for e_ in range(1, 32):
                        TTOP("dve", pend[:, e_:e_ + 1], pend[:, e_ - 1:e_], padded[:, e_:e_ + 1], ALU.add, [pend, padded], [pend])
                    TTOP("dve", pstart[:], pend[:], padded[:], ALU.subtract, [pend, padded], [pstart])
                    pcol = S.sb([32, 1], F32)
                    tmpd = S.sb([32, 32], F32)
                    TTOP("dve", tmpd[:], pend[0:32, :], ident32[0:32, 0:32], ALU.mult, [pend, ident32], [tmpd])
                    S.op("dve", lambda e: e.reduce_sum(out=pcol[:], in_=tmpd[:], axis=AX.X), [tmpd], [pcol])
                    cmp = S.sb([32, 128], F32)
                    TSC("dve", cmp[:], thr[:], pcol[:, 0:1], None, ALU.is_ge, None, [thr, pcol], [cmp])
                    ones32m = S.sb([32, 128], F32)
                    S.op("dve", lambda e: e.memset(ones32m[:], 1.0), [], [ones32m])
                    piota = S.sb([128, 1], F32)
                    DMA("sp", piota[:], k_piota, [], [piota])
                    MM(pl[:, 0:128], ones32m[0:32, :], cmp[:], True, True, [ones32m, cmp], [pl])
                    blkf = S.sb([128, 128], F32)
                    TSC("dve", blkf[:], pl[:, 0:128], 31.0, None, ALU.min, None, [pl], [blkf])
                    wif = S.sb([128, 128], F32)
                    same = S.sb([128, 128], F32)
                    S.op("dve", lambda e: e.memset(same[:], 0.0), [], [same])
                    TTOP("dve", same[:, 2:128], blkf[:, 2:128], blkf[:, 0:126], ALU.is_equal, [blkf], [same])
                    TSC("dve", same[:], same[:], SKIPBIG, None, ALU.mult, None, [same], [same])
                    STT("dve", wif[:], blkf[:], 1024.0, same[:], ALU.mult, ALU.add, [blkf, same], [wif])
                    TSC("dve", wif[:], wif[:], piota[:, 0:1], None, ALU.add, None, [wif, piota], [wif])
                    CP("dve", widx[:], wif[:], [wif], [widx])
                    STT("dve", wif[:], blkf[:], 512.0, same[:], ALU.mult, ALU.add, [blkf, same], [wif])
                    TSC("dve", wif[:], wif[:], piota[:, 0:1], None, ALU.add, None, [wif, piota], [wif])
                    CP("dve", w2idx[:], wif[:], [wif], [w2idx])
                    STT("dve", wif[:], blkf[:], 128.0, same[:], ALU.mult, ALU.add, [blkf, same], [wif])
                    TSC("dve", wif[:], wif[:], piota[:, 0:1], None, ALU.add, None, [wif, piota], [wif])
                    CP("dve", bidx[:], wif[:], [wif], [bidx])
                    oh = S.sb([128, 32], F32)
                    idxf = S.sb([128, 4], F32)
                    for t, (src, dst, row0, r) in enumerate(tiles):
                        TTOP("dve", pos[:, t, :], pos[:, t, :], pstart[:], ALU.add, [pos, pstart], [pos])
                        for k in range(4):
                            STT("dve", oh[:], lg[:, t, :], t8[:, t, k:k + 1], pos[:, t, :], ALU.is_equal, ALU.mult,
                                [lg, t8, pos], [oh])
                            S.op("dve", lambda e: e.reduce_sum(out=idxf[:, k:k + 1], in_=oh[:], axis=AX.X), [oh], [idxf])
                        CP("dve", idxi[:, t, :], idxf[:], [idxf], [idxi])
                        hb_ = hb[t % 2]
                        DMA("sp", hb_[:], h2d[t * 128:(t + 1) * 128, :], [h2d], [hb_])
                        for k in range(4):
                            S.dma("pool", lambda e: e.indirect_dma_start(
                                out=xsort[:, :], out_offset=bass.IndirectOffsetOnAxis(ap=idxi[:, t, k:k + 1], axis=0),
                                in_=hb_[:], in_offset=None), [hb_, idxi], [xsort])
                S.stack = ph
                S.barrier()
                if stop_after == ("route", l):
                    return
                with contextlib.ExitStack() as ph2:
                    S.stack = ph2
                    w1b = [S.sb([128, 8, 2 * D], BF16) for _ in range(2)]
                    w2b = [S.sb([128, 8, D], BF16) for _ in range(2)]
                    b1c = [S.sb([128, 16], F32) for _ in range(2)]
                    xs_ = [S.sb([128, 4, D], BF16) for _ in range(2)]
                    xsT2 = [S.sb([128, 8, TS], BF16) for _ in range(2)]
                    actT = S.sb([128, 8, TS], BF16)
                    b1p = [S.sb([128, 8], F32) for _ in range(2)]
                    linp = [S.sb([128, TS], F32) for _ in range(2)]
                    g32s = [S.sb([128, TS], F32) for _ in range(2)]
                    sgs = [S.sb([128, TS], F32) for _ in range(2)]
                    tts = [S.sb([128, TS], F32) for _ in range(2)]
                    yt = [S.sb([128, D], F32) for _ in range(2)]
                    pT = [S.ps([128, 128], BF16) for _ in range(2)]
                    ph_ = [S.ps([128, 512]) for _ in range(4)]
                    py = [S.ps([128, 512]) for _ in range(2)]
                    w1flat = exp_w1.rearrange("l e k n -> (l e k) n")
                    w2flat = exp_w2.rearrange("l e c p n -> (l e c p) n")
                    b1flat = exp_b1T.rearrange("l e p n -> (l e p) n")

                    if not hasattr(build, "_bnd") or build._bnd[0] is not nc:
                        rg_ = nc.gpsimd.alloc_register("bndreg")
                        nc.gpsimd.reg_mov(rg_, 2 * 32 * 1024 - 1)
                        build._bnd = (nc, rg_)
                    bnd_reg = build._bnd[1]

                    def load_w(j):
                        b = j % 2
                        for c in range(8):
                            S.dma("pool", lambda e: e.indirect_dma_start(
                                out=w1b[b][:, c, :], out_offset=None, in_=w1flat,
                                in_offset=bass.IndirectOffsetOnAxis(ap=widx[:, j:j + 1], axis=0),
                                element_offset=(l * 32 * 1024 + c * 128) * 2048, bounds_check=bnd_reg, oob_is_err=False), [widx], [w1b[b]])
                        for c2 in range(4):
                            S.dma("pool", lambda e: e.indirect_dma_start(
                                out=w2b[b][:, 2 * c2:2 * c2 + 2, :].rearrange("p c n -> p (c n)"), out_offset=None, in_=w2flat,
                                in_offset=bass.IndirectOffsetOnAxis(ap=w2idx[:, j:j + 1], axis=0),
                                element_offset=(l * 128 + c2) * 128 * 2048, bounds_check=bnd_reg, oob_is_err=False),
                                [w2idx], [w2b[b]])
                        S.dma("pool", lambda e: e.indirect_dma_start(
                            out=b1c[b][:], out_offset=None, in_=b1flat,
                            in_offset=bass.IndirectOffsetOnAxis(ap=bidx[:, j:j + 1], axis=0),
                            element_offset=l * 32 * 128 * 16, bounds_check=bnd_reg, oob_is_err=False), [bidx], [b1c[b]])

                    def load_x(j):
                        DMA("sp", xs_[j % 2][:], xsort[j * TS:(j + 1) * TS, :].rearrange("(a p) d -> p a d", p=128),
                            [xsort], [xs_[j % 2]])

                    load_w(0)
                    load_x(0)
                    for j in range(NSL):
                        b = j % 2
                        if j + 1 < NSL:
                            load_w(j + 1)
                            load_x(j + 1)
                        W1, W2, B1c, X, xsT = w1b[b], w2b[b], b1c[b], xs_[b], xsT2[b]
                        TSC("dve", b1p[b][:], B1c[:, 8:16], 1.0, None, ALU.add, None, [B1c], [b1p[b]])
                        for a in range(4):
                            p_ = pT[a % 2]
                            for c in range(8):
                                TR(p_[:, c * 128:(c + 1) * 128], X[:, a, c * 128:(c + 1) * 128], identb[:], [X, identb], [p_])
                            CP("act" if a % 2 else "dve", xsT[:, :, a * 128:(a + 1) * 128],
                               p_[:, 0:1024].rearrange("p (c t) -> p c t", c=8), [p_], [xsT])
                        pi = 0
                        for i in range(8):
                            k2 = i % 2
                            pl_ = ph_[pi % 4]
                            pi += 1
                            for c in range(8):
                                MM(pl_[:, :], W1[:, c, (8 + i) * 128:(9 + i) * 128], xsT[:, c, :], c == 0, c == 7, [W1, xsT], [pl_])
                            TSC("dve", linp[k2][:], pl_[:, :], b1p[b][:, i:i + 1], -6.0, ALU.add, ALU.max, [pl_, b1p[b]], [linp[k2]])
                            pg_ = ph_[pi % 4]
                            pi += 1
                            for c in range(8):
                                MM(pg_[:, :], W1[:, c, i * 128:(i + 1) * 128], xsT[:, c, :], c == 0, c == 7, [W1, xsT], [pg_])
                            TSC("dve", g32s[k2][:], pg_[:, :], B1c[:, i:i + 1], 7.0, ALU.add, ALU.min, [pg_, B1c], [g32s[k2]])
                            ACT(sgs[k2][:], g32s[k2][:], AF.Sigmoid, [g32s[k2]], [sgs[k2]], scale=1.702)
                            TTOP("dve", tts[k2][:], sgs[k2][:], g32s[k2][:], ALU.mult, [sgs[k2], g32s[k2]], [tts[k2]])
                            STT("dve", actT[:, i, :], linp[k2][:], 8.0, tts[k2][:], ALU.min, ALU.mult, [linp[k2], tts[k2]], [actT])
                        for a in range(4):
                            y_ = yt[a % 2]
                            for nbk in range(2):
                                p_ = py[nbk]
                                for f in range(8):
                                    MM(p_[:, :], actT[:, f, a * 128:(a + 1) * 128], W2[:, f, nbk * 512:(nbk + 1) * 512],
                                       f == 0, f == 7, [actT, W2], [p_])
                                CP("act", y_[:, nbk * 512:(nbk + 1) * 512], p_[:, :], [p_], [y_])
                            DMA("sp", ysort[j * TS + a * 128: j * TS + (a + 1) * 128, :], y_[:], [y_], [ysort])
                S.stack = ph
                S.barrier()
                with contextlib.ExitStack() as ph3:
                    S.stack = ph3
                    yk = [[S.sb([128, D], F32) for _ in range(4)] for _ in range(2)]
                    xt = [S.sb([128, D], F32) for _ in range(2)]
                    acc = S.sb([128, D], F32)
                    xn = [S.sb([128, D], F32) for _ in range(2)]
                    junk = S.sb([128, D], BF16)
                    ss = S.sb([128, 1], F32)
                    rs = S.sb([128, 1], F32)
                    fng = S.sb([128, D], F32)
                    DMA("sp", fng[:], final_g[0:1, :].partition_broadcast(128), [], [fng])
                    b2all = S.sb([32, D], F32)
                    DMA("sp", b2all[:], exp_b2[l], [], [b2all])
                    GTs = S.sb([32, 128], F32)
                    pGT = S.ps([128, 512])
                    pbias = [S.ps([128, 512]) for _ in range(2)]
                    for t, (src, dst, row0, r) in enumerate(tiles):
                        load_mod(r, 1)
                        x_ = xt[t % 2]
                        Y = yk[t % 2]
                        xo = xn[t % 2]
                        DMA("sp", x_[:], src[row0:row0 + 128, :], [src], [x_])
                        for k in range(4):
                            S.dma("pool", lambda e: e.indirect_dma_start(
                                out=Y[k][:], out_offset=None, in_=ysort[:, :],
                                in_offset=bass.IndirectOffsetOnAxis(ap=idxi[:, t, k:k + 1], axis=0)), [ysort, idxi], [Y[k]])
                        TR(pGT[0:32, 0:128], Gd[:, t, :], ident32[:], [Gd, ident32], [pGT])
                        CP("act", GTs[:], pGT[0:32, 0:128], [pGT], [GTs])
                        for nbk in range(2):
                            MM(pbias[nbk][:, :], GTs[0:32, :], b2all[0:32, nbk * 512:(nbk + 1) * 512], True, True,
                               [GTs, b2all], [pbias[nbk]])
                        TSC("dve", acc[:], Y[0][:], gate4[:, t, 0:1], None, ALU.mult, None, [Y[0], gate4], [acc])
                        for k in range(1, 4):
                            STT("dve" if k != 2 else "pool", acc[:], Y[k][:], gate4[:, t, k:k + 1], acc[:], ALU.mult, ALU.add,
                                [Y[k], gate4, acc], [acc])
                        for nbk in range(2):
                            TTOP("dve", acc[:, nbk * 512:(nbk + 1) * 512], acc[:, nbk * 512:(nbk + 1) * 512], pbias[nbk][:, :], ALU.add,
                                 [acc, pbias[nbk]], [acc])
                        TTOP("pool", acc[:], acc[:], G2[:], ALU.mult, [acc, G2], [acc])
                        TTOP("dve", xo[:], acc[:], x_[:], ALU.add, [acc, x_], [xo])
                        if final:
                            ACT(junk[:], xo[:], AF.Square, [xo], [junk, ss], accum_out=ss[:])
                            rstd_from(ss, D, rs)
                            STT("dve", xo[:], xo[:], rs[:, 0:1], fng[:], ALU.mult, ALU.mult, [xo, rs, fng], [xo])
                        DMA("sp", dst[row0:row0 + 128, :], xo[:], [xo], [dst])
            S.stack = gst
            S.barrier()

        def tiles_for(xsrc, xdst, csrc, cdst, with_ctx):
            tl = []
            for s in range(SPC):
                for t in range(L // 128):
                    tl.append((xsrc, xdst, s * L + t * 128, s))
            if with_ctx:
                for s in range(SPC):
                    for t in range(LC // 128):
                        tl.append((csrc, cdst, s * LC + t * 128, 4))
            return tl

        done = False
        for l in range(2):
            last = l == 1
            phase_ada(l)
            if stop_after == ("ada", 0):
                break
            if l == 0:
                phase_mix(0, False, x_in, xs1, c_in, cs1)
                if stop_after == ("mix", 0):
                    break
                phase_moe(0, False, tiles_for(xs1, xs2, cs1, cs2, True), False)
                if stop_after in (("route", 0), ("moe", 0)):
                    break
            else:
                phase_mix(1, True, xs2, xs1, cs2, None)
                phase_moe(1, True, tiles_for(xs1, out_t, None, None, False), True)
        if stop_after is not None:
            S.barrier()
            if stop_after[0] == "ada":
                DMA("sp", out_t[0:30, :].rearrange("(r k) d -> r (k d)", r=5), modrows[:, :], [modrows], [out_t])
            else:
                srcd = {"mix": xs1, "moe": xs2, "route": xs1}[stop_after[0]]
                import os
                for i_ in range((L if os.environ.get("MIXLIM") else SPC * L) // 128):
                    DMA("sp", out_t[i_ * 128:(i_ + 1) * 128, :], srcd[i_ * 128:(i_ + 1) * 128, :], [srcd], [out_t])
        S.finish()
        build.stats = (S.n_inst, S.n_wait)
    return nc


def _prep_shared(inp):
    f = lambda a: np.ascontiguousarray(np.asarray(a, dtype=np.float32))
    cols = _ext_cols()
    sh = {}
    sh["ada_w"] = f(inp["ada_w"])
    sh["ada_b"] = f(inp["ada_b"])
    sh["norm1_g"] = f(inp["norm1_g"])
    sh["norm2_g"] = f(inp["norm2_g"])
    sh["w_in_ext"] = f(np.asarray(inp["w_in"])[:, :, cols])
    sh["attn_sink"] = f(inp["attn_sink"])
    dw = np.asarray(inp["conv_dw_w"])
    sh["conv_dwT"] = f(dw.transpose(0, 2, 1).reshape(2, 2, 128, 31).transpose(0, 2, 1, 3))
    cc = np.stack([np.asarray(inp["conv_dw_b"]), np.asarray(inp["conv_ln_g"]), np.asarray(inp["conv_ln_b"])], 1)
    sh["convcols"] = f(cc.reshape(2, 3, 2, 128).transpose(0, 3, 1, 2))
    sh["conv_pw_w"] = f(inp["conv_pw_w"])
    sh["gmlp_ln_g"] = f(inp["gmlp_ln_g"])
    sh["gmlp_ln_b"] = f(inp["gmlp_ln_b"])
    sh["gmlp_wsT"] = f(np.asarray(inp["gmlp_ws"]).transpose(0, 1, 3, 2))
    sh["gmlp_bsT"] = f(np.asarray(inp["gmlp_bs"]).transpose(0, 2, 1))
    sh["pool_w"] = f(inp["pool_w"])
    sh["pool_scale"] = f(inp["pool_scale"])
    sh["gncol"] = f(np.asarray(inp["group_norm_g"]).reshape(2, 8, 128).transpose(0, 2, 1))
    sh["w_out"] = f(inp["w_out"])
    sh["router_w"] = f(inp["router_w"])
    sh["router_b"] = f(inp["router_b"])
    sh["exp_w1"] = f(inp["exp_w1"])
    sh["exp_b1T"] = f(np.asarray(inp["exp_b1"]).reshape(2, 32, 16, 128).transpose(0, 1, 3, 2))
    sh["exp_w2r"] = f(np.asarray(inp["exp_w2"]).reshape(2, 32, 4, 2, 128, D).transpose(0, 1, 2, 4, 3, 5).reshape(2, 32, 4, 128, 2 * D))
    sh["exp_b2"] = f(inp["exp_b2"])
    sh["final_norm_g"] = f(np.asarray(inp["final_norm_g"]).reshape(1, D))
    for k, v in _consts().items():
        sh["k_" + k] = f(v)
    return sh


def _in_maps(inp):
    sh = _prep_shared(inp)
    x = np.asarray(inp["x"], dtype=np.float32)
    c = np.asarray(inp["c"], dtype=np.float32)
    ctx = np.asarray(inp["ctx"], dtype=np.float32)
    cctx = np.asarray(inp["c_ctx"], dtype=np.float32)
    maps = []
    for i in range(NCORE):
        m = dict(sh)
        m["x"] = np.ascontiguousarray(x[i * SPC:(i + 1) * SPC].reshape(SPC * L, D))
        m["ctx"] = np.ascontiguousarray(ctx[i * SPC:(i + 1) * SPC].reshape(SPC * LC, D))
        c5 = np.concatenate([c[i * SPC:(i + 1) * SPC], cctx[None, :]], 0)
        m["cT"] = np.ascontiguousarray(c5.T.reshape(8, 128, 5).transpose(1, 0, 2))
        maps.append(m)
    return maps


def kernel(**inputs):
    nc = build()
    maps = _in_maps(inputs)
    res = run_bass_kernel_spmd(nc, maps, core_ids=list(range(NCORE)))
    outs = [np.asarray(r["out"]).reshape(SPC, L, D) for r in res.results]
    return np.concatenate(outs, 0).astype(np.float32)
```

```python
import contextlib
import numpy as np
import concourse.bass as bass
import concourse.mybir as mybir
from concourse.bass_utils import run_bass_kernel_spmd

F32 = mybir.dt.float32
BF16 = mybir.dt.bfloat16
I32 = mybir.dt.int32
AF = mybir.ActivationFunctionType
ALU = mybir.AluOpType
AX = mybir.AxisListType
POOL_ENG = mybir.EngineType.Pool

EPOCH = 16000
NSLOT = 8
NCORE = 8
SPC = 4
D = 1024
L = 2048
LC = 256
INW = 2432
EPS = 1e-6
TS = 512
import os as _os
SKIPBIG = float(_os.environ.get("SKIPBIG", "1.0e6"))


class Res:
    __slots__ = ("w", "r")

    def __init__(self):
        self.w = {}
        self.r = {}


class TT:
    __slots__ = ("t", "res", "excl")

    def __init__(self, t, excl=False):
        self.t = t
        self.res = Res()
        self.excl = excl

    def __getitem__(self, k):
        return self.t[k]


def _res(b):
    return b.res if isinstance(b, TT) else b


class Sched:
    def __init__(self, nc, stack):
        self.nc = nc
        self.stack = stack
        self.gstack = stack
        self.E = {}
        for name, e in (("pe", nc.tensor), ("act", nc.scalar), ("dve", nc.vector),
                        ("pool", nc.gpsimd), ("sp", nc.sync)):
            self.E[name] = dict(e=e, name=name, sems=[], cnt=0, waited={}, slots=None, slot_i=0)
        self.n_inst = 0
        self.n_wait = 0
        self.uid = 0

    def sem(self, name):
        return self.gstack.enter_context(self.nc.semaphore(name))

    def sb(self, shape, dt, name=None):
        self.uid += 1
        return TT(self.stack.enter_context(self.nc.sbuf_tensor(f"{name or 't'}_{self.uid}", list(shape), dt)))

    def ps(self, shape, dt=F32, name=None):
        self.uid += 1
        shape = [128, 512 if dt == F32 else 1024]
        return TT(excl=True, t=self.stack.enter_context(self.nc.psum_tensor(f"{name or 'p'}_{self.uid}", list(shape), dt)))

    def _wait(self, E, sem, val):
        key = id(sem)
        if E["waited"].get(key, 0) >= val:
            return
        E["waited"][key] = val
        E["e"].wait_ge(sem, val)
        self.n_wait += 1

    def _deps(self, E, reads, writes):
        deps = {}
        for b in reads:
            for k, v in _res(b).w.items():
                if k not in deps or deps[k][1] < v[1]:
                    deps[k] = v
        for b in writes:
            r = _res(b)
            for dd in (r.w, r.r):
                for k, v in dd.items():
                    if k not in deps or deps[k][1] < v[1]:
                        deps[k] = v
        own = E["sems"]
        for k, (sem, val) in deps.items():
            if E["name"] == "pe" and any(sem is s for s in own):
                continue
            self._wait(E, sem, val)

    def _record(self, tok, reads, writes):
        k = id(tok[0])
        for b in reads:
            _res(b).r[k] = tok
        for b in writes:
            r = _res(b)
            r.w = {k: tok}
            r.r = {}

    def _next_tok(self, E):
        n = E["cnt"]
        ep = n // EPOCH
        while len(E["sems"]) <= ep:
            E["sems"].append(self.sem(f"c_{E['name']}_{len(E['sems'])}"))
        E["cnt"] = n + 1
        return (E["sems"][ep], n % EPOCH + 1)

    def op(self, eng, fn, reads=(), writes=()):
        E = self.E[eng]
        ex = [b for b in reads if isinstance(b, TT) and b.excl]
        if ex:
            reads = [b for b in reads if not (isinstance(b, TT) and b.excl)]
            writes = list(writes) + ex
        self._deps(E, reads, writes)
        inst = fn(E["e"])
        tok = self._next_tok(E)
        inst.then_inc(tok[0], 1)
        self._record(tok, reads, writes)
        self.n_inst += 1

    def wait_for(self, eng, reads=(), writes=()):
        self._deps(self.E[eng], reads, writes)

    def dma(self, q, fn, reads=(), writes=()):
        E = self.E[q]
        if E["slots"] is None:
            E["slots"] = [[self.sem(f"d_{q}_{i}"), 0] for i in range(NSLOT)]
        self._deps(E, reads, writes)
        sl = E["slots"][E["slot_i"] % NSLOT]
        E["slot_i"] += 1
        if sl[1] > 0:
            self._wait(E, sl[0], sl[1])
        inst = fn(E["e"])
        sl[1] += 16
        inst.then_inc(sl[0], 16)
        self._record((sl[0], sl[1]), reads, writes)
        self.n_inst += 1

    def _all_toks(self):
        toks = []
        for E in self.E.values():
            if E["slots"]:
                for s, v in E["slots"]:
                    if v:
                        toks.append((s, v))
            n = E["cnt"]
            if n:
                toks.append((E["sems"][(n - 1) // EPOCH], (n - 1) % EPOCH + 1))
        return toks

    def barrier(self):
        toks = self._all_toks()
        for E in self.E.values():
            for s, v in toks:
                self._wait(E, s, v)

    def finish(self):
        self.barrier()


def _consts():
    c = {}
    c["ident"] = np.eye(128, dtype=np.float32)
    tq = np.arange(128)
    c["upper"] = (tq[:, None] < tq[None, :]).astype(np.float32)
    kj = np.arange(128)[:, None]
    qi = np.arange(128)[None, :]
    mL = (qi <= kj).astype(np.float32)
    mR = (kj <= qi).astype(np.float32)
    c["masks"] = np.stack([np.concatenate([mL, mL], 1), np.concatenate([mR, mR], 1)], 1)
    t = np.arange(L)
    row = (t // 64).astype(np.float32)
    col = (t % 64).astype(np.float32)
    inv = (10000.0 ** (-np.arange(0, 32, 2, dtype=np.float32) / 32)).astype(np.float32)
    ang_r = row[:, None] * inv[None, :]
    ang_c = col[:, None] * inv[None, :]
    cs = np.zeros((64, L), np.float32)
    sn = np.zeros((64, L), np.float32)
    for d in range(64):
        ang = ang_r if d < 32 else ang_c
        i = d % 16
        cs[d] = np.cos(ang[:, i])
        sn[d] = np.sin(ang[:, i]) * (-1.0 if (d % 32) < 16 else 1.0)
    c["ropeC"] = np.concatenate([cs, cs], 0)
    c["ropeS"] = np.concatenate([sn, sn], 0)
    PM = np.zeros((128, 20, 128), np.float32)
    LL = 512
    for gi, w in enumerate((2, 4, 8, 16)):
        M = np.zeros((LL, LL), np.float32)
        for tt in range(LL):
            lo = max(tt - w // 2, 0)
            hi = min(tt + w // 2, LL)
            M[lo:hi, tt] = 1.0 / (hi - lo)
            M[tt, tt] -= 1.0
        PM[:, gi * 5 + 0] = M[0:128, 128:256]
        PM[:, gi * 5 + 1] = M[256:384, 128:256]
        PM[:, gi * 5 + 2] = M[128:256, 128:256]
        PM[:, gi * 5 + 3] = M[0:128, 0:128]
        PM[:, gi * 5 + 4] = M[384:512, 384:512]
    c["poolM"] = PM
    c["slotthr"] = np.tile((np.arange(128, dtype=np.float32) * TS)[None, :], (32, 1))
    c["piota"] = np.arange(128, dtype=np.float32)[:, None]
    return c


def _ext_cols():
    def sw(off, n_heads):
        idx = []
        for h in range(n_heads):
            for d in range(64):
                sd = d + 16 if (d % 32) < 16 else d - 16
                idx.append(off + h * 64 + sd)
        return idx
    q = list(range(0, 256))
    qs = sw(0, 4)
    k0 = list(range(256, 320))
    k1 = list(range(320, 384))
    ks = sw(256, 2)
    k0s, k1s = ks[:64], ks[64:]
    cols = q + qs + k0 + k0 + k1 + k1 + k0s + k0s + k1s + k1s
    cols += list(range(512, 1024))
    cols += list(range(1024, 1536))
    cols += list(range(384, 512))
    cols += list(range(1536, 1792))
    assert len(cols) == INW
    return np.array(cols)


def build(stop_after=None):
    nc = bass.Bass("TRN2", target_bir_lowering=False)

    def din(name, shape, dt=F32):
        return nc.dram_tensor(name, list(shape), dt, kind="ExternalInput").ap()

    def dscr(name, shape, dt=F32):
        return TT(nc.dram_tensor(name, list(shape), dt, kind="Internal").ap())

    x_in = TT(din("x", [SPC * L, D]))
    c_in = TT(din("ctx", [SPC * LC, D]))
    cT_in = din("cT", [128, 8, 5])
    ada_w = din("ada_w", [2, D, 6 * D])
    ada_b = din("ada_b", [2, 6 * D])
    norm1_g = din("norm1_g", [2, D])
    norm2_g = din("norm2_g", [2, D])
    w_in = din("w_in_ext", [2, D, INW])
    attn_sink = din("attn_sink", [2, 4])
    dwT = din("conv_dwT", [2, 128, 2, 31])
    convcols = din("convcols", [2, 128, 3, 2])
    conv_pw = din("conv_pw_w", [2, 256, 256])
    gln_g = din("gmlp_ln_g", [2, 256])
    gln_b = din("gmlp_ln_b", [2, 256])
    gwsT = din("gmlp_wsT", [2, 4, 128, 128])
    gbsT = din("gmlp_bsT", [2, 128, 4])
    pool_w = din("pool_w", [2, 4, 64, 64])
    pool_scale = din("pool_scale", [2, 256])
    gncol = din("gncol", [2, 128, 8])
    w_out = din("w_out", [2, D, D])
    router_w = din("router_w", [2, D, 32])
    router_b = din("router_b", [2, 32])
    exp_w1 = din("exp_w1", [2, 32, D, 2 * D])
    exp_b1T = din("exp_b1T", [2, 32, 128, 16])
    exp_w2 = din("exp_w2r", [2, 32, 4, 128, 2 * D])
    exp_b2 = din("exp_b2", [2, 32, D])
    final_g = din("final_norm_g", [1, D])
    k_ident = din("k_ident", [128, 128])
    k_upper = din("k_upper", [128, 128])
    k_masks = din("k_masks", [128, 2, 256])
    k_ropeC = din("k_ropeC", [128, L])
    k_ropeS = din("k_ropeS", [128, L])
    k_poolM = din("k_poolM", [128, 20, 128])
    k_slotthr = din("k_slotthr", [32, 128])
    k_piota = din("k_piota", [128, 1])
    out_t = TT(nc.dram_tensor("out", [SPC * L, D], F32, kind="ExternalOutput").ap())

    xs1 = dscr("xs1", [SPC * L, D])
    xs2 = dscr("xs2", [SPC * L, D])
    cs1 = dscr("cs1", [SPC * LC, D])
    cs2 = dscr("cs2", [SPC * LC, D])
    modrows = dscr("modrows", [5, 6 * D])
    NTMAX = (SPC * (L + LC)) // 128
    NSLMAX = NTMAX * 128 * 4 // TS + 32
    h2d = dscr("h2d", [NTMAX * 128, D], BF16)
    xsort = dscr("xsort", [NSLMAX * TS, D], BF16)
    ysort = dscr("ysort", [NSLMAX * TS, D], F32)

    with contextlib.ExitStack() as gst:
        S = Sched(nc, gst)

        def ACT(out, in_, func, reads, writes, **kw):
            S.op("act", lambda e: e.activation(out=out, in_=in_, func=func, **kw), reads, writes)

        def MM(out, lhsT, rhs, start, stop, reads, writes):
            S.op("pe", lambda e: e.matmul(out, lhsT=lhsT, rhs=rhs, start=start, stop=stop), reads, writes)

        def TR(out, in_, ident, reads, writes):
            S.op("pe", lambda e: e.transpose(out=out, in_=in_, identity=ident), reads, writes)

        def TTOP(eng, out, in0, in1, op, reads, writes):
            S.op(eng, lambda e: e.tensor_tensor(out=out, in0=in0, in1=in1, op=op), reads, writes)

        def TSC(eng, out, in0, s1, s2, op0, op1, reads, writes):
            if op1 is None:
                S.op(eng, lambda e: e.tensor_scalar(out=out, in0=in0, scalar1=s1, scalar2=None, op0=op0), reads, writes)
            else:
                S.op(eng, lambda e: e.tensor_scalar(out=out, in0=in0, scalar1=s1, scalar2=s2, op0=op0, op1=op1),
                     reads, writes)

        def STT(eng, out, in0, scalar, in1, op0, op1, reads, writes):
            eng = "dve"
            S.op(eng, lambda e: e.scalar_tensor_tensor(out=out, in0=in0, scalar=scalar, in1=in1, op0=op0, op1=op1),
                 reads, writes)

        def CP(eng, out, in_, reads, writes):
            if eng == "act":
                S.op("act", lambda e: e.copy(out=out, in_=in_), reads, writes)
            else:
                S.op(eng, lambda e: e.tensor_copy(out=out, in_=in_), reads, writes)

        def DMA(q, out, in_, reads, writes):
            S.dma(q, lambda e: e.dma_start(out=out, in_=in_), reads, writes)

        def rstd_from(ssum, n, tmp):
            TSC("dve", tmp[:], ssum[:], 1.0 / n, EPS, ALU.mult, ALU.add, [ssum], [tmp])
            ACT(tmp[:], tmp[:], AF.Sqrt, [tmp], [tmp])
            S.op("dve", lambda e: e.reciprocal(out=tmp[:], in_=tmp[:]), [tmp], [tmp])

        ident32 = S.sb([128, 128], F32, "ident32")
        identb = S.sb([128, 128], BF16, "identb")
        DMA("sp", ident32[:], k_ident, [], [ident32])
        DMA("pool", identb[:], k_ident, [], [identb])

        def phase_ada(l):
            with contextlib.ExitStack() as ph:
                S.stack = ph
                cT = S.sb([128, 8, 5], F32)
                sc = S.sb([128, 8, 5], F32)
                DMA("sp", cT[:], cT_in, [], [cT])
                ACT(sc[:], cT[:], AF.Silu, [cT], [sc])
                rows = S.sb([5, 6 * D], F32)
                bb = S.sb([5, 6 * D], F32)
                DMA("sp", bb[:], ada_b[l:l + 1, :].partition_broadcast(5), [], [bb])
                wts = [S.sb([128, 8, 512], F32) for _ in range(2)]
                pms = [S.ps([128, 512]) for _ in range(2)]
                for n in range(12):
                    wt = wts[n % 2]
                    pm = pms[n % 2]
                    DMA("sp", wt[:], ada_w[l, :, n * 512:(n + 1) * 512].rearrange("(c p) n -> p c n", p=128), [], [wt])
                    for c in range(8):
                        MM(pm[0:5, :], sc[:, c, :], wt[:, c, :], c == 0, c == 7, [sc, wt], [pm])
                    TTOP("dve", rows[:, n * 512:(n + 1) * 512], pm[0:5, :], bb[:, n * 512:(n + 1) * 512], ALU.add,
                         [pm, bb], [rows])
                DMA("sp", modrows[:, :], rows[:], [rows], [modrows])
            S.stack = gst
            S.barrier()

        def phase_mix(l, last, xsrc, xdst, csrc, cdst):
            with contextlib.ExitStack() as ph:
                S.stack = ph
                win = S.sb([128, 8, INW], BF16, "win")
                for c in range(8):
                    for hf in range(2):
                        DMA("pool", win[:, c, hf * 1216:(hf + 1) * 1216], w_in[l, c * 128:(c + 1) * 128, hf * 1216:(hf + 1) * 1216],
                            [], [win])
                wout = S.sb([128, 8, D], BF16, "wout")
                gnc = S.sb([128, 8], F32)
                DMA("sp", gnc[:], gncol[l], [], [gnc])
                dg = S.sb([128, 2, 31, 128], BF16)
                dw = S.sb([128, 2, 31], F32)
                DMA("sp", dw[:], dwT[l], [], [dw])
                for cc_ in range(2):
                    for j_ in range(31):
                        TSC("pool" if j_ % 2 else "dve", dg[:, cc_, j_, :], identb[:], dw[:, cc_, j_:j_ + 1], None, ALU.mult, None,
                            [identb, dw], [dg])
                ccols = S.sb([128, 3, 2], F32)
                DMA("sp", ccols[:], convcols[l], [], [ccols])
                cpw = S.sb([128, 2, 256], BF16)
                DMA("pool", cpw[:], conv_pw[l].rearrange("(c p) n -> p c n", p=128), [], [cpw])
                wsT = S.sb([128, 4, 128], BF16)
                DMA("pool", wsT[:], gwsT[l].rearrange("h q p -> q h p"), [], [wsT])
                bsT = S.sb([128, 4], F32)
                DMA("sp", bsT[:], gbsT[l], [], [bsT])
                glg = S.sb([128, 256], F32)
                glb = S.sb([128, 256], F32)
                DMA("sp", glg[:], gln_g[l:l + 1, :].partition_broadcast(128), [], [glg])
                DMA("sp", glb[:], gln_b[l:l + 1, :].partition_broadcast(128), [], [glb])
                pw = S.sb([64, 4, 64], BF16)
                DMA("pool", pw[:], pool_w[l].rearrange("g c j -> c g j"), [], [pw])
                psc = S.sb([128, 256], F32)
                DMA("sp", psc[:], pool_scale[l:l + 1, :].partition_broadcast(128), [], [psc])
                poolM = S.sb([128, 20, 128], BF16)
                for hf in range(2):
                    DMA("pool", poolM[:, hf * 10:(hf + 1) * 10, :], k_poolM[:, hf * 10:(hf + 1) * 10, :], [], [poolM])
                masks = S.sb([128, 2, 256], BF16)
                DMA("pool", masks[:], k_masks, [], [masks])
                esink = S.sb([128, 4], F32)
                DMA("sp", esink[:], attn_sink[l:l + 1, :].partition_broadcast(128), [], [esink])
                ACT(esink[:], esink[:], AF.Exp, [esink], [esink])
                ropeC = S.sb([128, 512], F32)
                ropeS = S.sb([128, 512], F32)
                gn1 = S.sb([128, D], F32)
                DMA("sp", gn1[:], norm1_g[l:l + 1, :].partition_broadcast(128), [], [gn1])
                onesm = S.sb([128, 128], F32)
                S.op("dve", lambda e: e.memset(onesm[:], 1.0 / 256.0), [], [onesm])
                A1 = S.sb([128, D], F32)
                B1 = S.sb([128, D], F32)
                G1 = S.sb([128, D], F32)

                qT = [S.sb([128, L], BF16) for _ in range(2)]
                kT = [S.sb([128, L], BF16) for _ in range(2)]
                kcT = [S.sb([128, LC], BF16) for _ in range(2)]
                vaug = S.sb([128, 16, 2, 65], BF16)
                vcaug = S.sb([128, 2, 2, 65], BF16)
                S.op("pool", lambda e: e.memset(vaug[:], 1.0), [], [vaug])
                S.op("pool", lambda e: e.memset(vcaug[:], 1.0), [], [vcaug])
                convT = S.sb([128, 2, L + 30], BF16)
                S.op("pool", lambda e: e.memset(convT[:], 0.0), [], [convT])
                sTb = S.sb([128, 2, L], BF16)
                gmb = S.sb([128, 16, 256], BF16)
                poolh = S.sb([128, 16, 256], BF16)
                xt1_ = S.sb([128, D], F32)
                xt = [xt1_, xt1_]
                ss = S.sb([128, 1], F32)
                rs = S.sb([128, 1], F32)
                h32 = S.sb([128, D], F32)
                hb = S.sb([128, D], BF16)
                hT = S.sb([128, 8, 512], BF16)
                ftmp = [S.sb([128, 512], F32) for _ in range(3)]
                u32 = S.sb([128, 256], F32)
                v32 = S.sb([128, 256], F32)
                vnb = S.sb([128, 256], BF16)
                st2 = S.sb([128, 2], F32)
                st3 = S.sb([128, 2], F32)
                acc = S.sb([128, 2, 512], F32)
                mean_sb = S.sb([128, 512], F32)
                rstd_sb = S.sb([128, 512], F32)
                PT = [S.sb([128, 5, 256], BF16) for _ in range(2)]
                den = S.sb([128, 4], F32)
                mix = S.sb([128, D], F32)
                gss = S.sb([128, 4], F32)
                grs = S.sb([128, 4], F32)
                yb = S.sb([128, D], BF16)
                yT = S.sb([128, 8, 128], BF16)
                yTp = S.sb([64, 4, 128], BF16)
                xn = h32
                wtmp = [h32, mix]
                for c in range(8):
                    DMA("sp", wtmp[c % 2][:], w_out[l, c * 128:(c + 1) * 128, :], [], [wtmp[c % 2]])
                    TSC("dve", wout[:, c, :], wtmp[c % 2][:], gnc[:, c:c + 1], None, ALU.mult, None,
                        [wtmp[c % 2], gnc], [wout])
                pT = [S.ps([128, 128], BF16) for _ in range(2)]
                pf = [S.ps([128, 512]) for _ in range(2)]
                pa = S.ps([128, 512])
                pb = S.ps([128, 512])
                pg = S.ps([128, 512])
                pq = S.ps([128, 512])

                def load_mod(r):
                    DMA("sp", A1[:], modrows[r:r + 1, D:2 * D].partition_broadcast(128), [modrows], [A1])
                    DMA("sp", B1[:], modrows[r:r + 1, 0:D].partition_broadcast(128), [modrows], [B1])
                    DMA("sp", G1[:], modrows[r:r + 1, 2 * D:3 * D].partition_broadcast(128), [modrows], [G1])
                    STT("dve", A1[:], A1[:], 1.0, gn1[:], ALU.add, ALU.mult, [A1, gn1], [A1])

                cp_i = [0]

                def cp_alt(out, in_, reads, writes):
                    cp_i[0] += 1
                    CP("act" if cp_i[0] % 2 else "dve", out, in_, reads, writes)

                def seq(src, dst, row0, LL, is_ctx, do_pass2):
                    NT = LL // 128
                    GT = min(512, LL)
                    TG = GT // 128
                    KT = kcT if is_ctx else kT
                    VA = vcaug if is_ctx else vaug
                    for g in range(LL // GT):
                        for ti in range(TG):
                            t = g * TG + ti
                            x_ = xt[t % 2]
                            DMA("sp", x_[:], src[row0 + t * 128: row0 + (t + 1) * 128, :], [src], [x_])
                            ACT(yb[:], x_[:], AF.Square, [x_], [yb, ss], accum_out=ss[:])
                            rstd_from(ss, D, rs)
                            STT("dve", h32[:], x_[:], rs[:, 0:1], A1[:], ALU.mult, ALU.mult, [x_, rs, A1], [h32])
                            TTOP("dve", hb[:], h32[:], B1[:], ALU.add, [h32, B1], [hb])
                            p_ = pT[t % 2]
                            for c in range(8):
                                TR(p_[:, c * 128:(c + 1) * 128], hb[:, c * 128:(c + 1) * 128], identb[:], [hb, identb], [p_])
                            cp_alt(hT[:, :, ti * 128:(ti + 1) * 128], p_[:, 0:1024].rearrange("p (c t) -> p c t", c=8), [p_], [hT])
                        if not is_ctx:
                            DMA("sp", ropeC[:, :GT], k_ropeC[:, g * GT:(g + 1) * GT], [], [ropeC])
                            DMA("sp", ropeS[:, :GT], k_ropeS[:, g * GT:(g + 1) * GT], [], [ropeS])
                        order = [0, 2, 1, 3, 4, 6, 5, 7, 10, 8, 11, 9] if not is_ctx else [0, 1, 4, 5, 10, 8, 11, 9]
                        tsl = slice(g * GT, (g + 1) * GT)
                        for oi, fc in enumerate(order):
                            p_ = pf[oi % 2]
                            for c in range(8):
                                MM(p_[:, :GT], win[:, c, fc * 128:(fc + 1) * 128], hT[:, c, :GT], c == 0, c == 7,
                                   [win, hT], [p_])
                            if fc in (0, 1, 4, 5):
                                dstT = (qT if not is_ctx else qT)[fc] if fc < 2 else KT[fc - 4]
                                if is_ctx:
                                    cp_alt(dstT[:, tsl], p_[:, :GT], [p_], [dstT])
                                else:
                                    TTOP("dve", ftmp[0][:, :GT], p_[:, :GT], ropeC[:, :GT], ALU.mult, [p_, ropeC], [ftmp[0]])
                            elif fc in (2, 3, 6, 7):
                                dstT = qT[fc - 2] if fc < 4 else KT[fc - 6]
                                TTOP("dve", ftmp[1][:, :GT], p_[:, :GT], ropeS[:, :GT], ALU.mult, [p_, ropeS], [ftmp[1]])
                                TTOP("dve", dstT[:, tsl], ftmp[0][:, :GT], ftmp[1][:, :GT], ALU.add,
                                     [ftmp[0], ftmp[1]], [dstT])
                            elif fc in (10, 11):
                                ACT(ftmp[2][:, :GT], p_[:, :GT], AF.Sigmoid, [p_], [ftmp[2]])
                            else:
                                cc = fc - 8
                                TTOP("dve", convT[:, cc, 15 + g * GT: 15 + (g + 1) * GT], p_[:, :GT], ftmp[2][:, :GT],
                                     ALU.mult, [p_, ftmp[2]], [convT])
                        for ti in range(TG):
                            t = g * TG + ti
                            for c in range(8):
                                MM(pa[:, :], hT[:, c, ti * 128:(ti + 1) * 128], win[:, c, 1536:2048], c == 0, c == 7,
                                   [hT, win], [pa])
                            for c in range(8):
                                MM(pb[:, 0:384], hT[:, c, ti * 128:(ti + 1) * 128], win[:, c, 2048:2432], c == 0, c == 7,
                                   [hT, win], [pb])
                            S.op("act", lambda e: e.copy(out=VA[:, t, :, 0:64],
                                                          in_=pb[:, 0:128].rearrange("p (j d) -> p j d", j=2)), [pb], [VA])
                            CP("dve", poolh[:, t, :], pb[:, 128:384], [pb], [poolh])
                            CP("act", u32[:], pa[:, 0:256], [pa], [u32])
                            ACT(v32[:], pa[:, 256:512], AF.Copy, [pa], [v32, st2], accum_out=st2[:, 0:1])
                            ACT(yb[:, 0:256], pa[:, 256:512], AF.Square, [pa], [yb, st2], accum_out=st2[:, 1:2])
                            TSC("dve", st3[:, 0:1], st2[:, 0:1], 1.0 / 256, None, ALU.mult, None, [st2], [st3])
                            STT("dve", st3[:, 1:2], st3[:, 0:1], -1.0, st3[:, 0:1], ALU.mult, ALU.mult, [st3], [st3])
                            STT("dve", st3[:, 1:2], st2[:, 1:2], 1.0 / 256, st3[:, 1:2], ALU.mult, ALU.add, [st2, st3], [st3])
                            TSC("dve", st3[:, 1:2], st3[:, 1:2], EPS, None, ALU.add, None, [st3], [st3])
                            ACT(st3[:, 1:2], st3[:, 1:2], AF.Sqrt, [st3], [st3])
                            S.op("dve", lambda e: e.reciprocal(out=st3[:, 1:2], in_=st3[:, 1:2]), [st3], [st3])
                            TSC("dve", v32[:], v32[:], st3[:, 0:1], st3[:, 1:2], ALU.subtract, ALU.mult, [v32, st3], [v32])
                            TTOP("dve", v32[:], v32[:], glg[:], ALU.mult, [v32, glg], [v32])
                            TTOP("dve", vnb[:], v32[:], glb[:], ALU.add, [v32, glb], [vnb])
                            for hh in range(4):
                                MM(pg[:, hh * 64:(hh + 1) * 64], wsT[:, hh, :], vnb[:, hh * 64:(hh + 1) * 64], True, True,
                                   [wsT, vnb], [pg])
                            for hh in range(4):
                                STT("dve", gmb[:, t, hh * 64:(hh + 1) * 64], pg[:, hh * 64:(hh + 1) * 64], bsT[:, hh:hh + 1],
                                    u32[:, hh * 64:(hh + 1) * 64], ALU.add, ALU.mult, [pg, bsT, u32], [gmb])
                    if not do_pass2:
                        return
                    for g in range(LL // GT):
                        tsl = slice(g * GT, (g + 1) * GT)
                        for cc in range(2):
                            pc_ = pq if cc == 0 else pg
                            for j in range(31):
                                MM(pc_[:, :GT], dg[:, cc, j, :], convT[:, cc, g * GT + j: g * GT + j + GT], j == 0, j == 30,
                                   [dg, convT], [pc_])
                            TSC("dve", acc[:, cc, :GT], pc_[:, :GT], ccols[:, 0, cc:cc + 1], None, ALU.add, None, [pc_, ccols], [acc])
                        for cc in range(2):
                            MM(pq[:, :GT], onesm[:], acc[:, cc, :GT], cc == 0, cc == 1, [onesm, acc], [pq])
                        CP("act", mean_sb[:, :GT], pq[:, :GT], [pq], [mean_sb])
                        for cc in range(2):
                            ACT(ftmp[cc][:, :GT], acc[:, cc, :GT], AF.Square, [acc], [ftmp[cc]])
                        for cc in range(2):
                            MM(pq[:, :GT], onesm[:], ftmp[cc][:, :GT], cc == 0, cc == 1, [onesm, ftmp[cc]], [pq])
                        TTOP("dve", rstd_sb[:, :GT], mean_sb[:, :GT], mean_sb[:, :GT], ALU.mult, [mean_sb], [rstd_sb])
                        TTOP("dve", rstd_sb[:, :GT], pq[:, :GT], rstd_sb[:, :GT], ALU.subtract, [pq, rstd_sb], [rstd_sb])
                        TSC("dve", rstd_sb[:, :GT], rstd_sb[:, :GT], EPS, None, ALU.add, None, [rstd_sb], [rstd_sb])
                        ACT(rstd_sb[:, :GT], rstd_sb[:, :GT], AF.Sqrt, [rstd_sb], [rstd_sb])
                        S.op("dve", lambda e: e.reciprocal(out=rstd_sb[:, :GT], in_=rstd_sb[:, :GT]), [rstd_sb], [rstd_sb])
                        for cc in range(2):
                            TTOP("dve", acc[:, cc, :GT], acc[:, cc, :GT], mean_sb[:, :GT], ALU.subtract, [acc, mean_sb], [acc])
                            TTOP("dve", acc[:, cc, :GT], acc[:, cc, :GT], rstd_sb[:, :GT], ALU.mult, [acc, rstd_sb], [acc])
                            ACT(sTb[:, cc, tsl], acc[:, cc, :GT], AF.Silu, [acc, ccols], [sTb],
                                scale=ccols[:, 1, cc:cc + 1], bias=ccols[:, 2, cc:cc + 1])
                        for ti in range(TG):
                            t = g * TG + ti
                            qs = slice(t * 128, (t + 1) * 128)
                            x_ = xt[t % 2]
                            DMA("sp", x_[:], src[row0 + t * 128: row0 + (t + 1) * 128, :], [src], [x_])
                            blocks = []
                            for cb in range(2):
                                blocks.append((kcT, slice(cb * 128, (cb + 1) * 128), vcaug, cb, None))
                            if not is_ctx:
                                if t > 0:
                                    blocks.append((kT, slice((t - 1) * 128, t * 128), vaug, t - 1, 0))
                                blocks.append((kT, qs, vaug, t, None))
                                if t < NT - 1:
                                    blocks.append((kT, slice((t + 1) * 128, (t + 2) * 128), vaug, t + 1, 1))
                            nb = len(blocks)
                            for j in range(2):
                                P_ = PT[j]
                                for lo in range(0, nb, 4):
                                    hi = min(nb, lo + 4)
                                    for gq in range(2):
                                        pr = slice(64 * gq, 64 * gq + 64)
                                        psc_ = pa if gq == 0 else pq
                                        for bi in range(lo, hi):
                                            Ks, ksl, Vt, vi, mi = blocks[bi]
                                            MM(psc_[:, (bi - lo) * 128:(bi - lo + 1) * 128], Ks[j][pr, ksl], qT[j][pr, qs], True, True,
                                               [Ks[j], qT[j]], [psc_])
                                    for gq in range(2):
                                        psc_ = pa if gq == 0 else pq
                                        ACT(P_[:, lo:hi, gq * 128:(gq + 1) * 128],
                                            psc_[:, 0:(hi - lo) * 128].rearrange("p (b q) -> p b q", q=128), AF.Exp, [psc_], [P_], scale=0.125)
                                for bi, (Ks, ksl, Vt, vi, mi) in enumerate(blocks):
                                    if mi is not None:
                                        TTOP("dve", P_[:, bi, :], P_[:, bi, :], masks[:, mi, :], ALU.mult, [P_, masks], [P_])
                                for gq in range(2):
                                    hh = 2 * j + gq
                                    for bi, (Ks, ksl, Vt, vi, mi) in enumerate(blocks):
                                        MM(pb[:, hh * 65:(hh + 1) * 65], P_[:, bi, gq * 128:(gq + 1) * 128], Vt[:, vi, j, :],
                                           bi == 0, bi == nb - 1, [P_, Vt], [pb])
                            pb3 = pb[:, 0:260].rearrange("p (h d) -> p h d", h=4)
                            TTOP("dve", den[:], pb3[:, :, 64], esink[:], ALU.add, [pb, esink], [den])
                            S.op("dve", lambda e: e.reciprocal(out=den[:], in_=den[:]), [den], [den])
                            for hh in range(4):
                                if hh % 2 == 0:
                                    TSC("dve", mix[:, hh * 64:(hh + 1) * 64], pb[:, hh * 65: hh * 65 + 64], den[:, hh:hh + 1], None,
                                        ALU.mult, None, [pb, den], [mix])
                                else:
                                    ACT(mix[:, hh * 64:(hh + 1) * 64], pb[:, hh * 65: hh * 65 + 64], AF.Copy, [pb, den], [mix],
                                        scale=den[:, hh:hh + 1])
                            for cc in range(2):
                                MM(pg[:, 0:256], sTb[:, cc, qs], cpw[:, cc, :], cc == 0, cc == 1, [sTb, cpw], [pg])
                            CP("act", mix[:, 256:512], pg[:, 0:256], [pg], [mix])
                            CP("dve", mix[:, 512:768], gmb[:, t, :], [gmb], [mix])
                            for gi in range(4):
                                srcs = []
                                if t > 0:
                                    srcs.append((t - 1, gi * 5 + 0))
                                srcs.append((t, gi * 5 + (3 if t == 0 else (4 if t == NT - 1 else 2))))
                                if t < NT - 1:
                                    srcs.append((t + 1, gi * 5 + 1))
                                for si, (tt_, mi) in enumerate(srcs):
                                    MM(pq[0:64, gi * 128:(gi + 1) * 128], poolh[:, tt_, gi * 64:(gi + 1) * 64], poolM[:, mi, :],
                                       si == 0, si == len(srcs) - 1, [poolh, poolM], [pq])
                            CP("dve", yTp[:, :, :], pq[0:64, :].rearrange("p (g t) -> p g t", g=4), [pq], [yTp])
                            for gi in range(4):
                                MM(pg[:, 256 + gi * 64: 256 + (gi + 1) * 64], yTp[:, gi, :], pw[:, gi, :], True, True,
                                   [yTp, pw], [pg])
                            TTOP("dve", mix[:, 768:1024], pg[:, 256:512], psc[:], ALU.mult, [pg, psc], [mix])
                            for gi in range(4):
                                ACT(hb[:, 0:256], mix[:, gi * 256:(gi + 1) * 256], AF.Square, [mix], [hb, gss],
                                    accum_out=gss[:, gi:gi + 1])
                            rstd_from(gss, 256, grs)
                            for gi in range(4):
                                if gi % 2 == 0:
                                    TSC("dve", yb[:, gi * 256:(gi + 1) * 256], mix[:, gi * 256:(gi + 1) * 256], grs[:, gi:gi + 1],
                                        None, ALU.mult, None, [mix, grs], [yb])
                                else:
                                    TSC("dve", yb[:, gi * 256:(gi + 1) * 256], mix[:, gi * 256:(gi + 1) * 256], grs[:, gi:gi + 1],
                                        None, ALU.mult, None, [mix, grs], [yb])
                            p_ = pT[t % 2]
                            for c in range(8):
                                TR(p_[:, c * 128:(c + 1) * 128], yb[:, c * 128:(c + 1) * 128], identb[:], [yb, identb], [p_])
                            cp_alt(yT[:, :, :], p_[:, 0:1024].rearrange("p (c t) -> p c t", c=8), [p_], [yT])
                            for nbk in range(2):
                                p_ = pf[nbk]
                                for c in range(8):
                                    MM(p_[:, :], yT[:, c, :], wout[:, c, nbk * 512:(nbk + 1) * 512], c == 0, c == 7, [yT, wout], [p_])
                                TTOP("dve", xn[:, nbk * 512:(nbk + 1) * 512], p_[:, :], G1[:, nbk * 512:(nbk + 1) * 512], ALU.mult,
                                     [p_, G1], [xn])
                            TTOP("dve", xn[:], xn[:], x_[:], ALU.add, [xn, x_], [xn])
                            DMA("sp", dst[row0 + t * 128: row0 + (t + 1) * 128, :], xn[:], [xn], [dst])

                import os
                lim = os.environ.get("MIXLIM", "")
                for s in range(SPC):
                    load_mod(4)
                    seq(csrc, cdst, s * LC, LC, True, not last)
                    if lim == "c":
                        break
                    load_mod(s)
                    seq(xsrc, xdst, s * L, L, False, True)
                    if lim == "cx":
                        break
            S.stack = gst
            S.barrier()

        def phase_moe(l, last, tiles, final):
            NT = len(tiles)
            NSL = NT * 128 * 4 // TS + 32
            with contextlib.ExitStack() as ph:
                S.stack = ph
                idxi = S.sb([128, NT, 4], I32, "idxi")
                gate4 = S.sb([128, NT, 4], F32, "gate4")
                widx = S.sb([128, 128], I32, "widx")
                w2idx = S.sb([128, 128], I32, "w2idx")
                bidx = S.sb([128, 128], I32, "bidx")
                Gd = S.sb([128, NT, 32], F32, "Gd")
                A2 = S.sb([128, D], F32)
                B2 = S.sb([128, D], F32)
                G2 = S.sb([128, D], F32)
                gn2 = S.sb([128, D], F32)
                DMA("sp", gn2[:], norm2_g[l:l + 1, :].partition_broadcast(128), [], [gn2])
                cur = [None]

                def load_mod(r, which):
                    if cur[0] == (r, which):
                        return
                    cur[0] = (r, which)
                    if which == 0:
                        DMA("sp", A2[:], modrows[r:r + 1, 4 * D:5 * D].partition_broadcast(128), [modrows], [A2])
                        DMA("sp", B2[:], modrows[r:r + 1, 3 * D:4 * D].partition_broadcast(128), [modrows], [B2])
                        STT("dve", A2[:], A2[:], 1.0, gn2[:], ALU.add, ALU.mult, [A2, gn2], [A2])
                    else:
                        DMA("sp", G2[:], modrows[r:r + 1, 5 * D:6 * D].partition_broadcast(128), [modrows], [G2])

                with contextlib.ExitStack() as ph1:
                    S.stack = ph1
                    rw = S.sb([128, 8, 32], F32)
                    DMA("sp", rw[:], router_w[l].rearrange("(c p) n -> p c n", p=128), [], [rw])
                    rbb = S.sb([128, 32], F32)
                    DMA("sp", rbb[:], router_b[l:l + 1, :].partition_broadcast(128), [], [rbb])
                    upper = S.sb([128, 128], BF16)
                    DMA("pool", upper[:], k_upper, [], [upper])
                    onesb = S.sb([128, 128], BF16)
                    S.op("dve", lambda e: e.memset(onesb[:], 1.0), [], [onesb])
                    ones32 = S.sb([128, 1], F32)
                    S.op("dve", lambda e: e.memset(ones32[:], 1.0), [], [ones32])
                    thr = S.sb([32, 128], F32)
                    DMA("sp", thr[:], k_slotthr, [], [thr])
                    lg = S.sb([128, NT, 32], F32)
                    t8 = S.sb([128, NT, 8], F32)
                    pos = S.sb([128, NT, 32], F32)
                    base = S.sb([128, 32], F32)
                    S.op("dve", lambda e: e.memset(base[:], 0.0), [], [base])
                    xt = [S.sb([128, D], F32) for _ in range(2)]
                    junk = S.sb([128, D], BF16)
                    ss = S.sb([128, 1], F32)
                    rs = S.sb([128, 1], F32)
                    h32 = S.sb([128, D], F32)
                    hb = [S.sb([128, D], BF16) for _ in range(2)]
                    hT32 = S.sb([128, 8, 128], F32)
                    maskb = S.sb([128, 32], BF16)
                    zt = S.sb([128, 2048], BF16)
                    S.op("pool", lambda e: e.memset(zt[:], 0.0), [], [zt])
                    pT32 = [S.ps([128, 128], F32) for _ in range(2)]
                    pl = S.ps([128, 512])
                    pp = S.ps([128, 512])
                    xz = xsort[0:NSL * TS, :].rearrange("(n p r) d -> n p (r d)", p=128, r=2)
                    for n in range(NSL * TS // 256):
                        DMA("sp", xz[n], zt[:], [zt], [xsort])
                    for t, (src, dst, row0, r) in enumerate(tiles):
                        load_mod(r, 0)
                        x_ = xt[t % 2]
                        DMA("sp", x_[:], src[row0:row0 + 128, :], [src], [x_])
                        ACT(junk[:], x_[:], AF.Square, [x_], [junk, ss], accum_out=ss[:])
                        rstd_from(ss, D, rs)
                        STT("dve", h32[:], x_[:], rs[:, 0:1], A2[:], ALU.mult, ALU.mult, [x_, rs, A2], [h32])
                        TTOP("dve", h32[:], h32[:], B2[:], ALU.add, [h32, B2], [h32])
                        hb_ = hb[t % 2]
                        CP("act", hb_[:], h32[:], [h32], [hb_])
                        DMA("sp", h2d[t * 128:(t + 1) * 128, :], hb_[:], [hb_], [h2d])
                        for hf in range(2):
                            p_ = pT32[hf]
                            for cc in range(4):
                                c = hf * 4 + cc
                                TR(p_[:, cc * 128:(cc + 1) * 128], h32[:, c * 128:(c + 1) * 128], ident32[:], [h32, ident32], [p_])
                            CP("act" if hf else "dve", hT32[:, hf * 4:(hf + 1) * 4, :],
                               p_[:, 0:512].rearrange("p (c t) -> p c t", c=4), [p_], [hT32])
                        for c in range(8):
                            MM(pl[:, 0:32], hT32[:, c, :], rw[:, c, :], c == 0, c == 7, [hT32, rw], [pl])
                        TTOP("dve", lg[:, t, :], pl[:, 0:32], rbb[:], ALU.add, [pl, rbb], [lg])
                        S.op("dve", lambda e: e.max(out=t8[:, t, :], in_=lg[:, t, :]), [lg], [t8])
                        TSC("dve", maskb[:], lg[:, t, :], t8[:, t, 3:4], None, ALU.is_ge, None, [lg, t8], [maskb])
                        MM(pp[:, 0:32], upper[:], maskb[:], True, True, [upper, maskb], [pp])
                        MM(pp[:, 32:64], onesb[:], maskb[:], True, True, [onesb, maskb], [pp])
                        TTOP("dve", pos[:, t, :], pp[:, 0:32], base[:], ALU.add, [pp, base], [pos])
                        TTOP("dve", base[:], pp[:, 32:64], base[:], ALU.add, [pp, base], [base])
                        TSC("dve", ss[:], t8[:, t, 0:1], -1.0, None, ALU.mult, None, [t8], [ss])
                        ACT(gate4[:, t, :], t8[:, t, 0:4], AF.Exp, [t8, ss], [gate4, rs], bias=ss[:, 0:1], accum_out=rs[:, 0:1])
                        S.op("dve", lambda e: e.reciprocal(out=rs[:], in_=rs[:]), [rs], [rs])
                        TSC("dve", gate4[:, t, :], gate4[:, t, :], rs[:, 0:1], None, ALU.mult, None, [gate4, rs], [gate4])
                        ACT(Gd[:, t, :], lg[:, t, :], AF.Exp, [lg, ss], [Gd], bias=ss[:, 0:1])
                        TTOP("dve", Gd[:, t, :], Gd[:, t, :], maskb[:], ALU.mult, [Gd, maskb], [Gd])
                        TSC("dve", Gd[:, t, :], Gd[:, t, :], rs[:, 0:1], None, ALU.mult, None, [Gd, rs], [Gd])
                    padded = S.sb([128, 32], F32)
                    pend = S.sb([128, 32], F32)
                    pstart = S.sb([128, 32], F32)
                    tmpc = S.sb([128, 32], F32)
                    TSC("dve", padded[:], base[:], 0.0, None, ALU.is_gt, None, [base], [padded])
                    for jj in range(1, NT * 128 // TS + 1):
                        STT("dve", padded[:], base[:], float(TS * jj), padded[:], ALU.is_gt, ALU.add, [base, padded], [padded])
                    TSC("dve", padded[:], padded[:], float(TS), None, ALU.mult, None, [padded], [padded])
                    CP("dve", pend[:, 0:1], padded[:, 0:1], [padded], [pend])
                    for e_ in range(1, 32):
                        TTOP("dve", pend[:, e_:e_ + 1], pend[:, e_ - 1:e_], padded[:, e_:e_ + 1], ALU.add, [pend, padded], [pend])
                    TTOP("dve", pstart[:], pend[:], padded[:], ALU.subtract, [pend, padded], [pstart])
                    pcol = S.sb([32, 1], F32)
                    tmpd = S.sb([32, 32], F32)
                    TTOP("dve", tmpd[:], pend[0:32, :], ident32[0:32, 0:32], ALU.mult, [pend, ident32], [tmpd])
                    S.op("dve", lambda e: e.reduce_sum(out=pcol[:], in_=tmpd[:], axis=AX.X), [tmpd], [pcol])
                    cmp = S.sb([32, 128], F32)
                    TSC("dve", cmp[:], thr[:], pcol[:, 0:1], None, ALU.is_ge, None, [thr, pcol], [cmp])
                    ones32m = S.sb([32, 128], F32)
                    S.op("dve", lambda e: e.memset(ones32m[:], 1.0), [], [ones32m])
                    piota = S.sb([128, 1], F32)
                    DMA("sp", piota[:], k_piota, [], [piota])
                    MM(pl[:, 0:128], ones32m[0:32, :], cmp[:], True, True, [ones32m, cmp], [pl])
                    blkf = S.sb([128, 128], F32)
                    TSC("dve", blkf[:], pl[:, 0:128], 31.0, None, ALU.min, None, [pl], [blkf])
                    wif = S.sb([128, 128], F32)
                    same = S.sb([128, 128], F32)
                    S.op("dve", lambda e: e.memset(same[:], 0.0), [], [same])
                    TTOP("dve", same[:, 2:128], blkf[:, 2:128], blkf[:, 0:126], ALU.is_equal, [blkf], [same])
                    TSC("dve", same[:], same[:], SKIPBIG, None, ALU.mult, None, [same], [same])
                    STT("dve", wif[:], blkf[:], 1024.0, same[:], ALU.mult, ALU.add, [blkf, same], [wif])
                    TSC("dve", wif[:], wif[:], piota[:, 0:1], None, ALU.add, None, [wif, piota], [wif])
                    CP("dve", widx[:], wif[:], [wif], [widx])
                    STT("dve", wif[:], blkf[:], 512.0, same[:], ALU.mult, ALU.add, [blkf, same], [wif])
                    TSC("dve", wif[:], wif[:], piota[:, 0:1], None, ALU.add, None, [wif, piota], [wif])
                    CP("dve", w2idx[:], wif[:], [wif], [w2idx])
                    STT("dve", wif[:], blkf[:], 128.0, same[:], ALU.mult, ALU.add, [blkf, same], [wif])
                    TSC("dve", wif[:], wif[:], piota[:, 0:1], None, ALU.add, None, [wif, piota], [wif])
                    CP("dve", bidx[:], wif[:], [wif], [bidx])
                    oh = S.sb([128, 32], F32)
                    idxf = S.sb([128, 4], F32)
                    for t, (src, dst, row0, r) in enumerate(tiles):
                        TTOP("dve", pos[:, t, :], pos[:, t, :], pstart[:], ALU.add, [pos, pstart], [pos])
                        for k in range(4):
                            STT("dve", oh[:], lg[:, t, :], t8[:, t, k:k + 1], pos[:, t, :], ALU.is_equal, ALU.mult,
                                [lg, t8, pos], [oh])
                            S.op("dve", lambda e: e.reduce_sum(out=idxf[:, k:k + 1], in_=oh[:], axis=AX.X), [oh], [idxf])
                        CP("dve", idxi[:, t, :], idxf[:], [idxf], [idxi])
                        hb_ = hb[t % 2]
                        DMA("sp", hb_[:], h2d[t * 128:(t + 1) * 128, :], [h2d], [hb_])
                        for k in range(4):
                            S.dma("pool", lambda e: e.indirect_dma_start(
                                out=xsort[:, :], out_offset=bass.IndirectOffsetOnAxis(ap=idxi[:, t, k:k + 1], axis=0),
                                in_=hb_[:], in_offset=None), [hb_, idxi], [xsort])
                S.stack = ph
                S.barrier()
                if stop_after == ("route", l):
                    return
                with contextlib.ExitStack() as ph2:
                    S.stack = ph2
                    w1b = [S.sb([128, 8, 2 * D], BF16) for _ in range(2)]
                    w2b = [S.sb([128, 8, D], BF16) for _ in range(2)]
                    b1c = [S.sb([128, 16], F32) for _ in range(2)]
                    xs_ = [S.sb([128, 4, D], BF16) for _ in range(2)]
                    xsT2 = [S.sb([128, 8, TS], BF16) for _ in range(2)]
                    actT = S.sb([128, 8, TS], BF16)
                    b1p = [S.sb([128, 8], F32) for _ in range(2)]
                    linp = [S.sb([128, TS], F32) for _ in range(2)]
                    g32s = [S.sb([128, TS], F32) for _ in range(2)]
                    sgs = [S.sb([128, TS], F32) for _ in range(2)]
                    tts = [S.sb([128, TS], F32) for _ in range(2)]
                    yt = [S.sb([128, D], F32) for _ in range(2)]
                    pT = [S.ps([128, 128], BF16) for _ in range(2)]
                    ph_ = [S.ps([128, 512]) for _ in range(4)]
                    py = [S.ps([128, 512]) for _ in range(2)]
                    w1flat = exp_w1.rearrange("l e k n -> (l e k) n")
                    w2flat = exp_w2.rearrange("l e c p n -> (l e c p) n")
                    b1flat = exp_b1T.rearrange("l e p n -> (l e p) n")

                    if not hasattr(build, "_bnd") or build._bnd[0] is not nc:
                        rg_ = nc.gpsimd.alloc_register("bndreg")
                        nc.gpsimd.reg_mov(rg_, 2 * 32 * 1024 - 1)
                        build._bnd = (nc, rg_)
                    bnd_reg = build._bnd[1]

                    def load_w(j):
                        b = j % 2
                        for c in range(8):
                            S.dma("pool", lambda e: e.indirect_dma_start(
                                out=w1b[b][:, c, :], out_offset=None, in_=w1flat,
                                in_offset=bass.IndirectOffsetOnAxis(ap=widx[:, j:j + 1], axis=0),
                                element_offset=(l * 32 * 1024 + c * 128) * 2048, bounds_check=bnd_reg, oob_is_err=False), [widx], [w1b[b]])
                        for c2 in range(4):
                            S.dma("pool", lambda e: e.indirect_dma_start(
                                out=w2b[b][:, 2 * c2:2 * c2 + 2, :].rearrange("p c n -> p (c n)"), out_offset=None, in_=w2flat,
                                in_offset=bass.IndirectOffsetOnAxis(ap=w2idx[:, j:j + 1], axis=0),
                                element_offset=(l * 128 + c2) * 128 * 2048, bounds_check=bnd_reg, oob_is_err=False),
                                [w2idx], [w2b[b]])
                        S.dma("pool", lambda e: e.indirect_dma_start(
                            out=b1c[b][:], out_offset=None, in_=b1flat,
                            in_offset=bass.IndirectOffsetOnAxis(ap=bidx[:, j:j + 1], axis=0),
                            element_offset=l * 32 * 128 * 16, bounds_check=bnd_reg, oob_is_err=False), [bidx], [b1c[b]])

                    def load_x(j):
                        DMA("sp", xs_[j % 2][:], xsort[j * TS:(j + 1) * TS, :].rearrange("(a p) d -> p a d", p=128),
                            [xsort], [xs_[j % 2]])

                    load_w(0)
                    load_x(0)
                    for j in range(NSL):
                        b = j % 2
                        if j + 1 < NSL:
                            load_w(j + 1)
                            load_x(j + 1)
                        W1, W2, B1c, X, xsT = w1b[b], w2b[b], b1c[b], xs_[b], xsT2[b]
                        TSC("dve", b1p[b][:], B1c[:, 8:16], 1.0, None, ALU.add, None, [B1c], [b1p[b]])
                        for a in range(4):
                            p_ = pT[a % 2]
                            for c in range(8):
                                TR(p_[:, c * 128:(c + 1) * 128], X[:, a, c * 128:(c + 1) * 128], identb[:], [X, identb], [p_])
                            CP("act" if a % 2 else "dve", xsT[:, :, a * 128:(a + 1) * 128],
                               p_[:, 0:1024].rearrange("p (c t) -> p c t", c=8), [p_], [xsT])
                        pi = 0
                        for i in range(8):
                            k2 = i % 2
                            pl_ = ph_[pi % 4]
                            pi += 1
                            for c in range(8):
                                MM(pl_[:, :], W1[:, c, (8 + i) * 128:(9 + i) * 128], xsT[:, c, :], c == 0, c == 7, [W1, xsT], [pl_])
                            TSC("dve", linp[k2][:], pl_[:, :], b1p[b][:, i:i + 1], -6.0, ALU.add, ALU.max, [pl_, b1p[b]], [linp[k2]])
                            pg_ = ph_[pi % 4]
                            pi += 1
                            for c in range(8):
                                MM(pg_[:, :], W1[:, c, i * 128:(i + 1) * 128], xsT[:, c, :], c == 0, c == 7, [W1, xsT], [pg_])
                            TSC("dve", g32s[k2][:], pg_[:, :], B1c[:, i:i + 1], 7.0, ALU.add, ALU.min, [pg_, B1c], [g32s[k2]])
                            ACT(sgs[k2][:], g32s[k2][:], AF.Sigmoid, [g32s[k2]], [sgs[k2]], scale=1.702)
                            TTOP("dve", tts[k2][:], sgs[k2][:], g32s[k2][:], ALU.mult, [sgs[k2], g32s[k2]], [tts[k2]])
                            STT("dve", actT[:, i, :], linp[k2][:], 8.0, tts[k2][:], ALU.min, ALU.mult, [linp[k2], tts[k2]], [actT])
                        for a in range(4):
                            y_ = yt[a % 2]
                            for nbk in range(2):
                                p_ = py[nbk]
                                for f in range(8):
                                    MM(p_[:, :], actT[:, f, a * 128:(a + 1) * 128], W2[:, f, nbk * 512:(nbk + 1) * 512],
                                       f == 0, f == 7, [actT, W2], [p_])
                                CP("act", y_[:, nbk * 512:(nbk + 1) * 512], p_[:, :], [p_], [y_])
                            DMA("sp", ysort[j * TS + a * 128: j * TS + (a + 1) * 128, :], y_[:], [y_], [ysort])
                S.stack = ph
                S.barrier()
                with contextlib.ExitStack() as ph3:
                    S.stack = ph3
                    yk = [[S.sb([128, D], F32) for _ in range(4)] for _ in range(2)]
                    xt = [S.sb([128, D], F32) for _ in range(2)]
                    acc = S.sb([128, D], F32)
                    xn = [S.sb([128, D], F32) for _ in range(2)]
                    junk = S.sb([128, D], BF16)
                    ss = S.sb([128, 1], F32)
                    rs = S.sb([128, 1], F32)
                    fng = S.sb([128, D], F32)
                    DMA("sp", fng[:], final_g[0:1, :].partition_broadcast(128), [], [fng])
                    b2all = S.sb([32, D], F32)
                    DMA("sp", b2all[:], exp_b2[l], [], [b2all])
                    GTs = S.sb([32, 128], F32)
                    pGT = S.ps([128, 512])
                    pbias = [S.ps([128, 512]) for _ in range(2)]
                    for t, (src, dst, row0, r) in enumerate(tiles):
                        load_mod(r, 1)
                        x_ = xt[t % 2]
                        Y = yk[t % 2]
                        xo = xn[t % 2]
                        DMA("sp", x_[:], src[row0:row0 + 128, :], [src], [x_])
                        for k in range(4):
                            S.dma("pool", lambda e: e.indirect_dma_start(
                                out=Y[k][:], out_offset=None, in_=ysort[:, :],
                                in_offset=bass.IndirectOffsetOnAxis(ap=idxi[:, t, k:k + 1], axis=0)), [ysort, idxi], [Y[k]])
                        TR(pGT[0:32, 0:128], Gd[:, t, :], ident32[:], [Gd, ident32], [pGT])
                        CP("act", GTs[:], pGT[0:32, 0:128], [pGT], [GTs])
                        for nbk in range(2):
                            MM(pbias[nbk][:, :], GTs[0:32, :], b2all[0:32, nbk * 512:(nbk + 1) * 512], True, True,
                               [GTs, b2all], [pbias[nbk]])
                        TSC("dve", acc[:], Y[0][:], gate4[:, t, 0:1], None, ALU.mult, None, [Y[0], gate4], [acc])
                        for k in range(1, 4):
                            STT("dve" if k != 2 else "pool", acc[:], Y[k][:], gate4[:, t, k:k + 1], acc[:], ALU.mult, ALU.add,
                                [Y[k], gate4, acc], [acc])
                        for nbk in range(2):
                            TTOP("dve", acc[:, nbk * 512:(nbk + 1) * 512], acc[:, nbk * 512:(nbk + 1) * 512], pbias[nbk][:, :], ALU.add,
                                 [acc, pbias[nbk]], [acc])
                        TTOP("dve", acc[:], acc[:], G2[:], ALU.mult, [acc, G2], [acc])
                        TTOP("dve", xo[:], acc[:], x_[:], ALU.add, [acc, x_], [xo])
                        if final:
                            ACT(junk[:], xo[:], AF.Square, [xo], [junk, ss], accum_out=ss[:])
                            rstd_from(ss, D, rs)
                            STT("dve", xo[:], xo[:], rs[:, 0:1], fng[:], ALU.mult, ALU.mult, [xo, rs, fng], [xo])
                        DMA("sp", dst[row0:row0 + 128, :], xo[:], [xo], [dst])
            S.stack = gst
            S.barrier()

        def tiles_for(xsrc, xdst, csrc, cdst, with_ctx):
            tl = []
            for s in range(SPC):
                for t in range(L // 128):
                    tl.append((xsrc, xdst, s * L + t * 128, s))
            if with_ctx:
                for s in range(SPC):
                    for t in range(LC // 128):
                        tl.append((csrc, cdst, s * LC + t * 128, 4))
            return tl

        done = False
        for l in range(2):
            last = l == 1
            phase_ada(l)
            if stop_after == ("ada", 0):
                break
            if l == 0:
                phase_mix(0, False, x_in, xs1, c_in, cs1)
                if stop_after == ("mix", 0):
                    break
                phase_moe(0, False, tiles_for(xs1, xs2, cs1, cs2, True), False)
                if stop_after in (("route", 0), ("moe", 0)):
                    break
            else:
                phase_mix(1, True, xs2, xs1, cs2, None)
                phase_moe(1, True, tiles_for(xs1, out_t, None, None, False), True)
        if stop_after is not None:
            S.barrier()
            if stop_after[0] == "ada":
                DMA("sp", out_t[0:30, :].rearrange("(r k) d -> r (k d)", r=5), modrows[:, :], [modrows], [out_t])
            else:
                srcd = {"mix": xs1, "moe": xs2, "route": xs1}[stop_after[0]]
                import os
                for i_ in range((L if os.environ.get("MIXLIM") else SPC * L) // 128):
                    DMA("sp", out_t[i_ * 128:(i_ + 1) * 128, :], srcd[i_ * 128:(i_ + 1) * 128, :], [srcd], [out_t])
        S.finish()
        build.stats = (S.n_inst, S.n_wait)
    return nc


def _prep_shared(inp):
    f = lambda a: np.ascontiguousarray(np.asarray(a, dtype=np.float32))
    cols = _ext_cols()
    sh = {}
    sh["ada_w"] = f(inp["ada_w"])
    sh["ada_b"] = f(inp["ada_b"])
    sh["norm1_g"] = f(inp["norm1_g"])
    sh["norm2_g"] = f(inp["norm2_g"])
    sh["w_in_ext"] = f(np.asarray(inp["w_in"])[:, :, cols])
    sh["attn_sink"] = f(inp["attn_sink"])
    dw = np.asarray(inp["conv_dw_w"])
    sh["conv_dwT"] = f(dw.transpose(0, 2, 1).reshape(2, 2, 128, 31).transpose(0, 2, 1, 3))
    cc = np.stack([np.asarray(inp["conv_dw_b"]), np.asarray(inp["conv_ln_g"]), np.asarray(inp["conv_ln_b"])], 1)
    sh["convcols"] = f(cc.reshape(2, 3, 2, 128).transpose(0, 3, 1, 2))
    sh["conv_pw_w"] = f(inp["conv_pw_w"])
    sh["gmlp_ln_g"] = f(inp["gmlp_ln_g"])
    sh["gmlp_ln_b"] = f(inp["gmlp_ln_b"])
    sh["gmlp_wsT"] = f(np.asarray(inp["gmlp_ws"]).transpose(0, 1, 3, 2))
    sh["gmlp_bsT"] = f(np.asarray(inp["gmlp_bs"]).transpose(0, 2, 1))
    sh["pool_w"] = f(inp["pool_w"])
    sh["pool_scale"] = f(inp["pool_scale"])
    sh["gncol"] = f(np.asarray(inp["group_norm_g"]).reshape(2, 8, 128).transpose(0, 2, 1))
    sh["w_out"] = f(inp["w_out"])
    sh["router_w"] = f(inp["router_w"])
    sh["router_b"] = f(inp["router_b"])
    sh["exp_w1"] = f(inp["exp_w1"])
    sh["exp_b1T"] = f(np.asarray(inp["exp_b1"]).reshape(2, 32, 16, 128).transpose(0, 1, 3, 2))
    sh["exp_w2r"] = f(np.asarray(inp["exp_w2"]).reshape(2, 32, 4, 2, 128, D).transpose(0, 1, 2, 4, 3, 5).reshape(2, 32, 4, 128, 2 * D))
    sh["exp_b2"] = f(inp["exp_b2"])
    sh["final_norm_g"] = f(np.asarray(inp["final_norm_g"]).reshape(1, D))
    for k, v in _consts().items():
        sh["k_" + k] = f(v)
    return sh


def _in_maps(inp):
    sh = _prep_shared(inp)
    x = np.asarray(inp["x"], dtype=np.float32)
    c = np.asarray(inp["c"], dtype=np.float32)
    ctx = np.asarray(inp["ctx"], dtype=np.float32)
    cctx = np.asarray(inp["c_ctx"], dtype=np.float32)
    maps = []
    for i in range(NCORE):
        m = dict(sh)
        m["x"] = np.ascontiguousarray(x[i * SPC:(i + 1) * SPC].reshape(SPC * L, D))
        m["ctx"] = np.ascontiguousarray(ctx[i * SPC:(i + 1) * SPC].reshape(SPC * LC, D))
        c5 = np.concatenate([c[i * SPC:(i + 1) * SPC], cctx[None, :]], 0)
        m["cT"] = np.ascontiguousarray(c5.T.reshape(8, 128, 5).transpose(1, 0, 2))
        maps.append(m)
    return maps


def kernel(**inputs):
    nc = build()
    maps = _in_maps(inputs)
    res = run_bass_kernel_spmd(nc, maps, core_ids=list(range(NCORE)))
    outs = [np.asarray(r["out"]).reshape(SPC, L, D) for r in res.results]
    return np.concatenate(outs, 0).astype(np.float32)
```

```python
import contextlib
import numpy as np
import concourse.bass as bass
import concourse.mybir as mybir
from concourse.bass_utils import run_bass_kernel_spmd

F32 = mybir.dt.float32
BF16 = mybir.dt.bfloat16
I32 = mybir.dt.int32
AF = mybir.ActivationFunctionType
ALU = mybir.AluOpType
AX = mybir.AxisListType
POOL_ENG = mybir.EngineType.Pool

EPOCH = 16000
NSLOT = 8
NCORE = 8
SPC = 4
D = 1024
L = 2048
LC = 256
INW = 2432
EPS = 1e-6
TS = 512
import os as _os
SKIPBIG = float(_os.environ.get("SKIPBIG", "1.0e6"))


class Res:
    __slots__ = ("w", "r")

    def __init__(self):
        self.w = {}
        self.r = {}


class TT:
    __slots__ = ("t", "res", "excl")

    def __init__(self, t, excl=False):
        self.t = t
        self.res = Res()
        self.excl = excl

    def __getitem__(self, k):
        return self.t[k]


def _res(b):
    return b.res if isinstance(b, TT) else b


class Sched:
    def __init__(self, nc, stack):
        self.nc = nc
        self.stack = stack
        self.gstack = stack
        self.E = {}
        for name, e in (("pe", nc.tensor), ("act", nc.scalar), ("dve", nc.vector),
                        ("pool", nc.gpsimd), ("sp", nc.sync)):
            self.E[name] = dict(e=e, name=name, sems=[], cnt=0, waited={}, slots=None, slot_i=0)
        self.n_inst = 0
        self.n_wait = 0
        self.uid = 0

    def sem(self, name):
        return self.gstack.enter_context(self.nc.semaphore(name))

    def sb(self, shape, dt, name=None):
        self.uid += 1
        return TT(self.stack.enter_context(self.nc.sbuf_tensor(f"{name or 't'}_{self.uid}", list(shape), dt)))

    def ps(self, shape, dt=F32, name=None):
        self.uid += 1
        shape = [128, 512 if dt == F32 else 1024]
        return TT(excl=True, t=self.stack.enter_context(self.nc.psum_tensor(f"{name or 'p'}_{self.uid}", list(shape), dt)))

    def _wait(self, E, sem, val):
        key = id(sem)
        if E["waited"].get(key, 0) >= val:
            return
        E["waited"][key] = val
        E["e"].wait_ge(sem, val)
        self.n_wait += 1

    def _deps(self, E, reads, writes):
        deps = {}
        for b in reads:
            for k, v in _res(b).w.items():
                if k not in deps or deps[k][1] < v[1]:
                    deps[k] = v
        for b in writes:
            r = _res(b)
            for dd in (r.w, r.r):
                for k, v in dd.items():
                    if k not in deps or deps[k][1] < v[1]:
                        deps[k] = v
        own = E["sems"]
        for k, (sem, val) in deps.items():
            if E["name"] == "pe" and any(sem is s for s in own):
                continue
            self._wait(E, sem, val)

    def _record(self, tok, reads, writes):
        k = id(tok[0])
        for b in reads:
            _res(b).r[k] = tok
        for b in writes:
            r = _res(b)
            r.w = {k: tok}
            r.r = {}

    def _next_tok(self, E):
        n = E["cnt"]
        ep = n // EPOCH
        while len(E["sems"]) <= ep:
            E["sems"].append(self.sem(f"c_{E['name']}_{len(E['sems'])}"))
        E["cnt"] = n + 1
        return (E["sems"][ep], n % EPOCH + 1)

    def op(self, eng, fn, reads=(), writes=()):
        E = self.E[eng]
        ex = [b for b in reads if isinstance(b, TT) and b.excl]
        if ex:
            reads = [b for b in reads if not (isinstance(b, TT) and b.excl)]
            writes = list(writes) + ex
        self._deps(E, reads, writes)
        inst = fn(E["e"])
        tok = self._next_tok(E)
        inst.then_inc(tok[0], 1)
        self._record(tok, reads, writes)
        self.n_inst += 1

    def wait_for(self, eng, reads=(), writes=()):
        self._deps(self.E[eng], reads, writes)

    def dma(self, q, fn, reads=(), writes=()):
        E = self.E[q]
        if E["slots"] is None:
            E["slots"] = [[self.sem(f"d_{q}_{i}"), 0] for i in range(NSLOT)]
        self._deps(E, reads, writes)
        sl = E["slots"][E["slot_i"] % NSLOT]
        E["slot_i"] += 1
        if sl[1] > 0:
            self._wait(E, sl[0], sl[1])
        inst = fn(E["e"])
        sl[1] += 16
        inst.then_inc(sl[0], 16)
        self._record((sl[0], sl[1]), reads, writes)
        self.n_inst += 1

    def _all_toks(self):
        toks = []
        for E in self.E.values():
            if E["slots"]:
                for s, v in E["slots"]:
                    if v:
                        toks.append((s, v))
            n = E["cnt"]
            if n:
                toks.append((E["sems"][(n - 1) // EPOCH], (n - 1) % EPOCH + 1))
        return toks

    def barrier(self):
        toks = self._all_toks()
        for E in self.E.values():
            for s, v in toks:
                self._wait(E, s, v)

    def finish(self):
        self.barrier()


def _consts():
    c = {}
    c["ident"] = np.eye(128, dtype=np.float32)
    tq = np.arange(128)
    c["upper"] = (tq[:, None] < tq[None, :]).astype(np.float32)
    kj = np.arange(128)[:, None]
    qi = np.arange(128)[None, :]
    mL = (qi <= kj).astype(np.float32)
    mR = (kj <= qi).astype(np.float32)
    c["masks"] = np.stack([np.concatenate([mL, mL], 1), np.concatenate([mR, mR], 1)], 1)
    t = np.arange(L)
    row = (t // 64).astype(np.float32)
    col = (t % 64).astype(np.float32)
    inv = (10000.0 ** (-np.arange(0, 32, 2, dtype=np.float32) / 32)).astype(np.float32)
    ang_r = row[:, None] * inv[None, :]
    ang_c = col[:, None] * inv[None, :]
    cs = np.zeros((64, L), np.float32)
    sn = np.zeros((64, L), np.float32)
    for d in range(64):
        ang = ang_r if d < 32 else ang_c
        i = d % 16
        cs[d] = np.cos(ang[:, i])
        sn[d] = np.sin(ang[:, i]) * (-1.0 if (d % 32) < 16 else 1.0)
    c["ropeC"] = np.concatenate([cs, cs], 0)
    c["ropeS"] = np.concatenate([sn, sn], 0)
    PM = np.zeros((128, 20, 128), np.float32)
    LL = 512
    for gi, w in enumerate((2, 4, 8, 16)):
        M = np.zeros((LL, LL), np.float32)
        for tt in range(LL):
            lo = max(tt - w // 2, 0)
            hi = min(tt + w // 2, LL)
            M[lo:hi, tt] = 1.0 / (hi - lo)
            M[tt, tt] -= 1.0
        PM[:, gi * 5 + 0] = M[0:128, 128:256]
        PM[:, gi * 5 + 1] = M[256:384, 128:256]
        PM[:, gi * 5 + 2] = M[128:256, 128:256]
        PM[:, gi * 5 + 3] = M[0:128, 0:128]
        PM[:, gi * 5 + 4] = M[384:512, 384:512]
    c["poolM"] = PM
    c["slotthr"] = np.tile((np.arange(128, dtype=np.float32) * TS)[None, :], (32, 1))
    c["piota"] = np.arange(128, dtype=np.float32)[:, None]
    return c


def _ext_cols():
    def sw(off, n_heads):
        idx = []
        for h in range(n_heads):
            for d in range(64):
                sd = d + 16 if (d % 32) < 16 else d - 16
                idx.append(off + h * 64 + sd)
        return idx
    q = list(range(0, 256))
    qs = sw(0, 4)
    k0 = list(range(256, 320))
    k1 = list(range(320, 384))
    ks = sw(256, 2)
    k0s, k1s = ks[:64], ks[64:]
    cols = q + qs + k0 + k0 + k1 + k1 + k0s + k0s + k1s + k1s
    cols += list(range(512, 1024))
    cols += list(range(1024, 1536))
    cols += list(range(384, 512))
    cols += list(range(1536, 1792))
    assert len(cols) == INW
    return np.array(cols)


def build(stop_after=None):
    nc = bass.Bass("TRN2", target_bir_lowering=False)

    def din(name, shape, dt=F32):
        return nc.dram_tensor(name, list(shape), dt, kind="ExternalInput").ap()

    def dscr(name, shape, dt=F32):
        return TT(nc.dram_tensor(name, list(shape), dt, kind="Internal").ap())

    x_in = TT(din("x", [SPC * L, D]))
    c_in = TT(din("ctx", [SPC * LC, D]))
    cT_in = din("cT", [128, 8, 5])
    ada_w = din("ada_w", [2, D, 6 * D])
    ada_b = din("ada_b", [2, 6 * D])
    norm1_g = din("norm1_g", [2, D])
    norm2_g = din("norm2_g", [2, D])
    w_in = din("w_in_ext", [2, D, INW])
    attn_sink = din("attn_sink", [2, 4])
    dwT = din("conv_dwT", [2, 128, 2, 31])
    convcols = din("convcols", [2, 128, 3, 2])
    conv_pw = din("conv_pw_w", [2, 256, 256])
    gln_g = din("gmlp_ln_g", [2, 256])
    gln_b = din("gmlp_ln_b", [2, 256])
    gwsT = din("gmlp_wsT", [2, 4, 128, 128])
    gbsT = din("gmlp_bsT", [2, 128, 4])
    pool_w = din("pool_w", [2, 4, 64, 64])
    pool_scale = din("pool_scale", [2, 256])
    gncol = din("gncol", [2, 128, 8])
    w_out = din("w_out", [2, D, D])
    router_w = din("router_w", [2, D, 32])
    router_b = din("router_b", [2, 32])
    exp_w1 = din("exp_w1", [2, 32, D, 2 * D])
    exp_b1T = din("exp_b1T", [2, 32, 128, 16])
    exp_w2 = din("exp_w2r", [2, 32, 4, 128, 2 * D])
    exp_b2 = din("exp_b2", [2, 32, D])
    final_g = din("final_norm_g", [1, D])
    k_ident = din("k_ident", [128, 128])
    k_upper = din("k_upper", [128, 128])
    k_masks = din("k_masks", [128, 2, 256])
    k_ropeC = din("k_ropeC", [128, L])
    k_ropeS = din("k_ropeS", [128, L])
    k_poolM = din("k_poolM", [128, 20, 128])
    k_slotthr = din("k_slotthr", [32, 128])
    k_piota = din("k_piota", [128, 1])
    out_t = TT(nc.dram_tensor("out", [SPC * L, D], F32, kind="ExternalOutput").ap())

    xs1 = dscr("xs1", [SPC * L, D])
    xs2 = dscr("xs2", [SPC * L, D])
    cs1 = dscr("cs1", [SPC * LC, D])
    cs2 = dscr("cs2", [SPC * LC, D])
    modrows = dscr("modrows", [5, 6 * D])
    NTMAX = (SPC * (L + LC)) // 128
    NSLMAX = NTMAX * 128 * 4 // TS + 32
    h2d = dscr("h2d", [NTMAX * 128, D], BF16)
    xsort = dscr("xsort", [NSLMAX * TS, D], BF16)
    ysort = dscr("ysort", [NSLMAX * TS, D], F32)

    with contextlib.ExitStack() as gst:
        S = Sched(nc, gst)

        def ACT(out, in_, func, reads, writes, **kw):
            S.op("act", lambda e: e.activation(out=out, in_=in_, func=func, **kw), reads, writes)

        def MM(out, lhsT, rhs, start, stop, reads, writes):
            S.op("pe", lambda e: e.matmul(out, lhsT=lhsT, rhs=rhs, start=start, stop=stop), reads, writes)

        def TR(out, in_, ident, reads, writes):
            S.op("pe", lambda e: e.transpose(out=out, in_=in_, identity=ident), reads, writes)

        def TTOP(eng, out, in0, in1, op, reads, writes):
            S.op(eng, lambda e: e.tensor_tensor(out=out, in0=in0, in1=in1, op=op), reads, writes)

        def TSC(eng, out, in0, s1, s2, op0, op1, reads, writes):
            if op1 is None:
                S.op(eng, lambda e: e.tensor_scalar(out=out, in0=in0, scalar1=s1, scalar2=None, op0=op0), reads, writes)
            else:
                S.op(eng, lambda e: e.tensor_scalar(out=out, in0=in0, scalar1=s1, scalar2=s2, op0=op0, op1=op1),
                     reads, writes)

        def STT(eng, out, in0, scalar, in1, op0, op1, reads, writes):
            eng = "dve"
            S.op(eng, lambda e: e.scalar_tensor_tensor(out=out, in0=in0, scalar=scalar, in1=in1, op0=op0, op1=op1),
                 reads, writes)

        def CP(eng, out, in_, reads, writes):
            if eng == "act":
                S.op("act", lambda e: e.copy(out=out, in_=in_), reads, writes)
            else:
                S.op(eng, lambda e: e.tensor_copy(out=out, in_=in_), reads, writes)

        def DMA(q, out, in_, reads, writes):
            S.dma(q, lambda e: e.dma_start(out=out, in_=in_), reads, writes)

        def rstd_from(ssum, n, tmp):
            TSC("dve", tmp[:], ssum[:], 1.0 / n, EPS, ALU.mult, ALU.add, [ssum], [tmp])
            ACT(tmp[:], tmp[:], AF.Sqrt, [tmp], [tmp])
            S.op("dve", lambda e: e.reciprocal(out=tmp[:], in_=tmp[:]), [tmp], [tmp])

        ident32 = S.sb([128, 128], F32, "ident32")
        identb = S.sb([128, 128], BF16, "identb")
        DMA("sp", ident32[:], k_ident, [], [ident32])
        DMA("pool", identb[:], k_ident, [], [identb])

        def phase_ada(l):
            with contextlib.ExitStack() as ph:
                S.stack = ph
                cT = S.sb([128, 8, 5], F32)
                sc = S.sb([128, 8, 5], F32)
                DMA("sp", cT[:], cT_in, [], [cT])
                ACT(sc[:], cT[:], AF.Silu, [cT], [sc])
                rows = S.sb([5, 6 * D], F32)
                bb = S.sb([5, 6 * D], F32)
                DMA("sp", bb[:], ada_b[l:l + 1, :].partition_broadcast(5), [], [bb])
                wts = [S.sb([128, 8, 512], F32) for _ in range(2)]
                pms = [S.ps([128, 512]) for _ in range(2)]
                for n in range(12):
                    wt = wts[n % 2]
                    pm = pms[n % 2]
                    DMA("sp", wt[:], ada_w[l, :, n * 512:(n + 1) * 512].rearrange("(c p) n -> p c n", p=128), [], [wt])
                    for c in range(8):
                        MM(pm[0:5, :], sc[:, c, :], wt[:, c, :], c == 0, c == 7, [sc, wt], [pm])
                    TTOP("dve", rows[:, n * 512:(n + 1) * 512], pm[0:5, :], bb[:, n * 512:(n + 1) * 512], ALU.add,
                         [pm, bb], [rows])
                DMA("sp", modrows[:, :], rows[:], [rows], [modrows])
            S.stack = gst
            S.barrier()

        def phase_mix(l, last, xsrc, xdst, csrc, cdst):
            with contextlib.ExitStack() as ph:
                S.stack = ph
                win = S.sb([128, 8, INW], BF16, "win")
                for c in range(8):
                    for hf in range(2):
                        DMA("pool", win[:, c, hf * 1216:(hf + 1) * 1216], w_in[l, c * 128:(c + 1) * 128, hf * 1216:(hf + 1) * 1216],
                            [], [win])
                wout = S.sb([128, 8, D], BF16, "wout")
                gnc = S.sb([128, 8], F32)
                DMA("sp", gnc[:], gncol[l], [], [gnc])
                dg = S.sb([128, 2, 31, 128], BF16)
                dw = S.sb([128, 2, 31], F32)
                DMA("sp", dw[:], dwT[l], [], [dw])
                for cc_ in range(2):
                    for j_ in range(31):
                        TSC("pool" if j_ % 2 else "dve", dg[:, cc_, j_, :], identb[:], dw[:, cc_, j_:j_ + 1], None, ALU.mult, None,
                            [identb, dw], [dg])
                ccols = S.sb([128, 3, 2], F32)
                DMA("sp", ccols[:], convcols[l], [], [ccols])
                cpw = S.sb([128, 2, 256], BF16)
                DMA("pool", cpw[:], conv_pw[l].rearrange("(c p) n -> p c n", p=128), [], [cpw])
                wsT = S.sb([128, 4, 128], BF16)
                DMA("pool", wsT[:], gwsT[l].rearrange("h q p -> q h p"), [], [wsT])
                bsT = S.sb([128, 4], F32)
                DMA("sp", bsT[:], gbsT[l], [], [bsT])
                glg = S.sb([128, 256], F32)
                glb = S.sb([128, 256], F32)
                DMA("sp", glg[:], gln_g[l:l + 1, :].partition_broadcast(128), [], [glg])
                DMA("sp", glb[:], gln_b[l:l + 1, :].partition_broadcast(128), [], [glb])
                pw = S.sb([64, 4, 64], BF16)
                DMA("pool", pw[:], pool_w[l].rearrange("g c j -> c g j"), [], [pw])
                psc = S.sb([128, 256], F32)
                DMA("sp", psc[:], pool_scale[l:l + 1, :].partition_broadcast(128), [], [psc])
                poolM = S.sb([128, 20, 128], BF16)
                for hf in range(2):
                    DMA("pool", poolM[:, hf * 10:(hf + 1) * 10, :], k_poolM[:, hf * 10:(hf + 1) * 10, :], [], [poolM])
                masks = S.sb([128, 2, 256], BF16)
                DMA("pool", masks[:], k_masks, [], [masks])
                esink = S.sb([128, 4], F32)
                DMA("sp", esink[:], attn_sink[l:l + 1, :].partition_broadcast(128), [], [esink])
                ACT(esink[:], esink[:], AF.Exp, [esink], [esink])
                ropeC = S.sb([128, 512], F32)
                ropeS = S.sb([128, 512], F32)
                gn1 = S.sb([128, D], F32)
                DMA("sp", gn1[:], norm1_g[l:l + 1, :].partition_broadcast(128), [], [gn1])
                onesm = S.sb([128, 128], F32)
                S.op("dve", lambda e: e.memset(onesm[:], 1.0 / 256.0), [], [onesm])
                A1 = S.sb([128, D], F32)
                B1 = S.sb([128, D], F32)
                G1 = S.sb([128, D], F32)

                qT = [S.sb([128, L], BF16) for _ in range(2)]
                kT = [S.sb([128, L], BF16) for _ in range(2)]
                kcT = [S.sb([128, LC], BF16) for _ in range(2)]
                vaug = S.sb([128, 16, 2, 65], BF16)
                vcaug = S.sb([128, 2, 2, 65], BF16)
                S.op("pool", lambda e: e.memset(vaug[:], 1.0), [], [vaug])
                S.op("pool", lambda e: e.memset(vcaug[:], 1.0), [], [vcaug])
                convT = S.sb([128, 2, L + 30], BF16)
                S.op("pool", lambda e: e.memset(convT[:], 0.0), [], [convT])
                sTb = S.sb([128, 2, L], BF16)
                gmb = S.sb([128, 16, 256], BF16)
                poolh = S.sb([128, 16, 256], BF16)
                xt1_ = S.sb([128, D], F32)
                xt = [xt1_, xt1_]
                ss = S.sb([128, 1], F32)
                rs = S.sb([128, 1], F32)
                h32 = S.sb([128, D], F32)
                hb = S.sb([128, D], BF16)
                hT = S.sb([128, 8, 512], BF16)
                ftmp = [S.sb([128, 512], F32) for _ in range(3)]
                u32 = S.sb([128, 256], F32)
                v32 = S.sb([128, 256], F32)
                vnb = S.sb([128, 256], BF16)
                st2 = S.sb([128, 2], F32)
                st3 = S.sb([128, 2], F32)
                acc = S.sb([128, 2, 512], F32)
                mean_sb = S.sb([128, 512], F32)
                rstd_sb = S.sb([128, 512], F32)
                PT = [S.sb([128, 5, 256], BF16) for _ in range(2)]
                den = S.sb([128, 4], F32)
                mix = S.sb([128, D], F32)
                gss = S.sb([128, 4], F32)
                grs = S.sb([128, 4], F32)
                yb = S.sb([128, D], BF16)
                yT = S.sb([128, 8, 128], BF16)
                yTp = S.sb([64, 4, 128], BF16)
                xn = h32
                wtmp = [h32, mix]
                for c in range(8):
                    DMA("sp", wtmp[c % 2][:], w_out[l, c * 128:(c + 1) * 128, :], [], [wtmp[c % 2]])
                    TSC("dve", wout[:, c, :], wtmp[c % 2][:], gnc[:, c:c + 1], None, ALU.mult, None,
                        [wtmp[c % 2], gnc], [wout])
                pT = [S.ps([128, 128], BF16) for _ in range(2)]
                pf = [S.ps([128, 512]) for _ in range(2)]
                pa = S.ps([128, 512])
                pb = S.ps([128, 512])
                pg = S.ps([128, 512])
                pq = S.ps([128, 512])

                def load_mod(r):
                    DMA("sp", A1[:], modrows[r:r + 1, D:2 * D].partition_broadcast(128), [modrows], [A1])
                    DMA("sp", B1[:], modrows[r:r + 1, 0:D].partition_broadcast(128), [modrows], [B1])
                    DMA("sp", G1[:], modrows[r:r + 1, 2 * D:3 * D].partition_broadcast(128), [modrows], [G1])
                    STT("dve", A1[:], A1[:], 1.0, gn1[:], ALU.add, ALU.mult, [A1, gn1], [A1])

                cp_i = [0]

                def cp_alt(out, in_, reads, writes):
                    cp_i[0] += 1
                    CP("act" if cp_i[0] % 2 else "dve", out, in_, reads, writes)

                def seq(src, dst, row0, LL, is_ctx, do_pass2):
                    NT = LL // 128
                    GT = min(512, LL)
                    TG = GT // 128
                    KT = kcT if is_ctx else kT
                    VA = vcaug if is_ctx else vaug
                    for g in range(LL // GT):
                        for ti in range(TG):
                            t = g * TG + ti
                            x_ = xt[t % 2]
                            DMA("sp", x_[:], src[row0 + t * 128: row0 + (t + 1) * 128, :], [src], [x_])
                            ACT(yb[:], x_[:], AF.Square, [x_], [yb, ss], accum_out=ss[:])
                            rstd_from(ss, D, rs)
                            STT("dve", h32[:], x_[:], rs[:, 0:1], A1[:], ALU.mult, ALU.mult, [x_, rs, A1], [h32])
                            TTOP("dve", hb[:], h32[:], B1[:], ALU.add, [h32, B1], [hb])
                            p_ = pT[t % 2]
                            for c in range(8):
                                TR(p_[:, c * 128:(c + 1) * 128], hb[:, c * 128:(c + 1) * 128], identb[:], [hb, identb], [p_])
                            cp_alt(hT[:, :, ti * 128:(ti + 1) * 128], p_[:, 0:1024].rearrange("p (c t) -> p c t", c=8), [p_], [hT])
                        if not is_ctx:
                            DMA("sp", ropeC[:, :GT], k_ropeC[:, g * GT:(g + 1) * GT], [], [ropeC])
                            DMA("sp", ropeS[:, :GT], k_ropeS[:, g * GT:(g + 1) * GT], [], [ropeS])
                        order = [0, 2, 1, 3, 4, 6, 5, 7, 10, 8, 11, 9] if not is_ctx else [0, 1, 4, 5, 10, 8, 11, 9]
                        tsl = slice(g * GT, (g + 1) * GT)
                        for oi, fc in enumerate(order):
                            p_ = pf[oi % 2]
                            for c in range(8):
                                MM(p_[:, :GT], win[:, c, fc * 128:(fc + 1) * 128], hT[:, c, :GT], c == 0, c == 7,
                                   [win, hT], [p_])
                            if fc in (0, 1, 4, 5):
                                dstT = (qT if not is_ctx else qT)[fc] if fc < 2 else KT[fc - 4]
                                if is_ctx:
                                    cp_alt(dstT[:, tsl], p_[:, :GT], [p_], [dstT])
                                else:
                                    TTOP("dve", ftmp[0][:, :GT], p_[:, :GT], ropeC[:, :GT], ALU.mult, [p_, ropeC], [ftmp[0]])
                            elif fc in (2, 3, 6, 7):
                                dstT = qT[fc - 2] if fc < 4 else KT[fc - 6]
                                TTOP("dve", ftmp[1][:, :GT], p_[:, :GT], ropeS[:, :GT], ALU.mult, [p_, ropeS], [ftmp[1]])
                                TTOP("dve", dstT[:, tsl], ftmp[0][:, :GT], ftmp[1][:, :GT], ALU.add,
                                     [ftmp[0], ftmp[1]], [dstT])
                            elif fc in (10, 11):
                                ACT(ftmp[2][:, :GT], p_[:, :GT], AF.Sigmoid, [p_], [ftmp[2]])
                            else:
                                cc = fc - 8
                                TTOP("dve", convT[:, cc, 15 + g * GT: 15 + (g + 1) * GT], p_[:, :GT], ftmp[2][:, :GT],
                                     ALU.mult, [p_, ftmp[2]], [convT])
                        for ti in range(TG):
                            t = g * TG + ti
                            for c in range(8):
                                MM(pa[:, :], hT[:, c, ti * 128:(ti + 1) * 128], win[:, c, 1536:2048], c == 0, c == 7,
                                   [hT, win], [pa])
                            for c in range(8):
                                MM(pb[:, 0:384], hT[:, c, ti * 128:(ti + 1) * 128], win[:, c, 2048:2432], c == 0, c == 7,
                                   [hT, win], [pb])
                            S.op("act", lambda e: e.copy(out=VA[:, t, :, 0:64],
                                                          in_=pb[:, 0:128].rearrange("p (j d) -> p j d", j=2)), [pb], [VA])
                            CP("dve", poolh[:, t, :], pb[:, 128:384], [pb], [poolh])
                            CP("act", u32[:], pa[:, 0:256], [pa], [u32])
                            ACT(v32[:], pa[:, 256:512], AF.Copy, [pa], [v32, st2], accum_out=st2[:, 0:1])
                            ACT(yb[:, 0:256], pa[:, 256:512], AF.Square, [pa], [yb, st2], accum_out=st2[:, 1:2])
                            TSC("dve", st3[:, 0:1], st2[:, 0:1], 1.0 / 256, None, ALU.mult, None, [st2], [st3])
                            STT("dve", st3[:, 1:2], st3[:, 0:1], -1.0, st3[:, 0:1], ALU.mult, ALU.mult, [st3], [st3])
                            STT("dve", st3[:, 1:2], st2[:, 1:2], 1.0 / 256, st3[:, 1:2], ALU.mult, ALU.add, [st2, st3], [st3])
                            TSC("dve", st3[:, 1:2], st3[:, 1:2], EPS, None, ALU.add, None, [st3], [st3])
                            ACT(st3[:, 1:2], st3[:, 1:2], AF.Sqrt, [st3], [st3])
                            S.op("dve", lambda e: e.reciprocal(out=st3[:, 1:2], in_=st3[:, 1:2]), [st3], [st3])
                            TSC("dve", v32[:], v32[:], st3[:, 0:1], st3[:, 1:2], ALU.subtract, ALU.mult, [v32, st3], [v32])
                            TTOP("dve", v32[:], v32[:], glg[:], ALU.mult, [v32, glg], [v32])
                            TTOP("dve", vnb[:], v32[:], glb[:], ALU.add, [v32, glb], [vnb])
                            for hh in range(4):
                                MM(pg[:, hh * 64:(hh + 1) * 64], wsT[:, hh, :], vnb[:, hh * 64:(hh + 1) * 64], True, True,
                                   [wsT, vnb], [pg])
                            for hh in range(4):
                                STT("dve", gmb[:, t, hh * 64:(hh + 1) * 64], pg[:, hh * 64:(hh + 1) * 64], bsT[:, hh:hh + 1],
                                    u32[:, hh * 64:(hh + 1) * 64], ALU.add, ALU.mult, [pg, bsT, u32], [gmb])
                    if not do_pass2:
                        return
                    for g in range(LL // GT):
                        tsl = slice(g * GT, (g + 1) * GT)
                        for cc in range(2):
                            pc_ = pq if cc == 0 else pg
                            for j in range(31):
                                MM(pc_[:, :GT], dg[:, cc, j, :], convT[:, cc, g * GT + j: g * GT + j + GT], j == 0, j == 30,
                                   [dg, convT], [pc_])
                            TSC("dve", acc[:, cc, :GT], pc_[:, :GT], ccols[:, 0, cc:cc + 1], None, ALU.add, None, [pc_, ccols], [acc])
                        for cc in range(2):
                            MM(pq[:, :GT], onesm[:], acc[:, cc, :GT], cc == 0, cc == 1, [onesm, acc], [pq])
                        CP("act", mean_sb[:, :GT], pq[:, :GT], [pq], [mean_sb])
                        for cc in range(2):
                            ACT(ftmp[cc][:, :GT], acc[:, cc, :GT], AF.Square, [acc], [ftmp[cc]])
                        for cc in range(2):
                            MM(pq[:, :GT], onesm[:], ftmp[cc][:, :GT], cc == 0, cc == 1, [onesm, ftmp[cc]], [pq])
                        TTOP("dve", rstd_sb[:, :GT], mean_sb[:, :GT], mean_sb[:, :GT], ALU.mult, [mean_sb], [rstd_sb])
                        TTOP("dve", rstd_sb[:, :GT], pq[:, :GT], rstd_sb[:, :GT], ALU.subtract, [pq, rstd_sb], [rstd_sb])
                        TSC("dve", rstd_sb[:, :GT], rstd_sb[:, :GT], EPS, None, ALU.add, None, [rstd_sb], [rstd_sb])
                        ACT(rstd_sb[:, :GT], rstd_sb[:, :GT], AF.Sqrt, [rstd_sb], [rstd_sb])
                        S.op("dve", lambda e: e.reciprocal(out=rstd_sb[:, :GT], in_=rstd_sb[:, :GT]), [rstd_sb], [rstd_sb])
                        for cc in range(2):
                            TTOP("dve", acc[:, cc, :GT], acc[:, cc, :GT], mean_sb[:, :GT], ALU.subtract, [acc, mean_sb], [acc])
                            TTOP("dve", acc[:, cc, :GT], acc[:, cc, :GT], rstd_sb[:, :GT], ALU.mult, [acc, rstd_sb], [acc])
                            ACT(sTb[:, cc, tsl], acc[:, cc, :GT], AF.Silu, [acc, ccols], [sTb],
                                scale=ccols[:, 1, cc:cc + 1], bias=ccols[:, 2, cc:cc + 1])
                        for ti in range(TG):
                            t = g * TG + ti
                            qs = slice(t * 128, (t + 1) * 128)
                            x_ = xt[t % 2]
                            DMA("sp", x_[:], src[row0 + t * 128: row0 + (t + 1) * 128, :], [src], [x_])
                            blocks = []
                            for cb in range(2):
                                blocks.append((kcT, slice(cb * 128, (cb + 1) * 128), vcaug, cb, None))
                            if not is_ctx:
                                if t > 0:
                                    blocks.append((kT, slice((t - 1) * 128, t * 128), vaug, t - 1, 0))
                                blocks.append((kT, qs, vaug, t, None))
                                if t < NT - 1:
                                    blocks.append((kT, slice((t + 1) * 128, (t + 2) * 128), vaug, t + 1, 1))
                            nb = len(blocks)
                            for j in range(2):
                                P_ = PT[j]
                                for lo in range(0, nb, 4):
                                    hi = min(nb, lo + 4)
                                    for gq in range(2):
                                        pr = slice(64 * gq, 64 * gq + 64)
                                        psc_ = pa if gq == 0 else pq
                                        for bi in range(lo, hi):
                                            Ks, ksl, Vt, vi, mi = blocks[bi]
                                            MM(psc_[:, (bi - lo) * 128:(bi - lo + 1) * 128], Ks[j][pr, ksl], qT[j][pr, qs], True, True,
                                               [Ks[j], qT[j]], [psc_])
                                    for gq in range(2):
                                        psc_ = pa if gq == 0 else pq
                                        ACT(P_[:, lo:hi, gq * 128:(gq + 1) * 128],
                                            psc_[:, 0:(hi - lo) * 128].rearrange("p (b q) -> p b q", q=128), AF.Exp, [psc_], [P_], scale=0.125)
                                for bi, (Ks, ksl, Vt, vi, mi) in enumerate(blocks):
                                    if mi is not None:
                                        TTOP("dve", P_[:, bi, :], P_[:, bi, :], masks[:, mi, :], ALU.mult, [P_, masks], [P_])
                                for gq in range(2):
                                    hh = 2 * j + gq
                                    for bi, (Ks, ksl, Vt, vi, mi) in enumerate(blocks):
                                        MM(pb[:, hh * 65:(hh + 1) * 65], P_[:, bi, gq * 128:(gq + 1) * 128], Vt[:, vi, j, :],
                                           bi == 0, bi == nb - 1, [P_, Vt], [pb])
                            pb3 = pb[:, 0:260].rearrange("p (h d) -> p h d", h=4)
                            TTOP("dve", den[:], pb3[:, :, 64], esink[:], ALU.add, [pb, esink], [den])
                            S.op("dve", lambda e: e.reciprocal(out=den[:], in_=den[:]), [den], [den])
                            for hh in range(4):
                                if hh % 2 == 0:
                                    TSC("dve", mix[:, hh * 64:(hh + 1) * 64], pb[:, hh * 65: hh * 65 + 64], den[:, hh:hh + 1], None,
                                        ALU.mult, None, [pb, den], [mix])
                                else:
                                    ACT(mix[:, hh * 64:(hh + 1) * 64], pb[:, hh * 65: hh * 65 + 64], AF.Copy, [pb, den], [mix],
                                        scale=den[:, hh:hh + 1])
                            for cc in range(2):
                                MM(pg[:, 0:256], sTb[:, cc, qs], cpw[:, cc, :], cc == 0, cc == 1, [sTb, cpw], [pg])
                            CP("act", mix[:, 256:512], pg[:, 0:256], [pg], [mix])
                            CP("dve", mix[:, 512:768], gmb[:, t, :], [gmb], [mix])
                            for gi in range(4):
                                srcs = []
                                if t > 0:
                                    srcs.append((t - 1, gi * 5 + 0))
                                srcs.append((t, gi * 5 + (3 if t == 0 else (4 if t == NT - 1 else 2))))
                                if t < NT - 1:
                                    srcs.append((t + 1, gi * 5 + 1))
                                for si, (tt_, mi) in enumerate(srcs):
                                    MM(pq[0:64, gi * 128:(gi + 1) * 128], poolh[:, tt_, gi * 64:(gi + 1) * 64], poolM[:, mi, :],
                                       si == 0, si == len(srcs) - 1, [poolh, poolM], [pq])
                            CP("dve", yTp[:, :, :], pq[0:64, :].rearrange("p (g t) -> p g t", g=4), [pq], [yTp])
                            for gi in range(4):
                                MM(pg[:, 256 + gi * 64: 256 + (gi + 1) * 64], yTp[:, gi, :], pw[:, gi, :], True, True,
                                   [yTp, pw], [pg])
                            TTOP("dve", mix[:, 768:1024], pg[:, 256:512], psc[:], ALU.mult, [pg, psc], [mix])
                            for gi in range(4):
                                ACT(hb[:, 0:256], mix[:, gi * 256:(gi + 1) * 256], AF.Square, [mix], [hb, gss],
                                    accum_out=gss[:, gi:gi + 1])
                            rstd_from(gss, 256, grs)
                            for gi in range(4):
                                if gi % 2 == 0:
                                    TSC("dve", yb[:, gi * 256:(gi + 1) * 256], mix[:, gi * 256:(gi + 1) * 256], grs[:, gi:gi + 1],
                                        None, ALU.mult, None, [mix, grs], [yb])
                                else:
                                    TSC("dve", yb[:, gi * 256:(gi + 1) * 256], mix[:, gi * 256:(gi + 1) * 256], grs[:, gi:gi + 1],
                                        None, ALU.mult, None, [mix, grs], [yb])
                            p_ = pT[t % 2]
                            for c in range(8):
                                TR(p_[:, c * 128:(c + 1) * 128], yb[:, c * 128:(c + 1) * 128], identb[:], [yb, identb], [p_])
                            cp_alt(yT[:, :, :], p_[:, 0:1024].rearrange("p (c t) -> p c t", c=8), [p_], [yT])
                            for nbk in range(2):
                                p_ = pf[nbk]
                                for c in range(8):
                                    MM(p_[:, :], yT[:, c, :], wout[:, c, nbk * 512:(nbk + 1) * 512], c == 0, c == 7, [yT, wout], [p_])
                                TTOP("dve", xn[:, nbk * 512:(nbk + 1) * 512], p_[:, :], G1[:, nbk * 512:(nbk + 1) * 512], ALU.mult,
                                     [p_, G1], [xn])
                            TTOP("dve", xn[:], xn[:], x_[:], ALU.add, [xn, x_], [xn])
                            DMA("sp", dst[row0 + t * 128: row0 + (t + 1) * 128, :], xn[:], [xn], [dst])

                import os
                lim = os.environ.get("MIXLIM", "")
                for s in range(SPC):
                    load_mod(4)
                    seq(csrc, cdst, s * LC, LC, True, not last)
                    if lim == "c":
                        break
                    load_mod(s)
                    seq(xsrc, xdst, s * L, L, False, True)
                    if lim == "cx":
                        break
            S.stack = gst
            S.barrier()

        def phase_moe(l, last, tiles, final):
            NT = len(tiles)
            NSL = NT * 128 * 4 // TS + 32
            with contextlib.ExitStack() as ph:
                S.stack = ph
                idxi = S.sb([128, NT, 4], I32, "idxi")
                gate4 = S.sb([128, NT, 4], F32, "gate4")
                widx = S.sb([128, 128], I32, "widx")
                w2idx = S.sb([128, 128], I32, "w2idx")
                bidx = S.sb([128, 128], I32, "bidx")
                Gd = S.sb([128, NT, 32], F32, "Gd")
                A2 = S.sb([128, D], F32)
                B2 = S.sb([128, D], F32)
                G2 = S.sb([128, D], F32)
                gn2 = S.sb([128, D], F32)
                DMA("sp", gn2[:], norm2_g[l:l + 1, :].partition_broadcast(128), [], [gn2])
                cur = [None]

                def load_mod(r, which):
                    if cur[0] == (r, which):
                        return
                    cur[0] = (r, which)
                    if which == 0:
                        DMA("sp", A2[:], modrows[r:r + 1, 4 * D:5 * D].partition_broadcast(128), [modrows], [A2])
                        DMA("sp", B2[:], modrows[r:r + 1, 3 * D:4 * D].partition_broadcast(128), [modrows], [B2])
                        STT("dve", A2[:], A2[:], 1.0, gn2[:], ALU.add, ALU.mult, [A2, gn2], [A2])
                    else:
                        DMA("sp", G2[:], modrows[r:r + 1, 5 * D:6 * D].partition_broadcast(128), [modrows], [G2])

                with contextlib.ExitStack() as ph1:
                    S.stack = ph1
                    rw = S.sb([128, 8, 32], F32)
                    DMA("sp", rw[:], router_w[l].rearrange("(c p) n -> p c n", p=128), [], [rw])
                    rbb = S.sb([128, 32], F32)
                    DMA("sp", rbb[:], router_b[l:l + 1, :].partition_broadcast(128), [], [rbb])
                    upper = S.sb([128, 128], BF16)
                    DMA("pool", upper[:], k_upper, [], [upper])
                    onesb = S.sb([128, 128], BF16)
                    S.op("dve", lambda e: e.memset(onesb[:], 1.0), [], [onesb])
                    ones32 = S.sb([128, 1], F32)
                    S.op("dve", lambda e: e.memset(ones32[:], 1.0), [], [ones32])
                    thr = S.sb([32, 128], F32)
                    DMA("sp", thr[:], k_slotthr, [], [thr])
                    lg = S.sb([128, NT, 32], F32)
                    t8 = S.sb([128, NT, 8], F32)
                    pos = S.sb([128, NT, 32], F32)
                    base = S.sb([128, 32], F32)
                    S.op("dve", lambda e: e.memset(base[:], 0.0), [], [base])
                    xt = [S.sb([128, D], F32) for _ in range(2)]
                    junk = S.sb([128, D], BF16)
                    ss = S.sb([128, 1], F32)
                    rs = S.sb([128, 1], F32)
                    h32 = S.sb([128, D], F32)
                    hb = [S.sb([128, D], BF16) for _ in range(2)]
                    hT32 = S.sb([128, 8, 128], F32)
                    maskb = S.sb([128, 32], BF16)
                    zt = S.sb([128, 2048], BF16)
                    S.op("pool", lambda e: e.memset(zt[:], 0.0), [], [zt])
                    pT32 = [S.ps([128, 128], F32) for _ in range(2)]
                    pl = S.ps([128, 512])
                    pp = S.ps([128, 512])
                    xz = xsort[0:NSL * TS, :].rearrange("(n p r) d -> n p (r d)", p=128, r=2)
                    zlist = list(range(NSL * TS // 256))
                    zper = -(-len(zlist) // (NT // 2))
                    h32s = [h32, S.sb([128, D], F32)]
                    hT32s = [hT32, S.sb([128, 8, 128], F32)]
                    junks = [junk, S.sb([128, D], BF16)]
                    sss = [ss, S.sb([128, 1], F32)]
                    rss = [rs, S.sb([128, 1], F32)]
                    maskbs = [maskb, S.sb([128, 32], BF16)]
                    pT32s = [pT32, [S.ps([128, 128], F32) for _ in range(2)]]
                    pls = [pl, S.ps([128, 512])]
                    pps = [pp, S.ps([128, 512])]

                    def route_tile(t, k):
                        src, dst, row0, r = tiles[t]
                        x_, h32_, hb_, hT_, jk, ss_, rs_, mk = xt[k], h32s[k], hb[k], hT32s[k], junks[k], sss[k], rss[k], maskbs[k]
                        pTk, pl_, pp_ = pT32s[k], pls[k], pps[k]
                        DMA("sp", x_[:], src[row0:row0 + 128, :], [src], [x_])
                        yield
                        ACT(jk[:], x_[:], AF.Square, [x_], [jk, ss_], accum_out=ss_[:])
                        yield
                        TSC("dve", rs_[:], ss_[:], 1.0 / D, EPS, ALU.mult, ALU.add, [ss_], [rs_])
                        yield
                        ACT(rs_[:], rs_[:], AF.Sqrt, [rs_], [rs_])
                        yield
                        S.op("dve", lambda e: e.reciprocal(out=rs_[:], in_=rs_[:]), [rs_], [rs_])
                        yield
                        STT("dve", h32_[:], x_[:], rs_[:, 0:1], A2[:], ALU.mult, ALU.mult, [x_, rs_, A2], [h32_])
                        yield
                        TTOP("dve", h32_[:], h32_[:], B2[:], ALU.add, [h32_, B2], [h32_])
                        yield
                        CP("act", hb_[:], h32_[:], [h32_], [hb_])
                        DMA("sp", h2d[t * 128:(t + 1) * 128, :], hb_[:], [hb_], [h2d])
                        yield
                        for hf in range(2):
                            p_ = pTk[hf]
                            for cc in range(4):
                                c = hf * 4 + cc
                                TR(p_[:, cc * 128:(cc + 1) * 128], h32_[:, c * 128:(c + 1) * 128], ident32[:], [h32_, ident32], [p_])
                            CP("act" if hf else "dve", hT_[:, hf * 4:(hf + 1) * 4, :],
                               p_[:, 0:512].rearrange("p (c t) -> p c t", c=4), [p_], [hT_])
                            yield
                        for c in range(8):
                            MM(pl_[:, 0:32], hT_[:, c, :], rw[:, c, :], c == 0, c == 7, [hT_, rw], [pl_])
                        yield
                        TTOP("dve", lg[:, t, :], pl_[:, 0:32], rbb[:], ALU.add, [pl_, rbb], [lg])
                        yield
                        S.op("dve", lambda e: e.max(out=t8[:, t, :], in_=lg[:, t, :]), [lg], [t8])
                        yield
                        TSC("dve", mk[:], lg[:, t, :], t8[:, t, 3:4], None, ALU.is_ge, None, [lg, t8], [mk])
                        yield
                        MM(pp_[:, 0:32], upper[:], mk[:], True, True, [upper, mk], [pp_])
                        MM(pp_[:, 32:64], onesb[:], mk[:], True, True, [onesb, mk], [pp_])
                        yield
                        TSC("dve", ss_[:], t8[:, t, 0:1], -1.0, None, ALU.mult, None, [t8], [ss_])
                        yield
                        ACT(gate4[:, t, :], t8[:, t, 0:4], AF.Exp, [t8, ss_], [gate4, rs_], bias=ss_[:, 0:1], accum_out=rs_[:, 0:1])
                        yield
                        S.op("dve", lambda e: e.reciprocal(out=rs_[:], in_=rs_[:]), [rs_], [rs_])
                        yield
                        TSC("dve", gate4[:, t, :], gate4[:, t, :], rs_[:, 0:1], None, ALU.mult, None, [gate4, rs_], [gate4])
                        ACT(Gd[:, t, :], lg[:, t, :], AF.Exp, [lg, ss_], [Gd], bias=ss_[:, 0:1])
                        yield
                        TTOP("dve", Gd[:, t, :], Gd[:, t, :], mk[:], ALU.mult, [Gd, mk], [Gd])
                        yield
                        TSC("dve", Gd[:, t, :], Gd[:, t, :], rs_[:, 0:1], None, ALU.mult, None, [Gd, rs_], [Gd])
                        TTOP("dve", pos[:, t, :], pp_[:, 0:32], base[:], ALU.add, [pp_, base], [pos])
                        TTOP("dve", base[:], pp_[:, 32:64], base[:], ALU.add, [pp_, base], [base])
                        yield

                    for t0 in range(0, NT, 2):
                        load_mod(tiles[t0][3], 0)
                        alive = [route_tile(t0, 0), route_tile(t0 + 1, 1)]
                        while alive:
                            for g_ in list(alive):
                                try:
                                    next(g_)
                                except StopIteration:
                                    alive.remove(g_)
                        for _z in range(zper):
                            if zlist:
                                DMA("sp", xz[zlist.pop()], zt[:], [zt], [xsort])
                    while zlist:
                        DMA("sp", xz[zlist.pop()], zt[:], [zt], [xsort])
                    padded = S.sb([128, 32], F32)
                    pend = S.sb([128, 32], F32)
                    pstart = S.sb([128, 32], F32)
                    tmpc = S.sb([128, 32], F32)
                    TSC("dve", padded[:], base[:], 0.0, None, ALU.is_gt, None, [base], [padded])
                    for jj in range(1, NT * 128 // TS + 1):
                        STT("dve", padded[:], base[:], float(TS * jj), padded[:], ALU.is_gt, ALU.add, [base, padded], [padded])
                    TSC("dve", padded[:], padded[:], float(TS), None, ALU.mult, None, [padded], [padded])
                    CP("dve", pend[:, 0:1], padded[:, 0:1], [padded], [pend])
                    for e_ in range(1, 32):
                        TTOP("dve", pend[:, e_:e_ + 1], pend[:, e_ - 1:e_], padded[:, e_:e_ + 1], ALU.add, [pend, padded], [pend])
                    TTOP("dve", pstart[:], pend[:], padded[:], ALU.subtract, [pend, padded], [pstart])
                    pcol = S.sb([32, 1], F32)
                    tmpd = S.sb([32, 32], F32)
                    TTOP("dve", tmpd[:], pend[0:32, :], ident32[0:32, 0:32], ALU.mult, [pend, ident32], [tmpd])
                    S.op("dve", lambda e: e.reduce_sum(out=pcol[:], in_=tmpd[:], axis=AX.X), [tmpd], [pcol])
                    cmp = S.sb([32, 128], F32)
                    TSC("dve", cmp[:], thr[:], pcol[:, 0:1], None, ALU.is_ge, None, [thr, pcol], [cmp])
                    ones32m = S.sb([32, 128], F32)
                    S.op("dve", lambda e: e.memset(ones32m[:], 1.0), [], [ones32m])
                    piota = S.sb([128, 1], F32)
                    DMA("sp", piota[:], k_piota, [], [piota])
                    MM(pl[:, 0:128], ones32m[0:32, :], cmp[:], True, True, [ones32m, cmp], [pl])
                    blkf = S.sb([128, 128], F32)
                    TSC("dve", blkf[:], pl[:, 0:128], 31.0, None, ALU.min, None, [pl], [blkf])
                    wif = S.sb([128, 128], F32)
                    same = S.sb([128, 128], F32)
                    S.op("dve", lambda e: e.memset(same[:], 0.0), [], [same])
                    TTOP("dve", same[:, 2:128], blkf[:, 2:128], blkf[:, 0:126], ALU.is_equal, [blkf], [same])
                    TSC("dve", same[:], same[:], SKIPBIG, None, ALU.mult, None, [same], [same])
                    STT("dve", wif[:], blkf[:], 1024.0, same[:], ALU.mult, ALU.add, [blkf, same], [wif])
                    TSC("dve", wif[:], wif[:], piota[:, 0:1], None, ALU.add, None, [wif, piota], [wif])
                    CP("dve", widx[:], wif[:], [wif], [widx])
                    STT("dve", wif[:], blkf[:], 512.0, same[:], ALU.mult, ALU.add, [blkf, same], [wif])
                    TSC("dve", wif[:], wif[:], piota[:, 0:1], None, ALU.add, None, [wif, piota], [wif])
                    CP("dve", w2idx[:], wif[:], [wif], [w2idx])
                    STT("dve", wif[:], blkf[:], 128.0, same[:], ALU.mult, ALU.add, [blkf, same], [wif])
                    TSC("dve", wif[:], wif[:], piota[:, 0:1], None, ALU.add, None, [wif, piota], [wif])
                    CP("dve", bidx[:], wif[:], [wif], [bidx])
                    oh = S.sb([128, 32], F32)
                    idxf = S.sb([128, 4], F32)
                    for t, (src, dst, row0, r) in enumerate(tiles):
                        TTOP("dve", pos[:, t, :], pos[:, t, :], pstart[:], ALU.add, [pos, pstart], [pos])
                        for k in range(4):
                            STT("dve", oh[:], lg[:, t, :], t8[:, t, k:k + 1], pos[:, t, :], ALU.is_equal, ALU.mult,
                                [lg, t8, pos], [oh])
                            S.op("dve", lambda e: e.reduce_sum(out=idxf[:, k:k + 1], in_=oh[:], axis=AX.X), [oh], [idxf])
                        CP("dve", idxi[:, t, :], idxf[:], [idxf], [idxi])
                        hb_ = hb[t % 2]
                        DMA("sp", hb_[:], h2d[t * 128:(t + 1) * 128, :], [h2d], [hb_])
                        for k in range(4):
                            S.dma("pool", lambda e: e.indirect_dma_start(
                                out=xsort[:, :], out_offset=bass.IndirectOffsetOnAxis(ap=idxi[:, t, k:k + 1], axis=0),
                                in_=hb_[:], in_offset=None), [hb_, idxi], [xsort])
                S.stack = ph
                S.barrier()
                if stop_after == ("route", l):
                    return
                with contextlib.ExitStack() as ph2:
                    S.stack = ph2
                    w1b = [S.sb([128, 8, 2 * D], BF16) for _ in range(2)]
                    w2b = [S.sb([128, 8, D], BF16) for _ in range(2)]
                    b1c = [S.sb([128, 16], F32) for _ in range(2)]
                    xs_ = [S.sb([128, 4, D], BF16) for _ in range(2)]
                    xsT2 = [S.sb([128, 8, TS], BF16) for _ in range(2)]
                    actT = S.sb([128, 8, TS], BF16)
                    b1p = [S.sb([128, 8], F32) for _ in range(2)]
                    linp = [S.sb([128, TS], F32) for _ in range(2)]
                    g32s = [S.sb([128, TS], F32) for _ in range(2)]
                    sgs = [S.sb([128, TS], F32) for _ in range(2)]
                    tts = [S.sb([128, TS], F32) for _ in range(2)]
                    yt = [S.sb([128, D], F32) for _ in range(2)]
                    pT = [S.ps([128, 128], BF16) for _ in range(2)]
                    ph_ = [S.ps([128, 512]) for _ in range(4)]
                    py = [S.ps([128, 512]) for _ in range(2)]
                    w1flat = exp_w1.rearrange("l e k n -> (l e k) n")
                    w2flat = exp_w2.rearrange("l e c p n -> (l e c p) n")
                    b1flat = exp_b1T.rearrange("l e p n -> (l e p) n")

                    if not hasattr(build, "_bnd") or build._bnd[0] is not nc:
                        rg_ = nc.gpsimd.alloc_register("bndreg")
                        nc.gpsimd.reg_mov(rg_, 2 * 32 * 1024 - 1)
                        build._bnd = (nc, rg_)
                    bnd_reg = build._bnd[1]

                    def load_w(j):
                        b = j % 2
                        for c in range(8):
                            S.dma("pool", lambda e: e.indirect_dma_start(
                                out=w1b[b][:, c, :], out_offset=None, in_=w1flat,
                                in_offset=bass.IndirectOffsetOnAxis(ap=widx[:, j:j + 1], axis=0),
                                element_offset=(l * 32 * 1024 + c * 128) * 2048, bounds_check=bnd_reg, oob_is_err=False), [widx], [w1b[b]])
                        for c2 in range(4):
                            S.dma("pool", lambda e: e.indirect_dma_start(
                                out=w2b[b][:, 2 * c2:2 * c2 + 2, :].rearrange("p c n -> p (c n)"), out_offset=None, in_=w2flat,
                                in_offset=bass.IndirectOffsetOnAxis(ap=w2idx[:, j:j + 1], axis=0),
                                element_offset=(l * 128 + c2) * 128 * 2048, bounds_check=bnd_reg, oob_is_err=False),
                                [w2idx], [w2b[b]])
                        S.dma("pool", lambda e: e.indirect_dma_start(
                            out=b1c[b][:], out_offset=None, in_=b1flat,
                            in_offset=bass.IndirectOffsetOnAxis(ap=bidx[:, j:j + 1], axis=0),
                            element_offset=l * 32 * 128 * 16, bounds_check=bnd_reg, oob_is_err=False), [bidx], [b1c[b]])

                    def load_x(j):
                        DMA("sp", xs_[j % 2][:], xsort[j * TS:(j + 1) * TS, :].rearrange("(a p) d -> p a d", p=128),
                            [xsort], [xs_[j % 2]])

                    load_w(0)
                    load_x(0)
                    for j in range(NSL):
                        b = j % 2
                        if j + 1 < NSL:
                            load_w(j + 1)
                            load_x(j + 1)
                        W1, W2, B1c, X, xsT = w1b[b], w2b[b], b1c[b], xs_[b], xsT2[b]
                        TSC("dve", b1p[b][:], B1c[:, 8:16], 1.0, None, ALU.add, None, [B1c], [b1p[b]])
                        for a in range(4):
                            p_ = pT[a % 2]
                            for c in range(8):
                                TR(p_[:, c * 128:(c + 1) * 128], X[:, a, c * 128:(c + 1) * 128], identb[:], [X, identb], [p_])
                            CP("act" if a % 2 else "dve", xsT[:, :, a * 128:(a + 1) * 128],
                               p_[:, 0:1024].rearrange("p (c t) -> p c t", c=8), [p_], [xsT])
                        pi = 0
                        for i in range(8):
                            k2 = i % 2
                            pl_ = ph_[pi % 4]
                            pi += 1
                            for c in range(8):
                                MM(pl_[:, :], W1[:, c, (8 + i) * 128:(9 + i) * 128], xsT[:, c, :], c == 0, c == 7, [W1, xsT], [pl_])
                            TSC("dve", linp[k2][:], pl_[:, :], b1p[b][:, i:i + 1], -6.0, ALU.add, ALU.max, [pl_, b1p[b]], [linp[k2]])
                            pg_ = ph_[pi % 4]
                            pi += 1
                            for c in range(8):
                                MM(pg_[:, :], W1[:, c, i * 128:(i + 1) * 128], xsT[:, c, :], c == 0, c == 7, [W1, xsT], [pg_])
                            TSC("dve", g32s[k2][:], pg_[:, :], B1c[:, i:i + 1], 7.0, ALU.add, ALU.min, [pg_, B1c], [g32s[k2]])
                            ACT(sgs[k2][:], g32s[k2][:], AF.Sigmoid, [g32s[k2]], [sgs[k2]], scale=1.702)
                            TTOP("dve", tts[k2][:], sgs[k2][:], g32s[k2][:], ALU.mult, [sgs[k2], g32s[k2]], [tts[k2]])
                            STT("dve", actT[:, i, :], linp[k2][:], 8.0, tts[k2][:], ALU.min, ALU.mult, [linp[k2], tts[k2]], [actT])
                        for a in range(4):
                            y_ = yt[a % 2]
                            for nbk in range(2):
                                p_ = py[nbk]
                                for f in range(8):
                                    MM(p_[:, :], actT[:, f, a * 128:(a + 1) * 128], W2[:, f, nbk * 512:(nbk + 1) * 512],
                                       f == 0, f == 7, [actT, W2], [p_])
                                CP("act", y_[:, nbk * 512:(nbk + 1) * 512], p_[:, :], [p_], [y_])
                            DMA("sp", ysort[j * TS + a * 128: j * TS + (a + 1) * 128, :], y_[:], [y_], [ysort])
                S.stack = ph
                S.barrier()
                with contextlib.ExitStack() as ph3:
                    S.stack = ph3
                    yk = [[S.sb([128, D], F32) for _ in range(4)] for _ in range(2)]
                    xt = [S.sb([128, D], F32) for _ in range(2)]
                    acc = S.sb([128, D], F32)
                    xn = [S.sb([128, D], F32) for _ in range(2)]
                    junk = S.sb([128, D], BF16)
                    ss = S.sb([128, 1], F32)
                    rs = S.sb([128, 1], F32)
                    fng = S.sb([128, D], F32)
                    DMA("sp", fng[:], final_g[0:1, :].partition_broadcast(128), [], [fng])
                    b2all = S.sb([32, D], F32)
                    DMA("sp", b2all[:], exp_b2[l], [], [b2all])
                    GTs = S.sb([32, 128], F32)
                    pGT = S.ps([128, 512])
                    pbias = [S.ps([128, 512]) for _ in range(2)]
                    for t, (src, dst, row0, r) in enumerate(tiles):
                        load_mod(r, 1)
                        x_ = xt[t % 2]
                        Y = yk[t % 2]
                        xo = xn[t % 2]
                        DMA("sp", x_[:], src[row0:row0 + 128, :], [src], [x_])
                        for k in range(4):
                            S.dma("pool", lambda e: e.indirect_dma_start(
                                out=Y[k][:], out_offset=None, in_=ysort[:, :],
                                in_offset=bass.IndirectOffsetOnAxis(ap=idxi[:, t, k:k + 1], axis=0)), [ysort, idxi], [Y[k]])
                        TR(pGT[0:32, 0:128], Gd[:, t, :], ident32[:], [Gd, ident32], [pGT])
                        CP("act", GTs[:], pGT[0:32, 0:128], [pGT], [GTs])
                        for nbk in range(2):
                            MM(pbias[nbk][:, :], GTs[0:32, :], b2all[0:32, nbk * 512:(nbk + 1) * 512], True, True,
                               [GTs, b2all], [pbias[nbk]])
                        TSC("dve", acc[:], Y[0][:], gate4[:, t, 0:1], None, ALU.mult, None, [Y[0], gate4], [acc])
                        for k in range(1, 4):
                            STT("dve" if k != 2 else "pool", acc[:], Y[k][:], gate4[:, t, k:k + 1], acc[:], ALU.mult, ALU.add,
                                [Y[k], gate4, acc], [acc])
                        for nbk in range(2):
                            TTOP("dve", acc[:, nbk * 512:(nbk + 1) * 512], acc[:, nbk * 512:(nbk + 1) * 512], pbias[nbk][:, :], ALU.add,
                                 [acc, pbias[nbk]], [acc])
                        TTOP("dve", acc[:], acc[:], G2[:], ALU.mult, [acc, G2], [acc])
                        TTOP("dve", xo[:], acc[:], x_[:], ALU.add, [acc, x_], [xo])
                        if final:
                            ACT(junk[:], xo[:], AF.Square, [xo], [junk, ss], accum_out=ss[:])
                            rstd_from(ss, D, rs)
                            STT("dve", xo[:], xo[:], rs[:, 0:1], fng[:], ALU.mult, ALU.mult, [xo, rs, fng], [xo])
                        DMA("sp", dst[row0:row0 + 128, :], xo[:], [xo], [dst])
            S.stack = gst
            S.barrier()

        def tiles_for(xsrc, xdst, csrc, cdst, with_ctx):
            tl = []
            for s in range(SPC):
                for t in range(L // 128):
                    tl.append((xsrc, xdst, s * L + t * 128, s))
            if with_ctx:
                for s in range(SPC):
                    for t in range(LC // 128):
                        tl.append((csrc, cdst, s * LC + t * 128, 4))
            return tl

        done = False
        for l in range(2):
            last = l == 1
            phase_ada(l)
            if stop_after == ("ada", 0):
                break
            if l == 0:
                phase_mix(0, False, x_in, xs1, c_in, cs1)
                if stop_after == ("mix", 0):
                    break
                phase_moe(0, False, tiles_for(xs1, xs2, cs1, cs2, True), False)
                if stop_after in (("route", 0), ("moe", 0)):
                    break
            else:
                phase_mix(1, True, xs2, xs1, cs2, None)
                phase_moe(1, True, tiles_for(xs1, out_t, None, None, False), True)
        if stop_after is not None:
            S.barrier()
            if stop_after[0] == "ada":
                DMA("sp", out_t[0:30, :].rearrange("(r k) d -> r (k d)", r=5), modrows[:, :], [modrows], [out_t])
            else:
                srcd = {"mix": xs1, "moe": xs2, "route": xs1}[stop_after[0]]
                import os
                for i_ in range((L if os.environ.get("MIXLIM") else SPC * L) // 128):
                    DMA("sp", out_t[i_ * 128:(i_ + 1) * 128, :], srcd[i_ * 128:(i_ + 1) * 128, :], [srcd], [out_t])
        S.finish()
        build.stats = (S.n_inst, S.n_wait)
    return nc


def _prep_shared(inp):
    f = lambda a: np.ascontiguousarray(np.asarray(a, dtype=np.float32))
    cols = _ext_cols()
    sh = {}
    sh["ada_w"] = f(inp["ada_w"])
    sh["ada_b"] = f(inp["ada_b"])
    sh["norm1_g"] = f(inp["norm1_g"])
    sh["norm2_g"] = f(inp["norm2_g"])
    sh["w_in_ext"] = f(np.asarray(inp["w_in"])[:, :, cols])
    sh["attn_sink"] = f(inp["attn_sink"])
    dw = np.asarray(inp["conv_dw_w"])
    sh["conv_dwT"] = f(dw.transpose(0, 2, 1).reshape(2, 2, 128, 31).transpose(0, 2, 1, 3))
    cc = np.stack([np.asarray(inp["conv_dw_b"]), np.asarray(inp["conv_ln_g"]), np.asarray(inp["conv_ln_b"])], 1)
    sh["convcols"] = f(cc.reshape(2, 3, 2, 128).transpose(0, 3, 1, 2))
    sh["conv_pw_w"] = f(inp["conv_pw_w"])
    sh["gmlp_ln_g"] = f(inp["gmlp_ln_g"])
    sh["gmlp_ln_b"] = f(inp["gmlp_ln_b"])
    sh["gmlp_wsT"] = f(np.asarray(inp["gmlp_ws"]).transpose(0, 1, 3, 2))
    sh["gmlp_bsT"] = f(np.asarray(inp["gmlp_bs"]).transpose(0, 2, 1))
    sh["pool_w"] = f(inp["pool_w"])
    sh["pool_scale"] = f(inp["pool_scale"])
    sh["gncol"] = f(np.asarray(inp["group_norm_g"]).reshape(2, 8, 128).transpose(0, 2, 1))
    sh["w_out"] = f(inp["w_out"])
    sh["router_w"] = f(inp["router_w"])
    sh["router_b"] = f(inp["router_b"])
    sh["exp_w1"] = f(inp["exp_w1"])
    sh["exp_b1T"] = f(np.asarray(inp["exp_b1"]).reshape(2, 32, 16, 128).transpose(0, 1, 3, 2))
    sh["exp_w2r"] = f(np.asarray(inp["exp_w2"]).reshape(2, 32, 4, 2, 128, D).transpose(0, 1, 2, 4, 3, 5).reshape(2, 32, 4, 128, 2 * D))
    sh["exp_b2"] = f(inp["exp_b2"])
    sh["final_norm_g"] = f(np.asarray(inp["final_norm_g"]).reshape(1, D))
    for k, v in _consts().items():
        sh["k_" + k] = f(v)
    return sh


def _in_maps(inp):
    sh = _prep_shared(inp)
    x = np.asarray(inp["x"], dtype=np.float32)
    c = np.asarray(inp["c"], dtype=np.float32)
    ctx = np.asarray(inp["ctx"], dtype=np.float32)
    cctx = np.asarray(inp["c_ctx"], dtype=np.float32)
    maps = []
    for i in range(NCORE):
        m = dict(sh)
        m["x"] = np.ascontiguousarray(x[i * SPC:(i + 1) * SPC].reshape(SPC * L, D))
        m["ctx"] = np.ascontiguousarray(ctx[i * SPC:(i + 1) * SPC].reshape(SPC * LC, D))
        c5 = np.concatenate([c[i * SPC:(i + 1) * SPC], cctx[None, :]], 0)
        m["cT"] = np.ascontiguousarray(c5.T.reshape(8, 128, 5).transpose(1, 0, 2))
        maps.append(m)
    return maps


def kernel(**inputs):
    nc = build()
    maps = _in_maps(inputs)
    res = run_bass_kernel_spmd(nc, maps, core_ids=list(range(NCORE)))
    outs = [np.asarray(r["out"]).reshape(SPC, L, D) for r in res.results]
    return np.concatenate(outs, 0).astype(np.float32)
```

```python
import contextlib
import numpy as np
import concourse.bass as bass
import concourse.mybir as mybir
from concourse.bass_utils import run_bass_kernel_spmd

F32 = mybir.dt.float32
BF16 = mybir.dt.bfloat16
I32 = mybir.dt.int32
AF = mybir.ActivationFunctionType
ALU = mybir.AluOpType
AX = mybir.AxisListType
POOL_ENG = mybir.EngineType.Pool

EPOCH = 16000
NSLOT = 8
NCORE = 8
SPC = 4
D = 1024
L = 2048
LC = 256
INW = 2432
EPS = 1e-6
TS = 512
import os as _os
SKIPBIG = float(_os.environ.get("SKIPBIG", "1.0e6"))


class Res:
    __slots__ = ("w", "r")

    def __init__(self):
        self.w = {}
        self.r = {}


class TT:
    __slots__ = ("t", "res", "excl")

    def __init__(self, t, excl=False):
        self.t = t
        self.res = Res()
        self.excl = excl

    def __getitem__(self, k):
        return self.t[k]


def _res(b):
    return b.res if isinstance(b, TT) else b


class Sched:
    def __init__(self, nc, stack):
        self.nc = nc
        self.stack = stack
        self.gstack = stack
        self.E = {}
        for name, e in (("pe", nc.tensor), ("act", nc.scalar), ("dve", nc.vector),
                        ("pool", nc.gpsimd), ("sp", nc.sync)):
            self.E[name] = dict(e=e, name=name, sems=[], cnt=0, waited={}, slots=None, slot_i=0)
        self.n_inst = 0
        self.n_wait = 0
        self.uid = 0

    def sem(self, name):
        return self.gstack.enter_context(self.nc.semaphore(name))

    def sb(self, shape, dt, name=None):
        self.uid += 1
        return TT(self.stack.enter_context(self.nc.sbuf_tensor(f"{name or 't'}_{self.uid}", list(shape), dt)))

    def ps(self, shape, dt=F32, name=None):
        self.uid += 1
        shape = [128, 512 if dt == F32 else 1024]
        return TT(excl=True, t=self.stack.enter_context(self.nc.psum_tensor(f"{name or 'p'}_{self.uid}", list(shape), dt)))

    def _wait(self, E, sem, val):
        key = id(sem)
        if E["waited"].get(key, 0) >= val:
            return
        E["waited"][key] = val
        E["e"].wait_ge(sem, val)
        self.n_wait += 1

    def _deps(self, E, reads, writes):
        deps = {}
        for b in reads:
            for k, v in _res(b).w.items():
                if k not in deps or deps[k][1] < v[1]:
                    deps[k] = v
        for b in writes:
            r = _res(b)
            for dd in (r.w, r.r):
                for k, v in dd.items():
                    if k not in deps or deps[k][1] < v[1]:
                        deps[k] = v
        own = E["sems"]
        for k, (sem, val) in deps.items():
            if E["name"] == "pe" and any(sem is s for s in own):
                continue
            self._wait(E, sem, val)

    def _record(self, tok, reads, writes):
        k = id(tok[0])
        for b in reads:
            _res(b).r[k] = tok
        for b in writes:
            r = _res(b)
            r.w = {k: tok}
            r.r = {}

    def _next_tok(self, E):
        n = E["cnt"]
        ep = n // EPOCH
        while len(E["sems"]) <= ep:
            E["sems"].append(self.sem(f"c_{E['name']}_{len(E['sems'])}"))
        E["cnt"] = n + 1
        return (E["sems"][ep], n % EPOCH + 1)

    def op(self, eng, fn, reads=(), writes=()):
        E = self.E[eng]
        ex = [b for b in reads if isinstance(b, TT) and b.excl]
        if ex:
            reads = [b for b in reads if not (isinstance(b, TT) and b.excl)]
            writes = list(writes) + ex
        self._deps(E, reads, writes)
        inst = fn(E["e"])
        tok = self._next_tok(E)
        inst.then_inc(tok[0], 1)
        self._record(tok, reads, writes)
        self.n_inst += 1

    def wait_for(self, eng, reads=(), writes=()):
        self._deps(self.E[eng], reads, writes)

    def dma(self, q, fn, reads=(), writes=()):
        E = self.E[q]
        if E["slots"] is None:
            E["slots"] = [[self.sem(f"d_{q}_{i}"), 0] for i in range(NSLOT)]
        self._deps(E, reads, writes)
        sl = E["slots"][E["slot_i"] % NSLOT]
        E["slot_i"] += 1
        if sl[1] > 0:
            self._wait(E, sl[0], sl[1])
        inst = fn(E["e"])
        sl[1] += 16
        inst.then_inc(sl[0], 16)
        self._record((sl[0], sl[1]), reads, writes)
        self.n_inst += 1

    def _all_toks(self):
        toks = []
        for E in self.E.values():
            if E["slots"]:
                for s, v in E["slots"]:
                    if v:
                        toks.append((s, v))
            n = E["cnt"]
            if n:
                toks.append((E["sems"][(n - 1) // EPOCH], (n - 1) % EPOCH + 1))
        return toks

    def barrier(self):
        toks = self._all_toks()
        for E in self.E.values():
            for s, v in toks:
                self._wait(E, s, v)

    def finish(self):
        self.barrier()


def _consts():
    c = {}
    c["ident"] = np.eye(128, dtype=np.float32)
    tq = np.arange(128)
    c["upper"] = (tq[:, None] < tq[None, :]).astype(np.float32)
    kj = np.arange(128)[:, None]
    qi = np.arange(128)[None, :]
    mL = (qi <= kj).astype(np.float32)
    mR = (kj <= qi).astype(np.float32)
    c["masks"] = np.stack([np.concatenate([mL, mL], 1), np.concatenate([mR, mR], 1)], 1)
    t = np.arange(L)
    row = (t // 64).astype(np.float32)
    col = (t % 64).astype(np.float32)
    inv = (10000.0 ** (-np.arange(0, 32, 2, dtype=np.float32) / 32)).astype(np.float32)
    ang_r = row[:, None] * inv[None, :]
    ang_c = col[:, None] * inv[None, :]
    cs = np.zeros((64, L), np.float32)
    sn = np.zeros((64, L), np.float32)
    for d in range(64):
        ang = ang_r if d < 32 else ang_c
        i = d % 16
        cs[d] = np.cos(ang[:, i])
        sn[d] = np.sin(ang[:, i]) * (-1.0 if (d % 32) < 16 else 1.0)
    c["ropeC"] = np.concatenate([cs, cs], 0)
    c["ropeS"] = np.concatenate([sn, sn], 0)
    PM = np.zeros((128, 20, 128), np.float32)
    LL = 512
    for gi, w in enumerate((2, 4, 8, 16)):
        M = np.zeros((LL, LL), np.float32)
        for tt in range(LL):
            lo = max(tt - w // 2, 0)
            hi = min(tt + w // 2, LL)
            M[lo:hi, tt] = 1.0 / (hi - lo)
            M[tt, tt] -= 1.0
        PM[:, gi * 5 + 0] = M[0:128, 128:256]
        PM[:, gi * 5 + 1] = M[256:384, 128:256]
        PM[:, gi * 5 + 2] = M[128:256, 128:256]
        PM[:, gi * 5 + 3] = M[0:128, 0:128]
        PM[:, gi * 5 + 4] = M[384:512, 384:512]
    c["poolM"] = PM
    c["slotthr"] = np.tile((np.arange(128, dtype=np.float32) * TS)[None, :], (32, 1))
    c["piota"] = np.arange(128, dtype=np.float32)[:, None]
    return c


def _ext_cols():
    def sw(off, n_heads):
        idx = []
        for h in range(n_heads):
            for d in range(64):
                sd = d + 16 if (d % 32) < 16 else d - 16
                idx.append(off + h * 64 + sd)
        return idx
    q = list(range(0, 256))
    qs = sw(0, 4)
    k0 = list(range(256, 320))
    k1 = list(range(320, 384))
    ks = sw(256, 2)
    k0s, k1s = ks[:64], ks[64:]
    cols = q + qs + k0 + k0 + k1 + k1 + k0s + k0s + k1s + k1s
    cols += list(range(512, 1024))
    cols += list(range(1024, 1536))
    cols += list(range(384, 512))
    cols += list(range(1536, 1792))
    assert len(cols) == INW
    return np.array(cols)


def build(stop_after=None):
    nc = bass.Bass("TRN2", target_bir_lowering=False)

    def din(name, shape, dt=F32):
        return nc.dram_tensor(name, list(shape), dt, kind="ExternalInput").ap()

    def dscr(name, shape, dt=F32):
        return TT(nc.dram_tensor(name, list(shape), dt, kind="Internal").ap())

    x_in = TT(din("x", [SPC * L, D]))
    c_in = TT(din("ctx", [SPC * LC, D]))
    cT_in = din("cT", [128, 8, 5])
    ada_w = din("ada_w", [2, D, 6 * D])
    ada_b = din("ada_b", [2, 6 * D])
    norm1_g = din("norm1_g", [2, D])
    norm2_g = din("norm2_g", [2, D])
    w_in = din("w_in_ext", [2, D, INW])
    attn_sink = din("attn_sink", [2, 4])
    dwT = din("conv_dwT", [2, 128, 2, 31])
    convcols = din("convcols", [2, 128, 3, 2])
    conv_pw = din("conv_pw_w", [2, 256, 256])
    gln_g = din("gmlp_ln_g", [2, 256])
    gln_b = din("gmlp_ln_b", [2, 256])
    gwsT = din("gmlp_wsT", [2, 4, 128, 128])
    gbsT = din("gmlp_bsT", [2, 128, 4])
    pool_w = din("pool_w", [2, 4, 64, 64])
    pool_scale = din("pool_scale", [2, 256])
    gncol = din("gncol", [2, 128, 8])
    w_out = din("w_out", [2, D, D])
    router_w = din("router_w", [2, D, 32])
    router_b = din("router_b", [2, 32])
    exp_w1 = din("exp_w1", [2, 32, D, 2 * D])
    exp_b1T = din("exp_b1T", [2, 32, 128, 16])
    exp_w2 = din("exp_w2r", [2, 32, 4, 128, 2 * D])
    exp_b2 = din("exp_b2", [2, 32, D])
    final_g = din("final_norm_g", [1, D])
    k_ident = din("k_ident", [128, 128])
    k_upper = din("k_upper", [128, 128])
    k_masks = din("k_masks", [128, 2, 256])
    k_ropeC = din("k_ropeC", [128, L])
    k_ropeS = din("k_ropeS", [128, L])
    k_poolM = din("k_poolM", [128, 20, 128])
    k_slotthr = din("k_slotthr", [32, 128])
    k_piota = din("k_piota", [128, 1])
    out_t = TT(nc.dram_tensor("out", [SPC * L, D], F32, kind="ExternalOutput").ap())

    xs1 = dscr("xs1", [SPC * L, D])
    xs2 = dscr("xs2", [SPC * L, D])
    cs1 = dscr("cs1", [SPC * LC, D])
    cs2 = dscr("cs2", [SPC * LC, D])
    modrows = dscr("modrows", [5, 6 * D])
    NTMAX = (SPC * (L + LC)) // 128
    NSLMAX = NTMAX * 128 * 4 // TS + 32
    h2d = dscr("h2d", [NTMAX * 128, D], BF16)
    xsort = dscr("xsort", [NSLMAX * TS, D], BF16)
    ysort = dscr("ysort", [NSLMAX * TS, D], F32)

    with contextlib.ExitStack() as gst:
        S = Sched(nc, gst)

        def ACT(out, in_, func, reads, writes, **kw):
            S.op("act", lambda e: e.activation(out=out, in_=in_, func=func, **kw), reads, writes)

        def MM(out, lhsT, rhs, start, stop, reads, writes):
            S.op("pe", lambda e: e.matmul(out, lhsT=lhsT, rhs=rhs, start=start, stop=stop), reads, writes)

        def TR(out, in_, ident, reads, writes):
            S.op("pe", lambda e: e.transpose(out=out, in_=in_, identity=ident), reads, writes)

        def TTOP(eng, out, in0, in1, op, reads, writes):
            S.op(eng, lambda e: e.tensor_tensor(out=out, in0=in0, in1=in1, op=op), reads, writes)

        def TSC(eng, out, in0, s1, s2, op0, op1, reads, writes):
            if op1 is None:
                S.op(eng, lambda e: e.tensor_scalar(out=out, in0=in0, scalar1=s1, scalar2=None, op0=op0), reads, writes)
            else:
                S.op(eng, lambda e: e.tensor_scalar(out=out, in0=in0, scalar1=s1, scalar2=s2, op0=op0, op1=op1),
                     reads, writes)

        def STT(eng, out, in0, scalar, in1, op0, op1, reads, writes):
            eng = "dve"
            S.op(eng, lambda e: e.scalar_tensor_tensor(out=out, in0=in0, scalar=scalar, in1=in1, op0=op0, op1=op1),
                 reads, writes)

        def CP(eng, out, in_, reads, writes):
            if eng == "act":
                S.op("act", lambda e: e.copy(out=out, in_=in_), reads, writes)
            else:
                S.op(eng, lambda e: e.tensor_copy(out=out, in_=in_), reads, writes)

        def DMA(q, out, in_, reads, writes):
            S.dma(q, lambda e: e.dma_start(out=out, in_=in_), reads, writes)

        def rstd_from(ssum, n, tmp):
            TSC("dve", tmp[:], ssum[:], 1.0 / n, EPS, ALU.mult, ALU.add, [ssum], [tmp])
            ACT(tmp[:], tmp[:], AF.Sqrt, [tmp], [tmp])
            S.op("dve", lambda e: e.reciprocal(out=tmp[:], in_=tmp[:]), [tmp], [tmp])

        ident32 = S.sb([128, 128], F32, "ident32")
        identb = S.sb([128, 128], BF16, "identb")
        DMA("sp", ident32[:], k_ident, [], [ident32])
        DMA("pool", identb[:], k_ident, [], [identb])

        def phase_ada(l):
            with contextlib.ExitStack() as ph:
                S.stack = ph
                cT = S.sb([128, 8, 5], F32)
                sc = S.sb([128, 8, 5], F32)
                DMA("sp", cT[:], cT_in, [], [cT])
                ACT(sc[:], cT[:], AF.Silu, [cT], [sc])
                rows = S.sb([5, 6 * D], F32)
                bb = S.sb([5, 6 * D], F32)
                DMA("sp", bb[:], ada_b[l:l + 1, :].partition_broadcast(5), [], [bb])
                wts = [S.sb([128, 8, 512], F32) for _ in range(2)]
                pms = [S.ps([128, 512]) for _ in range(2)]
                for n in range(12):
                    wt = wts[n % 2]
                    pm = pms[n % 2]
                    DMA("sp", wt[:], ada_w[l, :, n * 512:(n + 1) * 512].rearrange("(c p) n -> p c n", p=128), [], [wt])
                    for c in range(8):
                        MM(pm[0:5, :], sc[:, c, :], wt[:, c, :], c == 0, c == 7, [sc, wt], [pm])
                    TTOP("dve", rows[:, n * 512:(n + 1) * 512], pm[0:5, :], bb[:, n * 512:(n + 1) * 512], ALU.add,
                         [pm, bb], [rows])
                DMA("sp", modrows[:, :], rows[:], [rows], [modrows])
            S.stack = gst
            S.barrier()

        def phase_mix(l, last, xsrc, xdst, csrc, cdst):
            with contextlib.ExitStack() as ph:
                S.stack = ph
                win = S.sb([128, 8, INW], BF16, "win")
                for c in range(8):
                    for hf in range(2):
                        DMA("pool", win[:, c, hf * 1216:(hf + 1) * 1216], w_in[l, c * 128:(c + 1) * 128, hf * 1216:(hf + 1) * 1216],
                            [], [win])
                wout = S.sb([128, 8, D], BF16, "wout")
                gnc = S.sb([128, 8], F32)
                DMA("sp", gnc[:], gncol[l], [], [gnc])
                dg = S.sb([128, 2, 31, 128], BF16)
                dw = S.sb([128, 2, 31], F32)
                DMA("sp", dw[:], dwT[l], [], [dw])
                for cc_ in range(2):
                    for j_ in range(31):
                        TSC("pool" if j_ % 2 else "dve", dg[:, cc_, j_, :], identb[:], dw[:, cc_, j_:j_ + 1], None, ALU.mult, None,
                            [identb, dw], [dg])
                ccols = S.sb([128, 3, 2], F32)
                DMA("sp", ccols[:], convcols[l], [], [ccols])
                cpw = S.sb([128, 2, 256], BF16)
                DMA("pool", cpw[:], conv_pw[l].rearrange("(c p) n -> p c n", p=128), [], [cpw])
                wsT = S.sb([128, 4, 128], BF16)
                DMA("pool", wsT[:], gwsT[l].rearrange("h q p -> q h p"), [], [wsT])
                bsT = S.sb([128, 4], F32)
                DMA("sp", bsT[:], gbsT[l], [], [bsT])
                glg = S.sb([128, 256], F32)
                glb = S.sb([128, 256], F32)
                DMA("sp", glg[:], gln_g[l:l + 1, :].partition_broadcast(128), [], [glg])
                DMA("sp", glb[:], gln_b[l:l + 1, :].partition_broadcast(128), [], [glb])
                pw = S.sb([64, 4, 64], BF16)
                DMA("pool", pw[:], pool_w[l].rearrange("g c j -> c g j"), [], [pw])
                psc = S.sb([128, 256], F32)
                DMA("sp", psc[:], pool_scale[l:l + 1, :].partition_broadcast(128), [], [psc])
                poolM = S.sb([128, 20, 128], BF16)
                for hf in range(2):
                    DMA("pool", poolM[:, hf * 10:(hf + 1) * 10, :], k_poolM[:, hf * 10:(hf + 1) * 10, :], [], [poolM])
                masks = S.sb([128, 2, 256], BF16)
                DMA("pool", masks[:], k_masks, [], [masks])
                esink = S.sb([128, 4], F32)
                DMA("sp", esink[:], attn_sink[l:l + 1, :].partition_broadcast(128), [], [esink])
                ACT(esink[:], esink[:], AF.Exp, [esink], [esink])
                ropeC = S.sb([128, 512], F32)
                ropeS = S.sb([128, 512], F32)
                gn1 = S.sb([128, D], F32)
                DMA("sp", gn1[:], norm1_g[l:l + 1, :].partition_broadcast(128), [], [gn1])
                onesm = S.sb([128, 128], F32)
                S.op("dve", lambda e: e.memset(onesm[:], 1.0 / 256.0), [], [onesm])
                A1 = S.sb([128, D], F32)
                B1 = S.sb([128, D], F32)
                G1 = S.sb([128, D], F32)

                qT = [S.sb([128, L], BF16) for _ in range(2)]
                kT = [S.sb([128, L], BF16) for _ in range(2)]
                kcT = [S.sb([128, LC], BF16) for _ in range(2)]
                vaug = S.sb([128, 16, 2, 65], BF16)
                vcaug = S.sb([128, 2, 2, 65], BF16)
                S.op("pool", lambda e: e.memset(vaug[:], 1.0), [], [vaug])
                S.op("pool", lambda e: e.memset(vcaug[:], 1.0), [], [vcaug])
                convT = S.sb([128, 2, L + 30], BF16)
                S.op("pool", lambda e: e.memset(convT[:], 0.0), [], [convT])
                sTb = S.sb([128, 2, L], BF16)
                gmb = S.sb([128, 16, 256], BF16)
                poolh = S.sb([128, 16, 256], BF16)
                xt1_ = S.sb([128, D], F32)
                xt = [xt1_, xt1_]
                ss = S.sb([128, 1], F32)
                rs = S.sb([128, 1], F32)
                h32 = S.sb([128, D], F32)
                hb = S.sb([128, D], BF16)
                hT = S.sb([128, 8, 512], BF16)
                ftmp = [S.sb([128, 512], F32) for _ in range(3)]
                u32 = S.sb([128, 256], F32)
                v32 = S.sb([128, 256], F32)
                vnb = S.sb([128, 256], BF16)
                st2 = S.sb([128, 2], F32)
                st3 = S.sb([128, 2], F32)
                acc = S.sb([128, 2, 512], F32)
                mean_sb = S.sb([128, 512], F32)
                rstd_sb = S.sb([128, 512], F32)
                PT = [S.sb([128, 5, 256], BF16) for _ in range(2)]
                den = S.sb([128, 4], F32)
                mix = S.sb([128, D], F32)
                gss = S.sb([128, 4], F32)
                grs = S.sb([128, 4], F32)
                yb = S.sb([128, D], BF16)
                yT = S.sb([128, 8, 128], BF16)
                yTp = S.sb([64, 4, 128], BF16)
                xn = h32
                wtmp = [h32, mix]
                for c in range(8):
                    DMA("sp", wtmp[c % 2][:], w_out[l, c * 128:(c + 1) * 128, :], [], [wtmp[c % 2]])
                    TSC("dve", wout[:, c, :], wtmp[c % 2][:], gnc[:, c:c + 1], None, ALU.mult, None,
                        [wtmp[c % 2], gnc], [wout])
                pT = [S.ps([128, 128], BF16) for _ in range(2)]
                pf = [S.ps([128, 512]) for _ in range(2)]
                pa = S.ps([128, 512])
                pb = S.ps([128, 512])
                pg = S.ps([128, 512])
                pq = S.ps([128, 512])

                def load_mod(r):
                    DMA("sp", A1[:], modrows[r:r + 1, D:2 * D].partition_broadcast(128), [modrows], [A1])
                    DMA("sp", B1[:], modrows[r:r + 1, 0:D].partition_broadcast(128), [modrows], [B1])
                    DMA("sp", G1[:], modrows[r:r + 1, 2 * D:3 * D].partition_broadcast(128), [modrows], [G1])
                    STT("dve", A1[:], A1[:], 1.0, gn1[:], ALU.add, ALU.mult, [A1, gn1], [A1])

                cp_i = [0]

                def cp_alt(out, in_, reads, writes):
                    cp_i[0] += 1
                    CP("act" if cp_i[0] % 2 else "dve", out, in_, reads, writes)

                def seq(src, dst, row0, LL, is_ctx, do_pass2):
                    NT = LL // 128
                    GT = min(512, LL)
                    TG = GT // 128
                    KT = kcT if is_ctx else kT
                    VA = vcaug if is_ctx else vaug
                    for g in range(LL // GT):
                        for ti in range(TG):
                            t = g * TG + ti
                            x_ = xt[t % 2]
                            DMA("sp", x_[:], src[row0 + t * 128: row0 + (t + 1) * 128, :], [src], [x_])
                            ACT(yb[:], x_[:], AF.Square, [x_], [yb, ss], accum_out=ss[:])
                            rstd_from(ss, D, rs)
                            STT("dve", h32[:], x_[:], rs[:, 0:1], A1[:], ALU.mult, ALU.mult, [x_, rs, A1], [h32])
                            TTOP("dve", hb[:], h32[:], B1[:], ALU.add, [h32, B1], [hb])
                            p_ = pT[t % 2]
                            for c in range(8):
                                TR(p_[:, c * 128:(c + 1) * 128], hb[:, c * 128:(c + 1) * 128], identb[:], [hb, identb], [p_])
                            cp_alt(hT[:, :, ti * 128:(ti + 1) * 128], p_[:, 0:1024].rearrange("p (c t) -> p c t", c=8), [p_], [hT])
                        if not is_ctx:
                            DMA("sp", ropeC[:, :GT], k_ropeC[:, g * GT:(g + 1) * GT], [], [ropeC])
                            DMA("sp", ropeS[:, :GT], k_ropeS[:, g * GT:(g + 1) * GT], [], [ropeS])
                        order = [0, 2, 1, 3, 4, 6, 5, 7, 10, 8, 11, 9] if not is_ctx else [0, 1, 4, 5, 10, 8, 11, 9]
                        tsl = slice(g * GT, (g + 1) * GT)
                        for oi, fc in enumerate(order):
                            p_ = pf[oi % 2]
                            for c in range(8):
                                MM(p_[:, :GT], win[:, c, fc * 128:(fc + 1) * 128], hT[:, c, :GT], c == 0, c == 7,
                                   [win, hT], [p_])
                            if fc in (0, 1, 4, 5):
                                dstT = (qT if not is_ctx else qT)[fc] if fc < 2 else KT[fc - 4]
                                if is_ctx:
                                    cp_alt(dstT[:, tsl], p_[:, :GT], [p_], [dstT])
                                else:
                                    TTOP("dve", ftmp[0][:, :GT], p_[:, :GT], ropeC[:, :GT], ALU.mult, [p_, ropeC], [ftmp[0]])
                            elif fc in (2, 3, 6, 7):
                                dstT = qT[fc - 2] if fc < 4 else KT[fc - 6]
                                TTOP("dve", ftmp[1][:, :GT], p_[:, :GT], ropeS[:, :GT], ALU.mult, [p_, ropeS], [ftmp[1]])
                                TTOP("dve", dstT[:, tsl], ftmp[0][:, :GT], ftmp[1][:, :GT], ALU.add,
                                     [ftmp[0], ftmp[1]], [dstT])
                            elif fc in (10, 11):
                                ACT(ftmp[2][:, :GT], p_[:, :GT], AF.Sigmoid, [p_], [ftmp[2]])
                            else:
                                cc = fc - 8
                                TTOP("dve", convT[:, cc, 15 + g * GT: 15 + (g + 1) * GT], p_[:, :GT], ftmp[2][:, :GT],
                                     ALU.mult, [p_, ftmp[2]], [convT])
                        for ti in range(TG):
                            t = g * TG + ti
                            for c in range(8):
                                MM(pa[:, :], hT[:, c, ti * 128:(ti + 1) * 128], win[:, c, 1536:2048], c == 0, c == 7,
                                   [hT, win], [pa])
                            for c in range(8):
                                MM(pb[:, 0:384], hT[:, c, ti * 128:(ti + 1) * 128], win[:, c, 2048:2432], c == 0, c == 7,
                                   [hT, win], [pb])
                            S.op("act", lambda e: e.copy(out=VA[:, t, :, 0:64],
                                                          in_=pb[:, 0:128].rearrange("p (j d) -> p j d", j=2)), [pb], [VA])
                            CP("dve", poolh[:, t, :], pb[:, 128:384], [pb], [poolh])
                            CP("act", u32[:], pa[:, 0:256], [pa], [u32])
                            ACT(v32[:], pa[:, 256:512], AF.Copy, [pa], [v32, st2], accum_out=st2[:, 0:1])
                            ACT(yb[:, 0:256], pa[:, 256:512], AF.Square, [pa], [yb, st2], accum_out=st2[:, 1:2])
                            TSC("dve", st3[:, 0:1], st2[:, 0:1], 1.0 / 256, None, ALU.mult, None, [st2], [st3])
                            STT("dve", st3[:, 1:2], st3[:, 0:1], -1.0, st3[:, 0:1], ALU.mult, ALU.mult, [st3], [st3])
                            STT("dve", st3[:, 1:2], st2[:, 1:2], 1.0 / 256, st3[:, 1:2], ALU.mult, ALU.add, [st2, st3], [st3])
                            TSC("dve", st3[:, 1:2], st3[:, 1:2], EPS, None, ALU.add, None, [st3], [st3])
                            ACT(st3[:, 1:2], st3[:, 1:2], AF.Sqrt, [st3], [st3])
                            S.op("dve", lambda e: e.reciprocal(out=st3[:, 1:2], in_=st3[:, 1:2]), [st3], [st3])
                            TSC("dve", v32[:], v32[:], st3[:, 0:1], st3[:, 1:2], ALU.subtract, ALU.mult, [v32, st3], [v32])
                            TTOP("dve", v32[:], v32[:], glg[:], ALU.mult, [v32, glg], [v32])
                            TTOP("dve", vnb[:], v32[:], glb[:], ALU.add, [v32, glb], [vnb])
                            for hh in range(4):
                                MM(pg[:, hh * 64:(hh + 1) * 64], wsT[:, hh, :], vnb[:, hh * 64:(hh + 1) * 64], True, True,
                                   [wsT, vnb], [pg])
                            for hh in range(4):
                                STT("dve", gmb[:, t, hh * 64:(hh + 1) * 64], pg[:, hh * 64:(hh + 1) * 64], bsT[:, hh:hh + 1],
                                    u32[:, hh * 64:(hh + 1) * 64], ALU.add, ALU.mult, [pg, bsT, u32], [gmb])
                    if not do_pass2:
                        return
                    for g in range(LL // GT):
                        tsl = slice(g * GT, (g + 1) * GT)
                        for cc in range(2):
                            pc_ = pq if cc == 0 else pg
                            for j in range(31):
                                MM(pc_[:, :GT], dg[:, cc, j, :], convT[:, cc, g * GT + j: g * GT + j + GT], j == 0, j == 30,
                                   [dg, convT], [pc_])
                            TSC("dve", acc[:, cc, :GT], pc_[:, :GT], ccols[:, 0, cc:cc + 1], None, ALU.add, None, [pc_, ccols], [acc])
                        for cc in range(2):
                            MM(pq[:, :GT], onesm[:], acc[:, cc, :GT], cc == 0, cc == 1, [onesm, acc], [pq])
                        CP("act", mean_sb[:, :GT], pq[:, :GT], [pq], [mean_sb])
                        for cc in range(2):
                            ACT(ftmp[cc][:, :GT], acc[:, cc, :GT], AF.Square, [acc], [ftmp[cc]])
                        for cc in range(2):
                            MM(pq[:, :GT], onesm[:], ftmp[cc][:, :GT], cc == 0, cc == 1, [onesm, ftmp[cc]], [pq])
                        TTOP("dve", rstd_sb[:, :GT], mean_sb[:, :GT], mean_sb[:, :GT], ALU.mult, [mean_sb], [rstd_sb])
                        TTOP("dve", rstd_sb[:, :GT], pq[:, :GT], rstd_sb[:, :GT], ALU.subtract, [pq, rstd_sb], [rstd_sb])
                        TSC("dve", rstd_sb[:, :GT], rstd_sb[:, :GT], EPS, None, ALU.add, None, [rstd_sb], [rstd_sb])
                        ACT(rstd_sb[:, :GT], rstd_sb[:, :GT], AF.Sqrt, [rstd_sb], [rstd_sb])
                        S.op("dve", lambda e: e.reciprocal(out=rstd_sb[:, :GT], in_=rstd_sb[:, :GT]), [rstd_sb], [rstd_sb])
                        for cc in range(2):
                            TTOP("dve", acc[:, cc, :GT], acc[:, cc, :GT], mean_sb[:, :GT], ALU.subtract, [acc, mean_sb], [acc])
                            TTOP("dve", acc[:, cc, :GT], acc[:, cc, :GT], rstd_sb[:, :GT], ALU.mult, [acc, rstd_sb], [acc])
                            ACT(sTb[:, cc, tsl], acc[:, cc, :GT], AF.Silu, [acc, ccols], [sTb],
                                scale=ccols[:, 1, cc:cc + 1], bias=ccols[:, 2, cc:cc + 1])
                        for ti in range(TG):
                            t = g * TG + ti
                            qs = slice(t * 128, (t + 1) * 128)
                            x_ = xt[t % 2]
                            DMA("sp", x_[:], src[row0 + t * 128: row0 + (t + 1) * 128, :], [src], [x_])
                            blocks = []
                            for cb in range(2):
                                blocks.append((kcT, slice(cb * 128, (cb + 1) * 128), vcaug, cb, None))
                            if not is_ctx:
                                if t > 0:
                                    blocks.append((kT, slice((t - 1) * 128, t * 128), vaug, t - 1, 0))
                                blocks.append((kT, qs, vaug, t, None))
                                if t < NT - 1:
                                    blocks.append((kT, slice((t + 1) * 128, (t + 2) * 128), vaug, t + 1, 1))
                            nb = len(blocks)
                            for j in range(2):
                                P_ = PT[j]
                                for lo in range(0, nb, 4):
                                    hi = min(nb, lo + 4)
                                    for gq in range(2):
                                        pr = slice(64 * gq, 64 * gq + 64)
                                        psc_ = pa if gq == 0 else pq
                                        for bi in range(lo, hi):
                                            Ks, ksl, Vt, vi, mi = blocks[bi]
                                            MM(psc_[:, (bi - lo) * 128:(bi - lo + 1) * 128], Ks[j][pr, ksl], qT[j][pr, qs], True, True,
                                               [Ks[j], qT[j]], [psc_])
                                    for gq in range(2):
                                        psc_ = pa if gq == 0 else pq
                                        ACT(P_[:, lo:hi, gq * 128:(gq + 1) * 128],
                                            psc_[:, 0:(hi - lo) * 128].rearrange("p (b q) -> p b q", q=128), AF.Exp, [psc_], [P_], scale=0.125)
                                for bi, (Ks, ksl, Vt, vi, mi) in enumerate(blocks):
                                    if mi is not None:
                                        TTOP("dve", P_[:, bi, :], P_[:, bi, :], masks[:, mi, :], ALU.mult, [P_, masks], [P_])
                                for gq in range(2):
                                    hh = 2 * j + gq
                                    for bi, (Ks, ksl, Vt, vi, mi) in enumerate(blocks):
                                        MM(pb[:, hh * 65:(hh + 1) * 65], P_[:, bi, gq * 128:(gq + 1) * 128], Vt[:, vi, j, :],
                                           bi == 0, bi == nb - 1, [P_, Vt], [pb])
                            pb3 = pb[:, 0:260].rearrange("p (h d) -> p h d", h=4)
                            TTOP("dve", den[:], pb3[:, :, 64], esink[:], ALU.add, [pb, esink], [den])
                            S.op("dve", lambda e: e.reciprocal(out=den[:], in_=den[:]), [den], [den])
                            for hh in range(4):
                                if hh % 2 == 0:
                                    TSC("dve", mix[:, hh * 64:(hh + 1) * 64], pb[:, hh * 65: hh * 65 + 64], den[:, hh:hh + 1], None,
                                        ALU.mult, None, [pb, den], [mix])
                                else:
                                    ACT(mix[:, hh * 64:(hh + 1) * 64], pb[:, hh * 65: hh * 65 + 64], AF.Copy, [pb, den], [mix],
                                        scale=den[:, hh:hh + 1])
                            for cc in range(2):
                                MM(pg[:, 0:256], sTb[:, cc, qs], cpw[:, cc, :], cc == 0, cc == 1, [sTb, cpw], [pg])
                            CP("act", mix[:, 256:512], pg[:, 0:256], [pg], [mix])
                            CP("dve", mix[:, 512:768], gmb[:, t, :], [gmb], [mix])
                            for gi in range(4):
                                srcs = []
                                if t > 0:
                                    srcs.append((t - 1, gi * 5 + 0))
                                srcs.append((t, gi * 5 + (3 if t == 0 else (4 if t == NT - 1 else 2))))
                                if t < NT - 1:
                                    srcs.append((t + 1, gi * 5 + 1))
                                for si, (tt_, mi) in enumerate(srcs):
                                    MM(pq[0:64, gi * 128:(gi + 1) * 128], poolh[:, tt_, gi * 64:(gi + 1) * 64], poolM[:, mi, :],
                                       si == 0, si == len(srcs) - 1, [poolh, poolM], [pq])
                            CP("dve", yTp[:, :, :], pq[0:64, :].rearrange("p (g t) -> p g t", g=4), [pq], [yTp])
                            for gi in range(4):
                                MM(pg[:, 256 + gi * 64: 256 + (gi + 1) * 64], yTp[:, gi, :], pw[:, gi, :], True, True,
                                   [yTp, pw], [pg])
                            TTOP("dve", mix[:, 768:1024], pg[:, 256:512], psc[:], ALU.mult, [pg, psc], [mix])
                            for gi in range(4):
                                ACT(hb[:, 0:256], mix[:, gi * 256:(gi + 1) * 256], AF.Square, [mix], [hb, gss],
                                    accum_out=gss[:, gi:gi + 1])
                            rstd_from(gss, 256, grs)
                            for gi in range(4):
                                if gi % 2 == 0:
                                    TSC("dve", yb[:, gi * 256:(gi + 1) * 256], mix[:, gi * 256:(gi + 1) * 256], grs[:, gi:gi + 1],
                                        None, ALU.mult, None, [mix, grs], [yb])
                                else:
                                    TSC("dve", yb[:, gi * 256:(gi + 1) * 256], mix[:, gi * 256:(gi + 1) * 256], grs[:, gi:gi + 1],
                                        None, ALU.mult, None, [mix, grs], [yb])
                            p_ = pT[t % 2]
                            for c in range(8):
                                TR(p_[:, c * 128:(c + 1) * 128], yb[:, c * 128:(c + 1) * 128], identb[:], [yb, identb], [p_])
                            cp_alt(yT[:, :, :], p_[:, 0:1024].rearrange("p (c t) -> p c t", c=8), [p_], [yT])
                            for nbk in range(2):
                                p_ = pf[nbk]
                                for c in range(8):
                                    MM(p_[:, :], yT[:, c, :], wout[:, c, nbk * 512:(nbk + 1) * 512], c == 0, c == 7, [yT, wout], [p_])
                                TTOP("dve", xn[:, nbk * 512:(nbk + 1) * 512], p_[:, :], G1[:, nbk * 512:(nbk + 1) * 512], ALU.mult,
                                     [p_, G1], [xn])
                            TTOP("dve", xn[:], xn[:], x_[:], ALU.add, [xn, x_], [xn])
                            DMA("sp", dst[row0 + t * 128: row0 + (t + 1) * 128, :], xn[:], [xn], [dst])

                import os
                lim = os.environ.get("MIXLIM", "")
                for s in range(SPC):
                    load_mod(4)
                    seq(csrc, cdst, s * LC, LC, True, not last)
                    if lim == "c":
                        break
                    load_mod(s)
                    seq(xsrc, xdst, s * L, L, False, True)
                    if lim == "cx":
                        break
            S.stack = gst
            S.barrier()

        def phase_moe(l, last, tiles, final):
            NT = len(tiles)
            NSL = NT * 128 * 4 // TS + 32
            with contextlib.ExitStack() as ph:
                S.stack = ph
                idxi = S.sb([128, NT, 4], I32, "idxi")
                gate4 = S.sb([128, NT, 4], F32, "gate4")
                widx = S.sb([128, 128], I32, "widx")
                w2idx = S.sb([128, 128], I32, "w2idx")
                bidx = S.sb([128, 128], I32, "bidx")
                Gd = S.sb([128, NT, 32], F32, "Gd")
                A2 = S.sb([128, D], F32)
                B2 = S.sb([128, D], F32)
                G2 = S.sb([128, D], F32)
                gn2 = S.sb([128, D], F32)
                DMA("sp", gn2[:], norm2_g[l:l + 1, :].partition_broadcast(128), [], [gn2])
                cur = [None]

                def load_mod(r, which):
                    if cur[0] == (r, which):
                        return
                    cur[0] = (r, which)
                    if which == 0:
                        DMA("sp", A2[:], modrows[r:r + 1, 4 * D:5 * D].partition_broadcast(128), [modrows], [A2])
                        DMA("sp", B2[:], modrows[r:r + 1, 3 * D:4 * D].partition_broadcast(128), [modrows], [B2])
                        STT("dve", A2[:], A2[:], 1.0, gn2[:], ALU.add, ALU.mult, [A2, gn2], [A2])
                    else:
                        DMA("sp", G2[:], modrows[r:r + 1, 5 * D:6 * D].partition_broadcast(128), [modrows], [G2])

                with contextlib.ExitStack() as ph1:
                    S.stack = ph1
                    rw = S.sb([128, 8, 32], F32)
                    DMA("sp", rw[:], router_w[l].rearrange("(c p) n -> p c n", p=128), [], [rw])
                    rbb = S.sb([128, 32], F32)
                    DMA("sp", rbb[:], router_b[l:l + 1, :].partition_broadcast(128), [], [rbb])
                    upper = S.sb([128, 128], BF16)
                    DMA("pool", upper[:], k_upper, [], [upper])
                    onesb = S.sb([128, 128], BF16)
                    S.op("dve", lambda e: e.memset(onesb[:], 1.0), [], [onesb])
                    ones32 = S.sb([128, 1], F32)
                    S.op("dve", lambda e: e.memset(ones32[:], 1.0), [], [ones32])
                    thr = S.sb([32, 128], F32)
                    DMA("sp", thr[:], k_slotthr, [], [thr])
                    lg = S.sb([128, NT, 32], F32)
                    t8 = S.sb([128, NT, 8], F32)
                    pos = S.sb([128, NT, 32], F32)
                    base = S.sb([128, 32], F32)
                    S.op("dve", lambda e: e.memset(base[:], 0.0), [], [base])
                    xt = [S.sb([128, D], F32) for _ in range(2)]
                    junk = S.sb([128, D], BF16)
                    ss = S.sb([128, 1], F32)
                    rs = S.sb([128, 1], F32)
                    h32 = S.sb([128, D], F32)
                    hb = [S.sb([128, D], BF16) for _ in range(2)]
                    hT32 = S.sb([128, 8, 128], F32)
                    maskb = S.sb([128, 32], BF16)
                    zt = S.sb([128, 2048], BF16)
                    S.op("pool", lambda e: e.memset(zt[:], 0.0), [], [zt])
                    pT32 = [S.ps([128, 128], F32) for _ in range(2)]
                    pl = S.ps([128, 512])
                    pp = S.ps([128, 512])
                    xz = xsort[0:NSL * TS, :].rearrange("(n p r) d -> n p (r d)", p=128, r=2)
                    zlist = list(range(NSL * TS // 256))
                    zper = -(-len(zlist) // (NT // 2))
                    h32s = [h32, S.sb([128, D], F32)]
                    hT32s = [hT32, S.sb([128, 8, 128], F32)]
                    junks = [junk, S.sb([128, D], BF16)]
                    sss = [ss, S.sb([128, 1], F32)]
                    rss = [rs, S.sb([128, 1], F32)]
                    maskbs = [maskb, S.sb([128, 32], BF16)]
                    pT32s = [pT32, [S.ps([128, 128], F32) for _ in range(2)]]
                    pls = [pl, S.ps([128, 512])]
                    pps = [pp, S.ps([128, 512])]

                    def route_tile(t, k):
                        src, dst, row0, r = tiles[t]
                        x_, h32_, hb_, hT_, jk, ss_, rs_, mk = xt[k], h32s[k], hb[k], hT32s[k], junks[k], sss[k], rss[k], maskbs[k]
                        pTk, pl_, pp_ = pT32s[k], pls[k], pps[k]
                        DMA("sp", x_[:], src[row0:row0 + 128, :], [src], [x_])
                        yield
                        ACT(jk[:], x_[:], AF.Square, [x_], [jk, ss_], accum_out=ss_[:])
                        yield
                        TSC("dve", rs_[:], ss_[:], 1.0 / D, EPS, ALU.mult, ALU.add, [ss_], [rs_])
                        yield
                        ACT(rs_[:], rs_[:], AF.Sqrt, [rs_], [rs_])
                        yield
                        S.op("dve", lambda e: e.reciprocal(out=rs_[:], in_=rs_[:]), [rs_], [rs_])
                        yield
                        STT("dve", h32_[:], x_[:], rs_[:, 0:1], A2[:], ALU.mult, ALU.mult, [x_, rs_, A2], [h32_])
                        yield
                        TTOP("dve", h32_[:], h32_[:], B2[:], ALU.add, [h32_, B2], [h32_])
                        yield
                        CP("act", hb_[:], h32_[:], [h32_], [hb_])
                        DMA("sp", h2d[t * 128:(t + 1) * 128, :], hb_[:], [hb_], [h2d])
                        yield
                        for hf in range(2):
                            p_ = pTk[hf]
                            for cc in range(4):
                                c = hf * 4 + cc
                                TR(p_[:, cc * 128:(cc + 1) * 128], h32_[:, c * 128:(c + 1) * 128], ident32[:], [h32_, ident32], [p_])
                            CP("act" if hf else "dve", hT_[:, hf * 4:(hf + 1) * 4, :],
                               p_[:, 0:512].rearrange("p (c t) -> p c t", c=4), [p_], [hT_])
                            yield
                        for c in range(8):
                            MM(pl_[:, 0:32], hT_[:, c, :], rw[:, c, :], c == 0, c == 7, [hT_, rw], [pl_])
                        yield
                        TTOP("dve", lg[:, t, :], pl_[:, 0:32], rbb[:], ALU.add, [pl_, rbb], [lg])
                        yield
                        S.op("dve", lambda e: e.max(out=t8[:, t, :], in_=lg[:, t, :]), [lg], [t8])
                        yield
                        TSC("dve", mk[:], lg[:, t, :], t8[:, t, 3:4], None, ALU.is_ge, None, [lg, t8], [mk])
                        yield
                        MM(pp_[:, 0:32], upper[:], mk[:], True, True, [upper, mk], [pp_])
                        MM(pp_[:, 32:64], onesb[:], mk[:], True, True, [onesb, mk], [pp_])
                        yield
                        TSC("dve", ss_[:], t8[:, t, 0:1], -1.0, None, ALU.mult, None, [t8], [ss_])
                        yield
                        ACT(gate4[:, t, :], t8[:, t, 0:4], AF.Exp, [t8, ss_], [gate4, rs_], bias=ss_[:, 0:1], accum_out=rs_[:, 0:1])
                        yield
                        S.op("dve", lambda e: e.reciprocal(out=rs_[:], in_=rs_[:]), [rs_], [rs_])
                        yield
                        TSC("dve", gate4[:, t, :], gate4[:, t, :], rs_[:, 0:1], None, ALU.mult, None, [gate4, rs_], [gate4])
                        ACT(Gd[:, t, :], lg[:, t, :], AF.Exp, [lg, ss_], [Gd], bias=ss_[:, 0:1])
                        yield
                        TTOP("dve", Gd[:, t, :], Gd[:, t, :], mk[:], ALU.mult, [Gd, mk], [Gd])
                        yield
                        TSC("dve", Gd[:, t, :], Gd[:, t, :], rs_[:, 0:1], None, ALU.mult, None, [Gd, rs_], [Gd])
                        TTOP("dve", pos[:, t, :], pp_[:, 0:32], base[:], ALU.add, [pp_, base], [pos])
                        TTOP("dve", base[:], pp_[:, 32:64], base[:], ALU.add, [pp_, base], [base])
                        yield

                    for t0 in range(0, NT, 2):
                        load_mod(tiles[t0][3], 0)
                        alive = [route_tile(t0, 0), route_tile(t0 + 1, 1)]
                        while alive:
                            for g_ in list(alive):
                                try:
                                    next(g_)
                                except StopIteration:
                                    alive.remove(g_)
                        for _z in range(zper):
                            if zlist:
                                DMA("sp", xz[zlist.pop()], zt[:], [zt], [xsort])
                    while zlist:
                        DMA("sp", xz[zlist.pop()], zt[:], [zt], [xsort])
                    padded = S.sb([128, 32], F32)
                    pend = S.sb([128, 32], F32)
                    pstart = S.sb([128, 32], F32)
                    tmpc = S.sb([128, 32], F32)
                    TSC("dve", padded[:], base[:], 0.0, None, ALU.is_gt, None, [base], [padded])
                    for jj in range(1, NT * 128 // TS + 1):
                        STT("dve", padded[:], base[:], float(TS * jj), padded[:], ALU.is_gt, ALU.add, [base, padded], [padded])
                    TSC("dve", padded[:], padded[:], float(TS), None, ALU.mult, None, [padded], [padded])
                    CP("dve", pend[:, 0:1], padded[:, 0:1], [padded], [pend])
                    for e_ in range(1, 32):
                        TTOP("dve", pend[:, e_:e_ + 1], pend[:, e_ - 1:e_], padded[:, e_:e_ + 1], ALU.add, [pend, padded], [pend])
                    TTOP("dve", pstart[:], pend[:], padded[:], ALU.subtract, [pend, padded], [pstart])
                    pcol = S.sb([32, 1], F32)
                    tmpd = S.sb([32, 32], F32)
                    TTOP("dve", tmpd[:], pend[0:32, :], ident32[0:32, 0:32], ALU.mult, [pend, ident32], [tmpd])
                    S.op("dve", lambda e: e.reduce_sum(out=pcol[:], in_=tmpd[:], axis=AX.X), [tmpd], [pcol])
                    cmp = S.sb([32, 128], F32)
                    TSC("dve", cmp[:], thr[:], pcol[:, 0:1], None, ALU.is_ge, None, [thr, pcol], [cmp])
                    ones32m = S.sb([32, 128], F32)
                    S.op("dve", lambda e: e.memset(ones32m[:], 1.0), [], [ones32m])
                    piota = S.sb([128, 1], F32)
                    DMA("sp", piota[:], k_piota, [], [piota])
                    MM(pl[:, 0:128], ones32m[0:32, :], cmp[:], True, True, [ones32m, cmp], [pl])
                    blkf = S.sb([128, 128], F32)
                    TSC("dve", blkf[:], pl[:, 0:128], 31.0, None, ALU.min, None, [pl], [blkf])
                    wif = S.sb([128, 128], F32)
                    same = S.sb([128, 128], F32)
                    S.op("dve", lambda e: e.memset(same[:], 0.0), [], [same])
                    TTOP("dve", same[:, 1:128], blkf[:, 1:128], blkf[:, 0:127], ALU.is_equal, [blkf], [same])
                    S.op("dve", lambda e: e.memset(same[:, NSL // 2:NSL // 2 + 1], 0.0), [], [same])
                    TSC("dve", same[:], same[:], SKIPBIG, None, ALU.mult, None, [same], [same])
                    STT("dve", wif[:], blkf[:], 1024.0, same[:], ALU.mult, ALU.add, [blkf, same], [wif])
                    TSC("dve", wif[:], wif[:], piota[:, 0:1], None, ALU.add, None, [wif, piota], [wif])
                    CP("dve", widx[:], wif[:], [wif], [widx])
                    STT("dve", wif[:], blkf[:], 512.0, same[:], ALU.mult, ALU.add, [blkf, same], [wif])
                    TSC("dve", wif[:], wif[:], piota[:, 0:1], None, ALU.add, None, [wif, piota], [wif])
                    CP("dve", w2idx[:], wif[:], [wif], [w2idx])
                    STT("dve", wif[:], blkf[:], 128.0, same[:], ALU.mult, ALU.add, [blkf, same], [wif])
                    TSC("dve", wif[:], wif[:], piota[:, 0:1], None, ALU.add, None, [wif, piota], [wif])
                    CP("dve", bidx[:], wif[:], [wif], [bidx])
                    oh = S.sb([128, 32], F32)
                    idxf = S.sb([128, 4], F32)
                    for t, (src, dst, row0, r) in enumerate(tiles):
                        TTOP("dve", pos[:, t, :], pos[:, t, :], pstart[:], ALU.add, [pos, pstart], [pos])
                        for k in range(4):
                            STT("dve", oh[:], lg[:, t, :], t8[:, t, k:k + 1], pos[:, t, :], ALU.is_equal, ALU.mult,
                                [lg, t8, pos], [oh])
                            S.op("dve", lambda e: e.reduce_sum(out=idxf[:, k:k + 1], in_=oh[:], axis=AX.X), [oh], [idxf])
                        CP("dve", idxi[:, t, :], idxf[:], [idxf], [idxi])
                        hb_ = hb[t % 2]
                        DMA("sp", hb_[:], h2d[t * 128:(t + 1) * 128, :], [h2d], [hb_])
                        for k in range(4):
                            S.dma("pool", lambda e: e.indirect_dma_start(
                                out=xsort[:, :], out_offset=bass.IndirectOffsetOnAxis(ap=idxi[:, t, k:k + 1], axis=0),
                                in_=hb_[:], in_offset=None), [hb_, idxi], [xsort])
                S.stack = ph
                S.barrier()
                if stop_after == ("route", l):
                    return
                with contextlib.ExitStack() as ph2:
                    S.stack = ph2
                    w1b = [S.sb([128, 8, 2 * D], BF16) for _ in range(2)]
                    w2b = [S.sb([128, 8, D], BF16) for _ in range(2)]
                    b1c = [S.sb([128, 16], F32) for _ in range(2)]
                    xs_ = [S.sb([128, 4, D], BF16) for _ in range(2)]
                    xsT2 = [S.sb([128, 8, TS], BF16) for _ in range(2)]
                    actT = S.sb([128, 8, TS], BF16)
                    b1p = [S.sb([128, 8], F32) for _ in range(2)]
                    linp = [S.sb([128, TS], F32) for _ in range(2)]
                    g32s = [S.sb([128, TS], F32) for _ in range(2)]
                    sgs = [S.sb([128, TS], F32) for _ in range(2)]
                    tts = [S.sb([128, TS], F32) for _ in range(2)]
                    yt = [S.sb([128, D], F32) for _ in range(2)]
                    pT = [S.ps([128, 128], BF16) for _ in range(2)]
                    ph_ = [S.ps([128, 512]) for _ in range(4)]
                    py = [S.ps([128, 512]) for _ in range(2)]
                    w1flat = exp_w1.rearrange("l e k n -> (l e k) n")
                    w2flat = exp_w2.rearrange("l e c p n -> (l e c p) n")
                    b1flat = exp_b1T.rearrange("l e p n -> (l e p) n")

                    if not hasattr(build, "_bnd") or build._bnd[0] is not nc:
                        rg_ = nc.gpsimd.alloc_register("bndreg")
                        nc.gpsimd.reg_mov(rg_, 2 * 32 * 1024 - 1)
                        build._bnd = (nc, rg_)
                    bnd_reg = build._bnd[1]

                    HS = NSL // 2
                    order = []
                    for q_ in range(HS):
                        order += [q_, HS + q_]

                    def load_w(j):
                        b = 0 if j < HS else 1
                        for c in range(8):
                            S.dma("pool", lambda e: e.indirect_dma_start(
                                out=w1b[b][:, c, :], out_offset=None, in_=w1flat,
                                in_offset=bass.IndirectOffsetOnAxis(ap=widx[:, j:j + 1], axis=0),
                                element_offset=(l * 32 * 1024 + c * 128) * 2048, bounds_check=bnd_reg, oob_is_err=False), [widx], [w1b[b]])
                        for c2 in range(4):
                            S.dma("pool", lambda e: e.indirect_dma_start(
                                out=w2b[b][:, 2 * c2:2 * c2 + 2, :].rearrange("p c n -> p (c n)"), out_offset=None, in_=w2flat,
                                in_offset=bass.IndirectOffsetOnAxis(ap=w2idx[:, j:j + 1], axis=0),
                                element_offset=(l * 128 + c2) * 128 * 2048, bounds_check=bnd_reg, oob_is_err=False),
                                [w2idx], [w2b[b]])
                        S.dma("pool", lambda e: e.indirect_dma_start(
                            out=b1c[b][:], out_offset=None, in_=b1flat,
                            in_offset=bass.IndirectOffsetOnAxis(ap=bidx[:, j:j + 1], axis=0),
                            element_offset=l * 32 * 128 * 16, bounds_check=bnd_reg, oob_is_err=False), [bidx], [b1c[b]])

                    def load_x(j, i):
                        DMA("sp", xs_[i % 2][:], xsort[j * TS:(j + 1) * TS, :].rearrange("(a p) d -> p a d", p=128),
                            [xsort], [xs_[i % 2]])

                    load_w(order[0])
                    load_x(order[0], 0)
                    for i_, j in enumerate(order):
                        b = 0 if j < HS else 1
                        if i_ + 1 < NSL:
                            load_w(order[i_ + 1])
                            load_x(order[i_ + 1], i_ + 1)
                        W1, W2, B1c, X, xsT = w1b[b], w2b[b], b1c[b], xs_[i_ % 2], xsT2[i_ % 2]
                        TSC("dve", b1p[b][:], B1c[:, 8:16], 1.0, None, ALU.add, None, [B1c], [b1p[b]])
                        for a in range(4):
                            p_ = pT[a % 2]
                            for c in range(8):
                                TR(p_[:, c * 128:(c + 1) * 128], X[:, a, c * 128:(c + 1) * 128], identb[:], [X, identb], [p_])
                            CP("act" if a % 2 else "dve", xsT[:, :, a * 128:(a + 1) * 128],
                               p_[:, 0:1024].rearrange("p (c t) -> p c t", c=8), [p_], [xsT])
                        pi = 0
                        for i in range(8):
                            k2 = i % 2
                            pl_ = ph_[pi % 4]
                            pi += 1
                            for c in range(8):
                                MM(pl_[:, :], W1[:, c, (8 + i) * 128:(9 + i) * 128], xsT[:, c, :], c == 0, c == 7, [W1, xsT], [pl_])
                            TSC("dve", linp[k2][:], pl_[:, :], b1p[b][:, i:i + 1], -6.0, ALU.add, ALU.max, [pl_, b1p[b]], [linp[k2]])
                            pg_ = ph_[pi % 4]
                            pi += 1
                            for c in range(8):
                                MM(pg_[:, :], W1[:, c, i * 128:(i + 1) * 128], xsT[:, c, :], c == 0, c == 7, [W1, xsT], [pg_])
                            TSC("dve", g32s[k2][:], pg_[:, :], B1c[:, i:i + 1], 7.0, ALU.add, ALU.min, [pg_, B1c], [g32s[k2]])
                            ACT(sgs[k2][:], g32s[k2][:], AF.Sigmoid, [g32s[k2]], [sgs[k2]], scale=1.702)
                            TTOP("dve", tts[k2][:], sgs[k2][:], g32s[k2][:], ALU.mult, [sgs[k2], g32s[k2]], [tts[k2]])
                            STT("dve", actT[:, i, :], linp[k2][:], 8.0, tts[k2][:], ALU.min, ALU.mult, [linp[k2], tts[k2]], [actT])
                        for a in range(4):
                            y_ = yt[a % 2]
                            for nbk in range(2):
                                p_ = py[nbk]
                                for f in range(8):
                                    MM(p_[:, :], actT[:, f, a * 128:(a + 1) * 128], W2[:, f, nbk * 512:(nbk + 1) * 512],
                                       f == 0, f == 7, [actT, W2], [p_])
                                CP("act", y_[:, nbk * 512:(nbk + 1) * 512], p_[:, :], [p_], [y_])
                            DMA("sp", ysort[j * TS + a * 128: j * TS + (a + 1) * 128, :], y_[:], [y_], [ysort])
                S.stack = ph
                S.barrier()
                with contextlib.ExitStack() as ph3:
                    S.stack = ph3
                    yk = [[S.sb([128, D], F32) for _ in range(4)] for _ in range(2)]
                    xt = [S.sb([128, D], F32) for _ in range(2)]
                    acc = S.sb([128, D], F32)
                    xn = [S.sb([128, D], F32) for _ in range(2)]
                    junk = S.sb([128, D], BF16)
                    ss = S.sb([128, 1], F32)
                    rs = S.sb([128, 1], F32)
                    fng = S.sb([128, D], F32)
                    DMA("sp", fng[:], final_g[0:1, :].partition_broadcast(128), [], [fng])
                    b2all = S.sb([32, D], F32)
                    DMA("sp", b2all[:], exp_b2[l], [], [b2all])
                    GTs = S.sb([32, 128], F32)
                    pGT = S.ps([128, 512])
                    pbias = [S.ps([128, 512]) for _ in range(2)]
                    for t, (src, dst, row0, r) in enumerate(tiles):
                        load_mod(r, 1)
                        x_ = xt[t % 2]
                        Y = yk[t % 2]
                        xo = xn[t % 2]
                        DMA("sp", x_[:], src[row0:row0 + 128, :], [src], [x_])
                        for k in range(4):
                            S.dma("pool", lambda e: e.indirect_dma_start(
                                out=Y[k][:], out_offset=None, in_=ysort[:, :],
                                in_offset=bass.IndirectOffsetOnAxis(ap=idxi[:, t, k:k + 1], axis=0)), [ysort, idxi], [Y[k]])
                        TR(pGT[0:32, 0:128], Gd[:, t, :], ident32[:], [Gd, ident32], [pGT])
                        CP("act", GTs[:], pGT[0:32, 0:128], [pGT], [GTs])
                        for nbk in range(2):
                            MM(pbias[nbk][:, :], GTs[0:32, :], b2all[0:32, nbk * 512:(nbk + 1) * 512], True, True,
                               [GTs, b2all], [pbias[nbk]])
                        TSC("dve", acc[:], Y[0][:], gate4[:, t, 0:1], None, ALU.mult, None, [Y[0], gate4], [acc])
                        for k in range(1, 4):
                            STT("dve" if k != 2 else "pool", acc[:], Y[k][:], gate4[:, t, k:k + 1], acc[:], ALU.mult, ALU.add,
                                [Y[k], gate4, acc], [acc])
                        for nbk in range(2):
                            TTOP("dve", acc[:, nbk * 512:(nbk + 1) * 512], acc[:, nbk * 512:(nbk + 1) * 512], pbias[nbk][:, :], ALU.add,
                                 [acc, pbias[nbk]], [acc])
                        TTOP("dve", acc[:], acc[:], G2[:], ALU.mult, [acc, G2], [acc])
                        TTOP("dve", xo[:], acc[:], x_[:], ALU.add, [acc, x_], [xo])
                        if final:
                            ACT(junk[:], xo[:], AF.Square, [xo], [junk, ss], accum_out=ss[:])
                            rstd_from(ss, D, rs)
                            STT("dve", xo[:], xo[:], rs[:, 0:1], fng[:], ALU.mult, ALU.mult, [xo, rs, fng], [xo])
                        DMA("sp", dst[row0:row0 + 128, :], xo[:], [xo], [dst])
            S.stack = gst
            S.barrier()

        def tiles_for(xsrc, xdst, csrc, cdst, with_ctx):
            tl = []
            for s in range(SPC):
                for t in range(L // 128):
                    tl.append((xsrc, xdst, s * L + t * 128, s))
            if with_ctx:
                for s in range(SPC):
                    for t in range(LC // 128):
                        tl.append((csrc, cdst, s * LC + t * 128, 4))
            return tl

        done = False
        for l in range(2):
            last = l == 1
            phase_ada(l)
            if stop_after == ("ada", 0):
                break
            if l == 0:
                phase_mix(0, False, x_in, xs1, c_in, cs1)
                if stop_after == ("mix", 0):
                    break
                phase_moe(0, False, tiles_for(xs1, xs2, cs1, cs2, True), False)
                if stop_after in (("route", 0), ("moe", 0)):
                    break
            else:
                phase_mix(1, True, xs2, xs1, cs2, None)
                phase_moe(1, True, tiles_for(xs1, out_t, None, None, False), True)
        if stop_after is not None:
            S.barrier()
            if stop_after[0] == "ada":
                DMA("sp", out_t[0:30, :].rearrange("(r k) d -> r (k d)", r=5), modrows[:, :], [modrows], [out_t])
            else:
                srcd = {"mix": xs1, "moe": xs2, "route": xs1}[stop_after[0]]
                import os
                for i_ in range((L if os.environ.get("MIXLIM") else SPC * L) // 128):
                    DMA("sp", out_t[i_ * 128:(i_ + 1) * 128, :], srcd[i_ * 128:(i_ + 1) * 128, :], [srcd], [out_t])
        S.finish()
        build.stats = (S.n_inst, S.n_wait)
    return nc


def _prep_shared(inp):
    f = lambda a: np.ascontiguousarray(np.asarray(a, dtype=np.float32))
    cols = _ext_cols()
    sh = {}
    sh["ada_w"] = f(inp["ada_w"])
    sh["ada_b"] = f(inp["ada_b"])
    sh["norm1_g"] = f(inp["norm1_g"])
    sh["norm2_g"] = f(inp["norm2_g"])
    sh["w_in_ext"] = f(np.asarray(inp["w_in"])[:, :, cols])
    sh["attn_sink"] = f(inp["attn_sink"])
    dw = np.asarray(inp["conv_dw_w"])
    sh["conv_dwT"] = f(dw.transpose(0, 2, 1).reshape(2, 2, 128, 31).transpose(0, 2, 1, 3))
    cc = np.stack([np.asarray(inp["conv_dw_b"]), np.asarray(inp["conv_ln_g"]), np.asarray(inp["conv_ln_b"])], 1)
    sh["convcols"] = f(cc.reshape(2, 3, 2, 128).transpose(0, 3, 1, 2))
    sh["conv_pw_w"] = f(inp["conv_pw_w"])
    sh["gmlp_ln_g"] = f(inp["gmlp_ln_g"])
    sh["gmlp_ln_b"] = f(inp["gmlp_ln_b"])
    sh["gmlp_wsT"] = f(np.asarray(inp["gmlp_ws"]).transpose(0, 1, 3, 2))
    sh["gmlp_bsT"] = f(np.asarray(inp["gmlp_bs"]).transpose(0, 2, 1))
    sh["pool_w"] = f(inp["pool_w"])
    sh["pool_scale"] = f(inp["pool_scale"])
    sh["gncol"] = f(np.asarray(inp["group_norm_g"]).reshape(2, 8, 128).transpose(0, 2, 1))
    sh["w_out"] = f(inp["w_out"])
    sh["router_w"] = f(inp["router_w"])
    sh["router_b"] = f(inp["router_b"])
    sh["exp_w1"] = f(inp["exp_w1"])
    sh["exp_b1T"] = f(np.asarray(inp["exp_b1"]).reshape(2, 32, 16, 128).transpose(0, 1, 3, 2))
    sh["exp_w2r"] = f(np.asarray(inp["exp_w2"]).reshape(2, 32, 4, 2, 128, D).transpose(0, 1, 2, 4, 3, 5).reshape(2, 32, 4, 128, 2 * D))
    sh["exp_b2"] = f(inp["exp_b2"])
    sh["final_norm_g"] = f(np.asarray(inp["final_norm_g"]).reshape(1, D))
    for k, v in _consts().items():
        sh["k_" + k] = f(v)
    return sh


def _in_maps(inp):
    sh = _prep_shared(inp)
    x = np.asarray(inp["x"], dtype=np.float32)
    c = np.asarray(inp["c"], dtype=np.float32)
    ctx = np.asarray(inp["ctx"], dtype=np.float32)
    cctx = np.asarray(inp["c_ctx"], dtype=np.float32)
    maps = []
    for i in range(NCORE):
        m = dict(sh)
        m["x"] = np.ascontiguousarray(x[i * SPC:(i + 1) * SPC].reshape(SPC * L, D))
        m["ctx"] = np.ascontiguousarray(ctx[i * SPC:(i + 1) * SPC].reshape(SPC * LC, D))
        c5 = np.concatenate([c[i * SPC:(i + 1) * SPC], cctx[None, :]], 0)
        m["cT"] = np.ascontiguousarray(c5.T.reshape(8, 128, 5).transpose(1, 0, 2))
        maps.append(m)
    return maps


def kernel(**inputs):
    nc = build()
    maps = _in_maps(inputs)
    res = run_bass_kernel_spmd(nc, maps, core_ids=list(range(NCORE)))
    outs = [np.asarray(r["out"]).reshape(SPC, L, D) for r in res.results]
    return np.concatenate(outs, 0).astype(np.float32)
```

```python
import contextlib
import numpy as np
import concourse.bass as bass
import concourse.mybir as mybir
from concourse.bass_utils import run_bass_kernel_spmd

F32 = mybir.dt.float32
BF16 = mybir.dt.bfloat16
I32 = mybir.dt.int32
AF = mybir.ActivationFunctionType
ALU = mybir.AluOpType
AX = mybir.AxisListType
POOL_ENG = mybir.EngineType.Pool

EPOCH = 16000
NSLOT = 8
NCORE = 8
SPC = 4
D = 1024
L = 2048
LC = 256
INW = 2432
EPS = 1e-6
TS = 512
import os as _os
SKIPBIG = float(_os.environ.get("SKIPBIG", "1.0e6"))


class Res:
    __slots__ = ("w", "r")

    def __init__(self):
        self.w = {}
        self.r = {}


class TT:
    __slots__ = ("t", "res", "excl")

    def __init__(self, t, excl=False):
        self.t = t
        self.res = Res()
        self.excl = excl

    def __getitem__(self, k):
        return self.t[k]


def _res(b):
    return b.res if isinstance(b, TT) else b


class Sched:
    def __init__(self, nc, stack):
        self.nc = nc
        self.stack = stack
        self.gstack = stack
        self.E = {}
        for name, e in (("pe", nc.tensor), ("act", nc.scalar), ("dve", nc.vector),
                        ("pool", nc.gpsimd), ("sp", nc.sync)):
            self.E[name] = dict(e=e, name=name, sems=[], cnt=0, waited={}, slots=None, slot_i=0)
        self.n_inst = 0
        self.n_wait = 0
        self.uid = 0

    def sem(self, name):
        return self.gstack.enter_context(self.nc.semaphore(name))

    def sb(self, shape, dt, name=None):
        self.uid += 1
        return TT(self.stack.enter_context(self.nc.sbuf_tensor(f"{name or 't'}_{self.uid}", list(shape), dt)))

    def ps(self, shape, dt=F32, name=None):
        self.uid += 1
        shape = [128, 512 if dt == F32 else 1024]
        return TT(excl=True, t=self.stack.enter_context(self.nc.psum_tensor(f"{name or 'p'}_{self.uid}", list(shape), dt)))

    def _wait(self, E, sem, val):
        key = id(sem)
        if E["waited"].get(key, 0) >= val:
            return
        E["waited"][key] = val
        E["e"].wait_ge(sem, val)
        self.n_wait += 1

    def _deps(self, E, reads, writes):
        deps = {}
        for b in reads:
            for k, v in _res(b).w.items():
                if k not in deps or deps[k][1] < v[1]:
                    deps[k] = v
        for b in writes:
            r = _res(b)
            for dd in (r.w, r.r):
                for k, v in dd.items():
                    if k not in deps or deps[k][1] < v[1]:
                        deps[k] = v
        own = E["sems"]
        for k, (sem, val) in deps.items():
            if E["name"] == "pe" and any(sem is s for s in own):
                continue
            self._wait(E, sem, val)

    def _record(self, tok, reads, writes):
        k = id(tok[0])
        for b in reads:
            _res(b).r[k] = tok
        for b in writes:
            r = _res(b)
            r.w = {k: tok}
            r.r = {}

    def _next_tok(self, E):
        n = E["cnt"]
        ep = n // EPOCH
        while len(E["sems"]) <= ep:
            E["sems"].append(self.sem(f"c_{E['name']}_{len(E['sems'])}"))
        E["cnt"] = n + 1
        return (E["sems"][ep], n % EPOCH + 1)

    def op(self, eng, fn, reads=(), writes=()):
        E = self.E[eng]
        ex = [b for b in reads if isinstance(b, TT) and b.excl]
        if ex:
            reads = [b for b in reads if not (isinstance(b, TT) and b.excl)]
            writes = list(writes) + ex
        self._deps(E, reads, writes)
        inst = fn(E["e"])
        tok = self._next_tok(E)
        inst.then_inc(tok[0], 1)
        self._record(tok, reads, writes)
        self.n_inst += 1

    def wait_for(self, eng, reads=(), writes=()):
        self._deps(self.E[eng], reads, writes)

    def dma(self, q, fn, reads=(), writes=()):
        E = self.E[q]
        if E["slots"] is None:
            E["slots"] = [[self.sem(f"d_{q}_{i}"), 0] for i in range(NSLOT)]
        self._deps(E, reads, writes)
        sl = E["slots"][E["slot_i"] % NSLOT]
        E["slot_i"] += 1
        if sl[1] > 0:
            self._wait(E, sl[0], sl[1])
        inst = fn(E["e"])
        sl[1] += 16
        inst.then_inc(sl[0], 16)
        self._record((sl[0], sl[1]), reads, writes)
        self.n_inst += 1

    def _all_toks(self):
        toks = []
        for E in self.E.values():
            if E["slots"]:
                for s, v in E["slots"]:
                    if v:
                        toks.append((s, v))
            n = E["cnt"]
            if n:
                toks.append((E["sems"][(n - 1) // EPOCH], (n - 1) % EPOCH + 1))
        return toks

    def barrier(self):
        toks = self._all_toks()
        for E in self.E.values():
            for s, v in toks:
                self._wait(E, s, v)

    def finish(self):
        self.barrier()


def _consts():
    c = {}
    c["ident"] = np.eye(128, dtype=np.float32)
    tq = np.arange(128)
    c["upper"] = (tq[:, None] < tq[None, :]).astype(np.float32)
    kj = np.arange(128)[:, None]
    qi = np.arange(128)[None, :]
    mL = (qi <= kj).astype(np.float32)
    mR = (kj <= qi).astype(np.float32)
    c["masks"] = np.stack([np.concatenate([mL, mL], 1), np.concatenate([mR, mR], 1)], 1)
    t = np.arange(L)
    row = (t // 64).astype(np.float32)
    col = (t % 64).astype(np.float32)
    inv = (10000.0 ** (-np.arange(0, 32, 2, dtype=np.float32) / 32)).astype(np.float32)
    ang_r = row[:, None] * inv[None, :]
    ang_c = col[:, None] * inv[None, :]
    cs = np.zeros((64, L), np.float32)
    sn = np.zeros((64, L), np.float32)
    for d in range(64):
        ang = ang_r if d < 32 else ang_c
        i = d % 16
        cs[d] = np.cos(ang[:, i])
        sn[d] = np.sin(ang[:, i]) * (-1.0 if (d % 32) < 16 else 1.0)
    c["ropeC"] = np.concatenate([cs, cs], 0)
    c["ropeS"] = np.concatenate([sn, sn], 0)
    PM = np.zeros((128, 20, 128), np.float32)
    LL = 512
    for gi, w in enumerate((2, 4, 8, 16)):
        M = np.zeros((LL, LL), np.float32)
        for tt in range(LL):
            lo = max(tt - w // 2, 0)
            hi = min(tt + w // 2, LL)
            M[lo:hi, tt] = 1.0 / (hi - lo)
            M[tt, tt] -= 1.0
        PM[:, gi * 5 + 0] = M[0:128, 128:256]
        PM[:, gi * 5 + 1] = M[256:384, 128:256]
        PM[:, gi * 5 + 2] = M[128:256, 128:256]
        PM[:, gi * 5 + 3] = M[0:128, 0:128]
        PM[:, gi * 5 + 4] = M[384:512, 384:512]
    c["poolM"] = PM
    c["slotthr"] = np.tile((np.arange(128, dtype=np.float32) * TS)[None, :], (32, 1))
    c["piota"] = np.arange(128, dtype=np.float32)[:, None]
    return c


def _ext_cols():
    def sw(off, n_heads):
        idx = []
        for h in range(n_heads):
            for d in range(64):
                sd = d + 16 if (d % 32) < 16 else d - 16
                idx.append(off + h * 64 + sd)
        return idx
    q = list(range(0, 256))
    qs = sw(0, 4)
    k0 = list(range(256, 320))
    k1 = list(range(320, 384))
    ks = sw(256, 2)
    k0s, k1s = ks[:64], ks[64:]
    cols = q + qs + k0 + k0 + k1 + k1 + k0s + k0s + k1s + k1s
    cols += list(range(512, 1024))
    cols += list(range(1024, 1536))
    cols += list(range(384, 512))
    cols += list(range(1536, 1792))
    assert len(cols) == INW
    return np.array(cols)


def build(stop_after=None):
    nc = bass.Bass("TRN2", target_bir_lowering=False)

    def din(name, shape, dt=F32):
        return nc.dram_tensor(name, list(shape), dt, kind="ExternalInput").ap()

    def dscr(name, shape, dt=F32):
        return TT(nc.dram_tensor(name, list(shape), dt, kind="Internal").ap())

    x_in = TT(din("x", [SPC * L, D]))
    c_in = TT(din("ctx", [SPC * LC, D]))
    cT_in = din("cT", [128, 8, 5])
    ada_w = din("ada_w", [2, D, 6 * D])
    ada_b = din("ada_b", [2, 6 * D])
    norm1_g = din("norm1_g", [2, D])
    norm2_g = din("norm2_g", [2, D])
    w_in = din("w_in_ext", [2, D, INW])
    attn_sink = din("attn_sink", [2, 4])
    dwT = din("conv_dwT", [2, 128, 2, 31])
    convcols = din("convcols", [2, 128, 3, 2])
    conv_pw = din("conv_pw_w", [2, 256, 256])
    gln_g = din("gmlp_ln_g", [2, 256])
    gln_b = din("gmlp_ln_b", [2, 256])
    gwsT = din("gmlp_wsT", [2, 4, 128, 128])
    gbsT = din("gmlp_bsT", [2, 128, 4])
    pool_w = din("pool_w", [2, 4, 64, 64])
    pool_scale = din("pool_scale", [2, 256])
    gncol = din("gncol", [2, 128, 8])
    w_out = din("w_out", [2, D, D])
    router_w = din("router_w", [2, D, 32])
    router_b = din("router_b", [2, 32])
    exp_w1 = din("exp_w1", [2, 32, D, 2 * D])
    exp_b1T = din("exp_b1T", [2, 32, 128, 16])
    exp_w2 = din("exp_w2r", [2, 32, 4, 128, 2 * D])
    exp_b2 = din("exp_b2", [2, 32, D])
    final_g = din("final_norm_g", [1, D])
    k_ident = din("k_ident", [128, 128])
    k_upper = din("k_upper", [128, 128])
    k_masks = din("k_masks", [128, 2, 256])
    k_ropeC = din("k_ropeC", [128, L])
    k_ropeS = din("k_ropeS", [128, L])
    k_poolM = din("k_poolM", [128, 20, 128])
    k_slotthr = din("k_slotthr", [32, 128])
    k_piota = din("k_piota", [128, 1])
    out_t = TT(nc.dram_tensor("out", [SPC * L, D], F32, kind="ExternalOutput").ap())

    xs1 = dscr("xs1", [SPC * L, D])
    xs2 = dscr("xs2", [SPC * L, D])
    cs1 = dscr("cs1", [SPC * LC, D])
    cs2 = dscr("cs2", [SPC * LC, D])
    modrows = dscr("modrows", [5, 6 * D])
    NTMAX = (SPC * (L + LC)) // 128
    NSLMAX = NTMAX * 128 * 4 // TS + 32
    h2d = dscr("h2d", [NTMAX * 128, D], BF16)
    xsort = dscr("xsort", [NSLMAX * TS, D], BF16)
    ysort = dscr("ysort", [NSLMAX * TS, D], F32)

    with contextlib.ExitStack() as gst:
        S = Sched(nc, gst)

        def ACT(out, in_, func, reads, writes, **kw):
            S.op("act", lambda e: e.activation(out=out, in_=in_, func=func, **kw), reads, writes)

        def MM(out, lhsT, rhs, start, stop, reads, writes):
            S.op("pe", lambda e: e.matmul(out, lhsT=lhsT, rhs=rhs, start=start, stop=stop), reads, writes)

        def TR(out, in_, ident, reads, writes):
            S.op("pe", lambda e: e.transpose(out=out, in_=in_, identity=ident), reads, writes)

        def TTOP(eng, out, in0, in1, op, reads, writes):
            S.op(eng, lambda e: e.tensor_tensor(out=out, in0=in0, in1=in1, op=op), reads, writes)

        def TSC(eng, out, in0, s1, s2, op0, op1, reads, writes):
            if op1 is None:
                S.op(eng, lambda e: e.tensor_scalar(out=out, in0=in0, scalar1=s1, scalar2=None, op0=op0), reads, writes)
            else:
                S.op(eng, lambda e: e.tensor_scalar(out=out, in0=in0, scalar1=s1, scalar2=s2, op0=op0, op1=op1),
                     reads, writes)

        def STT(eng, out, in0, scalar, in1, op0, op1, reads, writes):
            eng = "dve"
            S.op(eng, lambda e: e.scalar_tensor_tensor(out=out, in0=in0, scalar=scalar, in1=in1, op0=op0, op1=op1),
                 reads, writes)

        def CP(eng, out, in_, reads, writes):
            if eng == "act":
                S.op("act", lambda e: e.copy(out=out, in_=in_), reads, writes)
            else:
                S.op(eng, lambda e: e.tensor_copy(out=out, in_=in_), reads, writes)

        def DMA(q, out, in_, reads, writes):
            S.dma(q, lambda e: e.dma_start(out=out, in_=in_), reads, writes)

        def rstd_from(ssum, n, tmp):
            TSC("dve", tmp[:], ssum[:], 1.0 / n, EPS, ALU.mult, ALU.add, [ssum], [tmp])
            ACT(tmp[:], tmp[:], AF.Sqrt, [tmp], [tmp])
            S.op("dve", lambda e: e.reciprocal(out=tmp[:], in_=tmp[:]), [tmp], [tmp])

        ident32 = S.sb([128, 128], F32, "ident32")
        identb = S.sb([128, 128], BF16, "identb")
        DMA("sp", ident32[:], k_ident, [], [ident32])
        DMA("pool", identb[:], k_ident, [], [identb])

        def phase_ada(l):
            with contextlib.ExitStack() as ph:
                S.stack = ph
                cT = S.sb([128, 8, 5], F32)
                sc = S.sb([128, 8, 5], F32)
                DMA("sp", cT[:], cT_in, [], [cT])
                ACT(sc[:], cT[:], AF.Silu, [cT], [sc])
                rows = S.sb([5, 6 * D], F32)
                bb = S.sb([5, 6 * D], F32)
                DMA("sp", bb[:], ada_b[l:l + 1, :].partition_broadcast(5), [], [bb])
                wts = [S.sb([128, 8, 512], F32) for _ in range(2)]
                pms = [S.ps([128, 512]) for _ in range(2)]
                for n in range(12):
                    wt = wts[n % 2]
                    pm = pms[n % 2]
                    DMA("sp", wt[:], ada_w[l, :, n * 512:(n + 1) * 512].rearrange("(c p) n -> p c n", p=128), [], [wt])
                    for c in range(8):
                        MM(pm[0:5, :], sc[:, c, :], wt[:, c, :], c == 0, c == 7, [sc, wt], [pm])
                    TTOP("dve", rows[:, n * 512:(n + 1) * 512], pm[0:5, :], bb[:, n * 512:(n + 1) * 512], ALU.add,
                         [pm, bb], [rows])
                DMA("sp", modrows[:, :], rows[:], [rows], [modrows])
            S.stack = gst
            S.barrier()

        def phase_mix(l, last, xsrc, xdst, csrc, cdst):
            with contextlib.ExitStack() as ph:
                S.stack = ph
                win = S.sb([128, 8, INW], BF16, "win")
                for c in range(8):
                    for hf in range(2):
                        DMA("pool", win[:, c, hf * 1216:(hf + 1) * 1216], w_in[l, c * 128:(c + 1) * 128, hf * 1216:(hf + 1) * 1216],
                            [], [win])
                wout = S.sb([128, 8, D], BF16, "wout")
                gnc = S.sb([128, 8], F32)
                DMA("sp", gnc[:], gncol[l], [], [gnc])
                dg = S.sb([128, 2, 31, 128], BF16)
                dw = S.sb([128, 2, 31], F32)
                DMA("sp", dw[:], dwT[l], [], [dw])
                for cc_ in range(2):
                    for j_ in range(31):
                        TSC("pool" if j_ % 2 else "dve", dg[:, cc_, j_, :], identb[:], dw[:, cc_, j_:j_ + 1], None, ALU.mult, None,
                            [identb, dw], [dg])
                ccols = S.sb([128, 3, 2], F32)
                DMA("sp", ccols[:], convcols[l], [], [ccols])
                cpw = S.sb([128, 2, 256], BF16)
                DMA("pool", cpw[:], conv_pw[l].rearrange("(c p) n -> p c n", p=128), [], [cpw])
                wsT = S.sb([128, 4, 128], BF16)
                DMA("pool", wsT[:], gwsT[l].rearrange("h q p -> q h p"), [], [wsT])
                bsT = S.sb([128, 4], F32)
                DMA("sp", bsT[:], gbsT[l], [], [bsT])
                glg = S.sb([128, 256], F32)
                glb = S.sb([128, 256], F32)
                DMA("sp", glg[:], gln_g[l:l + 1, :].partition_broadcast(128), [], [glg])
                DMA("sp", glb[:], gln_b[l:l + 1, :].partition_broadcast(128), [], [glb])
                pw = S.sb([64, 4, 64], BF16)
                DMA("pool", pw[:], pool_w[l].rearrange("g c j -> c g j"), [], [pw])
                psc = S.sb([128, 256], F32)
                DMA("sp", psc[:], pool_scale[l:l + 1, :].partition_broadcast(128), [], [psc])
                poolM = S.sb([128, 20, 128], BF16)
                for hf in range(2):
                    DMA("pool", poolM[:, hf * 10:(hf + 1) * 10, :], k_poolM[:, hf * 10:(hf + 1) * 10, :], [], [poolM])
                masks = S.sb([128, 2, 256], BF16)
                DMA("pool", masks[:], k_masks, [], [masks])
                esink = S.sb([128, 4], F32)
                DMA("sp", esink[:], attn_sink[l:l + 1, :].partition_broadcast(128), [], [esink])
                ACT(esink[:], esink[:], AF.Exp, [esink], [esink])
                ropeC = S.sb([128, 512], F32)
                ropeS = S.sb([128, 512], F32)
                gn1 = S.sb([128, D], F32)
                DMA("sp", gn1[:], norm1_g[l:l + 1, :].partition_broadcast(128), [], [gn1])
                onesm = S.sb([128, 128], F32)
                S.op("dve", lambda e: e.memset(onesm[:], 1.0 / 256.0), [], [onesm])
                A1 = S.sb([128, D], F32)
                B1 = S.sb([128, D], F32)
                G1 = S.sb([128, D], F32)

                qT = [S.sb([128, L], BF16) for _ in range(2)]
                kT = [S.sb([128, L], BF16) for _ in range(2)]
                kcT = [S.sb([128, LC], BF16) for _ in range(2)]
                vaug = S.sb([128, 16, 2, 65], BF16)
                vcaug = S.sb([128, 2, 2, 65], BF16)
                S.op("pool", lambda e: e.memset(vaug[:], 1.0), [], [vaug])
                S.op("pool", lambda e: e.memset(vcaug[:], 1.0), [], [vcaug])
                convT = S.sb([128, 2, L + 30], BF16)
                S.op("pool", lambda e: e.memset(convT[:], 0.0), [], [convT])
                sTb = S.sb([128, 2, L], BF16)
                gmb = S.sb([128, 16, 256], BF16)
                poolh = S.sb([128, 16, 256], BF16)
                xt1_ = S.sb([128, D], F32)
                xt = [xt1_, xt1_]
                ss = S.sb([128, 1], F32)
                rs = S.sb([128, 1], F32)
                h32 = S.sb([128, D], F32)
                hb = S.sb([128, D], BF16)
                hT = S.sb([128, 8, 512], BF16)
                ftmp = [S.sb([128, 512], F32) for _ in range(3)]
                u32 = S.sb([128, 256], F32)
                v32 = S.sb([128, 256], F32)
                vnb = S.sb([128, 256], BF16)
                st2 = S.sb([128, 2], F32)
                st3 = S.sb([128, 2], F32)
                acc = S.sb([128, 2, 512], F32)
                mean_sb = S.sb([128, 512], F32)
                rstd_sb = S.sb([128, 512], F32)
                PT = [S.sb([128, 5, 256], BF16) for _ in range(2)]
                den = S.sb([128, 4], F32)
                mix = S.sb([128, D], F32)
                gss = S.sb([128, 4], F32)
                grs = S.sb([128, 4], F32)
                yb = S.sb([128, D], BF16)
                yT = S.sb([128, 8, 128], BF16)
                yTp = S.sb([64, 4, 128], BF16)
                xn = h32
                wtmp = [h32, mix]
                for c in range(8):
                    DMA("sp", wtmp[c % 2][:], w_out[l, c * 128:(c + 1) * 128, :], [], [wtmp[c % 2]])
                    TSC("dve", wout[:, c, :], wtmp[c % 2][:], gnc[:, c:c + 1], None, ALU.mult, None,
                        [wtmp[c % 2], gnc], [wout])
                pT = [S.ps([128, 128], BF16) for _ in range(2)]
                pf = [S.ps([128, 512]) for _ in range(2)]
                pa = S.ps([128, 512])
                pb = S.ps([128, 512])
                pg = S.ps([128, 512])
                pq = S.ps([128, 512])

                def load_mod(r):
                    DMA("sp", A1[:], modrows[r:r + 1, D:2 * D].partition_broadcast(128), [modrows], [A1])
                    DMA("sp", B1[:], modrows[r:r + 1, 0:D].partition_broadcast(128), [modrows], [B1])
                    DMA("sp", G1[:], modrows[r:r + 1, 2 * D:3 * D].partition_broadcast(128), [modrows], [G1])
                    STT("dve", A1[:], A1[:], 1.0, gn1[:], ALU.add, ALU.mult, [A1, gn1], [A1])

                cp_i = [0]

                def cp_alt(out, in_, reads, writes):
                    cp_i[0] += 1
                    CP("act" if cp_i[0] % 2 else "dve", out, in_, reads, writes)

                def seq(src, dst, row0, LL, is_ctx, do_pass2):
                    NT = LL // 128
                    GT = min(512, LL)
                    TG = GT // 128
                    KT = kcT if is_ctx else kT
                    VA = vcaug if is_ctx else vaug
                    for g in range(LL // GT):
                        for ti in range(TG):
                            t = g * TG + ti
                            x_ = xt[t % 2]
                            DMA("sp", x_[:], src[row0 + t * 128: row0 + (t + 1) * 128, :], [src], [x_])
                            ACT(yb[:], x_[:], AF.Square, [x_], [yb, ss], accum_out=ss[:])
                            rstd_from(ss, D, rs)
                            STT("dve", h32[:], x_[:], rs[:, 0:1], A1[:], ALU.mult, ALU.mult, [x_, rs, A1], [h32])
                            TTOP("dve", hb[:], h32[:], B1[:], ALU.add, [h32, B1], [hb])
                            p_ = pT[t % 2]
                            for c in range(8):
                                TR(p_[:, c * 128:(c + 1) * 128], hb[:, c * 128:(c + 1) * 128], identb[:], [hb, identb], [p_])
                            cp_alt(hT[:, :, ti * 128:(ti + 1) * 128], p_[:, 0:1024].rearrange("p (c t) -> p c t", c=8), [p_], [hT])
                        if not is_ctx:
                            DMA("sp", ropeC[:, :GT], k_ropeC[:, g * GT:(g + 1) * GT], [], [ropeC])
                            DMA("sp", ropeS[:, :GT], k_ropeS[:, g * GT:(g + 1) * GT], [], [ropeS])
                        order = [0, 2, 1, 3, 4, 6, 5, 7, 10, 8, 11, 9] if not is_ctx else [0, 1, 4, 5, 10, 8, 11, 9]
                        tsl = slice(g * GT, (g + 1) * GT)
                        for oi, fc in enumerate(order):
                            p_ = pf[oi % 2]
                            for c in range(8):
                                MM(p_[:, :GT], win[:, c, fc * 128:(fc + 1) * 128], hT[:, c, :GT], c == 0, c == 7,
                                   [win, hT], [p_])
                            if fc in (0, 1, 4, 5):
                                dstT = (qT if not is_ctx else qT)[fc] if fc < 2 else KT[fc - 4]
                                if is_ctx:
                                    cp_alt(dstT[:, tsl], p_[:, :GT], [p_], [dstT])
                                else:
                                    TTOP("dve", ftmp[0][:, :GT], p_[:, :GT], ropeC[:, :GT], ALU.mult, [p_, ropeC], [ftmp[0]])
                            elif fc in (2, 3, 6, 7):
                                dstT = qT[fc - 2] if fc < 4 else KT[fc - 6]
                                TTOP("dve", ftmp[1][:, :GT], p_[:, :GT], ropeS[:, :GT], ALU.mult, [p_, ropeS], [ftmp[1]])
                                TTOP("dve", dstT[:, tsl], ftmp[0][:, :GT], ftmp[1][:, :GT], ALU.add,
                                     [ftmp[0], ftmp[1]], [dstT])
                            elif fc in (10, 11):
                                ACT(ftmp[2][:, :GT], p_[:, :GT], AF.Sigmoid, [p_], [ftmp[2]])
                            else:
                                cc = fc - 8
                                TTOP("dve", convT[:, cc, 15 + g * GT: 15 + (g + 1) * GT], p_[:, :GT], ftmp[2][:, :GT],
                                     ALU.mult, [p_, ftmp[2]], [convT])
                        for ti in range(TG):
                            t = g * TG + ti
                            for c in range(8):
                                MM(pa[:, :], hT[:, c, ti * 128:(ti + 1) * 128], win[:, c, 1536:2048], c == 0, c == 7,
                                   [hT, win], [pa])
                            for c in range(8):
                                MM(pb[:, 0:384], hT[:, c, ti * 128:(ti + 1) * 128], win[:, c, 2048:2432], c == 0, c == 7,
                                   [hT, win], [pb])
                            S.op("act", lambda e: e.copy(out=VA[:, t, :, 0:64],
                                                          in_=pb[:, 0:128].rearrange("p (j d) -> p j d", j=2)), [pb], [VA])
                            CP("dve", poolh[:, t, :], pb[:, 128:384], [pb], [poolh])
                            CP("act", u32[:], pa[:, 0:256], [pa], [u32])
                            ACT(v32[:], pa[:, 256:512], AF.Copy, [pa], [v32, st2], accum_out=st2[:, 0:1])
                            ACT(yb[:, 0:256], pa[:, 256:512], AF.Square, [pa], [yb, st2], accum_out=st2[:, 1:2])
                            TSC("dve", st3[:, 0:1], st2[:, 0:1], 1.0 / 256, None, ALU.mult, None, [st2], [st3])
                            STT("dve", st3[:, 1:2], st3[:, 0:1], -1.0, st3[:, 0:1], ALU.mult, ALU.mult, [st3], [st3])
                            STT("dve", st3[:, 1:2], st2[:, 1:2], 1.0 / 256, st3[:, 1:2], ALU.mult, ALU.add, [st2, st3], [st3])
                            TSC("dve", st3[:, 1:2], st3[:, 1:2], EPS, None, ALU.add, None, [st3], [st3])
                            ACT(st3[:, 1:2], st3[:, 1:2], AF.Sqrt, [st3], [st3])
                            S.op("dve", lambda e: e.reciprocal(out=st3[:, 1:2], in_=st3[:, 1:2]), [st3], [st3])
                            TSC("dve", v32[:], v32[:], st3[:, 0:1], st3[:, 1:2], ALU.subtract, ALU.mult, [v32, st3], [v32])
                            TTOP("dve", v32[:], v32[:], glg[:], ALU.mult, [v32, glg], [v32])
                            TTOP("dve", vnb[:], v32[:], glb[:], ALU.add, [v32, glb], [vnb])
                            for hh in range(4):
                                MM(pg[:, hh * 64:(hh + 1) * 64], wsT[:, hh, :], vnb[:, hh * 64:(hh + 1) * 64], True, True,
                                   [wsT, vnb], [pg])
                            for hh in range(4):
                                STT("dve", gmb[:, t, hh * 64:(hh + 1) * 64], pg[:, hh * 64:(hh + 1) * 64], bsT[:, hh:hh + 1],
                                    u32[:, hh * 64:(hh + 1) * 64], ALU.add, ALU.mult, [pg, bsT, u32], [gmb])
                    if not do_pass2:
                        return
                    for g in range(LL // GT):
                        tsl = slice(g * GT, (g + 1) * GT)
                        for cc in range(2):
                            pc_ = pq if cc == 0 else pg
                            for j in range(31):
                                MM(pc_[:, :GT], dg[:, cc, j, :], convT[:, cc, g * GT + j: g * GT + j + GT], j == 0, j == 30,
                                   [dg, convT], [pc_])
                            TSC("dve", acc[:, cc, :GT], pc_[:, :GT], ccols[:, 0, cc:cc + 1], None, ALU.add, None, [pc_, ccols], [acc])
                        for cc in range(2):
                            MM(pq[:, :GT], onesm[:], acc[:, cc, :GT], cc == 0, cc == 1, [onesm, acc], [pq])
                        CP("act", mean_sb[:, :GT], pq[:, :GT], [pq], [mean_sb])
                        for cc in range(2):
                            ACT(ftmp[cc][:, :GT], acc[:, cc, :GT], AF.Square, [acc], [ftmp[cc]])
                        for cc in range(2):
                            MM(pq[:, :GT], onesm[:], ftmp[cc][:, :GT], cc == 0, cc == 1, [onesm, ftmp[cc]], [pq])
                        TTOP("dve", rstd_sb[:, :GT], mean_sb[:, :GT], mean_sb[:, :GT], ALU.mult, [mean_sb], [rstd_sb])
                        TTOP("dve", rstd_sb[:, :GT], pq[:, :GT], rstd_sb[:, :GT], ALU.subtract, [pq, rstd_sb], [rstd_sb])
                        TSC("dve", rstd_sb[:, :GT], rstd_sb[:, :GT], EPS, None, ALU.add, None, [rstd_sb], [rstd_sb])
                        ACT(rstd_sb[:, :GT], rstd_sb[:, :GT], AF.Sqrt, [rstd_sb], [rstd_sb])
                        S.op("dve", lambda e: e.reciprocal(out=rstd_sb[:, :GT], in_=rstd_sb[:, :GT]), [rstd_sb], [rstd_sb])
                        for cc in range(2):
                            TTOP("dve", acc[:, cc, :GT], acc[:, cc, :GT], mean_sb[:, :GT], ALU.subtract, [acc, mean_sb], [acc])
                            TTOP("dve", acc[:, cc, :GT], acc[:, cc, :GT], rstd_sb[:, :GT], ALU.mult, [acc, rstd_sb], [acc])
                            ACT(sTb[:, cc, tsl], acc[:, cc, :GT], AF.Silu, [acc, ccols], [sTb],
                                scale=ccols[:, 1, cc:cc + 1], bias=ccols[:, 2, cc:cc + 1])
                        for ti in range(TG):
                            t = g * TG + ti
                            qs = slice(t * 128, (t + 1) * 128)
                            x_ = xt[t % 2]
                            DMA("sp", x_[:], src[row0 + t * 128: row0 + (t + 1) * 128, :], [src], [x_])
                            blocks = []
                            for cb in range(2):
                                blocks.append((kcT, slice(cb * 128, (cb + 1) * 128), vcaug, cb, None))
                            if not is_ctx:
                                if t > 0:
                                    blocks.append((kT, slice((t - 1) * 128, t * 128), vaug, t - 1, 0))
                                blocks.append((kT, qs, vaug, t, None))
                                if t < NT - 1:
                                    blocks.append((kT, slice((t + 1) * 128, (t + 2) * 128), vaug, t + 1, 1))
                            nb = len(blocks)
                            for j in range(2):
                                P_ = PT[j]
                                for lo in range(0, nb, 4):
                                    hi = min(nb, lo + 4)
                                    for gq in range(2):
                                        pr = slice(64 * gq, 64 * gq + 64)
                                        psc_ = pa if gq == 0 else pq
                                        for bi in range(lo, hi):
                                            Ks, ksl, Vt, vi, mi = blocks[bi]
                                            MM(psc_[:, (bi - lo) * 128:(bi - lo + 1) * 128], Ks[j][pr, ksl], qT[j][pr, qs], True, True,
                                               [Ks[j], qT[j]], [psc_])
                                    for gq in range(2):
                                        psc_ = pa if gq == 0 else pq
                                        ACT(P_[:, lo:hi, gq * 128:(gq + 1) * 128],
                                            psc_[:, 0:(hi - lo) * 128].rearrange("p (b q) -> p b q", q=128), AF.Exp, [psc_], [P_], scale=0.125)
                                for bi, (Ks, ksl, Vt, vi, mi) in enumerate(blocks):
                                    if mi is not None:
                                        TTOP("dve", P_[:, bi, :], P_[:, bi, :], masks[:, mi, :], ALU.mult, [P_, masks], [P_])
                                for gq in range(2):
                                    hh = 2 * j + gq
                                    for bi, (Ks, ksl, Vt, vi, mi) in enumerate(blocks):
                                        MM(pb[:, hh * 65:(hh + 1) * 65], P_[:, bi, gq * 128:(gq + 1) * 128], Vt[:, vi, j, :],
                                           bi == 0, bi == nb - 1, [P_, Vt], [pb])
                            pb3 = pb[:, 0:260].rearrange("p (h d) -> p h d", h=4)
                            TTOP("dve", den[:], pb3[:, :, 64], esink[:], ALU.add, [pb, esink], [den])
                            S.op("dve", lambda e: e.reciprocal(out=den[:], in_=den[:]), [den], [den])
                            for hh in range(4):
                                if hh % 2 == 0:
                                    TSC("dve", mix[:, hh * 64:(hh + 1) * 64], pb[:, hh * 65: hh * 65 + 64], den[:, hh:hh + 1], None,
                                        ALU.mult, None, [pb, den], [mix])
                                else:
                                    ACT(mix[:, hh * 64:(hh + 1) * 64], pb[:, hh * 65: hh * 65 + 64], AF.Copy, [pb, den], [mix],
                                        scale=den[:, hh:hh + 1])
                            for cc in range(2):
                                MM(pg[:, 0:256], sTb[:, cc, qs], cpw[:, cc, :], cc == 0, cc == 1, [sTb, cpw], [pg])
                            CP("act", mix[:, 256:512], pg[:, 0:256], [pg], [mix])
                            CP("dve", mix[:, 512:768], gmb[:, t, :], [gmb], [mix])
                            for gi in range(4):
                                srcs = []
                                if t > 0:
                                    srcs.append((t - 1, gi * 5 + 0))
                                srcs.append((t, gi * 5 + (3 if t == 0 else (4 if t == NT - 1 else 2))))
                                if t < NT - 1:
                                    srcs.append((t + 1, gi * 5 + 1))
                                for si, (tt_, mi) in enumerate(srcs):
                                    MM(pq[0:64, gi * 128:(gi + 1) * 128], poolh[:, tt_, gi * 64:(gi + 1) * 64], poolM[:, mi, :],
                                       si == 0, si == len(srcs) - 1, [poolh, poolM], [pq])
                            CP("dve", yTp[:, :, :], pq[0:64, :].rearrange("p (g t) -> p g t", g=4), [pq], [yTp])
                            for gi in range(4):
                                MM(pg[:, 256 + gi * 64: 256 + (gi + 1) * 64], yTp[:, gi, :], pw[:, gi, :], True, True,
                                   [yTp, pw], [pg])
                            TTOP("dve", mix[:, 768:1024], pg[:, 256:512], psc[:], ALU.mult, [pg, psc], [mix])
                            for gi in range(4):
                                ACT(hb[:, 0:256], mix[:, gi * 256:(gi + 1) * 256], AF.Square, [mix], [hb, gss],
                                    accum_out=gss[:, gi:gi + 1])
                            rstd_from(gss, 256, grs)
                            for gi in range(4):
                                if gi % 2 == 0:
                                    TSC("dve", yb[:, gi * 256:(gi + 1) * 256], mix[:, gi * 256:(gi + 1) * 256], grs[:, gi:gi + 1],
                                        None, ALU.mult, None, [mix, grs], [yb])
                                else:
                                    TSC("dve", yb[:, gi * 256:(gi + 1) * 256], mix[:, gi * 256:(gi + 1) * 256], grs[:, gi:gi + 1],
                                        None, ALU.mult, None, [mix, grs], [yb])
                            p_ = pT[t % 2]
                            for c in range(8):
                                TR(p_[:, c * 128:(c + 1) * 128], yb[:, c * 128:(c + 1) * 128], identb[:], [yb, identb], [p_])
                            cp_alt(yT[:, :, :], p_[:, 0:1024].rearrange("p (c t) -> p c t", c=8), [p_], [yT])
                            for nbk in range(2):
                                p_ = pf[nbk]
                                for c in range(8):
                                    MM(p_[:, :], yT[:, c, :], wout[:, c, nbk * 512:(nbk + 1) * 512], c == 0, c == 7, [yT, wout], [p_])
                                TTOP("dve", xn[:, nbk * 512:(nbk + 1) * 512], p_[:, :], G1[:, nbk * 512:(nbk + 1) * 512], ALU.mult,
                                     [p_, G1], [xn])
                            TTOP("dve", xn[:], xn[:], x_[:], ALU.add, [xn, x_], [xn])
                            DMA("sp", dst[row0 + t * 128: row0 + (t + 1) * 128, :], xn[:], [xn], [dst])

                import os
                lim = os.environ.get("MIXLIM", "")
                for s in range(SPC):
                    load_mod(4)
                    seq(csrc, cdst, s * LC, LC, True, not last)
                    if lim == "c":
                        break
                    load_mod(s)
                    seq(xsrc, xdst, s * L, L, False, True)
                    if lim == "cx":
                        break
            S.stack = gst
            S.barrier()

        def phase_moe(l, last, tiles, final):
            NT = len(tiles)
            NSL = NT * 128 * 4 // TS + 32
            with contextlib.ExitStack() as ph:
                S.stack = ph
                idxi = S.sb([128, NT, 4], I32, "idxi")
                gate4 = S.sb([128, NT, 4], F32, "gate4")
                widx = S.sb([128, 128], I32, "widx")
                w2idx = S.sb([128, 128], I32, "w2idx")
                bidx = S.sb([128, 128], I32, "bidx")
                Gd = S.sb([128, NT, 32], F32, "Gd")
                A2 = S.sb([128, D], F32)
                B2 = S.sb([128, D], F32)
                G2 = S.sb([128, D], F32)
                gn2 = S.sb([128, D], F32)
                DMA("sp", gn2[:], norm2_g[l:l + 1, :].partition_broadcast(128), [], [gn2])
                cur = [None]

                def load_mod(r, which):
                    if cur[0] == (r, which):
                        return
                    cur[0] = (r, which)
                    if which == 0:
                        DMA("sp", A2[:], modrows[r:r + 1, 4 * D:5 * D].partition_broadcast(128), [modrows], [A2])
                        DMA("sp", B2[:], modrows[r:r + 1, 3 * D:4 * D].partition_broadcast(128), [modrows], [B2])
                        STT("dve", A2[:], A2[:], 1.0, gn2[:], ALU.add, ALU.mult, [A2, gn2], [A2])
                    else:
                        DMA("sp", G2[:], modrows[r:r + 1, 5 * D:6 * D].partition_broadcast(128), [modrows], [G2])

                with contextlib.ExitStack() as ph1:
                    S.stack = ph1
                    rw = S.sb([128, 8, 32], F32)
                    DMA("sp", rw[:], router_w[l].rearrange("(c p) n -> p c n", p=128), [], [rw])
                    rbb = S.sb([128, 32], F32)
                    DMA("sp", rbb[:], router_b[l:l + 1, :].partition_broadcast(128), [], [rbb])
                    upper = S.sb([128, 128], BF16)
                    DMA("pool", upper[:], k_upper, [], [upper])
                    onesb = S.sb([128, 128], BF16)
                    S.op("dve", lambda e: e.memset(onesb[:], 1.0), [], [onesb])
                    ones32 = S.sb([128, 1], F32)
                    S.op("dve", lambda e: e.memset(ones32[:], 1.0), [], [ones32])
                    thr = S.sb([32, 128], F32)
                    DMA("sp", thr[:], k_slotthr, [], [thr])
                    lg = S.sb([128, NT, 32], F32)
                    t8 = S.sb([128, NT, 8], F32)
                    pos = S.sb([128, NT, 32], F32)
                    base = S.sb([128, 32], F32)
                    S.op("dve", lambda e: e.memset(base[:], 0.0), [], [base])
                    xt = [S.sb([128, D], F32) for _ in range(2)]
                    junk = S.sb([128, D], BF16)
                    ss = S.sb([128, 1], F32)
                    rs = S.sb([128, 1], F32)
                    h32 = S.sb([128, D], F32)
                    hb = [S.sb([128, D], BF16) for _ in range(2)]
                    hT32 = S.sb([128, 8, 128], F32)
                    maskb = S.sb([128, 32], BF16)
                    zt = S.sb([128, 2048], BF16)
                    S.op("pool", lambda e: e.memset(zt[:], 0.0), [], [zt])
                    pT32 = [S.ps([128, 128], F32) for _ in range(2)]
                    pl = S.ps([128, 512])
                    pp = S.ps([128, 512])
                    xz = xsort[0:NSL * TS, :].rearrange("(n p r) d -> n p (r d)", p=128, r=2)
                    zlist = list(range(NSL * TS // 256))
                    zper = -(-len(zlist) // (NT // 2))
                    h32s = [h32, S.sb([128, D], F32)]
                    hT32s = [hT32, S.sb([128, 8, 128], F32)]
                    junks = [junk, S.sb([128, D], BF16)]
                    sss = [ss, S.sb([128, 1], F32)]
                    rss = [rs, S.sb([128, 1], F32)]
                    maskbs = [maskb, S.sb([128, 32], BF16)]
                    pT32s = [pT32, [S.ps([128, 128], F32) for _ in range(2)]]
                    pls = [pl, S.ps([128, 512])]
                    pps = [pp, S.ps([128, 512])]

                    def route_tile(t, k):
                        src, dst, row0, r = tiles[t]
                        x_, h32_, hb_, hT_, jk, ss_, rs_, mk = xt[k], h32s[k], hb[k], hT32s[k], junks[k], sss[k], rss[k], maskbs[k]
                        pTk, pl_, pp_ = pT32s[k], pls[k], pps[k]
                        DMA("sp", x_[:], src[row0:row0 + 128, :], [src], [x_])
                        yield
                        ACT(jk[:], x_[:], AF.Square, [x_], [jk, ss_], accum_out=ss_[:])
                        yield
                        TSC("dve", rs_[:], ss_[:], 1.0 / D, EPS, ALU.mult, ALU.add, [ss_], [rs_])
                        yield
                        ACT(rs_[:], rs_[:], AF.Sqrt, [rs_], [rs_])
                        yield
                        S.op("dve", lambda e: e.reciprocal(out=rs_[:], in_=rs_[:]), [rs_], [rs_])
                        yield
                        STT("dve", h32_[:], x_[:], rs_[:, 0:1], A2[:], ALU.mult, ALU.mult, [x_, rs_, A2], [h32_])
                        yield
                        TTOP("dve", h32_[:], h32_[:], B2[:], ALU.add, [h32_, B2], [h32_])
                        yield
                        CP("act", hb_[:], h32_[:], [h32_], [hb_])
                        DMA("sp", h2d[t * 128:(t + 1) * 128, :], hb_[:], [hb_], [h2d])
                        yield
                        for hf in range(2):
                            p_ = pTk[hf]
                            for cc in range(4):
                                c = hf * 4 + cc
                                TR(p_[:, cc * 128:(cc + 1) * 128], h32_[:, c * 128:(c + 1) * 128], ident32[:], [h32_, ident32], [p_])
                            CP("act" if hf else "dve", hT_[:, hf * 4:(hf + 1) * 4, :],
                               p_[:, 0:512].rearrange("p (c t) -> p c t", c=4), [p_], [hT_])
                            yield
                        for c in range(8):
                            MM(pl_[:, 0:32], hT_[:, c, :], rw[:, c, :], c == 0, c == 7, [hT_, rw], [pl_])
                        yield
                        TTOP("dve", lg[:, t, :], pl_[:, 0:32], rbb[:], ALU.add, [pl_, rbb], [lg])
                        yield
                        S.op("dve", lambda e: e.max(out=t8[:, t, :], in_=lg[:, t, :]), [lg], [t8])
                        yield
                        TSC("dve", mk[:], lg[:, t, :], t8[:, t, 3:4], None, ALU.is_ge, None, [lg, t8], [mk])
                        yield
                        MM(pp_[:, 0:32], upper[:], mk[:], True, True, [upper, mk], [pp_])
                        MM(pp_[:, 32:64], onesb[:], mk[:], True, True, [onesb, mk], [pp_])
                        yield
                        TSC("dve", ss_[:], t8[:, t, 0:1], -1.0, None, ALU.mult, None, [t8], [ss_])
                        yield
                        ACT(gate4[:, t, :], t8[:, t, 0:4], AF.Exp, [t8, ss_], [gate4, rs_], bias=ss_[:, 0:1], accum_out=rs_[:, 0:1])
                        yield
                        S.op("dve", lambda e: e.reciprocal(out=rs_[:], in_=rs_[:]), [rs_], [rs_])
                        yield
                        TSC("dve", gate4[:, t, :], gate4[:, t, :], rs_[:, 0:1], None, ALU.mult, None, [gate4, rs_], [gate4])
                        ACT(Gd[:, t, :], lg[:, t, :], AF.Exp, [lg, ss_], [Gd], bias=ss_[:, 0:1])
                        yield
                        TTOP("dve", Gd[:, t, :], Gd[:, t, :], mk[:], ALU.mult, [Gd, mk], [Gd])
                        yield
                        TSC("dve", Gd[:, t, :], Gd[:, t, :], rs_[:, 0:1], None, ALU.mult, None, [Gd, rs_], [Gd])
                        TTOP("dve", pos[:, t, :], pp_[:, 0:32], base[:], ALU.add, [pp_, base], [pos])
                        TTOP("dve", base[:], pp_[:, 32:64], base[:], ALU.add, [pp_, base], [base])
                        yield

                    for t0 in range(0, NT, 2):
                        load_mod(tiles[t0][3], 0)
                        alive = [route_tile(t0, 0), route_tile(t0 + 1, 1)]
                        while alive:
                            for g_ in list(alive):
                                try:
                                    next(g_)
                                except StopIteration:
                                    alive.remove(g_)
                        for _z in range(zper):
                            if zlist:
                                DMA("sp", xz[zlist.pop()], zt[:], [zt], [xsort])
                    while zlist:
                        DMA("sp", xz[zlist.pop()], zt[:], [zt], [xsort])
                    padded = S.sb([128, 32], F32)
                    pend = S.sb([128, 32], F32)
                    pstart = S.sb([128, 32], F32)
                    tmpc = S.sb([128, 32], F32)
                    TSC("dve", padded[:], base[:], 0.0, None, ALU.is_gt, None, [base], [padded])
                    for jj in range(1, NT * 128 // TS + 1):
                        STT("dve", padded[:], base[:], float(TS * jj), padded[:], ALU.is_gt, ALU.add, [base, padded], [padded])
                    TSC("dve", padded[:], padded[:], float(TS), None, ALU.mult, None, [padded], [padded])
                    CP("dve", pend[:, 0:1], padded[:, 0:1], [padded], [pend])
                    for e_ in range(1, 32):
                        TTOP("dve", pend[:, e_:e_ + 1], pend[:, e_ - 1:e_], padded[:, e_:e_ + 1], ALU.add, [pend, padded], [pend])
                    TTOP("dve", pstart[:], pend[:], padded[:], ALU.subtract, [pend, padded], [pstart])
                    pcol = S.sb([32, 1], F32)
                    tmpd = S.sb([32, 32], F32)
                    TTOP("dve", tmpd[:], pend[0:32, :], ident32[0:32, 0:32], ALU.mult, [pend, ident32], [tmpd])
                    S.op("dve", lambda e: e.reduce_sum(out=pcol[:], in_=tmpd[:], axis=AX.X), [tmpd], [pcol])
                    cmp = S.sb([32, 128], F32)
                    TSC("dve", cmp[:], thr[:], pcol[:, 0:1], None, ALU.is_ge, None, [thr, pcol], [cmp])
                    ones32m = S.sb([32, 128], F32)
                    S.op("dve", lambda e: e.memset(ones32m[:], 1.0), [], [ones32m])
                    piota = S.sb([128, 1], F32)
                    DMA("sp", piota[:], k_piota, [], [piota])
                    MM(pl[:, 0:128], ones32m[0:32, :], cmp[:], True, True, [ones32m, cmp], [pl])
                    blkf = S.sb([128, 128], F32)
                    TSC("dve", blkf[:], pl[:, 0:128], 31.0, None, ALU.min, None, [pl], [blkf])
                    wif = S.sb([128, 128], F32)
                    same = S.sb([128, 128], F32)
                    S.op("dve", lambda e: e.memset(same[:], 0.0), [], [same])
                    TTOP("dve", same[:, 1:128], blkf[:, 1:128], blkf[:, 0:127], ALU.is_equal, [blkf], [same])
                    S.op("dve", lambda e: e.memset(same[:, NSL // 2:NSL // 2 + 1], 0.0), [], [same])
                    TSC("dve", same[:], same[:], SKIPBIG, None, ALU.mult, None, [same], [same])
                    STT("dve", wif[:], blkf[:], 1024.0, same[:], ALU.mult, ALU.add, [blkf, same], [wif])
                    TSC("dve", wif[:], wif[:], piota[:, 0:1], None, ALU.add, None, [wif, piota], [wif])
                    CP("dve", widx[:], wif[:], [wif], [widx])
                    STT("dve", wif[:], blkf[:], 512.0, same[:], ALU.mult, ALU.add, [blkf, same], [wif])
                    TSC("dve", wif[:], wif[:], piota[:, 0:1], None, ALU.add, None, [wif, piota], [wif])
                    CP("dve", w2idx[:], wif[:], [wif], [w2idx])
                    STT("dve", wif[:], blkf[:], 128.0, same[:], ALU.mult, ALU.add, [blkf, same], [wif])
                    TSC("dve", wif[:], wif[:], piota[:, 0:1], None, ALU.add, None, [wif, piota], [wif])
                    CP("dve", bidx[:], wif[:], [wif], [bidx])
                    oh = S.sb([128, 32], F32)
                    idxf = S.sb([128, 4], F32)
                    for t, (src, dst, row0, r) in enumerate(tiles):
                        TTOP("dve", pos[:, t, :], pos[:, t, :], pstart[:], ALU.add, [pos, pstart], [pos])
                        for k in range(4):
                            STT("dve", oh[:], lg[:, t, :], t8[:, t, k:k + 1], pos[:, t, :], ALU.is_equal, ALU.mult,
                                [lg, t8, pos], [oh])
                            S.op("dve", lambda e: e.reduce_sum(out=idxf[:, k:k + 1], in_=oh[:], axis=AX.X), [oh], [idxf])
                        CP("dve", idxi[:, t, :], idxf[:], [idxf], [idxi])
                        hb_ = hb[t % 2]
                        DMA("sp", hb_[:], h2d[t * 128:(t + 1) * 128, :], [h2d], [hb_])
                        for k in range(4):
                            S.dma("pool", lambda e: e.indirect_dma_start(
                                out=xsort[:, :], out_offset=bass.IndirectOffsetOnAxis(ap=idxi[:, t, k:k + 1], axis=0),
                                in_=hb_[:], in_offset=None), [hb_, idxi], [xsort])
                S.stack = ph
                S.barrier()
                if stop_after == ("route", l):
                    return
                with contextlib.ExitStack() as ph2:
                    S.stack = ph2
                    w1b = [S.sb([128, 8, 2 * D], BF16) for _ in range(2)]
                    w2b = [S.sb([128, 8, D], BF16) for _ in range(2)]
                    b1c = [S.sb([128, 16], F32) for _ in range(2)]
                    xs_ = [S.sb([128, 4, D], BF16) for _ in range(2)]
                    xsT2 = [S.sb([128, 8, TS], BF16) for _ in range(2)]
                    actT = S.sb([128, 8, TS], BF16)
                    b1p = [S.sb([128, 8], F32) for _ in range(2)]
                    linp = [S.sb([128, TS], F32) for _ in range(2)]
                    g32s = [S.sb([128, TS], F32) for _ in range(2)]
                    sgs = [S.sb([128, TS], F32) for _ in range(2)]
                    tts = [S.sb([128, TS], F32) for _ in range(2)]
                    yt = [S.sb([128, D], F32) for _ in range(2)]
                    pT = [S.ps([128, 128], BF16) for _ in range(2)]
                    ph_ = [S.ps([128, 512]) for _ in range(4)]
                    py = [S.ps([128, 512]) for _ in range(2)]
                    w1flat = exp_w1.rearrange("l e k n -> (l e k) n")
                    w2flat = exp_w2.rearrange("l e c p n -> (l e c p) n")
                    b1flat = exp_b1T.rearrange("l e p n -> (l e p) n")

                    if not hasattr(build, "_bnd") or build._bnd[0] is not nc:
                        rg_ = nc.gpsimd.alloc_register("bndreg")
                        nc.gpsimd.reg_mov(rg_, 2 * 32 * 1024 - 1)
                        build._bnd = (nc, rg_)
                    bnd_reg = build._bnd[1]

                    HS = NSL // 2
                    order = []
                    for q_ in range(HS):
                        order += [q_, HS + q_]

                    def load_w(j):
                        b = 0 if j < HS else 1
                        for c in range(8):
                            S.dma("pool", lambda e: e.indirect_dma_start(
                                out=w1b[b][:, c, :], out_offset=None, in_=w1flat,
                                in_offset=bass.IndirectOffsetOnAxis(ap=widx[:, j:j + 1], axis=0),
                                element_offset=(l * 32 * 1024 + c * 128) * 2048, bounds_check=bnd_reg, oob_is_err=False), [widx], [w1b[b]])
                        for c2 in range(4):
                            S.dma("pool", lambda e: e.indirect_dma_start(
                                out=w2b[b][:, 2 * c2:2 * c2 + 2, :].rearrange("p c n -> p (c n)"), out_offset=None, in_=w2flat,
                                in_offset=bass.IndirectOffsetOnAxis(ap=w2idx[:, j:j + 1], axis=0),
                                element_offset=(l * 128 + c2) * 128 * 2048, bounds_check=bnd_reg, oob_is_err=False),
                                [w2idx], [w2b[b]])
                        S.dma("pool", lambda e: e.indirect_dma_start(
                            out=b1c[b][:], out_offset=None, in_=b1flat,
                            in_offset=bass.IndirectOffsetOnAxis(ap=bidx[:, j:j + 1], axis=0),
                            element_offset=l * 32 * 128 * 16, bounds_check=bnd_reg, oob_is_err=False), [bidx], [b1c[b]])

                    def load_x(j, i):
                        DMA("sp", xs_[i % 2][:], xsort[j * TS:(j + 1) * TS, :].rearrange("(a p) d -> p a d", p=128),
                            [xsort], [xs_[i % 2]])

                    def do_transposes(ii):
                        X, xsT = xs_[ii % 2], xsT2[ii % 2]
                        for a in range(4):
                            p_ = pT[a % 2]
                            for c in range(8):
                                TR(p_[:, c * 128:(c + 1) * 128], X[:, a, c * 128:(c + 1) * 128], identb[:], [X, identb], [p_])
                            CP("act" if a % 2 else "dve", xsT[:, :, a * 128:(a + 1) * 128],
                               p_[:, 0:1024].rearrange("p (c t) -> p c t", c=8), [p_], [xsT])

                    load_w(order[0])
                    load_x(order[0], 0)
                    do_transposes(0)
                    for i_, j in enumerate(order):
                        b = 0 if j < HS else 1
                        if i_ + 1 < NSL:
                            load_w(order[i_ + 1])
                            load_x(order[i_ + 1], i_ + 1)
                        W1, W2, B1c, X, xsT = w1b[b], w2b[b], b1c[b], xs_[i_ % 2], xsT2[i_ % 2]
                        TSC("dve", b1p[b][:], B1c[:, 8:16], 1.0, None, ALU.add, None, [B1c], [b1p[b]])
                        pi = 0
                        for i in range(8):
                            k2 = i % 2
                            pl_ = ph_[pi % 4]
                            pi += 1
                            for c in range(8):
                                MM(pl_[:, :], W1[:, c, (8 + i) * 128:(9 + i) * 128], xsT[:, c, :], c == 0, c == 7, [W1, xsT], [pl_])
                            TSC("dve", linp[k2][:], pl_[:, :], b1p[b][:, i:i + 1], -6.0, ALU.add, ALU.max, [pl_, b1p[b]], [linp[k2]])
                            pg_ = ph_[pi % 4]
                            pi += 1
                            for c in range(8):
                                MM(pg_[:, :], W1[:, c, i * 128:(i + 1) * 128], xsT[:, c, :], c == 0, c == 7, [W1, xsT], [pg_])
                            TSC("dve", g32s[k2][:], pg_[:, :], B1c[:, i:i + 1], 7.0, ALU.add, ALU.min, [pg_, B1c], [g32s[k2]])
                            ACT(sgs[k2][:], g32s[k2][:], AF.Sigmoid, [g32s[k2]], [sgs[k2]], scale=1.702)
                            TTOP("dve", tts[k2][:], sgs[k2][:], g32s[k2][:], ALU.mult, [sgs[k2], g32s[k2]], [tts[k2]])
                            STT("dve", actT[:, i, :], linp[k2][:], 8.0, tts[k2][:], ALU.min, ALU.mult, [linp[k2], tts[k2]], [actT])
                        if i_ + 1 < NSL:
                            do_transposes(i_ + 1)
                        for a in range(4):
                            y_ = yt[a % 2]
                            for nbk in range(2):
                                p_ = py[nbk]
                                for f in range(8):
                                    MM(p_[:, :], actT[:, f, a * 128:(a + 1) * 128], W2[:, f, nbk * 512:(nbk + 1) * 512],
                                       f == 0, f == 7, [actT, W2], [p_])
                                CP("act", y_[:, nbk * 512:(nbk + 1) * 512], p_[:, :], [p_], [y_])
                            DMA("sp", ysort[j * TS + a * 128: j * TS + (a + 1) * 128, :], y_[:], [y_], [ysort])
                S.stack = ph
                S.barrier()
                with contextlib.ExitStack() as ph3:
                    S.stack = ph3
                    yk = [[S.sb([128, D], F32) for _ in range(4)] for _ in range(2)]
                    xt = [S.sb([128, D], F32) for _ in range(2)]
                    acc = S.sb([128, D], F32)
                    xn = [S.sb([128, D], F32) for _ in range(2)]
                    junk = S.sb([128, D], BF16)
                    ss = S.sb([128, 1], F32)
                    rs = S.sb([128, 1], F32)
                    fng = S.sb([128, D], F32)
                    DMA("sp", fng[:], final_g[0:1, :].partition_broadcast(128), [], [fng])
                    b2all = S.sb([32, D], F32)
                    DMA("sp", b2all[:], exp_b2[l], [], [b2all])
                    GTs = S.sb([32, 128], F32)
                    pGT = S.ps([128, 512])
                    pbias = [S.ps([128, 512]) for _ in range(2)]
                    for t, (src, dst, row0, r) in enumerate(tiles):
                        load_mod(r, 1)
                        x_ = xt[t % 2]
                        Y = yk[t % 2]
                        xo = xn[t % 2]
                        DMA("sp", x_[:], src[row0:row0 + 128, :], [src], [x_])
                        for k in range(4):
                            S.dma("pool", lambda e: e.indirect_dma_start(
                                out=Y[k][:], out_offset=None, in_=ysort[:, :],
                                in_offset=bass.IndirectOffsetOnAxis(ap=idxi[:, t, k:k + 1], axis=0)), [ysort, idxi], [Y[k]])
                        TR(pGT[0:32, 0:128], Gd[:, t, :], ident32[:], [Gd, ident32], [pGT])
                        CP("act", GTs[:], pGT[0:32, 0:128], [pGT], [GTs])
                        for nbk in range(2):
                            MM(pbias[nbk][:, :], GTs[0:32, :], b2all[0:32, nbk * 512:(nbk + 1) * 512], True, True,
                               [GTs, b2all], [pbias[nbk]])
                        TSC("dve", acc[:], Y[0][:], gate4[:, t, 0:1], None, ALU.mult, None, [Y[0], gate4], [acc])
                        for k in range(1, 4):
                            STT("dve" if k != 2 else "pool", acc[:], Y[k][:], gate4[:, t, k:k + 1], acc[:], ALU.mult, ALU.add,
                                [Y[k], gate4, acc], [acc])
                        for nbk in range(2):
                            TTOP("dve", acc[:, nbk * 512:(nbk + 1) * 512], acc[:, nbk * 512:(nbk + 1) * 512], pbias[nbk][:, :], ALU.add,
                                 [acc, pbias[nbk]], [acc])
                        TTOP("dve", acc[:], acc[:], G2[:], ALU.mult, [acc, G2], [acc])
                        TTOP("dve", xo[:], acc[:], x_[:], ALU.add, [acc, x_], [xo])
                        if final:
                            ACT(junk[:], xo[:], AF.Square, [xo], [junk, ss], accum_out=ss[:])
                            rstd_from(ss, D, rs)
                            STT("dve", xo[:], xo[:], rs[:, 0:1], fng[:], ALU.mult, ALU.mult, [xo, rs, fng], [xo])
                        DMA("sp", dst[row0:row0 + 128, :], xo[:], [xo], [dst])
            S.stack = gst
            S.barrier()

        def tiles_for(xsrc, xdst, csrc, cdst, with_ctx):
            tl = []
            for s in range(SPC):
                for t in range(L // 128):
                    tl.append((xsrc, xdst, s * L + t * 128, s))
            if with_ctx:
                for s in range(SPC):
                    for t in range(LC // 128):
                        tl.append((csrc, cdst, s * LC + t * 128, 4))
            return tl

        done = False
        for l in range(2):
            last = l == 1
            phase_ada(l)
            if stop_after == ("ada", 0):
                break
            if l == 0:
                phase_mix(0, False, x_in, xs1, c_in, cs1)
                if stop_after == ("mix", 0):
                    break
                phase_moe(0, False, tiles_for(xs1, xs2, cs1, cs2, True), False)
                if stop_after in (("route", 0), ("moe", 0)):
                    break
            else:
                phase_mix(1, True, xs2, xs1, cs2, None)
                phase_moe(1, True, tiles_for(xs1, out_t, None, None, False), True)
        if stop_after is not None:
            S.barrier()
            if stop_after[0] == "ada":
                DMA("sp", out_t[0:30, :].rearrange("(r k) d -> r (k d)", r=5), modrows[:, :], [modrows], [out_t])
            else:
                srcd = {"mix": xs1, "moe": xs2, "route": xs1}[stop_after[0]]
                import os
                for i_ in range((L if os.environ.get("MIXLIM") else SPC * L) // 128):
                    DMA("sp", out_t[i_ * 128:(i_ + 1) * 128, :], srcd[i_ * 128:(i_ + 1) * 128, :], [srcd], [out_t])
        S.finish()
        build.stats = (S.n_inst, S.n_wait)
    return nc


def _prep_shared(inp):
    f = lambda a: np.ascontiguousarray(np.asarray(a, dtype=np.float32))
    cols = _ext_cols()
    sh = {}
    sh["ada_w"] = f(inp["ada_w"])
    sh["ada_b"] = f(inp["ada_b"])
    sh["norm1_g"] = f(inp["norm1_g"])
    sh["norm2_g"] = f(inp["norm2_g"])
    sh["w_in_ext"] = f(np.asarray(inp["w_in"])[:, :, cols])
    sh["attn_sink"] = f(inp["attn_sink"])
    dw = np.asarray(inp["conv_dw_w"])
    sh["conv_dwT"] = f(dw.transpose(0, 2, 1).reshape(2, 2, 128, 31).transpose(0, 2, 1, 3))
    cc = np.stack([np.asarray(inp["conv_dw_b"]), np.asarray(inp["conv_ln_g"]), np.asarray(inp["conv_ln_b"])], 1)
    sh["convcols"] = f(cc.reshape(2, 3, 2, 128).transpose(0, 3, 1, 2))
    sh["conv_pw_w"] = f(inp["conv_pw_w"])
    sh["gmlp_ln_g"] = f(inp["gmlp_ln_g"])
    sh["gmlp_ln_b"] = f(inp["gmlp_ln_b"])
    sh["gmlp_wsT"] = f(np.asarray(inp["gmlp_ws"]).transpose(0, 1, 3, 2))
    sh["gmlp_bsT"] = f(np.asarray(inp["gmlp_bs"]).transpose(0, 2, 1))
    sh["pool_w"] = f(inp["pool_w"])
    sh["pool_scale"] = f(inp["pool_scale"])
    sh["gncol"] = f(np.asarray(inp["group_norm_g"]).reshape(2, 8, 128).transpose(0, 2, 1))
    sh["w_out"] = f(inp["w_out"])
    sh["router_w"] = f(inp["router_w"])
    sh["router_b"] = f(inp["router_b"])
    sh["exp_w1"] = f(inp["exp_w1"])
    sh["exp_b1T"] = f(np.asarray(inp["exp_b1"]).reshape(2, 32, 16, 128).transpose(0, 1, 3, 2))
    sh["exp_w2r"] = f(np.asarray(inp["exp_w2"]).reshape(2, 32, 4, 2, 128, D).transpose(0, 1, 2, 4, 3, 5).reshape(2, 32, 4, 128, 2 * D))
    sh["exp_b2"] = f(inp["exp_b2"])
    sh["final_norm_g"] = f(np.asarray(inp["final_norm_g"]).reshape(1, D))
    for k, v in _consts().items():
        sh["k_" + k] = f(v)
    return sh


def _in_maps(inp):
    sh = _prep_shared(inp)
    x = np.asarray(inp["x"], dtype=np.float32)
    c = np.asarray(inp["c"], dtype=np.float32)
    ctx = np.asarray(inp["ctx"], dtype=np.float32)
    cctx = np.asarray(inp["c_ctx"], dtype=np.float32)
    maps = []
    for i in range(NCORE):
        m = dict(sh)
        m["x"] = np.ascontiguousarray(x[i * SPC:(i + 1) * SPC].reshape(SPC * L, D))
        m["ctx"] = np.ascontiguousarray(ctx[i * SPC:(i + 1) * SPC].reshape(SPC * LC, D))
        c5 = np.concatenate([c[i * SPC:(i + 1) * SPC], cctx[None, :]], 0)
        m["cT"] = np.ascontiguousarray(c5.T.reshape(8, 128, 5).transpose(1, 0, 2))
        maps.append(m)
    return maps


def kernel(**inputs):
    nc = build()
    maps = _in_maps(inputs)
    res = run_bass_kernel_spmd(nc, maps, core_ids=list(range(NCORE)))
    outs = [np.asarray(r["out"]).reshape(SPC, L, D) for r in res.results]
    return np.concatenate(outs, 0).astype(np.float32)
```

```python
import contextlib
import numpy as np
import concourse.bass as bass
import concourse.mybir as mybir
from concourse.bass_utils import run_bass_kernel_spmd

F32 = mybir.dt.float32
BF16 = mybir.dt.bfloat16
I32 = mybir.dt.int32
AF = mybir.ActivationFunctionType
ALU = mybir.AluOpType
AX = mybir.AxisListType
POOL_ENG = mybir.EngineType.Pool

EPOCH = 16000
NSLOT = 8
NCORE = 8
SPC = 4
D = 1024
L = 2048
LC = 256
INW = 2432
EPS = 1e-6
TS = 512
import os as _os
SKIPBIG = float(_os.environ.get("SKIPBIG", "1.0e6"))


class Res:
    __slots__ = ("w", "r")

    def __init__(self):
        self.w = {}
        self.r = {}


class TT:
    __slots__ = ("t", "res", "excl")

    def __init__(self, t, excl=False):
        self.t = t
        self.res = Res()
        self.excl = excl

    def __getitem__(self, k):
        return self.t[k]


def _res(b):
    return b.res if isinstance(b, TT) else b


class Sched:
    def __init__(self, nc, stack):
        self.nc = nc
        self.stack = stack
        self.gstack = stack
        self.E = {}
        for name, e in (("pe", nc.tensor), ("act", nc.scalar), ("dve", nc.vector),
                        ("pool", nc.gpsimd), ("sp", nc.sync)):
            self.E[name] = dict(e=e, name=name, sems=[], cnt=0, waited={}, slots=None, slot_i=0)
        self.n_inst = 0
        self.n_wait = 0
        self.uid = 0

    def sem(self, name):
        return self.gstack.enter_context(self.nc.semaphore(name))

    def sb(self, shape, dt, name=None):
        self.uid += 1
        return TT(self.stack.enter_context(self.nc.sbuf_tensor(f"{name or 't'}_{self.uid}", list(shape), dt)))

    def ps(self, shape, dt=F32, name=None):
        self.uid += 1
        shape = [128, 512 if dt == F32 else 1024]
        return TT(excl=True, t=self.stack.enter_context(self.nc.psum_tensor(f"{name or 'p'}_{self.uid}", list(shape), dt)))

    def _wait(self, E, sem, val):
        key = id(sem)
        if E["waited"].get(key, 0) >= val:
            return
        E["waited"][key] = val
        E["e"].wait_ge(sem, val)
        self.n_wait += 1

    def _deps(self, E, reads, writes):
        deps = {}
        for b in reads:
            for k, v in _res(b).w.items():
                if k not in deps or deps[k][1] < v[1]:
                    deps[k] = v
        for b in writes:
            r = _res(b)
            for dd in (r.w, r.r):
                for k, v in dd.items():
                    if k not in deps or deps[k][1] < v[1]:
                        deps[k] = v
        own = E["sems"]
        for k, (sem, val) in deps.items():
            if E["name"] == "pe" and any(sem is s for s in own):
                continue
            self._wait(E, sem, val)

    def _record(self, tok, reads, writes):
        k = id(tok[0])
        for b in reads:
            _res(b).r[k] = tok
        for b in writes:
            r = _res(b)
            r.w = {k: tok}
            r.r = {}

    def _next_tok(self, E):
        n = E["cnt"]
        ep = n // EPOCH
        while len(E["sems"]) <= ep:
            E["sems"].append(self.sem(f"c_{E['name']}_{len(E['sems'])}"))
        E["cnt"] = n + 1
        return (E["sems"][ep], n % EPOCH + 1)

    def op(self, eng, fn, reads=(), writes=()):
        E = self.E[eng]
        ex = [b for b in reads if isinstance(b, TT) and b.excl]
        if ex:
            reads = [b for b in reads if not (isinstance(b, TT) and b.excl)]
            writes = list(writes) + ex
        self._deps(E, reads, writes)
        inst = fn(E["e"])
        tok = self._next_tok(E)
        inst.then_inc(tok[0], 1)
        self._record(tok, reads, writes)
        self.n_inst += 1

    def wait_for(self, eng, reads=(), writes=()):
        self._deps(self.E[eng], reads, writes)

    def dma(self, q, fn, reads=(), writes=()):
        E = self.E[q]
        if E["slots"] is None:
            E["slots"] = [[self.sem(f"d_{q}_{i}"), 0] for i in range(NSLOT)]
        self._deps(E, reads, writes)
        sl = E["slots"][E["slot_i"] % NSLOT]
        E["slot_i"] += 1
        if sl[1] > 0:
            self._wait(E, sl[0], sl[1])
        inst = fn(E["e"])
        sl[1] += 16
        inst.then_inc(sl[0], 16)
        self._record((sl[0], sl[1]), reads, writes)
        self.n_inst += 1

    def _all_toks(self):
        toks = []
        for E in self.E.values():
            if E["slots"]:
                for s, v in E["slots"]:
                    if v:
                        toks.append((s, v))
            n = E["cnt"]
            if n:
                toks.append((E["sems"][(n - 1) // EPOCH], (n - 1) % EPOCH + 1))
        return toks

    def barrier(self):
        toks = self._all_toks()
        for E in self.E.values():
            for s, v in toks:
                self._wait(E, s, v)

    def finish(self):
        self.barrier()


def _consts():
    c = {}
    c["ident"] = np.eye(128, dtype=np.float32)
    tq = np.arange(128)
    c["upper"] = (tq[:, None] < tq[None, :]).astype(np.float32)
    kj = np.arange(128)[:, None]
    qi = np.arange(128)[None, :]
    mL = (qi <= kj).astype(np.float32)
    mR = (kj <= qi).astype(np.float32)
    c["masks"] = np.stack([np.concatenate([mL, mL], 1), np.concatenate([mR, mR], 1)], 1)
    t = np.arange(L)
    row = (t // 64).astype(np.float32)
    col = (t % 64).astype(np.float32)
    inv = (10000.0 ** (-np.arange(0, 32, 2, dtype=np.float32) / 32)).astype(np.float32)
    ang_r = row[:, None] * inv[None, :]
    ang_c = col[:, None] * inv[None, :]
    cs = np.zeros((64, L), np.float32)
    sn = np.zeros((64, L), np.float32)
    for d in range(64):
        ang = ang_r if d < 32 else ang_c
        i = d % 16
        cs[d] = np.cos(ang[:, i])
        sn[d] = np.sin(ang[:, i]) * (-1.0 if (d % 32) < 16 else 1.0)
    c["ropeC"] = np.concatenate([cs, cs], 0)
    c["ropeS"] = np.concatenate([sn, sn], 0)
    PM = np.zeros((128, 20, 128), np.float32)
    LL = 512
    for gi, w in enumerate((2, 4, 8, 16)):
        M = np.zeros((LL, LL), np.float32)
        for tt in range(LL):
            lo = max(tt - w // 2, 0)
            hi = min(tt + w // 2, LL)
            M[lo:hi, tt] = 1.0 / (hi - lo)
            M[tt, tt] -= 1.0
        PM[:, gi * 5 + 0] = M[0:128, 128:256]
        PM[:, gi * 5 + 1] = M[256:384, 128:256]
        PM[:, gi * 5 + 2] = M[128:256, 128:256]
        PM[:, gi * 5 + 3] = M[0:128, 0:128]
        PM[:, gi * 5 + 4] = M[384:512, 384:512]
    c["poolM"] = PM
    c["slotthr"] = np.tile((np.arange(128, dtype=np.float32) * TS)[None, :], (32, 1))
    c["piota"] = np.arange(128, dtype=np.float32)[:, None]
    return c


def _ext_cols():
    def sw(off, n_heads):
        idx = []
        for h in range(n_heads):
            for d in range(64):
                sd = d + 16 if (d % 32) < 16 else d - 16
                idx.append(off + h * 64 + sd)
        return idx
    q = list(range(0, 256))
    qs = sw(0, 4)
    k0 = list(range(256, 320))
    k1 = list(range(320, 384))
    ks = sw(256, 2)
    k0s, k1s = ks[:64], ks[64:]
    cols = q + qs + k0 + k0 + k1 + k1 + k0s + k0s + k1s + k1s
    cols += list(range(512, 1024))
    cols += list(range(1024, 1536))
    cols += list(range(384, 512))
    cols += list(range(1536, 1792))
    assert len(cols) == INW
    return np.array(cols)


def build(stop_after=None):
    nc = bass.Bass("TRN2", target_bir_lowering=False)

    def din(name, shape, dt=F32):
        return nc.dram_tensor(name, list(shape), dt, kind="ExternalInput").ap()

    def dscr(name, shape, dt=F32):
        return TT(nc.dram_tensor(name, list(shape), dt, kind="Internal").ap())

    x_in = TT(din("x", [SPC * L, D]))
    c_in = TT(din("ctx", [SPC * LC, D]))
    cT_in = din("cT", [128, 8, 5])
    ada_w = din("ada_w", [2, D, 6 * D])
    ada_b = din("ada_b", [2, 6 * D])
    norm1_g = din("norm1_g", [2, D])
    norm2_g = din("norm2_g", [2, D])
    w_in = din("w_in_ext", [2, D, INW])
    attn_sink = din("attn_sink", [2, 4])
    dwT = din("conv_dwT", [2, 128, 2, 31])
    convcols = din("convcols", [2, 128, 3, 2])
    conv_pw = din("conv_pw_w", [2, 256, 256])
    gln_g = din("gmlp_ln_g", [2, 256])
    gln_b = din("gmlp_ln_b", [2, 256])
    gwsT = din("gmlp_wsT", [2, 4, 128, 128])
    gbsT = din("gmlp_bsT", [2, 128, 4])
    pool_w = din("pool_w", [2, 4, 64, 64])
    pool_scale = din("pool_scale", [2, 256])
    gncol = din("gncol", [2, 128, 8])
    w_out = din("w_out", [2, D, D])
    router_w = din("router_w", [2, D, 32])
    router_b = din("router_b", [2, 32])
    exp_w1 = din("exp_w1", [2, 32, D, 2 * D])
    exp_b1T = din("exp_b1T", [2, 32, 128, 16])
    exp_w2 = din("exp_w2r", [2, 32, 4, 128, 2 * D])
    exp_b2 = din("exp_b2", [2, 32, D])
    final_g = din("final_norm_g", [1, D])
    k_ident = din("k_ident", [128, 128])
    k_upper = din("k_upper", [128, 128])
    k_masks = din("k_masks", [128, 2, 256])
    k_ropeC = din("k_ropeC", [128, L])
    k_ropeS = din("k_ropeS", [128, L])
    k_poolM = din("k_poolM", [128, 20, 128])
    k_slotthr = din("k_slotthr", [32, 128])
    k_piota = din("k_piota", [128, 1])
    out_t = TT(nc.dram_tensor("out", [SPC * L, D], F32, kind="ExternalOutput").ap())

    xs1 = dscr("xs1", [SPC * L, D])
    xs2 = dscr("xs2", [SPC * L, D])
    cs1 = dscr("cs1", [SPC * LC, D])
    cs2 = dscr("cs2", [SPC * LC, D])
    modrows = dscr("modrows", [5, 6 * D])
    NTMAX = (SPC * (L + LC)) // 128
    NSLMAX = NTMAX * 128 * 4 // TS + 32
    h2d = dscr("h2d", [NTMAX * 128, D], BF16)
    xsort = dscr("xsort", [NSLMAX * TS, D], BF16)
    ysort = dscr("ysort", [NSLMAX * TS, D], F32)

    with contextlib.ExitStack() as gst:
        S = Sched(nc, gst)

        def ACT(out, in_, func, reads, writes, **kw):
            S.op("act", lambda e: e.activation(out=out, in_=in_, func=func, **kw), reads, writes)

        def MM(out, lhsT, rhs, start, stop, reads, writes):
            S.op("pe", lambda e: e.matmul(out, lhsT=lhsT, rhs=rhs, start=start, stop=stop), reads, writes)

        def TR(out, in_, ident, reads, writes):
            S.op("pe", lambda e: e.transpose(out=out, in_=in_, identity=ident), reads, writes)

        def TTOP(eng, out, in0, in1, op, reads, writes):
            S.op(eng, lambda e: e.tensor_tensor(out=out, in0=in0, in1=in1, op=op), reads, writes)

        def TSC(eng, out, in0, s1, s2, op0, op1, reads, writes):
            if op1 is None:
                S.op(eng, lambda e: e.tensor_scalar(out=out, in0=in0, scalar1=s1, scalar2=None, op0=op0), reads, writes)
            else:
                S.op(eng, lambda e: e.tensor_scalar(out=out, in0=in0, scalar1=s1, scalar2=s2, op0=op0, op1=op1),
                     reads, writes)

        def STT(eng, out, in0, scalar, in1, op0, op1, reads, writes):
            eng = "dve"
            S.op(eng, lambda e: e.scalar_tensor_tensor(out=out, in0=in0, scalar=scalar, in1=in1, op0=op0, op1=op1),
                 reads, writes)

        def CP(eng, out, in_, reads, writes):
            if eng == "act":
                S.op("act", lambda e: e.copy(out=out, in_=in_), reads, writes)
            else:
                S.op(eng, lambda e: e.tensor_copy(out=out, in_=in_), reads, writes)

        def DMA(q, out, in_, reads, writes):
            S.dma(q, lambda e: e.dma_start(out=out, in_=in_), reads, writes)

        def rstd_from(ssum, n, tmp):
            TSC("dve", tmp[:], ssum[:], 1.0 / n, EPS, ALU.mult, ALU.add, [ssum], [tmp])
            ACT(tmp[:], tmp[:], AF.Sqrt, [tmp], [tmp])
            S.op("dve", lambda e: e.reciprocal(out=tmp[:], in_=tmp[:]), [tmp], [tmp])

        ident32 = S.sb([128, 128], F32, "ident32")
        identb = S.sb([128, 128], BF16, "identb")
        DMA("sp", ident32[:], k_ident, [], [ident32])
        DMA("pool", identb[:], k_ident, [], [identb])

        def phase_ada(l):
            with contextlib.ExitStack() as ph:
                S.stack = ph
                cT = S.sb([128, 8, 5], F32)
                sc = S.sb([128, 8, 5], F32)
                DMA("sp", cT[:], cT_in, [], [cT])
                ACT(sc[:], cT[:], AF.Silu, [cT], [sc])
                rows = S.sb([5, 6 * D], F32)
                bb = S.sb([5, 6 * D], F32)
                DMA("sp", bb[:], ada_b[l:l + 1, :].partition_broadcast(5), [], [bb])
                wts = [S.sb([128, 8, 512], F32) for _ in range(2)]
                pms = [S.ps([128, 512]) for _ in range(2)]
                for n in range(12):
                    wt = wts[n % 2]
                    pm = pms[n % 2]
                    DMA("sp", wt[:], ada_w[l, :, n * 512:(n + 1) * 512].rearrange("(c p) n -> p c n", p=128), [], [wt])
                    for c in range(8):
                        MM(pm[0:5, :], sc[:, c, :], wt[:, c, :], c == 0, c == 7, [sc, wt], [pm])
                    TTOP("dve", rows[:, n * 512:(n + 1) * 512], pm[0:5, :], bb[:, n * 512:(n + 1) * 512], ALU.add,
                         [pm, bb], [rows])
                DMA("sp", modrows[:, :], rows[:], [rows], [modrows])
            S.stack = gst
            S.barrier()

        def phase_mix(l, last, xsrc, xdst, csrc, cdst):
            with contextlib.ExitStack() as ph:
                S.stack = ph
                win = S.sb([128, 8, INW], BF16, "win")
                for c in range(8):
                    for hf in range(2):
                        DMA("pool", win[:, c, hf * 1216:(hf + 1) * 1216], w_in[l, c * 128:(c + 1) * 128, hf * 1216:(hf + 1) * 1216],
                            [], [win])
                wout = S.sb([128, 8, D], BF16, "wout")
                gnc = S.sb([128, 8], F32)
                DMA("sp", gnc[:], gncol[l], [], [gnc])
                dg = S.sb([128, 2, 31, 128], BF16)
                dw = S.sb([128, 2, 31], F32)
                DMA("sp", dw[:], dwT[l], [], [dw])
                for cc_ in range(2):
                    for j_ in range(31):
                        TSC("pool" if j_ % 2 else "dve", dg[:, cc_, j_, :], identb[:], dw[:, cc_, j_:j_ + 1], None, ALU.mult, None,
                            [identb, dw], [dg])
                ccols = S.sb([128, 3, 2], F32)
                DMA("sp", ccols[:], convcols[l], [], [ccols])
                cpw = S.sb([128, 2, 256], BF16)
                DMA("pool", cpw[:], conv_pw[l].rearrange("(c p) n -> p c n", p=128), [], [cpw])
                wsT = S.sb([128, 4, 128], BF16)
                DMA("pool", wsT[:], gwsT[l].rearrange("h q p -> q h p"), [], [wsT])
                bsT = S.sb([128, 4], F32)
                DMA("sp", bsT[:], gbsT[l], [], [bsT])
                glg = S.sb([128, 256], F32)
                glb = S.sb([128, 256], F32)
                DMA("sp", glg[:], gln_g[l:l + 1, :].partition_broadcast(128), [], [glg])
                DMA("sp", glb[:], gln_b[l:l + 1, :].partition_broadcast(128), [], [glb])
                pw = S.sb([64, 4, 64], BF16)
                DMA("pool", pw[:], pool_w[l].rearrange("g c j -> c g j"), [], [pw])
                psc = S.sb([128, 256], F32)
                DMA("sp", psc[:], pool_scale[l:l + 1, :].partition_broadcast(128), [], [psc])
                poolM = S.sb([128, 20, 128], BF16)
                for hf in range(2):
                    DMA("pool", poolM[:, hf * 10:(hf + 1) * 10, :], k_poolM[:, hf * 10:(hf + 1) * 10, :], [], [poolM])
                masks = S.sb([128, 2, 256], BF16)
                DMA("pool", masks[:], k_masks, [], [masks])
                esink = S.sb([128, 4], F32)
                DMA("sp", esink[:], attn_sink[l:l + 1, :].partition_broadcast(128), [], [esink])
                ACT(esink[:], esink[:], AF.Exp, [esink], [esink])
                ropeC = S.sb([128, 512], F32)
                ropeS = S.sb([128, 512], F32)
                gn1 = S.sb([128, D], F32)
                DMA("sp", gn1[:], norm1_g[l:l + 1, :].partition_broadcast(128), [], [gn1])
                onesm = S.sb([128, 128], F32)
                S.op("dve", lambda e: e.memset(onesm[:], 1.0 / 256.0), [], [onesm])
                A1 = S.sb([128, D], F32)
                B1 = S.sb([128, D], F32)
                G1 = S.sb([128, D], F32)

                qT = [S.sb([128, L], BF16) for _ in range(2)]
                kT = [S.sb([128, L], BF16) for _ in range(2)]
                kcT = [S.sb([128, LC], BF16) for _ in range(2)]
                vaug = S.sb([128, 16, 2, 65], BF16)
                vcaug = S.sb([128, 2, 2, 65], BF16)
                S.op("pool", lambda e: e.memset(vaug[:], 1.0), [], [vaug])
                S.op("pool", lambda e: e.memset(vcaug[:], 1.0), [], [vcaug])
                convT = S.sb([128, 2, L + 30], BF16)
                S.op("pool", lambda e: e.memset(convT[:], 0.0), [], [convT])
                sTb = S.sb([128, 2, L], BF16)
                gmb = S.sb([128, 16, 256], BF16)
                poolh = S.sb([128, 16, 256], BF16)
                xt1_ = S.sb([128, D], F32)
                xt = [xt1_, xt1_]
                ss = S.sb([128, 1], F32)
                rs = S.sb([128, 1], F32)
                h32 = S.sb([128, D], F32)
                hb = S.sb([128, D], BF16)
                hT = S.sb([128, 8, 512], BF16)
                ftmp = [S.sb([128, 512], F32) for _ in range(3)]
                u32 = S.sb([128, 256], F32)
                v32 = S.sb([128, 256], F32)
                vnb = S.sb([128, 256], BF16)
                st2 = S.sb([128, 2], F32)
                st3 = S.sb([128, 2], F32)
                acc = S.sb([128, 2, 512], F32)
                mean_sb = S.sb([128, 512], F32)
                rstd_sb = S.sb([128, 512], F32)
                PT = [S.sb([128, 5, 256], BF16) for _ in range(2)]
                den = S.sb([128, 4], F32)
                mix = S.sb([128, D], F32)
                gss = S.sb([128, 4], F32)
                grs = S.sb([128, 4], F32)
                yb = S.sb([128, D], BF16)
                yT = S.sb([128, 8, 128], BF16)
                yTp = S.sb([64, 4, 128], BF16)
                xn = h32
                wtmp = [h32, mix]
                for c in range(8):
                    DMA("sp", wtmp[c % 2][:], w_out[l, c * 128:(c + 1) * 128, :], [], [wtmp[c % 2]])
                    TSC("dve", wout[:, c, :], wtmp[c % 2][:], gnc[:, c:c + 1], None, ALU.mult, None,
                        [wtmp[c % 2], gnc], [wout])
                pT = [S.ps([128, 128], BF16) for _ in range(2)]
                pf = [S.ps([128, 512]) for _ in range(2)]
                pa = S.ps([128, 512])
                pb = S.ps([128, 512])
                pg = S.ps([128, 512])
                pq = S.ps([128, 512])

                def load_mod(r):
                    DMA("sp", A1[:], modrows[r:r + 1, D:2 * D].partition_broadcast(128), [modrows], [A1])
                    DMA("sp", B1[:], modrows[r:r + 1, 0:D].partition_broadcast(128), [modrows], [B1])
                    DMA("sp", G1[:], modrows[r:r + 1, 2 * D:3 * D].partition_broadcast(128), [modrows], [G1])
                    STT("dve", A1[:], A1[:], 1.0, gn1[:], ALU.add, ALU.mult, [A1, gn1], [A1])

                cp_i = [0]

                def cp_alt(out, in_, reads, writes):
                    cp_i[0] += 1
                    CP("act" if cp_i[0] % 2 else "dve", out, in_, reads, writes)

                def seq(src, dst, row0, LL, is_ctx, do_pass2):
                    NT = LL // 128
                    GT = min(512, LL)
                    TG = GT // 128
                    KT = kcT if is_ctx else kT
                    VA = vcaug if is_ctx else vaug
                    for g in range(LL // GT):
                        for ti in range(TG):
                            t = g * TG + ti
                            x_ = xt[t % 2]
                            DMA("sp", x_[:], src[row0 + t * 128: row0 + (t + 1) * 128, :], [src], [x_])
                            ACT(yb[:], x_[:], AF.Square, [x_], [yb, ss], accum_out=ss[:])
                            rstd_from(ss, D, rs)
                            STT("dve", h32[:], x_[:], rs[:, 0:1], A1[:], ALU.mult, ALU.mult, [x_, rs, A1], [h32])
                            TTOP("dve", hb[:], h32[:], B1[:], ALU.add, [h32, B1], [hb])
                            p_ = pT[t % 2]
                            for c in range(8):
                                TR(p_[:, c * 128:(c + 1) * 128], hb[:, c * 128:(c + 1) * 128], identb[:], [hb, identb], [p_])
                            cp_alt(hT[:, :, ti * 128:(ti + 1) * 128], p_[:, 0:1024].rearrange("p (c t) -> p c t", c=8), [p_], [hT])
                        if not is_ctx:
                            DMA("sp", ropeC[:, :GT], k_ropeC[:, g * GT:(g + 1) * GT], [], [ropeC])
                            DMA("sp", ropeS[:, :GT], k_ropeS[:, g * GT:(g + 1) * GT], [], [ropeS])
                        order = [0, 2, 1, 3, 4, 6, 5, 7, 10, 8, 11, 9] if not is_ctx else [0, 1, 4, 5, 10, 8, 11, 9]
                        tsl = slice(g * GT, (g + 1) * GT)
                        for oi, fc in enumerate(order):
                            p_ = pf[oi % 2]
                            for c in range(8):
                                MM(p_[:, :GT], win[:, c, fc * 128:(fc + 1) * 128], hT[:, c, :GT], c == 0, c == 7,
                                   [win, hT], [p_])
                            if fc in (0, 1, 4, 5):
                                dstT = (qT if not is_ctx else qT)[fc] if fc < 2 else KT[fc - 4]
                                if is_ctx:
                                    cp_alt(dstT[:, tsl], p_[:, :GT], [p_], [dstT])
                                else:
                                    TTOP("dve", ftmp[0][:, :GT], p_[:, :GT], ropeC[:, :GT], ALU.mult, [p_, ropeC], [ftmp[0]])
                            elif fc in (2, 3, 6, 7):
                                dstT = qT[fc - 2] if fc < 4 else KT[fc - 6]
                                TTOP("dve", ftmp[1][:, :GT], p_[:, :GT], ropeS[:, :GT], ALU.mult, [p_, ropeS], [ftmp[1]])
                                TTOP("dve", dstT[:, tsl], ftmp[0][:, :GT], ftmp[1][:, :GT], ALU.add,
                                     [ftmp[0], ftmp[1]], [dstT])
                            elif fc in (10, 11):
                                ACT(ftmp[2][:, :GT], p_[:, :GT], AF.Sigmoid, [p_], [ftmp[2]])
                            else:
                                cc = fc - 8
                                TTOP("dve", convT[:, cc, 15 + g * GT: 15 + (g + 1) * GT], p_[:, :GT], ftmp[2][:, :GT],
                                     ALU.mult, [p_, ftmp[2]], [convT])
                        for ti in range(TG):
                            t = g * TG + ti
                            for c in range(8):
                                MM(pa[:, :], hT[:, c, ti * 128:(ti + 1) * 128], win[:, c, 1536:2048], c == 0, c == 7,
                                   [hT, win], [pa])
                            for c in range(8):
                                MM(pb[:, 0:384], hT[:, c, ti * 128:(ti + 1) * 128], win[:, c, 2048:2432], c == 0, c == 7,
                                   [hT, win], [pb])
                            S.op("act", lambda e: e.copy(out=VA[:, t, :, 0:64],
                                                          in_=pb[:, 0:128].rearrange("p (j d) -> p j d", j=2)), [pb], [VA])
                            CP("dve", poolh[:, t, :], pb[:, 128:384], [pb], [poolh])
                            CP("act", u32[:], pa[:, 0:256], [pa], [u32])
                            ACT(v32[:], pa[:, 256:512], AF.Copy, [pa], [v32, st2], accum_out=st2[:, 0:1])
                            ACT(yb[:, 0:256], pa[:, 256:512], AF.Square, [pa], [yb, st2], accum_out=st2[:, 1:2])
                            TSC("dve", st3[:, 0:1], st2[:, 0:1], 1.0 / 256, None, ALU.mult, None, [st2], [st3])
                            STT("dve", st3[:, 1:2], st3[:, 0:1], -1.0, st3[:, 0:1], ALU.mult, ALU.mult, [st3], [st3])
                            STT("dve", st3[:, 1:2], st2[:, 1:2], 1.0 / 256, st3[:, 1:2], ALU.mult, ALU.add, [st2, st3], [st3])
                            TSC("dve", st3[:, 1:2], st3[:, 1:2], EPS, None, ALU.add, None, [st3], [st3])
                            ACT(st3[:, 1:2], st3[:, 1:2], AF.Sqrt, [st3], [st3])
                            S.op("dve", lambda e: e.reciprocal(out=st3[:, 1:2], in_=st3[:, 1:2]), [st3], [st3])
                            TSC("dve", v32[:], v32[:], st3[:, 0:1], st3[:, 1:2], ALU.subtract, ALU.mult, [v32, st3], [v32])
                            TTOP("dve", v32[:], v32[:], glg[:], ALU.mult, [v32, glg], [v32])
                            TTOP("dve", vnb[:], v32[:], glb[:], ALU.add, [v32, glb], [vnb])
                            for hh in range(4):
                                MM(pg[:, hh * 64:(hh + 1) * 64], wsT[:, hh, :], vnb[:, hh * 64:(hh + 1) * 64], True, True,
                                   [wsT, vnb], [pg])
                            for hh in range(4):
                                STT("dve", gmb[:, t, hh * 64:(hh + 1) * 64], pg[:, hh * 64:(hh + 1) * 64], bsT[:, hh:hh + 1],
                                    u32[:, hh * 64:(hh + 1) * 64], ALU.add, ALU.mult, [pg, bsT, u32], [gmb])
                    if not do_pass2:
                        return
                    for g in range(LL // GT):
                        tsl = slice(g * GT, (g + 1) * GT)
                        for cc in range(2):
                            pc_ = pq if cc == 0 else pg
                            for j in range(31):
                                MM(pc_[:, :GT], dg[:, cc, j, :], convT[:, cc, g * GT + j: g * GT + j + GT], j == 0, j == 30,
                                   [dg, convT], [pc_])
                            TSC("dve", acc[:, cc, :GT], pc_[:, :GT], ccols[:, 0, cc:cc + 1], None, ALU.add, None, [pc_, ccols], [acc])
                        for cc in range(2):
                            MM(pq[:, :GT], onesm[:], acc[:, cc, :GT], cc == 0, cc == 1, [onesm, acc], [pq])
                        CP("act", mean_sb[:, :GT], pq[:, :GT], [pq], [mean_sb])
                        for cc in range(2):
                            ACT(ftmp[cc][:, :GT], acc[:, cc, :GT], AF.Square, [acc], [ftmp[cc]])
                        for cc in range(2):
                            MM(pq[:, :GT], onesm[:], ftmp[cc][:, :GT], cc == 0, cc == 1, [onesm, ftmp[cc]], [pq])
                        TTOP("dve", rstd_sb[:, :GT], mean_sb[:, :GT], mean_sb[:, :GT], ALU.mult, [mean_sb], [rstd_sb])
                        TTOP("dve", rstd_sb[:, :GT], pq[:, :GT], rstd_sb[:, :GT], ALU.subtract, [pq, rstd_sb], [rstd_sb])
                        TSC("dve", rstd_sb[:, :GT], rstd_sb[:, :GT], EPS, None, ALU.add, None, [rstd_sb], [rstd_sb])
                        ACT(rstd_sb[:, :GT], rstd_sb[:, :GT], AF.Sqrt, [rstd_sb], [rstd_sb])
                        S.op("dve", lambda e: e.reciprocal(out=rstd_sb[:, :GT], in_=rstd_sb[:, :GT]), [rstd_sb], [rstd_sb])
                        for cc in range(2):
                            TTOP("dve", acc[:, cc, :GT], acc[:, cc, :GT], mean_sb[:, :GT], ALU.subtract, [acc, mean_sb], [acc])
                            TTOP("dve", acc[:, cc, :GT], acc[:, cc, :GT], rstd_sb[:, :GT], ALU.mult, [acc, rstd_sb], [acc])
                            ACT(sTb[:, cc, tsl], acc[:, cc, :GT], AF.Silu, [acc, ccols], [sTb],
                                scale=ccols[:, 1, cc:cc + 1], bias=ccols[:, 2, cc:cc + 1])
                        for ti in range(TG):
                            t = g * TG + ti
                            qs = slice(t * 128, (t + 1) * 128)
                            x_ = xt[t % 2]
                            DMA("sp", x_[:], src[row0 + t * 128: row0 + (t + 1) * 128, :], [src], [x_])
                            blocks = []
                            for cb in range(2):
                                blocks.append((kcT, slice(cb * 128, (cb + 1) * 128), vcaug, cb, None))
                            if not is_ctx:
                                if t > 0:
                                    blocks.append((kT, slice((t - 1) * 128, t * 128), vaug, t - 1, 0))
                                blocks.append((kT, qs, vaug, t, None))
                                if t < NT - 1:
                                    blocks.append((kT, slice((t + 1) * 128, (t + 2) * 128), vaug, t + 1, 1))
                            nb = len(blocks)
                            for j in range(2):
                                P_ = PT[j]
                                for lo in range(0, nb, 4):
                                    hi = min(nb, lo + 4)
                                    for gq in range(2):
                                        pr = slice(64 * gq, 64 * gq + 64)
                                        psc_ = pa if gq == 0 else pq
                                        for bi in range(lo, hi):
                                            Ks, ksl, Vt, vi, mi = blocks[bi]
                                            MM(psc_[:, (bi - lo) * 128:(bi - lo + 1) * 128], Ks[j][pr, ksl], qT[j][pr, qs], True, True,
                                               [Ks[j], qT[j]], [psc_])
                                    for gq in range(2):
                                        psc_ = pa if gq == 0 else pq
                                        ACT(P_[:, lo:hi, gq * 128:(gq + 1) * 128],
                                            psc_[:, 0:(hi - lo) * 128].rearrange("p (b q) -> p b q", q=128), AF.Exp, [psc_], [P_], scale=0.125)
                                for bi, (Ks, ksl, Vt, vi, mi) in enumerate(blocks):
                                    if mi is not None:
                                        TTOP("dve", P_[:, bi, :], P_[:, bi, :], masks[:, mi, :], ALU.mult, [P_, masks], [P_])
                                for gq in range(2):
                                    hh = 2 * j + gq
                                    for bi, (Ks, ksl, Vt, vi, mi) in enumerate(blocks):
                                        MM(pb[:, hh * 65:(hh + 1) * 65], P_[:, bi, gq * 128:(gq + 1) * 128], Vt[:, vi, j, :],
                                           bi == 0, bi == nb - 1, [P_, Vt], [pb])
                            pb3 = pb[:, 0:260].rearrange("p (h d) -> p h d", h=4)
                            TTOP("dve", den[:], pb3[:, :, 64], esink[:], ALU.add, [pb, esink], [den])
                            S.op("dve", lambda e: e.reciprocal(out=den[:], in_=den[:]), [den], [den])
                            for hh in range(4):
                                if hh % 2 == 0:
                                    TSC("dve", mix[:, hh * 64:(hh + 1) * 64], pb[:, hh * 65: hh * 65 + 64], den[:, hh:hh + 1], None,
                                        ALU.mult, None, [pb, den], [mix])
                                else:
                                    ACT(mix[:, hh * 64:(hh + 1) * 64], pb[:, hh * 65: hh * 65 + 64], AF.Copy, [pb, den], [mix],
                                        scale=den[:, hh:hh + 1])
                            for cc in range(2):
                                MM(pg[:, 0:256], sTb[:, cc, qs], cpw[:, cc, :], cc == 0, cc == 1, [sTb, cpw], [pg])
                            CP("act", mix[:, 256:512], pg[:, 0:256], [pg], [mix])
                            CP("dve", mix[:, 512:768], gmb[:, t, :], [gmb], [mix])
                            for gi in range(4):
                                srcs = []
                                if t > 0:
                                    srcs.append((t - 1, gi * 5 + 0))
                                srcs.append((t, gi * 5 + (3 if t == 0 else (4 if t == NT - 1 else 2))))
                                if t < NT - 1:
                                    srcs.append((t + 1, gi * 5 + 1))
                                for si, (tt_, mi) in enumerate(srcs):
                                    MM(pq[0:64, gi * 128:(gi + 1) * 128], poolh[:, tt_, gi * 64:(gi + 1) * 64], poolM[:, mi, :],
                                       si == 0, si == len(srcs) - 1, [poolh, poolM], [pq])
                            CP("dve", yTp[:, :, :], pq[0:64, :].rearrange("p (g t) -> p g t", g=4), [pq], [yTp])
                            for gi in range(4):
                                MM(pg[:, 256 + gi * 64: 256 + (gi + 1) * 64], yTp[:, gi, :], pw[:, gi, :], True, True,
                                   [yTp, pw], [pg])
                            TTOP("dve", mix[:, 768:1024], pg[:, 256:512], psc[:], ALU.mult, [pg, psc], [mix])
                            for gi in range(4):
                                ACT(hb[:, 0:256], mix[:, gi * 256:(gi + 1) * 256], AF.Square, [mix], [hb, gss],
                                    accum_out=gss[:, gi:gi + 1])
                            rstd_from(gss, 256, grs)
                            for gi in range(4):
                                if gi % 2 == 0:
                                    TSC("dve", yb[:, gi * 256:(gi + 1) * 256], mix[:, gi * 256:(gi + 1) * 256], grs[:, gi:gi + 1],
                                        None, ALU.mult, None, [mix, grs], [yb])
                                else:
                                    TSC("dve", yb[:, gi * 256:(gi + 1) * 256], mix[:, gi * 256:(gi + 1) * 256], grs[:, gi:gi + 1],
                                        None, ALU.mult, None, [mix, grs], [yb])
                            p_ = pT[t % 2]
                            for c in range(8):
                                TR(p_[:, c * 128:(c + 1) * 128], yb[:, c * 128:(c + 1) * 128], identb[:], [yb, identb], [p_])
                            cp_alt(yT[:, :, :], p_[:, 0:1024].rearrange("p (c t) -> p c t", c=8), [p_], [yT])
                            for nbk in range(2):
                                p_ = pf[nbk]
                                for c in range(8):
                                    MM(p_[:, :], yT[:, c, :], wout[:, c, nbk * 512:(nbk + 1) * 512], c == 0, c == 7, [yT, wout], [p_])
                                TTOP("dve", xn[:, nbk * 512:(nbk + 1) * 512], p_[:, :], G1[:, nbk * 512:(nbk + 1) * 512], ALU.mult,
                                     [p_, G1], [xn])
                            TTOP("dve", xn[:], xn[:], x_[:], ALU.add, [xn, x_], [xn])
                            DMA("sp", dst[row0 + t * 128: row0 + (t + 1) * 128, :], xn[:], [xn], [dst])

                import os
                lim = os.environ.get("MIXLIM", "")
                for s in range(SPC):
                    load_mod(4)
                    seq(csrc, cdst, s * LC, LC, True, not last)
                    if lim == "c":
                        break
                    load_mod(s)
                    seq(xsrc, xdst, s * L, L, False, True)
                    if lim == "cx":
                        break
            S.stack = gst
            S.barrier()

        def phase_moe(l, last, tiles, final):
            NT = len(tiles)
            NSL = NT * 128 * 4 // TS + 32
            with contextlib.ExitStack() as ph:
                S.stack = ph
                idxi = S.sb([128, NT, 4], I32, "idxi")
                gate4 = S.sb([128, NT, 4], F32, "gate4")
                widx = S.sb([128, 128], I32, "widx")
                w2idx = S.sb([128, 128], I32, "w2idx")
                bidx = S.sb([128, 128], I32, "bidx")
                Gd = S.sb([128, NT, 32], F32, "Gd")
                A2 = S.sb([128, D], F32)
                B2 = S.sb([128, D], F32)
                G2 = S.sb([128, D], F32)
                gn2 = S.sb([128, D], F32)
                DMA("sp", gn2[:], norm2_g[l:l + 1, :].partition_broadcast(128), [], [gn2])
                cur = [None]

                def load_mod(r, which):
                    if cur[0] == (r, which):
                        return
                    cur[0] = (r, which)
                    if which == 0:
                        DMA("sp", A2[:], modrows[r:r + 1, 4 * D:5 * D].partition_broadcast(128), [modrows], [A2])
                        DMA("sp", B2[:], modrows[r:r + 1, 3 * D:4 * D].partition_broadcast(128), [modrows], [B2])
                        STT("dve", A2[:], A2[:], 1.0, gn2[:], ALU.add, ALU.mult, [A2, gn2], [A2])
                    else:
                        DMA("sp", G2[:], modrows[r:r + 1, 5 * D:6 * D].partition_broadcast(128), [modrows], [G2])

                with contextlib.ExitStack() as ph1:
                    S.stack = ph1
                    rw = S.sb([128, 8, 32], F32)
                    DMA("sp", rw[:], router_w[l].rearrange("(c p) n -> p c n", p=128), [], [rw])
                    rbb = S.sb([128, 32], F32)
                    DMA("sp", rbb[:], router_b[l:l + 1, :].partition_broadcast(128), [], [rbb])
                    upper = S.sb([128, 128], BF16)
                    DMA("pool", upper[:], k_upper, [], [upper])
                    onesb = S.sb([128, 128], BF16)
                    S.op("dve", lambda e: e.memset(onesb[:], 1.0), [], [onesb])
                    ones32 = S.sb([128, 1], F32)
                    S.op("dve", lambda e: e.memset(ones32[:], 1.0), [], [ones32])
                    thr = S.sb([32, 128], F32)
                    DMA("sp", thr[:], k_slotthr, [], [thr])
                    lg = S.sb([128, NT, 32], F32)
                    t8 = S.sb([128, NT, 8], F32)
                    pos = S.sb([128, NT, 32], F32)
                    base = S.sb([128, 32], F32)
                    S.op("dve", lambda e: e.memset(base[:], 0.0), [], [base])
                    xt = [S.sb([128, D], F32) for _ in range(2)]
                    junk = S.sb([128, D], BF16)
                    ss = S.sb([128, 1], F32)
                    rs = S.sb([128, 1], F32)
                    h32 = S.sb([128, D], F32)
                    hb = [S.sb([128, D], BF16) for _ in range(2)]
                    hT32 = S.sb([128, 8, 128], F32)
                    maskb = S.sb([128, 32], BF16)
                    zt = S.sb([128, 2048], BF16)
                    S.op("pool", lambda e: e.memset(zt[:], 0.0), [], [zt])
                    pT32 = [S.ps([128, 128], F32) for _ in range(2)]
                    pl = S.ps([128, 512])
                    pp = S.ps([128, 512])
                    xz = xsort[0:NSL * TS, :].rearrange("(n p r) d -> n p (r d)", p=128, r=2)
                    zlist = list(range(NSL * TS // 256))
                    zres = []
                    h2dres = [Res() for _ in range(NT)]
                    zper = -(-len(zlist) // (NT // 2))
                    h32s = [h32, S.sb([128, D], F32)]
                    hT32s = [hT32, S.sb([128, 8, 128], F32)]
                    junks = [junk, S.sb([128, D], BF16)]
                    sss = [ss, S.sb([128, 1], F32)]
                    rss = [rs, S.sb([128, 1], F32)]
                    maskbs = [maskb, S.sb([128, 32], BF16)]
                    pT32s = [pT32, [S.ps([128, 128], F32) for _ in range(2)]]
                    pls = [pl, S.ps([128, 512])]
                    pps = [pp, S.ps([128, 512])]

                    def route_tile(t, k):
                        src, dst, row0, r = tiles[t]
                        x_, h32_, hb_, hT_, jk, ss_, rs_, mk = xt[k], h32s[k], hb[k], hT32s[k], junks[k], sss[k], rss[k], maskbs[k]
                        pTk, pl_, pp_ = pT32s[k], pls[k], pps[k]
                        DMA("sp", x_[:], src[row0:row0 + 128, :], [src], [x_])
                        yield
                        ACT(jk[:], x_[:], AF.Square, [x_], [jk, ss_], accum_out=ss_[:])
                        yield
                        TSC("dve", rs_[:], ss_[:], 1.0 / D, EPS, ALU.mult, ALU.add, [ss_], [rs_])
                        yield
                        ACT(rs_[:], rs_[:], AF.Sqrt, [rs_], [rs_])
                        yield
                        S.op("dve", lambda e: e.reciprocal(out=rs_[:], in_=rs_[:]), [rs_], [rs_])
                        yield
                        STT("dve", h32_[:], x_[:], rs_[:, 0:1], A2[:], ALU.mult, ALU.mult, [x_, rs_, A2], [h32_])
                        yield
                        TTOP("dve", h32_[:], h32_[:], B2[:], ALU.add, [h32_, B2], [h32_])
                        yield
                        CP("act", hb_[:], h32_[:], [h32_], [hb_])
                        DMA("sp", h2d[t * 128:(t + 1) * 128, :], hb_[:], [hb_], [h2dres[t]])
                        yield
                        for hf in range(2):
                            p_ = pTk[hf]
                            for cc in range(4):
                                c = hf * 4 + cc
                                TR(p_[:, cc * 128:(cc + 1) * 128], h32_[:, c * 128:(c + 1) * 128], ident32[:], [h32_, ident32], [p_])
                            CP("act" if hf else "dve", hT_[:, hf * 4:(hf + 1) * 4, :],
                               p_[:, 0:512].rearrange("p (c t) -> p c t", c=4), [p_], [hT_])
                            yield
                        for c in range(8):
                            MM(pl_[:, 0:32], hT_[:, c, :], rw[:, c, :], c == 0, c == 7, [hT_, rw], [pl_])
                        yield
                        TTOP("dve", lg[:, t, :], pl_[:, 0:32], rbb[:], ALU.add, [pl_, rbb], [lg])
                        yield
                        S.op("dve", lambda e: e.max(out=t8[:, t, :], in_=lg[:, t, :]), [lg], [t8])
                        yield
                        TSC("dve", mk[:], lg[:, t, :], t8[:, t, 3:4], None, ALU.is_ge, None, [lg, t8], [mk])
                        yield
                        MM(pp_[:, 0:32], upper[:], mk[:], True, True, [upper, mk], [pp_])
                        MM(pp_[:, 32:64], onesb[:], mk[:], True, True, [onesb, mk], [pp_])
                        yield
                        TSC("dve", ss_[:], t8[:, t, 0:1], -1.0, None, ALU.mult, None, [t8], [ss_])
                        yield
                        ACT(gate4[:, t, :], t8[:, t, 0:4], AF.Exp, [t8, ss_], [gate4, rs_], bias=ss_[:, 0:1], accum_out=rs_[:, 0:1])
                        yield
                        S.op("dve", lambda e: e.reciprocal(out=rs_[:], in_=rs_[:]), [rs_], [rs_])
                        yield
                        TSC("dve", gate4[:, t, :], gate4[:, t, :], rs_[:, 0:1], None, ALU.mult, None, [gate4, rs_], [gate4])
                        ACT(Gd[:, t, :], lg[:, t, :], AF.Exp, [lg, ss_], [Gd], bias=ss_[:, 0:1])
                        yield
                        TTOP("dve", Gd[:, t, :], Gd[:, t, :], mk[:], ALU.mult, [Gd, mk], [Gd])
                        yield
                        TSC("dve", Gd[:, t, :], Gd[:, t, :], rs_[:, 0:1], None, ALU.mult, None, [Gd, rs_], [Gd])
                        TTOP("dve", pos[:, t, :], pp_[:, 0:32], base[:], ALU.add, [pp_, base], [pos])
                        TTOP("dve", base[:], pp_[:, 32:64], base[:], ALU.add, [pp_, base], [base])
                        yield

                    for t0 in range(0, NT, 2):
                        load_mod(tiles[t0][3], 0)
                        alive = [route_tile(t0, 0), route_tile(t0 + 1, 1)]
                        while alive:
                            for g_ in list(alive):
                                try:
                                    next(g_)
                                except StopIteration:
                                    alive.remove(g_)
                        for _z in range(zper):
                            if zlist:
                                zres.append(Res())
                                DMA("sp", xz[zlist.pop()], zt[:], [zt], [zres[-1]])
                    while zlist:
                        zres.append(Res())
                        DMA("sp", xz[zlist.pop()], zt[:], [zt], [zres[-1]])
                    padded = S.sb([128, 32], F32)
                    pend = S.sb([128, 32], F32)
                    pstart = S.sb([128, 32], F32)
                    tmpc = S.sb([128, 32], F32)
                    TSC("dve", padded[:], base[:], 0.0, None, ALU.is_gt, None, [base], [padded])
                    for jj in range(1, NT * 128 // TS + 1):
                        STT("dve", padded[:], base[:], float(TS * jj), padded[:], ALU.is_gt, ALU.add, [base, padded], [padded])
                    TSC("dve", padded[:], padded[:], float(TS), None, ALU.mult, None, [padded], [padded])
                    CP("dve", pend[:, 0:1], padded[:, 0:1], [padded], [pend])
                    for e_ in range(1, 32):
                        TTOP("dve", pend[:, e_:e_ + 1], pend[:, e_ - 1:e_], padded[:, e_:e_ + 1], ALU.add, [pend, padded], [pend])
                    TTOP("dve", pstart[:], pend[:], padded[:], ALU.subtract, [pend, padded], [pstart])
                    pcol = S.sb([32, 1], F32)
                    tmpd = S.sb([32, 32], F32)
                    TTOP("dve", tmpd[:], pend[0:32, :], ident32[0:32, 0:32], ALU.mult, [pend, ident32], [tmpd])
                    S.op("dve", lambda e: e.reduce_sum(out=pcol[:], in_=tmpd[:], axis=AX.X), [tmpd], [pcol])
                    cmp = S.sb([32, 128], F32)
                    TSC("dve", cmp[:], thr[:], pcol[:, 0:1], None, ALU.is_ge, None, [thr, pcol], [cmp])
                    ones32m = S.sb([32, 128], F32)
                    S.op("dve", lambda e: e.memset(ones32m[:], 1.0), [], [ones32m])
                    piota = S.sb([128, 1], F32)
                    DMA("sp", piota[:], k_piota, [], [piota])
                    MM(pl[:, 0:128], ones32m[0:32, :], cmp[:], True, True, [ones32m, cmp], [pl])
                    blkf = S.sb([128, 128], F32)
                    TSC("dve", blkf[:], pl[:, 0:128], 31.0, None, ALU.min, None, [pl], [blkf])
                    wif = S.sb([128, 128], F32)
                    same = S.sb([128, 128], F32)
                    S.op("dve", lambda e: e.memset(same[:], 0.0), [], [same])
                    TTOP("dve", same[:, 1:128], blkf[:, 1:128], blkf[:, 0:127], ALU.is_equal, [blkf], [same])
                    S.op("dve", lambda e: e.memset(same[:, NSL // 2:NSL // 2 + 1], 0.0), [], [same])
                    TSC("dve", same[:], same[:], SKIPBIG, None, ALU.mult, None, [same], [same])
                    STT("dve", wif[:], blkf[:], 1024.0, same[:], ALU.mult, ALU.add, [blkf, same], [wif])
                    TSC("dve", wif[:], wif[:], piota[:, 0:1], None, ALU.add, None, [wif, piota], [wif])
                    CP("dve", widx[:], wif[:], [wif], [widx])
                    STT("dve", wif[:], blkf[:], 512.0, same[:], ALU.mult, ALU.add, [blkf, same], [wif])
                    TSC("dve", wif[:], wif[:], piota[:, 0:1], None, ALU.add, None, [wif, piota], [wif])
                    CP("dve", w2idx[:], wif[:], [wif], [w2idx])
                    STT("dve", wif[:], blkf[:], 128.0, same[:], ALU.mult, ALU.add, [blkf, same], [wif])
                    TSC("dve", wif[:], wif[:], piota[:, 0:1], None, ALU.add, None, [wif, piota], [wif])
                    CP("dve", bidx[:], wif[:], [wif], [bidx])
                    oh = S.sb([128, 32], F32)
                    idxf = S.sb([128, 4], F32)
                    for t, (src, dst, row0, r) in enumerate(tiles):
                        TTOP("dve", pos[:, t, :], pos[:, t, :], pstart[:], ALU.add, [pos, pstart], [pos])
                        for k in range(4):
                            STT("dve", oh[:], lg[:, t, :], t8[:, t, k:k + 1], pos[:, t, :], ALU.is_equal, ALU.mult,
                                [lg, t8, pos], [oh])
                            S.op("dve", lambda e: e.reduce_sum(out=idxf[:, k:k + 1], in_=oh[:], axis=AX.X), [oh], [idxf])
                        CP("dve", idxi[:, t, :], idxf[:], [idxf], [idxi])
                        hb_ = hb[t % 2]
                        DMA("sp", hb_[:], h2d[t * 128:(t + 1) * 128, :], [h2dres[t]], [hb_])
                        for k in range(4):
                            S.dma("pool", lambda e: e.indirect_dma_start(
                                out=xsort[:, :], out_offset=bass.IndirectOffsetOnAxis(ap=idxi[:, t, k:k + 1], axis=0),
                                in_=hb_[:], in_offset=None), [hb_, idxi] + zres, [])
                S.stack = ph
                S.barrier()
                if stop_after == ("route", l):
                    return
                with contextlib.ExitStack() as ph2:
                    S.stack = ph2
                    w1b = [S.sb([128, 8, 2 * D], BF16) for _ in range(2)]
                    w2b = [S.sb([128, 8, D], BF16) for _ in range(2)]
                    b1c = [S.sb([128, 16], F32) for _ in range(2)]
                    xs_ = [S.sb([128, 4, D], BF16) for _ in range(2)]
                    xsT2 = [S.sb([128, 8, TS], BF16) for _ in range(2)]
                    actT = S.sb([128, 8, TS], BF16)
                    b1p = [S.sb([128, 8], F32) for _ in range(2)]
                    linp = [S.sb([128, TS], F32) for _ in range(2)]
                    g32s = [S.sb([128, TS], F32) for _ in range(2)]
                    sgs = [S.sb([128, TS], F32) for _ in range(2)]
                    tts = [S.sb([128, TS], F32) for _ in range(2)]
                    yt = [S.sb([128, D], F32) for _ in range(2)]
                    pT = [S.ps([128, 128], BF16) for _ in range(2)]
                    ph_ = [S.ps([128, 512]) for _ in range(4)]
                    py = [S.ps([128, 512]) for _ in range(2)]
                    w1flat = exp_w1.rearrange("l e k n -> (l e k) n")
                    w2flat = exp_w2.rearrange("l e c p n -> (l e c p) n")
                    b1flat = exp_b1T.rearrange("l e p n -> (l e p) n")

                    if not hasattr(build, "_bnd") or build._bnd[0] is not nc:
                        rg_ = nc.gpsimd.alloc_register("bndreg")
                        nc.gpsimd.reg_mov(rg_, 2 * 32 * 1024 - 1)
                        build._bnd = (nc, rg_)
                    bnd_reg = build._bnd[1]

                    HS = NSL // 2
                    order = []
                    for q_ in range(HS):
                        order += [q_, HS + q_]

                    def load_w(j):
                        b = 0 if j < HS else 1
                        for c in range(8):
                            S.dma("pool", lambda e: e.indirect_dma_start(
                                out=w1b[b][:, c, :], out_offset=None, in_=w1flat,
                                in_offset=bass.IndirectOffsetOnAxis(ap=widx[:, j:j + 1], axis=0),
                                element_offset=(l * 32 * 1024 + c * 128) * 2048, bounds_check=bnd_reg, oob_is_err=False), [widx], [w1b[b]])
                        for c2 in range(4):
                            S.dma("pool", lambda e: e.indirect_dma_start(
                                out=w2b[b][:, 2 * c2:2 * c2 + 2, :].rearrange("p c n -> p (c n)"), out_offset=None, in_=w2flat,
                                in_offset=bass.IndirectOffsetOnAxis(ap=w2idx[:, j:j + 1], axis=0),
                                element_offset=(l * 128 + c2) * 128 * 2048, bounds_check=bnd_reg, oob_is_err=False),
                                [w2idx], [w2b[b]])
                        S.dma("pool", lambda e: e.indirect_dma_start(
                            out=b1c[b][:], out_offset=None, in_=b1flat,
                            in_offset=bass.IndirectOffsetOnAxis(ap=bidx[:, j:j + 1], axis=0),
                            element_offset=l * 32 * 128 * 16, bounds_check=bnd_reg, oob_is_err=False), [bidx], [b1c[b]])

                    def load_x(j, i):
                        DMA("sp", xs_[i % 2][:], xsort[j * TS:(j + 1) * TS, :].rearrange("(a p) d -> p a d", p=128),
                            [xsort], [xs_[i % 2]])

                    def do_transposes(ii):
                        X, xsT = xs_[ii % 2], xsT2[ii % 2]
                        for a in range(4):
                            p_ = pT[a % 2]
                            for c in range(8):
                                TR(p_[:, c * 128:(c + 1) * 128], X[:, a, c * 128:(c + 1) * 128], identb[:], [X, identb], [p_])
                            CP("act" if a % 2 else "dve", xsT[:, :, a * 128:(a + 1) * 128],
                               p_[:, 0:1024].rearrange("p (c t) -> p c t", c=8), [p_], [xsT])

                    load_w(order[0])
                    load_x(order[0], 0)
                    do_transposes(0)
                    for i_, j in enumerate(order):
                        b = 0 if j < HS else 1
                        if i_ + 1 < NSL:
                            load_w(order[i_ + 1])
                            load_x(order[i_ + 1], i_ + 1)
                        W1, W2, B1c, X, xsT = w1b[b], w2b[b], b1c[b], xs_[i_ % 2], xsT2[i_ % 2]
                        TSC("dve", b1p[b][:], B1c[:, 8:16], 1.0, None, ALU.add, None, [B1c], [b1p[b]])
                        pi = 0
                        for i in range(8):
                            k2 = i % 2
                            pl_ = ph_[pi % 4]
                            pi += 1
                            for c in range(8):
                                MM(pl_[:, :], W1[:, c, (8 + i) * 128:(9 + i) * 128], xsT[:, c, :], c == 0, c == 7, [W1, xsT], [pl_])
                            TSC("dve", linp[k2][:], pl_[:, :], b1p[b][:, i:i + 1], -6.0, ALU.add, ALU.max, [pl_, b1p[b]], [linp[k2]])
                            pg_ = ph_[pi % 4]
                            pi += 1
                            for c in range(8):
                                MM(pg_[:, :], W1[:, c, i * 128:(i + 1) * 128], xsT[:, c, :], c == 0, c == 7, [W1, xsT], [pg_])
                            TSC("dve", g32s[k2][:], pg_[:, :], B1c[:, i:i + 1], 7.0, ALU.add, ALU.min, [pg_, B1c], [g32s[k2]])
                            ACT(sgs[k2][:], g32s[k2][:], AF.Sigmoid, [g32s[k2]], [sgs[k2]], scale=1.702)
                            TTOP("dve", tts[k2][:], sgs[k2][:], g32s[k2][:], ALU.mult, [sgs[k2], g32s[k2]], [tts[k2]])
                            STT("dve", actT[:, i, :], linp[k2][:], 8.0, tts[k2][:], ALU.min, ALU.mult, [linp[k2], tts[k2]], [actT])
                        if i_ + 1 < NSL:
                            do_transposes(i_ + 1)
                        for a in range(4):
                            y_ = yt[a % 2]
                            for nbk in range(2):
                                p_ = py[nbk]
                                for f in range(8):
                                    MM(p_[:, :], actT[:, f, a * 128:(a + 1) * 128], W2[:, f, nbk * 512:(nbk + 1) * 512],
                                       f == 0, f == 7, [actT, W2], [p_])
                                CP("act", y_[:, nbk * 512:(nbk + 1) * 512], p_[:, :], [p_], [y_])
                            DMA("sp", ysort[j * TS + a * 128: j * TS + (a + 1) * 128, :], y_[:], [y_], [Res()])
                S.stack = ph
                S.barrier()
                with contextlib.ExitStack() as ph3:
                    S.stack = ph3
                    yk = [[S.sb([128, D], F32) for _ in range(4)] for _ in range(2)]
                    xt = [S.sb([128, D], F32) for _ in range(2)]
                    acc = S.sb([128, D], F32)
                    xn = [S.sb([128, D], F32) for _ in range(2)]
                    junk = S.sb([128, D], BF16)
                    ss = S.sb([128, 1], F32)
                    rs = S.sb([128, 1], F32)
                    fng = S.sb([128, D], F32)
                    DMA("sp", fng[:], final_g[0:1, :].partition_broadcast(128), [], [fng])
                    b2all = S.sb([32, D], F32)
                    DMA("sp", b2all[:], exp_b2[l], [], [b2all])
                    GTs = S.sb([32, 128], F32)
                    pGT = S.ps([128, 512])
                    pbias = [S.ps([128, 512]) for _ in range(2)]
                    for t, (src, dst, row0, r) in enumerate(tiles):
                        load_mod(r, 1)
                        x_ = xt[t % 2]
                        Y = yk[t % 2]
                        xo = xn[t % 2]
                        DMA("sp", x_[:], src[row0:row0 + 128, :], [src], [x_])
                        for k in range(4):
                            S.dma("pool", lambda e: e.indirect_dma_start(
                                out=Y[k][:], out_offset=None, in_=ysort[:, :],
                                in_offset=bass.IndirectOffsetOnAxis(ap=idxi[:, t, k:k + 1], axis=0)), [ysort, idxi], [Y[k]])
                        TR(pGT[0:32, 0:128], Gd[:, t, :], ident32[:], [Gd, ident32], [pGT])
                        CP("act", GTs[:], pGT[0:32, 0:128], [pGT], [GTs])
                        for nbk in range(2):
                            MM(pbias[nbk][:, :], GTs[0:32, :], b2all[0:32, nbk * 512:(nbk + 1) * 512], True, True,
                               [GTs, b2all], [pbias[nbk]])
                        TSC("dve", acc[:], Y[0][:], gate4[:, t, 0:1], None, ALU.mult, None, [Y[0], gate4], [acc])
                        for k in range(1, 4):
                            STT("dve" if k != 2 else "pool", acc[:], Y[k][:], gate4[:, t, k:k + 1], acc[:], ALU.mult, ALU.add,
                                [Y[k], gate4, acc], [acc])
                        for nbk in range(2):
                            TTOP("dve", acc[:, nbk * 512:(nbk + 1) * 512], acc[:, nbk * 512:(nbk + 1) * 512], pbias[nbk][:, :], ALU.add,
                                 [acc, pbias[nbk]], [acc])
                        TTOP("dve", acc[:], acc[:], G2[:], ALU.mult, [acc, G2], [acc])
                        TTOP("dve", xo[:], acc[:], x_[:], ALU.add, [acc, x_], [xo])
                        if final:
                            ACT(junk[:], xo[:], AF.Square, [xo], [junk, ss], accum_out=ss[:])
                            rstd_from(ss, D, rs)
                            STT("dve", xo[:], xo[:], rs[:, 0:1], fng[:], ALU.mult, ALU.mult, [xo, rs, fng], [xo])
                        DMA("sp", dst[row0:row0 + 128, :], xo[:], [xo], [dst])
            S.stack = gst
            S.barrier()

        def tiles_for(xsrc, xdst, csrc, cdst, with_ctx):
            tl = []
            for s in range(SPC):
                for t in range(L // 128):
                    tl.append((xsrc, xdst, s * L + t * 128, s))
            if with_ctx:
                for s in range(SPC):
                    for t in range(LC // 128):
                        tl.append((csrc, cdst, s * LC + t * 128, 4))
            return tl

        done = False
        for l in range(2):
            last = l == 1
            phase_ada(l)
            if stop_after == ("ada", 0):
                break
            if l == 0:
                phase_mix(0, False, x_in, xs1, c_in, cs1)
                if stop_after == ("mix", 0):
                    break
                phase_moe(0, False, tiles_for(xs1, xs2, cs1, cs2, True), False)
                if stop_after in (("route", 0), ("moe", 0)):
                    break
            else:
                phase_mix(1, True, xs2, xs1, cs2, None)
                phase_moe(1, True, tiles_for(xs1, out_t, None, None, False), True)
        if stop_after is not None:
            S.barrier()
            if stop_after[0] == "ada":
                DMA("sp", out_t[0:30, :].rearrange("(r k) d -> r (k d)", r=5), modrows[:, :], [modrows], [out_t])
            else:
                srcd = {"mix": xs1, "moe": xs2, "route": xs1}[stop_after[0]]
                import os
                for i_ in range((L if os.environ.get("MIXLIM") else SPC * L) // 128):
                    DMA("sp", out_t[i_ * 128:(i_ + 1) * 128, :], srcd[i_ * 128:(i_ + 1) * 128, :], [srcd], [out_t])
        S.finish()
        build.stats = (S.n_inst, S.n_wait)
    return nc


def _prep_shared(inp):
    f = lambda a: np.ascontiguousarray(np.asarray(a, dtype=np.float32))
    cols = _ext_cols()
    sh = {}
    sh["ada_w"] = f(inp["ada_w"])
    sh["ada_b"] = f(inp["ada_b"])
    sh["norm1_g"] = f(inp["norm1_g"])
    sh["norm2_g"] = f(inp["norm2_g"])
    sh["w_in_ext"] = f(np.asarray(inp["w_in"])[:, :, cols])
    sh["attn_sink"] = f(inp["attn_sink"])
    dw = np.asarray(inp["conv_dw_w"])
    sh["conv_dwT"] = f(dw.transpose(0, 2, 1).reshape(2, 2, 128, 31).transpose(0, 2, 1, 3))
    cc = np.stack([np.asarray(inp["conv_dw_b"]), np.asarray(inp["conv_ln_g"]), np.asarray(inp["conv_ln_b"])], 1)
    sh["convcols"] = f(cc.reshape(2, 3, 2, 128).transpose(0, 3, 1, 2))
    sh["conv_pw_w"] = f(inp["conv_pw_w"])
    sh["gmlp_ln_g"] = f(inp["gmlp_ln_g"])
    sh["gmlp_ln_b"] = f(inp["gmlp_ln_b"])
    sh["gmlp_wsT"] = f(np.asarray(inp["gmlp_ws"]).transpose(0, 1, 3, 2))
    sh["gmlp_bsT"] = f(np.asarray(inp["gmlp_bs"]).transpose(0, 2, 1))
    sh["pool_w"] = f(inp["pool_w"])
    sh["pool_scale"] = f(inp["pool_scale"])
    sh["gncol"] = f(np.asarray(inp["group_norm_g"]).reshape(2, 8, 128).transpose(0, 2, 1))
    sh["w_out"] = f(inp["w_out"])
    sh["router_w"] = f(inp["router_w"])
    sh["router_b"] = f(inp["router_b"])
    sh["exp_w1"] = f(inp["exp_w1"])
    sh["exp_b1T"] = f(np.asarray(inp["exp_b1"]).reshape(2, 32, 16, 128).transpose(0, 1, 3, 2))
    sh["exp_w2r"] = f(np.asarray(inp["exp_w2"]).reshape(2, 32, 4, 2, 128, D).transpose(0, 1, 2, 4, 3, 5).reshape(2, 32, 4, 128, 2 * D))
    sh["exp_b2"] = f(inp["exp_b2"])
    sh["final_norm_g"] = f(np.asarray(inp["final_norm_g"]).reshape(1, D))
    for k, v in _consts().items():
        sh["k_" + k] = f(v)
    return sh


def _in_maps(inp):
    sh = _prep_shared(inp)
    x = np.asarray(inp["x"], dtype=np.float32)
    c = np.asarray(inp["c"], dtype=np.float32)
    ctx = np.asarray(inp["ctx"], dtype=np.float32)
    cctx = np.asarray(inp["c_ctx"], dtype=np.float32)
    maps = []
    for i in range(NCORE):
        m = dict(sh)
        m["x"] = np.ascontiguousarray(x[i * SPC:(i + 1) * SPC].reshape(SPC * L, D))
        m["ctx"] = np.ascontiguousarray(ctx[i * SPC:(i + 1) * SPC].reshape(SPC * LC, D))
        c5 = np.concatenate([c[i * SPC:(i + 1) * SPC], cctx[None, :]], 0)
        m["cT"] = np.ascontiguousarray(c5.T.reshape(8, 128, 5).transpose(1, 0, 2))
        maps.append(m)
    return maps


def kernel(**inputs):
    nc = build()
    maps = _in_maps(inputs)
    res = run_bass_kernel_spmd(nc, maps, core_ids=list(range(NCORE)))
    outs = [np.asarray(r["out"]).reshape(SPC, L, D) for r in res.results]
    return np.concatenate(outs, 0).astype(np.float32)
```
